# Optimizing a Trainium2 kernel written in Bass

```python
import jax
import jax.numpy as jnp
from jax import lax
import numpy as np

D_MODEL = 1024
BATCH = 2
SEQ = 16384
DEPTH = 2

MIX_W = D_MODEL // 2
N_BRANCHES = 3
RET_HEADS = 4
RET_DV = MIX_W // RET_HEADS
RET_DK = RET_DV // 2
RET_CHUNK = 128
ML_HEADS = 4
ML_DH = MIX_W // ML_HEADS
ML_CONV = 4
ML_QK_BLOCK = 4
ML_CHUNK = 128
ATT_PATTERNS = ((128, 1), (512, 4), (2048, 16))
ATT_GROUPS = len(ATT_PATTERNS)
ATT_HEADS = 4
ATT_DV = MIX_W // ATT_HEADS
ATT_DK = ATT_DV // 2
N_EXPERTS = 32
TOP_K = 4
D_FF = D_MODEL
SWIGLU_LIMIT = 7.0
SWIGLU_ALPHA = 1.702
MOE_BLOCK = 128
DN_ALPHA = (2.0 * DEPTH) ** 0.25
DN_BETA = (8.0 * DEPTH) ** -0.25
EPS = 1e-5

IN_SIZES = (RET_HEADS * RET_DK, RET_HEADS * RET_DK, MIX_W, MIX_W,
            MIX_W, ML_HEADS, ML_HEADS, MIX_W,
            ATT_GROUPS * ATT_HEADS * ATT_DK, ATT_GROUPS * ATT_HEADS * ATT_DK, ATT_GROUPS * ATT_HEADS * ATT_DV,
            N_BRANCHES * D_MODEL)
IN_SPLITS = tuple(int(v) for v in np.cumsum(IN_SIZES)[:-1])
D_IN = int(sum(IN_SIZES))

kernel_name = 'hybrid_retention_mlstm_dilattn_moe_deepnorm'


def _layer_norm(x, g, b):
    xf = x.astype(jnp.float32)
    mu = jnp.mean(xf, -1, keepdims=True)
    var = jnp.mean(jnp.square(xf - mu), -1, keepdims=True)
    return ((xf - mu) * lax.rsqrt(var + EPS) * g.astype(jnp.float32) + b.astype(jnp.float32)).astype(x.dtype)


def _head_norm(h, g):
    mu = jnp.mean(h, -1, keepdims=True)
    var = jnp.mean(jnp.square(h - mu), -1, keepdims=True)
    y = (h - mu) * lax.rsqrt(var + EPS)
    return y.reshape(h.shape[:-2] + (-1,)) * g.astype(jnp.float32)


def _causal_conv(x, w, b):
    k_, c_ = w.shape
    y = lax.conv_general_dilated(x, w[:, None, :].astype(x.dtype), window_strides=(1,), padding=[(k_ - 1, 0)],
                                 dimension_numbers=('NWC', 'WIO', 'NWC'), feature_group_count=c_)
    return y + b


def _retention(q, k, v):
    f32 = jnp.float32
    b_, s_, h_, dk = q.shape
    c = RET_CHUNK
    n = s_ // c
    log_gamma = jnp.log1p(-jnp.exp2(-5.0 - jnp.arange(h_, dtype=f32)))
    q = q.astype(f32).reshape(b_, n, c, h_, dk)
    k = k.astype(f32).reshape(b_, n, c, h_, dk) * (dk ** -0.5)
    v = v.astype(f32).reshape(b_, n, c, h_, -1)
    pos = jnp.arange(c, dtype=f32)
    diff = pos[:, None] - pos[None, :]
    decay = jnp.where(diff >= 0, jnp.exp(log_gamma[:, None, None] * jnp.maximum(diff, 0.0)), 0.0)
    inner = jnp.einsum('bnhij,bnjhe->bnihe', jnp.einsum('bnihd,bnjhd->bnhij', q, k) * decay, v)
    k_decay = jnp.exp(log_gamma[:, None] * (c - 1.0 - pos))
    q_decay = jnp.exp(log_gamma[:, None] * (pos + 1.0))
    chunk_kv = jnp.einsum('bnjhd,bnjhe,hj->nbhde', k, v, k_decay)
    chunk_decay = jnp.exp(log_gamma * c)[None, :, None, None]

    def step(state, kv):
        return chunk_decay * state + kv, state

    _, prev = lax.scan(step, jnp.zeros(chunk_kv.shape[1:], f32), chunk_kv)
    cross = jnp.einsum('bnihd,nbhde,hi->bnihe', q, prev, q_decay)
    return (inner + cross).reshape(b_, s_, h_, -1)


def _mlstm(q, k, v, i_pre, f_pre):
    f32 = jnp.float32
    b_, s_, h_, dh = q.shape
    c = ML_CHUNK
    n = s_ // c

    def chunks(t):
        return t.astype(f32).reshape(b_, n, c, h_, dh).transpose(0, 3, 1, 2, 4)

    q = chunks(q)
    k = chunks(k) * (dh ** -0.5)
    v = chunks(v)
    ig = i_pre.reshape(b_, n, c, h_).transpose(0, 3, 1, 2)
    lf = jax.nn.log_sigmoid(f_pre).reshape(b_, n, c, h_).transpose(0, 3, 1, 2)
    cum = jnp.cumsum(lf, axis=-1)
    tot = cum[..., -1]
    a = tot[..., None] - cum + ig
    a_max = jnp.max(a, -1)
    wa = jnp.exp(a - a_max[..., None])
    chunk_c = jnp.einsum('bhnl,bhnld,bhnle->nbhde', wa, k, v)
    chunk_n = jnp.einsum('bhnl,bhnld->nbhd', wa, k)

    def step(carry, inp):
        cm, nv, m = carry
        cc, cn, g, am = inp
        m_new = jnp.maximum(g + m, am)
        s_old = jnp.exp(g + m - m_new)
        s_new = jnp.exp(am - m_new)
        cm_new = s_old[..., None, None] * cm + s_new[..., None, None] * cc
        nv_new = s_old[..., None] * nv + s_new[..., None] * cn
        return (cm_new, nv_new, m_new), (cm, nv, m)

    init = (jnp.zeros((b_, h_, dh, dh), f32), jnp.zeros((b_, h_, dh), f32), jnp.zeros((b_, h_), f32))
    _, (c_prev, n_prev, m_prev) = lax.scan(step, init, (chunk_c, chunk_n, tot.transpose(2, 0, 1), a_max.transpose(2, 0, 1)))
    m_inter = cum + m_prev.transpose(1, 2, 0)[..., None]
    causal = jnp.tril(jnp.ones((c, c), bool))
    dlog = jnp.where(causal, cum[..., :, None] - cum[..., None, :] + ig[..., None, :], -jnp.inf)
    m_q = jnp.maximum(m_inter, jnp.max(dlog, -1))
    w_qk = jnp.exp(dlog - m_q[..., None]) * jnp.einsum('bhnjd,bhnsd->bhnjs', q, k)
    inter = jnp.exp(m_inter - m_q)
    num = jnp.einsum('bhnjs,bhnse->bhnje', w_qk, v) + inter[..., None] * jnp.einsum('bhnjd,nbhde->bhnje', q, c_prev)
    den = jnp.sum(w_qk, -1) + inter * jnp.einsum('bhnjd,nbhd->bhnj', q, n_prev)
    h = num / jnp.maximum(jnp.abs(den), jnp.exp(-m_q))[..., None]
    return h.transpose(0, 2, 3, 1, 4).reshape(b_, s_, h_, dh)


def _alibi_slopes(n):
    return jnp.exp2(-8.0 * jnp.arange(1, n + 1, dtype=jnp.float32) / n)


def _dilated_group(q, k, v, window, dilation, slopes):
    f32 = jnp.float32
    b_, s_, h_, dk = q.shape
    wb = window // dilation
    l_sub = s_ // dilation
    n_blk = -(-l_sub // wb)
    l_pad = n_blk * wb

    def strided(t):
        t = t.astype(f32).reshape(b_, l_sub, dilation, h_, -1).transpose(0, 2, 1, 3, 4).reshape(b_ * dilation, l_sub, h_, -1)
        t = jnp.pad(t, ((0, 0), (0, l_pad - l_sub), (0, 0), (0, 0)))
        return t.reshape(b_ * dilation, n_blk, wb, h_, -1)

    def unstrided(t):
        return t.reshape(b_, dilation, l_pad, h_, -1)[:, :, :l_sub].transpose(0, 2, 1, 3, 4).reshape(b_, s_, h_, -1)

    def shift(t):
        return jnp.pad(t, ((0, 0), (1, 0), (0, 0), (0, 0), (0, 0)))[:, :-1]

    qs = strided(q) * (dk ** -0.5)
    ks = strided(k)
    vs = strided(v)
    kk = jnp.concatenate([shift(ks), ks], axis=2)
    vv = jnp.concatenate([shift(vs), vs], axis=2)
    scores = jnp.einsum('znihd,znjhd->znhij', qs, kk)
    qi = jnp.arange(wb)[:, None]
    kj = jnp.arange(2 * wb)[None, :]
    delta = qi + wb - kj
    valid = ((delta >= 0) & (delta <= wb))[None] & ((jnp.arange(n_blk)[:, None, None] > 0) | (kj >= wb)[None])
    bias = -slopes[:, None, None] * (dilation * delta).astype(f32)
    scores = jnp.where(valid[:, None], scores + bias, -jnp.inf)
    lse = jax.nn.logsumexp(scores, axis=-1)
    out = jnp.einsum('znhij,znjhe->znihe', jnp.exp(scores - lse[..., None]), vv)
    return unstrided(out), unstrided(jnp.swapaxes(lse, 2, 3)[..., None])


def _dilated_attention(q, k, v):
    slopes = _alibi_slopes(ATT_GROUPS * ATT_HEADS).reshape(ATT_GROUPS, ATT_HEADS)
    outs, lses = [], []
    for g, (window, dilation) in enumerate(ATT_PATTERNS):
        o, l = _dilated_group(q[:, :, g], k[:, :, g], v[:, :, g], window, dilation, slopes[g])
        outs.append(o)
        lses.append(l)
    wts = jax.nn.softmax(jnp.stack(lses), axis=0)
    return jnp.sum(wts * jnp.stack(outs), axis=0)


def _moe(x, w_router, b_router, w_gate, b_gate, w_up, b_up, w_down, b_down):
    b_, s_, d_ = x.shape
    xt = x.reshape(-1, d_)
    t_ = xt.shape[0]
    logits = (xt @ w_router + b_router).astype(jnp.float32)
    top_val, top_idx = lax.top_k(logits, TOP_K)
    probs = jax.nn.softmax(top_val, axis=-1)
    n_assign = t_ * TOP_K
    flat_e = top_idx.reshape(-1)
    order = jnp.argsort(flat_e)
    e_sorted = flat_e[order]
    tok_sorted = (order // TOP_K).astype(jnp.int32)
    p_sorted = probs.reshape(-1)[order]
    counts = jnp.bincount(flat_e, length=N_EXPERTS)
    padded = (counts + MOE_BLOCK - 1) // MOE_BLOCK * MOE_BLOCK
    start = jnp.cumsum(counts) - counts
    pstart = jnp.cumsum(padded) - padded
    n_blocks = -(-n_assign // MOE_BLOCK) + N_EXPERTS
    dest = pstart[e_sorted] + jnp.arange(n_assign) - start[e_sorted]
    row_tok = jnp.zeros((n_blocks * MOE_BLOCK,), jnp.int32).at[dest].set(tok_sorted)
    row_p = jnp.zeros((n_blocks * MOE_BLOCK,), jnp.float32).at[dest].set(p_sorted)
    block_e = jnp.minimum(jnp.searchsorted(jnp.cumsum(padded) // MOE_BLOCK, jnp.arange(n_blocks), side='right'), N_EXPERTS - 1)

    def block(acc, inp):
        e, tok, p = inp
        xb = xt[tok]
        gate = jnp.minimum(xb @ w_gate[e] + b_gate[e], SWIGLU_LIMIT)
        up = jnp.clip(xb @ w_up[e] + b_up[e], -SWIGLU_LIMIT, SWIGLU_LIMIT)
        y = ((up + 1.0) * gate * jax.nn.sigmoid(SWIGLU_ALPHA * gate)) @ w_down[e] + b_down[e]
        return acc.at[tok].add(y * p[:, None].astype(y.dtype)), None

    out, _ = lax.scan(block, jnp.zeros_like(xt), (block_e, row_tok.reshape(n_blocks, MOE_BLOCK), row_p.reshape(n_blocks, MOE_BLOCK)))
    return out.reshape(b_, s_, d_)


def _layer(x, w_in, ret_gn, ml_conv_w, ml_conv_b, ml_wq, ml_wk, ml_wv, ml_bi, ml_bf, ml_gn, ml_skip,
           w_branch, w_out, ln1_g, ln1_b, w_router, b_router, w_gate, b_gate, w_up, b_up, w_down, b_down, ln2_g, ln2_b):
    f32 = jnp.float32
    b_, s_, d_ = x.shape
    (r_q, r_k, r_v, r_g, m_x, m_i, m_f, m_o, a_q, a_k, a_v, gates) = jnp.split(x @ w_in, IN_SPLITS, axis=-1)
    ret = _retention(r_q.reshape(b_, s_, RET_HEADS, RET_DK), r_k.reshape(b_, s_, RET_HEADS, RET_DK),
                     r_v.reshape(b_, s_, RET_HEADS, RET_DV))
    y_ret = _head_norm(ret, ret_gn) * jax.nn.silu(r_g.astype(f32))
    m_c = jax.nn.silu(_causal_conv(m_x, ml_conv_w, ml_conv_b))

    def blockdiag(t, w):
        return jnp.einsum('bsnc,ncd->bsnd', t.reshape(b_, s_, -1, ML_QK_BLOCK), w).reshape(b_, s_, ML_HEADS, ML_DH)

    h_ml = _mlstm(blockdiag(m_c, ml_wq), blockdiag(m_c, ml_wk), blockdiag(m_x, ml_wv),
                  m_i.astype(f32) + ml_bi.astype(f32), m_f.astype(f32) + ml_bf.astype(f32))
    y_ml = jax.nn.sigmoid(m_o.astype(f32)) * (_head_norm(h_ml, ml_gn) + ml_skip.astype(f32) * m_c.astype(f32))
    att = _dilated_attention(a_q.reshape(b_, s_, ATT_GROUPS, ATT_HEADS, ATT_DK),
                             a_k.reshape(b_, s_, ATT_GROUPS, ATT_HEADS, ATT_DK),
                             a_v.reshape(b_, s_, ATT_GROUPS, ATT_HEADS, ATT_DV))
    y_att = att.reshape(b_, s_, MIX_W)
    gate_pre = gates.reshape(b_, s_, N_BRANCHES, d_)
    merged = jax.nn.sigmoid(gate_pre[:, :, 0]) * (y_ret.astype(x.dtype) @ w_branch[0])
    merged = merged + jax.nn.sigmoid(gate_pre[:, :, 1]) * (y_ml.astype(x.dtype) @ w_branch[1])
    merged = merged + jax.nn.sigmoid(gate_pre[:, :, 2]) * (y_att.astype(x.dtype) @ w_branch[2])
    x = _layer_norm(DN_ALPHA * x + merged @ w_out, ln1_g, ln1_b)
    x = _layer_norm(DN_ALPHA * x + _moe(x, w_router, b_router, w_gate, b_gate, w_up, b_up, w_down, b_down), ln2_g, ln2_b)
    return x


def setup_inputs(seed: int = 0) -> dict:
    key = jax.random.key(seed)
    ks = jax.random.split(key, 26)
    f32 = jnp.float32
    L = DEPTH
    nb = MIX_W // ML_QK_BLOCK

    def nrm(k, shape, scale):
        return jax.random.normal(k, shape, f32) * scale

    return {
        'x': nrm(ks[0], (BATCH, SEQ, D_MODEL), 1.0),
        'w_in': nrm(ks[1], (L, D_MODEL, D_IN), D_MODEL ** -0.5),
        'ret_gn': 1.0 + nrm(ks[2], (L, MIX_W), 0.01),
        'ml_conv_w': nrm(ks[3], (L, ML_CONV, MIX_W), ML_CONV ** -0.5),
        'ml_conv_b': nrm(ks[4], (L, MIX_W), 0.01),
        'ml_wq': nrm(ks[5], (L, nb, ML_QK_BLOCK, ML_QK_BLOCK), ML_QK_BLOCK ** -0.5),
        'ml_wk': nrm(ks[6], (L, nb, ML_QK_BLOCK, ML_QK_BLOCK), ML_QK_BLOCK ** -0.5),
        'ml_wv': nrm(ks[7], (L, nb, ML_QK_BLOCK, ML_QK_BLOCK), ML_QK_BLOCK ** -0.5),
        'ml_bi': nrm(ks[8], (L, ML_HEADS), 0.1),
        'ml_bf': jnp.linspace(3.0, 6.0, ML_HEADS, dtype=f32) + nrm(ks[9], (L, ML_HEADS), 0.1),
        'ml_gn': 1.0 + nrm(ks[10], (L, MIX_W), 0.01),
        'ml_skip': 1.0 + nrm(ks[11], (L, MIX_W), 0.1),
        'w_branch': nrm(ks[12], (L, N_BRANCHES, MIX_W, D_MODEL), MIX_W ** -0.5 * DN_BETA),
        'w_out': nrm(ks[13], (L, D_MODEL, D_MODEL), D_MODEL ** -0.5 * DN_BETA),
        'ln1_g': 1.0 + nrm(ks[14], (L, D_MODEL), 0.01),
        'ln1_b': nrm(ks[15], (L, D_MODEL), 0.01),
        'w_router': nrm(ks[16], (L, D_MODEL, N_EXPERTS), D_MODEL ** -0.5),
        'b_router': nrm(ks[17], (L, N_EXPERTS), 0.01),
        'w_gate': nrm(ks[18], (L, N_EXPERTS, D_MODEL, D_FF), D_MODEL ** -0.5),
        'b_gate': nrm(ks[19], (L, N_EXPERTS, D_FF), 0.01),
        'w_up': nrm(ks[20], (L, N_EXPERTS, D_MODEL, D_FF), D_MODEL ** -0.5),
        'b_up': nrm(ks[21], (L, N_EXPERTS, D_FF), 0.01),
        'w_down': nrm(ks[22], (L, N_EXPERTS, D_FF, D_MODEL), D_FF ** -0.5 * DN_BETA),
        'b_down': nrm(ks[23], (L, N_EXPERTS, D_MODEL), 0.01),
        'ln2_g': 1.0 + nrm(ks[24], (L, D_MODEL), 0.01),
        'ln2_b': nrm(ks[25], (L, D_MODEL), 0.01),
    }


def reference(x, w_in, ret_gn, ml_conv_w, ml_conv_b, ml_wq, ml_wk, ml_wv, ml_bi, ml_bf, ml_gn, ml_skip,
              w_branch, w_out, ln1_g, ln1_b, w_router, b_router, w_gate, b_gate, w_up, b_up, w_down, b_down, ln2_g, ln2_b):
    for l in range(DEPTH):
        x = _layer(x, w_in[l], ret_gn[l], ml_conv_w[l], ml_conv_b[l], ml_wq[l], ml_wk[l], ml_wv[l], ml_bi[l], ml_bf[l],
                   ml_gn[l], ml_skip[l], w_branch[l], w_out[l], ln1_g[l], ln1_b[l], w_router[l], b_router[l],
                   w_gate[l], b_gate[l], w_up[l], b_up[l], w_down[l], b_down[l], ln2_g[l], ln2_b[l])
    return x
```

```python
import numpy as np
import concourse.bass as bass
import concourse.mybir as mybir
from concourse.bass_utils import run_bass_kernel_spmd
from contextlib import ExitStack

F32 = mybir.dt.float32
F32R = mybir.dt.float32r
BF16 = mybir.dt.bfloat16
I32 = mybir.dt.int32
U32 = mybir.dt.uint32
AF = mybir.ActivationFunctionType
ALU = mybir.AluOpType
AX = mybir.AxisListType

ENGS = ['pe', 'act', 'dve', 'pool', 'sp']


class Tile:
    def __init__(self, P, h, name, space='sb'):
        self.P = P
        self.h = h
        self.name = name
        self.space = space
        self.lw = {}
        self.rd = {}
        self.dsem = None
        self.dcnt = 0

    def __getitem__(self, k):
        return V(self, self.h[k])

    @property
    def v(self):
        return V(self, self.h[:])


class V:
    def __init__(self, tile, ap):
        self.tile = tile
        self.ap = ap

    def __getitem__(self, k):
        return V(self.tile, self.ap[k])

    def bitcast(self, dt):
        return V(self.tile, self.ap.bitcast(dt))

    def bc(self, shape):
        return V(self.tile, self.ap.to_broadcast(shape))

    def re(self, s, **kw):
        return V(self.tile, self.ap.rearrange(s, **kw))


def _ap(x):
    if isinstance(x, Tile):
        return x.h[:]
    return x.ap if isinstance(x, V) else x


class Prog:
    def __init__(self, nc, es, same_engine_sync=True):
        self.nc = nc
        self.es = es
        self.es_top = es
        self.all_tiles = []
        self.stream = {e: [] for e in ENGS}
        self.sems = {}
        self.cnt = {e: 0 for e in ENGS}
        self.known = {e: {} for e in ENGS}
        self.same = same_engine_sync
        self.nsem = 0
        for e in ['pe', 'act', 'dve', 'pool']:
            self.sems[e] = es.enter_context(nc.semaphore("s_" + e))
            self.nsem += 1
        self.ninst = 0
        self.nwait = 0

    def sb(self, name, shape, dt=F32):
        h = self.es.enter_context(self.nc.sbuf_tensor(name, list(shape), dt))
        return Tile(self, h, name)

    def ps(self, name, shape, dt=F32):
        h = self.es.enter_context(self.nc.psum_tensor(name, list(shape), dt))
        return Tile(self, h, name, 'ps')

    def dram(self, name, shape, dt=F32, kind="Internal"):
        h = self.nc.dram_tensor(name, list(shape), dt, kind=kind)
        return Tile(self, h.ap(), name, 'dram')

    def _tsem(self, t):
        if t.dsem is None:
            key = "d_" + t.name
            self.sems[key] = self.es_top.enter_context(self.nc.semaphore(key))
            self.nsem += 1
            t.dsem = key
            self.all_tiles.append(t)
        return t.dsem

    def _waits(self, eng, rt, wt):
        need = {}
        for t in rt:
            for s, v in t.lw.items():
                need[s] = max(need.get(s, 0), v)
        for t in wt:
            for s, v in t.lw.items():
                need[s] = max(need.get(s, 0), v)
            for s, v in t.rd.items():
                need[s] = max(need.get(s, 0), v)
        out = []
        kn = self.known[eng]
        for s, v in need.items():
            if s == eng and (eng == 'pe' or not self.same):
                continue
            if kn.get(s, 0) < v:
                kn[s] = v
                out.append((s, v))
        return out

    def _record(self, ev, rt, wt):
        s, v = ev
        for t in wt:
            t.lw[s] = max(t.lw.get(s, 0), v)
            t.rd = {}
        for t in rt:
            if t in wt:
                continue
            t.rd[s] = max(t.rd.get(s, 0), v)

    @staticmethod
    def _tiles(xs):
        out = []
        for x in xs:
            if x is None:
                continue
            t = x.tile if isinstance(x, V) else x
            if isinstance(t, Tile) and t not in out:
                out.append(t)
        return out

    def I(self, eng, fn, w=(), r=()):
        wt = self._tiles(w)
        rt = self._tiles(r)
        for t in rt:
            if t.space == 'ps' and t not in wt and eng != 'pe':
                wt.append(t)
        waits = self._waits(eng, rt, wt)
        self.cnt[eng] += 1
        ev = (eng, self.cnt[eng])
        self.stream[eng].append((waits, fn, (eng, 1)))
        self._record(ev, rt, wt)
        self.ninst += 1
        self.nwait += len(waits)

    def dma(self, q, out, in_, **kw):
        wt = self._tiles([out])
        rt = self._tiles([in_])
        owner = None
        for x in (out, in_):
            t_ = x.tile if isinstance(x, V) else (x if isinstance(x, Tile) else None)
            if t_ is not None and t_.space == 'sb':
                owner = t_
        if owner is None:
            for x in (out, in_):
                t_ = x.tile if isinstance(x, V) else (x if isinstance(x, Tile) else None)
                if t_ is not None and owner is None:
                    owner = t_
        key = self._tsem(owner)
        waits = self._waits(q, rt, wt)
        owner.dcnt += 16
        ev = (key, owner.dcnt)
        o, i = _ap(out), _ap(in_)
        self.stream[q].append((waits, lambda e: e.dma_start(out=o, in_=i, **kw), (key, 16)))
        self._record(ev, rt, wt)
        self.ninst += 1
        self.nwait += len(waits)
        return ev

    def wait_all(self, eng, tiles):
        ts = self._tiles(tiles)
        waits = self._waits(eng, ts, ts)
        self.stream[eng].append((waits, None, None))

    def mm(self, out, lhsT, rhs, start=True, stop=True, **kw):
        o, a, b = _ap(out), _ap(lhsT), _ap(rhs)
        self.I('pe', lambda e: e.matmul(o, a, b, start=start, stop=stop, **kw), w=[out], r=[lhsT, rhs])

    def tr(self, out, in_, ident):
        o, a, b = _ap(out), _ap(in_), _ap(ident)
        self.I('pe', lambda e: e.transpose(o, a, b), w=[out], r=[in_, ident])

    def act(self, out, in_, func, bias=None, scale=1.0, accum_out=None, eng='act'):
        o, a = _ap(out), _ap(in_)
        kw = {}
        if bias is not None:
            kw['bias'] = _ap(bias)
        if accum_out is not None:
            kw['accum_out'] = _ap(accum_out)
        sc = _ap(scale)
        self.I(eng, lambda e: e.activation(o, a, func, scale=sc, **kw),
               w=[out, accum_out], r=[in_, bias, scale if isinstance(scale, V) else None])

    def tt(self, eng, out, in0, in1, op):
        o, a, b = _ap(out), _ap(in0), _ap(in1)
        self.I(eng, lambda e: e.tensor_tensor(o, a, b, op), w=[out], r=[in0, in1])

    def ts(self, eng, out, in0, s1, s2=None, op0=ALU.mult, op1=None, accum_out=None):
        o, a = _ap(out), _ap(in0)
        x1, x2 = _ap(s1), _ap(s2)
        kw = {}
        if op1 is not None:
            kw['op1'] = op1
        if accum_out is not None:
            kw['accum_out'] = _ap(accum_out)
        self.I(eng, lambda e: e.tensor_scalar(o, a, x1, x2, op0, **kw), w=[out, accum_out],
               r=[in0, s1 if isinstance(s1, V) else None, s2 if isinstance(s2, V) else None])

    def stt(self, eng, out, in0, scalar, in1, op0, op1):
        o, a, b = _ap(out), _ap(in0), _ap(in1)
        s = _ap(scalar)
        self.I(eng, lambda e: e.scalar_tensor_tensor(o, a, s, b, op0, op1), w=[out],
               r=[in0, in1, scalar if isinstance(scalar, V) else None])

    def copy(self, eng, out, in_):
        o, a = _ap(out), _ap(in_)
        if eng == 'act':
            self.I(eng, lambda e: e.copy(o, a), w=[out], r=[in_])
        else:
            self.I(eng, lambda e: e.tensor_copy(o, a), w=[out], r=[in_])

    def memset(self, eng, out, val):
        o = _ap(out)
        self.I(eng, lambda e: e.memset(o, val), w=[out])

    def barrier(self):
        evs = {e: self.cnt[e] for e in ['pe', 'act', 'dve', 'pool'] if self.cnt[e] > 0}
        for t in self.all_tiles:
            if t.dcnt > 0:
                evs[t.dsem] = t.dcnt
        for eng in ENGS:
            kn = self.known[eng]
            waits = []
            for s_, v in evs.items():
                if kn.get(s_, 0) < v:
                    kn[s_] = v
                    waits.append((s_, v))
            if waits:
                self.stream[eng].append((waits, None, None))

    def scope(self):
        P = self

        class _S:
            def __enter__(self_):
                self_.old = P.es
                self_.st = ExitStack()
                self_.st.__enter__()
                P.es = self_.st
                return self_

            def __exit__(self_, *a):
                P.barrier()
                P.emit()
                P.es = self_.old
                self_.st.__exit__(None, None, None)
                return False
        return _S()

    def emit(self):
        nc = self.nc
        sems = self.sems
        with nc.Block() as block:
            def run(engobj, name):
                for waits, fn, inc in self.stream[name]:
                    for s, v in waits:
                        engobj.wait_ge(sems[s], v)
                    if fn is not None:
                        ins = fn(engobj)
                        ins.then_inc(sems[inc[0]], inc[1])

            @block.tensor
            def _(e):
                run(e, 'pe')

            @block.scalar
            def _(e):
                run(e, 'act')

            @block.vector
            def _(e):
                run(e, 'dve')

            @block.gpsimd
            def _(e):
                run(e, 'pool')

            @block.sync
            def _(e):
                run(e, 'sp')
        self.stream = {e: [] for e in ENGS}


D = 1024
MIXW = 512
NEXP = 32
DN_ALPHA = (2.0 * 2) ** 0.25
EPS = 1e-5
OFF = dict(r_q=0, r_k=256, r_v=512, r_g=1024, m_x=1536, m_i=2048, m_f=2052, m_o=2056,
           a_q=2568, a_k=3336, a_v=4104, gates=5640)
D_IN = 8712


class Ctx:
    pass


def make_ctx(P):
    C = Ctx()
    C.identf = P.sb("identf", [128, 128], F32)
    C.identb = P.sb("identb", [128, 128], BF16)
    C.onesb = P.sb("onesb", [128, 128], BF16)
    C.onesf = P.sb("onesf", [128, 128], F32)
    P.memset('pool', C.identf.v, 1.0)
    o = C.identf.v.ap
    P.I('pool', lambda e: e.affine_select(o, o, [[-1, 128]], ALU.is_equal, 0.0, base=0, channel_multiplier=1),
        w=[C.identf], r=[C.identf])
    P.copy('pool', C.identb.v, C.identf.v)
    P.memset('pool', C.onesb.v, 1.0)
    P.memset('pool', C.onesf.v, 1.0)
    C.ps = [P.ps("psb%d" % i, [128, 512], F32) for i in range(8)]
    return C


def load_w_cast(P, dst, src, q='pool'):
    cols = src.shape[-1]
    c0 = 0
    while c0 < cols:
        c1 = min(cols, c0 + 1024)
        P.dma(q, dst[:, :, c0:c1], src[:, :, c0:c1])
        c0 = c1


def bcast_rows(P, dst, src1d, q='act'):
    P.dma(q, dst, src1d.partition_broadcast(128))


def x_transpose(P, C, xt, outs, psl):
    for half in range(2):
        pt = psl[half]
        for c in range(4):
            k = half * 4 + c
            P.tr(pt[:, c * 128:(c + 1) * 128], xt[:, k * 128:(k + 1) * 128], C.identf.v)
        for (o, eng) in outs:
            P.copy(eng, o[:, half * 4:(half + 1) * 4, :], pt.v.re("p (c t) -> p c t", c=4))


def layer_norm(P, r, g_b, b_b, st, mv, rstd, eng2='pool'):
    for hf in range(2):
        a, b = st[:, hf, :].ap, r[:, hf * 512:(hf + 1) * 512].ap
        P.I('dve', (lambda a, b: (lambda e: e.bn_stats(a, b)))(a, b), w=[st], r=[r])
    a, b = mv.ap, st.ap
    P.I('dve', lambda e: e.bn_aggr(a, b), w=[mv], r=[st])
    P.ts('dve', rstd, mv[:, 1:2], EPS, None, op0=ALU.add)
    P.act(rstd, rstd, AF.Ln)
    P.act(rstd, rstd, AF.Exp, scale=-0.5)
    P.ts('dve', r, r, mv[:, 0:1], rstd, op0=ALU.subtract, op1=ALU.mult)
    P.tt(eng2, r, r, g_b, ALU.mult)
    P.tt(eng2, r, r, b_b, ALU.add)


def pass_merge(P, C, NT, x_d, yT_d, w_in_l, w_branch_l, w_out_l, ln_g, ln_b, x1_d, pfx="m"):
    wg = P.sb(pfx + "wg", [128, 8, 3072], BF16)
    wb = P.sb(pfx + "wb", [128, 12, 1024], BF16)
    wo = P.sb(pfx + "wo", [128, 8, 1024], BF16)
    load_w_cast(P, wg.v, w_in_l[:, OFF['gates']:D_IN].rearrange("(kc p) c -> p kc c", p=128))
    load_w_cast(P, wb.v, w_branch_l.rearrange("b (kc p) c -> p (b kc) c", p=128))
    load_w_cast(P, wo.v, w_out_l.rearrange("(kc p) c -> p kc c", p=128))
    gB = P.sb(pfx + "gB", [128, 1024]); bB = P.sb(pfx + "bB", [128, 1024])
    bcast_rows(P, gB.v, ln_g); bcast_rows(P, bB.v, ln_b)
    xts = [P.sb(pfx + "xt%d" % i, [128, 1024]) for i in range(2)]
    xTs = [P.sb(pfx + "xT%d" % i, [128, 8, 128], BF16) for i in range(2)]
    yTs = [[P.sb(pfx + "yT%d_%d" % (b, i), [128, 4, 128], BF16) for i in range(2)] for b in range(3)]
    mg = [P.sb(pfx + "mg%d" % i, [128, 1024]) for i in range(2)]
    mT = [P.sb(pfx + "mT%d" % i, [128, 8, 128], BF16) for i in range(2)]
    sg = [P.sb(pfx + "sg%d" % i, [128, 512]) for i in range(2)]
    tmp = [P.sb(pfx + "tmp%d" % i, [128, 512]) for i in range(2)]
    rr = [P.sb(pfx + "rr%d" % i, [128, 1024]) for i in range(2)]
    st = P.sb(pfx + "st", [128, 2, 6]); mv = P.sb(pfx + "mv", [128, 2]); rstd = P.sb(pfx + "rstd", [128, 1])
    k = 0
    for t in range(NT // 128):
        xt = xts[t % 2]; xT = xTs[t % 2]
        P.dma('sp', xt.v, x_d[t * 128:(t + 1) * 128, :])
        x_transpose(P, C, xt.v, [(xT.v, 'act')], [C.ps[0], C.ps[1]])
        for b in range(3):
            P.dma('act', yTs[b][t % 2].v, yT_d[b][:, :, t * 128:(t + 1) * 128])
        m = mg[t % 2]
        for b in range(3):
            for hf in range(2):
                pg = C.ps[2 + (k % 2)]; pb = C.ps[4 + (k % 2)]; s = sg[k % 2]; tm = tmp[k % 2]
                k += 1
                for kc in range(8):
                    P.mm(pg.v, xT[:, kc, :], wg[:, kc, b * 1024 + hf * 512: b * 1024 + (hf + 1) * 512],
                         start=(kc == 0), stop=(kc == 7))
                for kc in range(4):
                    P.mm(pb.v, yTs[b][t % 2][:, kc, :], wb[:, b * 4 + kc, hf * 512:(hf + 1) * 512],
                         start=(kc == 0), stop=(kc == 3))
                P.act(s.v, pg.v, AF.Sigmoid)
                msl = m[:, hf * 512:(hf + 1) * 512]
                if b == 0:
                    P.tt('dve', msl, s.v, pb.v, ALU.mult)
                else:
                    P.tt('dve', tm.v, s.v, pb.v, ALU.mult)
                    P.tt('pool', msl, msl, tm.v, ALU.add)
        x_transpose(P, C, m.v, [(mT[t % 2].v, 'act')], [C.ps[6], C.ps[7]])
        r = rr[t % 2]
        for hf in range(2):
            po = C.ps[2 + (k % 2)]
            k += 1
            for kc in range(8):
                P.mm(po.v, mT[t % 2][:, kc, :], wo[:, kc, hf * 512:(hf + 1) * 512], start=(kc == 0), stop=(kc == 7))
            P.stt('dve', r[:, hf * 512:(hf + 1) * 512], xt[:, hf * 512:(hf + 1) * 512], DN_ALPHA, po.v,
                  ALU.mult, ALU.add)
        layer_norm(P, r.v, gB.v, bB.v, st.v, mv.v, rstd.v)
        P.dma('sp', x1_d[t * 128:(t + 1) * 128, :], r.v)


def pass_moe(P, C, NT, x1_d, w_router, b_router, w_gate, b_gate, w_up, b_up, w_down, b_down, ln_g, ln_b, out_d,
             NE=NEXP, pfx="e", TGT=4, dbg=0):
    TG = TGT * 128
    gB = P.sb(pfx + "gB", [128, 1024]); bB = P.sb(pfx + "bB", [128, 1024])
    bcast_rows(P, gB.v, ln_g); bcast_rows(P, bB.v, ln_b)
    wr = P.sb(pfx + "wr", [128, 8, NE], F32R)
    wr0 = P.sb(pfx + "wr0", [128, 8, NE])
    P.dma('act', wr0.v, w_router.rearrange("(kc p) e -> p kc e", p=128))
    P.copy('dve', wr.v, wr0.v)
    brB = P.sb(pfx + "brB", [128, NE]); bcast_rows(P, brB.v, b_router)
    bgT = P.sb(pfx + "bgT", [128, NE, 8]); buT = P.sb(pfx + "buT", [128, NE, 8])
    bstage = P.sb(pfx + "bstage", [128, 128])
    if dbg in (3, 7):
        P.memset('dve', bgT.v, 0.0); P.memset('dve', buT.v, 0.0)
    for (dstT, src) in (((bgT, b_gate), (buT, b_up)) if dbg not in (3, 7) else ()):
        rows = NE * 8
        srcv = src.rearrange("e (fc p) -> (e fc) p", p=128)
        dv = dstT.v.re("p e fc -> p (e fc)")
        r0 = 0
        while r0 < rows:
            r1 = min(rows, r0 + 128)
            n = r1 - r0
            P.dma('act', bstage[0:n, :], srcv[r0:r1, :])
            pz = C.ps[7]
            P.tr(pz[:, 0:n], bstage[0:n, :], C.identf[0:n, 0:n])
            P.copy('dve', dv[:, r0:r1], pz[:, 0:n])
            r0 = r1
    bd = P.sb(pfx + "bd", [NE, 1024], F32R)
    bd0 = P.sb(pfx + "bd0", [NE, 1024])
    P.dma('act', bd0.v, b_down)
    P.copy('dve', bd.v, bd0.v)
    W = [[P.sb(pfx + "W%d_%d" % (j, i), [128, 8, 1024], BF16) for j in range(3)] for i in range(2)]
    xts = [P.sb(pfx + "xt%d" % i, [128, 1024]) for i in range(TGT)]
    xTg = P.sb(pfx + "xTg", [128, 8, TG], BF16)
    xT32 = P.sb(pfx + "xT32", [128, 8, 128], F32R)
    acc = P.sb(pfx + "acc", [128, TGT, 1024])
    pall = P.sb(pfx + "pall", [128, TGT, NE])
    actT = P.sb(pfx + "actT", [128, 8, TG], BF16)
    lg = P.sb(pfx + "lg", [128, NE]); t8 = P.sb(pfx + "t8", [128, 8]); msk = P.sb(pfx + "msk", [128, NE])
    ex = P.sb(pfx + "ex", [128, NE]); sm = P.sb(pfx + "sm", [128, 1]); nmx = P.sb(pfx + "nmx", [128, 1])
    pT = P.sb(pfx + "pT", [NE, 128], F32R)
    gt = [P.sb(pfx + "g%d" % i, [128, TG]) for i in range(2)]
    st_ = [P.sb(pfx + "s%d" % i, [128, TG]) for i in range(2)]
    ut = [P.sb(pfx + "u%d" % i, [128, TG]) for i in range(2)]
    st = P.sb(pfx + "st", [128, 2, 6]); mv = P.sb(pfx + "mv", [128, 2]); rstd = P.sb(pfx + "rstd", [128, 1])
    c1 = P.sb(pfx + "c1", [128, 1]); c7 = P.sb(pfx + "c7", [128, 1])
    P.memset('dve', c1.v, 1.0); P.memset('dve', c7.v, 7.0)
    rr = [P.sb(pfx + "rr%d" % i, [128, 1024]) for i in range(2)]
    tmpq = [P.sb(pfx + "tq%d" % i, [128, 512]) for i in range(2)]
    assert TG == 512
    if dbg == 5:
        P.wait_all('sp', [gB, bB, wr, brB, bd, bgT, buT])
        return
    wcnt = 0
    kk = 0
    for gi in range(NT // TG):
        for tt in range(TGT):
            t = gi * TGT + tt
            xt = xts[tt]
            P.dma('sp', xt.v, x1_d[t * 128:(t + 1) * 128, :])
            x_transpose(P, C, xt.v, [(xTg[:, :, tt * 128:(tt + 1) * 128], 'act')] + ([(xT32.v, 'dve')] if dbg != 3 else []), [C.ps[0], C.ps[1]])
            if dbg in (2, 3, 7, 8):
                P.memset('dve', acc[:, tt, :], 0.0)
                P.memset('dve', pall[:, tt, :], 0.25)
            else:
                pl = C.ps[6]
                for kc in range(8):
                    P.mm(pl[:, 0:NE], xT32[:, kc, :], wr[:, kc, :], start=(kc == 0), stop=(kc == 7))
                P.tt('dve', lg.v, pl[:, 0:NE], brB.v, ALU.add)
                a, b = t8.v.ap, lg.v.ap
                P.I('dve', (lambda a, b: (lambda e: e.max(out=a, in_=b)))(a, b), w=[t8], r=[lg])
                P.ts('dve', msk.v, lg.v, t8[:, 3:4], c1.v, op0=ALU.is_ge, op1=ALU.mult)
                P.ts('dve', nmx.v, t8[:, 0:1], -1.0, None, op0=ALU.mult)
                P.act(ex.v, lg.v, AF.Exp, bias=nmx.v)
                P.tt('dve', ex.v, ex.v, msk.v, ALU.mult)
                a2, b2 = sm.v.ap, ex.v.ap
                P.I('dve', (lambda a, b: (lambda e: e.reduce_sum(a, b, AX.X)))(a2, b2), w=[sm], r=[ex])
                a3 = sm.v.ap
                P.I('dve', (lambda a: (lambda e: e.reciprocal(a, a)))(a3), w=[sm], r=[sm])
                P.ts('dve', pall[:, tt, :], ex.v, sm.v, c1.v, op0=ALU.mult, op1=ALU.mult)
                P.tr(pl[0:NE, 128:256], pall[:, tt, :], C.identf.v)
                P.copy('dve', pT.v, pl[0:NE, 128:256])
                for hf in range(2):
                    pb = C.ps[7]
                    P.mm(pb.v, pT.v, bd[:, hf * 512:(hf + 1) * 512])
                    P.copy('dve', acc[:, tt, hf * 512:(hf + 1) * 512], pb.v)
        for e in range(NE if dbg not in (1, 3, 7, 8) else 0):
            Wg, Wu, Wd = W[wcnt % 2]
            wcnt += 1
            P.dma('pool', Wg.v, w_gate[e].rearrange("(kc p) f -> p kc f", p=128))
            P.dma('pool', Wu.v, w_up[e].rearrange("(kc p) f -> p kc f", p=128))
            P.dma('pool', Wd.v, w_down[e].rearrange("(kc p) f -> p kc f", p=128))
            for fc in range(8):
                pg = C.ps[(kk % 2) * 2]; pu = C.ps[(kk % 2) * 2 + 1]
                g = gt[kk % 2]; s = st_[kk % 2]; u = ut[kk % 2]
                kk += 1
                for kc in range(8):
                    P.mm(pg.v, Wg[:, kc, fc * 128:(fc + 1) * 128], xTg[:, kc, :], start=(kc == 0), stop=(kc == 7))
                for kc in range(8):
                    P.mm(pu.v, Wu[:, kc, fc * 128:(fc + 1) * 128], xTg[:, kc, :], start=(kc == 0), stop=(kc == 7))
                P.ts('dve', g.v, pg.v, bgT[:, e, fc:fc + 1], c7.v, op0=ALU.add, op1=ALU.min)
                P.act(s.v, g.v, AF.Sigmoid, scale=1.702)
                P.ts('dve', u.v, pu.v, buT[:, e, fc:fc + 1], c7.v, op0=ALU.add, op1=ALU.min)
                P.ts('dve', u.v, u.v, -7.0, 1.0, op0=ALU.max, op1=ALU.add)
                P.tt('dve', g.v, g.v, s.v, ALU.mult)
                P.tt('dve', actT[:, fc, :], g.v, u.v, ALU.mult)
            for tt in range(TGT):
                for hf in range(2):
                    py = C.ps[4 + (kk % 2)]
                    kk += 1
                    for fc in range(8):
                        P.mm(py.v, actT[:, fc, tt * 128:(tt + 1) * 128], Wd[:, fc, hf * 512:(hf + 1) * 512],
                             start=(fc == 0), stop=(fc == 7))
                    av = acc[:, tt, hf * 512:(hf + 1) * 512]
                    tq = tmpq[kk % 2]
                    P.ts('dve', tq.v, py.v, pall[:, tt, e:e + 1], c1.v, op0=ALU.mult, op1=ALU.mult)
                    P.tt('dve', av, av, tq.v, ALU.add)
        for tt in range(TGT):
            t = gi * TGT + tt
            r = rr[tt % 2].v
            P.stt('dve', r, xts[tt].v, DN_ALPHA, acc[:, tt, :], ALU.mult, ALU.add)
            layer_norm(P, r, gB.v, bB.v, st.v, mv.v, rstd.v)
            P.dma('sp', out_d[t * 128:(t + 1) * 128, :], r)


RET_GAMMA = [1.0 - 2.0 ** (-5.0 - h) for h in range(4)]


def host_consts_scan(NT):
    j = np.arange(128, dtype=np.float64)
    lg = np.log(np.array(RET_GAMMA, dtype=np.float64))
    aR = np.exp(lg[None, :] * (j[:, None] + 1.0)) * (64 ** -0.5)
    bR = np.exp(-lg[None, :] * (j[:, None] + 1.0))
    eR = np.zeros((128, 2)); gR = np.zeros((128, 2))
    for h in range(4):
        ps = (h % 2) * 64
        eR[ps:ps + 64, h // 2] = np.exp(lg[h] * 128.0)
        gR[ps:ps + 64, h // 2] = np.exp(lg[h] * float(NT))
    mask = (j[:, None] <= j[None, :]).astype(np.float64)
    return dict(aR=aR.astype(np.float32), bR=bR.astype(np.float32), eR=eR.astype(np.float32),
                gR=gR.astype(np.float32), mask=mask.astype(np.float32))


def host_blockdiag(w):
    out = np.zeros((4, 128, 128), dtype=np.float32)
    for h in range(4):
        for n in range(32):
            out[h, 4 * n:4 * n + 4, 4 * n:4 * n + 4] = w[32 * h + n]
    return out


class PSRot:
    def __init__(self, C, banks):
        self.C = C; self.banks = banks; self.i = 0

    def __call__(self):
        b = self.C.ps[self.banks[self.i % len(self.banks)]]
        self.i += 1
        return b


def small_T(P, C, dst, src2d, rows, stage, ps):
    P.dma('act', stage[0:rows, :], src2d)
    P.tr(ps[:, 0:rows], stage[0:rows, :], C.identf[0:rows, 0:rows])
    P.copy('dve', dst, ps[:, 0:rows])


def pass_scan(P, C, NT, x_d, xprev_d, w_in_l, prm, cst, init, outs, mode="full", pfx="s"):
    full = (mode == "full")
    NW = 2568
    W = P.sb(pfx + "W", [128, 8, NW], BF16)
    load_w_cast(P, W.v, w_in_l[:, 0:NW].rearrange("(kc p) c -> p kc c", p=128))
    BD = {}
    for nm in ("bdq", "bdk", "bdv"):
        BD[nm] = P.sb(pfx + nm, [128, 4, 128], BF16)
        P.dma('pool', BD[nm].v, prm[nm].rearrange("h i o -> i h o"))
    stage = P.sb(pfx + "stage", [128, 128])
    cwT = P.sb(pfx + "cwT", [128, 16]); cbT = P.sb(pfx + "cbT", [128, 4])
    small_T(P, C, cwT.v, prm["ml_conv_w"].rearrange("k (c p) -> (k c) p", p=128), 16, stage, C.ps[7])
    small_T(P, C, cbT.v, prm["ml_conv_b"].rearrange("(c p) -> c p", p=128), 4, stage, C.ps[7])
    biB = P.sb(pfx + "biB", [128, 4]); bfB = P.sb(pfx + "bfB", [128, 4])
    bcast_rows(P, biB.v, prm["ml_bi"]); bcast_rows(P, bfB.v, prm["ml_bf"])
    aR = P.sb(pfx + "aR", [128, 4]); bR = P.sb(pfx + "bR", [128, 4]); eR = P.sb(pfx + "eR", [128, 2]); gR = P.sb(pfx + "gR", [128, 2])
    for t_, n_ in ((aR, "aR"), (bR, "bR"), (eR, "eR"), (gR, "gR")):
        P.dma('act', t_.v, cst[n_])
    mask = P.sb(pfx + "mask", [128, 128]); P.dma('act', mask.v, cst["mask"])
    maskr = P.sb(pfx + "maskr", [128, 128], F32R); P.copy('dve', maskr.v, mask.v)
    onesr = P.sb(pfx + "onesr", [128, 128], F32R); P.copy('dve', onesr.v, C.onesf.v)
    c1 = P.sb(pfx + "c1", [128, 1]); P.memset('dve', c1.v, 1.0)
    if full:
        gnR = P.sb(pfx + "gnR", [128, 512]); gnM = P.sb(pfx + "gnM", [128, 512]); skM = P.sb(pfx + "skM", [128, 512])
        bcast_rows(P, gnR.v, prm["ret_gn"]); bcast_rows(P, gnM.v, prm["ml_gn"]); bcast_rows(P, skM.v, prm["ml_skip"])
    Sret = P.sb(pfx + "Sret", [128, 2, 128]); Sretb = P.sb(pfx + "Sretb", [128, 2, 128], BF16)
    Cml = P.sb(pfx + "Cml", [128, 4, 129]); Cmlb = P.sb(pfx + "Cmlb", [128, 4, 129], BF16)
    tmpS = P.sb(pfx + "tmpS", [128, 4, 129]); tmpS2 = P.sb(pfx + "tmpS2", [128, 4, 129])
    totacc = P.sb(pfx + "totacc", [128, 4])
    P.memset('dve', Sret.v, 0.0); P.memset('dve', Cml.v, 0.0); P.memset('dve', totacc.v, 0.0)
    if init is not None:
        sel = P.sb(pfx + "sel", [128, 3]); nsel = P.sb(pfx + "nsel", [128, 3])
        P.dma('act', sel.v, init["sel"]); P.dma('act', nsel.v, init["nsel"])
        Fr = P.sb(pfx + "Fr", [128, 2, 128]); Fm = P.sb(pfx + "Fm", [128, 4, 129]); tl = P.sb(pfx + "tl", [128, 4])
        Gm = P.sb(pfx + "Gm", [128, 4])
        for q in range(3):
            P.dma('act', Fr.v, init["Fret"][q]); P.dma('act', Fm.v, init["Fml"][q]); P.dma('act', tl.v, init["totL"][q])
            P.act(Gm.v, tl.v, AF.Exp, scale=-1.0)
            for hp in range(2):
                P.act(tmpS[:, hp, 0:128], Sret[:, hp, :], AF.Copy, scale=gR[:, hp:hp + 1])
                P.tt('dve', tmpS[:, hp, 0:128], tmpS[:, hp, 0:128], Fr[:, hp, :], ALU.add)
                P.act(tmpS[:, hp, 0:128], tmpS[:, hp, 0:128], AF.Copy, scale=sel[:, q:q + 1])
                P.act(tmpS2[:, hp, 0:128], Sret[:, hp, :], AF.Copy, scale=nsel[:, q:q + 1])
                P.tt('dve', Sret[:, hp, :], tmpS[:, hp, 0:128], tmpS2[:, hp, 0:128], ALU.add)
            for h in range(4):
                P.act(tmpS[:, h, :], Cml[:, h, :], AF.Copy, scale=Gm[:, h:h + 1])
                P.tt('dve', tmpS[:, h, :], tmpS[:, h, :], Fm[:, h, :], ALU.add)
                P.act(tmpS[:, h, :], tmpS[:, h, :], AF.Copy, scale=sel[:, q:q + 1])
                P.act(tmpS2[:, h, :], Cml[:, h, :], AF.Copy, scale=nsel[:, q:q + 1])
                P.tt('dve', Cml[:, h, :], tmpS[:, h, :], tmpS2[:, h, :], ALU.add)
    P.copy('dve', Sretb.v, Sret.v); P.copy('dve', Cmlb.v, Cml.v)
    SC = 512
    xts = [P.sb(pfx + "xt%d" % i, [128, 1024]) for i in range(2)]
    xT = P.sb(pfx + "xT", [128, 8, SC], BF16)
    rqT = P.sb(pfx + "rqT", [128, 2, SC], BF16); rkT = P.sb(pfx + "rkT", [128, 2, SC], BF16)
    mxT = P.sb(pfx + "mxT", [128, 4, 3 + SC]); mxb = P.sb(pfx + "mxb", [128, 4, SC], BF16)
    cva = P.sb(pfx + "cva", [128, SC]); cvb = P.sb(pfx + "cvb", [128, SC])
    mcT = P.sb(pfx + "mcT", [128, 4, SC], BF16)
    qmT = P.sb(pfx + "qmT", [128, 4, SC], BF16); kmT = P.sb(pfx + "kmT", [128, 4, SC], BF16)
    rk_tok = P.sb(pfx + "rk_tok", [128, 256], BF16); km_tok = P.sb(pfx + "km_tok", [128, 512], BF16)
    vpR = P.sb(pfx + "vpR", [128, 4, 128], BF16); vpM = P.sb(pfx + "vpM", [128, 4, 129], BF16)
    g8 = P.sb(pfx + "g8", [128, 8]); L1 = P.sb(pfx + "L1", [128, 4], F32R); e1 = P.sb(pfx + "e1", [128, 4])
    igt = P.sb(pfx + "igt", [128, 4]); aM = P.sb(pfx + "aM", [128, 4]); bM = P.sb(pfx + "bM", [128, 4]); eM = P.sb(pfx + "eM", [128, 4])
    tmp4 = P.sb(pfx + "tmp4", [128, 4])
    Pm = [P.sb(pfx + "Pm%d" % i, [128, 128], BF16) for i in range(2)]
    ot = [P.sb(pfx + "ot%d" % i, [128, 129]) for i in range(2)]
    hh = [P.sb(pfx + "hh%d" % i, [128, 128]) for i in range(2)]
    dn = P.sb(pfx + "dn", [128, 1]); st6 = P.sb(pfx + "st6", [128, 6]); mv = P.sb(pfx + "mv", [128, 2]); rs = P.sb(pfx + "rs", [128, 1])
    if full:
        yR = P.sb(pfx + "yR", [128, 512]); yM = P.sb(pfx + "yM", [128, 512])
        rg = P.sb(pfx + "rg", [128, 512]); mo = P.sb(pfx + "mo", [128, 512]); mct = P.sb(pfx + "mct", [128, 512])
        yTo = [P.sb(pfx + "yTo%d" % i, [128, 4, 128], BF16) for i in range(2)]
        ybf = P.sb(pfx + "ybf", [128, 512], BF16)
    nps = PSRot(C, [0, 1, 2, 3, 4, 5, 6, 7])
    P.dma('sp', xts[0].v, xprev_d)
    x_transpose(P, C, xts[0].v, [(xT[:, :, 0:128], 'act')], [nps(), nps()])
    for c in range(4):
        pz = nps()
        for kc in range(8):
            P.mm(pz[:, 0:128], W[:, kc, OFF['m_x'] + c * 128: OFF['m_x'] + (c + 1) * 128], xT[:, kc, 0:128],
                 start=(kc == 0), stop=(kc == 7))
        P.copy('dve', mxT[:, c, 0:3], pz[:, 125:128])
    lnscale = float(np.log(128 ** -0.5))
    for sc in range(NT // SC):
        for tt in range(4):
            t = sc * 4 + tt
            xt = xts[t % 2]
            P.dma('sp', xt.v, x_d[t * 128:(t + 1) * 128, :])
            x_transpose(P, C, xt.v, [(xT[:, :, tt * 128:(tt + 1) * 128], 'act')], [nps(), nps()])
        for (dst, off, nch, kind) in ((rqT, OFF['r_q'], 2, 'bf'), (rkT, OFF['r_k'], 2, 'bf'), (mxT, OFF['m_x'], 4, 'mx')):
            if not full and dst is rqT:
                continue
            for c in range(nch):
                pz = nps()
                for kc in range(8):
                    P.mm(pz.v, W[:, kc, off + c * 128: off + (c + 1) * 128], xT[:, kc, :], start=(kc == 0), stop=(kc == 7))
                if kind == 'bf':
                    P.copy('act', dst[:, c, :], pz.v)
                else:
                    P.copy('act', mxT[:, c, 3:3 + SC], pz.v)
                    P.copy('dve', mxb[:, c, :], pz.v)
        for c in range(4):
            P.ts('dve', cva.v, mxT[:, c, 3:3 + SC], cwT[:, 12 + c:13 + c], cbT[:, c:c + 1], op0=ALU.mult, op1=ALU.add)
            P.stt('dve', cvb.v, mxT[:, c, 2:2 + SC], cwT[:, 8 + c:9 + c], cva.v, ALU.mult, ALU.add)
            P.stt('dve', cva.v, mxT[:, c, 1:1 + SC], cwT[:, 4 + c:5 + c], cvb.v, ALU.mult, ALU.add)
            P.stt('dve', cvb.v, mxT[:, c, 0:SC], cwT[:, c:c + 1], cva.v, ALU.mult, ALU.add)
            P.act(mcT[:, c, :], cvb.v, AF.Silu)
            P.copy('dve', cva[:, 0:3], mxT[:, c, SC:SC + 3])
            P.copy('dve', mxT[:, c, 0:3], cva[:, 0:3])
        for (dst, bd) in ((qmT, BD["bdq"]), (kmT, BD["bdk"])):
            if not full and dst is qmT:
                continue
            for h in range(4):
                pz = nps()
                P.mm(pz.v, bd[:, h, :], mcT[:, h, :])
                P.copy('act', dst[:, h, :], pz.v)
        for tt in range(4):
            t = sc * 4 + tt
            ts_ = slice(tt * 128, (tt + 1) * 128)

            def tok_proj(off, n):
                pz = nps()
                for kc in range(8):
                    P.mm(pz[:, 0:n], xT[:, kc, ts_], W[:, kc, off:off + n], start=(kc == 0), stop=(kc == 7))
                return pz
            p_rk = tok_proj(OFF['r_k'], 256)
            P.copy('act', rk_tok.v, p_rk[:, 0:256])
            p_g8 = tok_proj(OFF['m_i'], 8)
            P.copy('dve', g8.v, p_g8[:, 0:8])
            P.tt('dve', igt.v, g8[:, 0:4], biB.v, ALU.add)
            P.tt('dve', tmp4.v, g8[:, 4:8], bfB.v, ALU.add)
            P.act(e1.v, tmp4.v, AF.Exp, scale=-1.0)
            P.ts('dve', e1.v, e1.v, 1.0, None, op0=ALU.add)
            P.act(L1.v, e1.v, AF.Ln)
            pc = nps()
            P.mm(pc[:, 0:4], maskr.v, L1.v)
            P.mm(pc[:, 8:12], onesr.v, L1.v)
            P.act(aM.v, pc[:, 0:4], AF.Exp, scale=-1.0, bias=lnscale)
            P.tt('dve', tmp4.v, igt.v, pc[:, 0:4], ALU.add)
            P.act(bM.v, tmp4.v, AF.Exp)
            P.act(eM.v, pc[:, 8:12], AF.Exp, scale=-1.0)
            P.tt('dve', totacc.v, totacc.v, pc[:, 8:12], ALU.add)
            p_rv = tok_proj(OFF['r_v'], 512)
            for h in range(4):
                P.act(vpR[:, h, :], p_rv[:, h * 128:(h + 1) * 128], AF.Copy, scale=bR[:, h:h + 1])
            p_vm = nps()
            for h in range(4):
                P.mm(p_vm[:, h * 128:(h + 1) * 128], mxb[:, h, ts_], BD["bdv"][:, h, :])
            for h in range(4):
                P.act(vpM[:, h, 0:128], p_vm[:, h * 128:(h + 1) * 128], AF.Copy, scale=bM[:, h:h + 1])
            P.copy('dve', vpM[:, :, 128:129], bM.v.re("p (h o) -> p h o", o=1))
            p_km = nps()
            for h in range(4):
                P.mm(p_km[:, h * 128:(h + 1) * 128], mcT[:, h, ts_], BD["bdk"][:, h, :])
            P.copy('act', km_tok.v, p_km.v)
            if full:
                p_rg = tok_proj(OFF['r_g'], 512)
                P.act(rg.v, p_rg.v, AF.Silu)
                p_mo = tok_proj(OFF['m_o'], 512)
                P.act(mo.v, p_mo.v, AF.Sigmoid)
                p_mc = nps()
                pmb = p_mc.v.bitcast(BF16)
                for c in range(4):
                    P.tr(pmb[:, c * 128:(c + 1) * 128], mcT[:, c, ts_], C.identb.v)
                P.tt('dve', mct.v, pmb[:, 0:512], skM.v, ALU.mult)
            kq = 0
            for h in range(4):
                psl = slice((h % 2) * 64, (h % 2) * 64 + 64); hp = h // 2
                if full:
                    p_st = nps()
                    P.mm(p_st[:, 0:128], rkT[psl, hp, ts_], rqT[psl, hp, ts_])
                    pm = Pm[kq % 2]; o = ot[kq % 2]; hx = hh[kq % 2]; kq += 1
                    P.tt('dve', pm.v, p_st[:, 0:128], mask.v, ALU.mult)
                    p_o = nps()
                    P.mm(p_o[:, 0:128], pm.v, vpR[:, h, :], start=True, stop=False)
                    P.mm(p_o[:, 0:128], rqT[psl, hp, ts_], Sretb[psl, hp, :], start=False, stop=True)
                    P.act(o[:, 0:128], p_o[:, 0:128], AF.Copy, scale=aR[:, h:h + 1])
                    head_norm(P, o[:, 0:128], hx.v, st6, mv, rs)
                    P.tt('dve', hx.v, hx.v, gnR[:, h * 128:(h + 1) * 128], ALU.mult)
                    P.tt('dve', yR[:, h * 128:(h + 1) * 128], hx.v, rg[:, h * 128:(h + 1) * 128], ALU.mult)
                p_kv = nps()
                P.mm(p_kv[:, 0:128], rk_tok[:, hp * 128:(hp + 1) * 128], vpR[:, h, :])
                P.tt('dve', tmpS[psl, hp, 0:128], Sret[psl, hp, :], p_kv[psl, 0:128], ALU.add)
                P.act(Sret[psl, hp, :], tmpS[psl, hp, 0:128], AF.Copy, scale=eR[psl, hp:hp + 1])
                P.copy('dve', Sretb[psl, hp, :], Sret[psl, hp, :])
            for h in range(4):
                if full:
                    p_st = nps()
                    P.mm(p_st[:, 0:128], kmT[:, h, ts_], qmT[:, h, ts_])
                    pm = Pm[kq % 2]; o = ot[kq % 2]; hx = hh[kq % 2]; kq += 1
                    P.tt('dve', pm.v, p_st[:, 0:128], mask.v, ALU.mult)
                    p_o = nps()
                    P.mm(p_o[:, 0:129], pm.v, vpM[:, h, :], start=True, stop=False)
                    P.mm(p_o[:, 0:129], qmT[:, h, ts_], Cmlb[:, h, :], start=False, stop=True)
                    P.act(o.v, p_o[:, 0:129], AF.Copy, scale=aM[:, h:h + 1])
                    P.act(dn.v, o[:, 128:129], AF.Abs)
                    P.ts('dve', dn.v, dn.v, 1.0, None, op0=ALU.max)
                    a_ = dn.v.ap
                    P.I('dve', (lambda a_: (lambda e: e.reciprocal(a_, a_)))(a_), w=[dn], r=[dn])
                    P.act(o[:, 0:128], o[:, 0:128], AF.Copy, scale=dn.v)
                    head_norm(P, o[:, 0:128], hx.v, st6, mv, rs)
                    P.tt('dve', hx.v, hx.v, gnM[:, h * 128:(h + 1) * 128], ALU.mult)
                    P.tt('dve', hx.v, hx.v, mct[:, h * 128:(h + 1) * 128], ALU.add)
                    P.tt('dve', yM[:, h * 128:(h + 1) * 128], hx.v, mo[:, h * 128:(h + 1) * 128], ALU.mult)
                p_kv = nps()
                P.mm(p_kv[:, 0:129], km_tok[:, h * 128:(h + 1) * 128], vpM[:, h, :])
                P.tt('dve', tmpS[:, h, :], Cml[:, h, :], p_kv[:, 0:129], ALU.add)
                P.act(Cml[:, h, :], tmpS[:, h, :], AF.Copy, scale=eM[:, h:h + 1])
                P.copy('dve', Cmlb[:, h, :], Cml[:, h, :])
            if full:
                for (ysrc, ydst) in ((yR, outs[0]), (yM, outs[1])):
                    P.copy('act', ybf.v, ysrc.v)
                    pz = nps(); pzb = pz.v.bitcast(BF16)
                    for c in range(4):
                        P.tr(pzb[:, c * 128:(c + 1) * 128], ybf[:, c * 128:(c + 1) * 128], C.identb.v)
                    yo = yTo[kq % 2]; kq += 1
                    P.copy('dve', yo.v, pzb[:, 0:512].re("p (c t) -> p c t", c=4))
                    P.dma('sp', ydst[:, :, t * 128:(t + 1) * 128], yo.v)
    if not full:
        P.dma('sp', outs[0].v, Sret.v)
        P.dma('sp', outs[1].v, Cml.v)
        P.dma('sp', outs[2].v, totacc.v)


def head_norm(P, src, dst, st6, mv, rs):
    a, b = st6.v.ap, src.ap
    P.I('dve', lambda e: e.bn_stats(a, b), w=[st6], r=[src])
    a2, b2 = mv.v.ap, st6.v.ap
    P.I('dve', lambda e: e.bn_aggr(a2, b2), w=[mv], r=[st6])
    P.ts('dve', rs.v, mv[:, 1:2], EPS, None, op0=ALU.add)
    P.act(rs.v, rs.v, AF.Ln)
    P.act(rs.v, rs.v, AF.Exp, scale=-0.5)
    P.ts('dve', dst, src, mv[:, 0:1], rs.v, op0=ALU.subtract, op1=ALU.mult)


ATT_PAT = ((128, 1), (512, 4), (2048, 16))
HALO = 2048


def host_consts_attn():
    slopes = np.exp2(-8.0 * np.arange(1, 13, dtype=np.float64) / 12.0).reshape(3, 4)
    s = np.arange(128)[:, None]; i = np.arange(128)[None, :]
    out = np.zeros((3, 2, 128, 4, 128), dtype=np.float32)
    for g, (win, d) in enumerate(ATT_PAT):
        for h in range(4):
            dcur = i - s
            b = np.where((dcur >= 0), -slopes[g, h] * d * dcur, -30000.0)
            out[g, 1, :, h, :] = b
            dprev = i + 128 - s
            b = np.where((dprev <= 128), -slopes[g, h] * d * dprev, -30000.0)
            out[g, 0, :, h, :] = b
    return out.reshape(3, 2, 128, 512)


def pass_attn(P, C, NT, xext_d, w_in_l, bias_d, hv_d, yT_out, pfx="a", dbg=0):
    accN = P.sb(pfx + "accN", [128, 4, NT]); accD = P.sb(pfx + "accD", [128, 4, NT])
    for h_ in range(4):
        for j_ in range(NT // 2048):
            P.memset('dve', accN[:, h_, j_ * 2048:(j_ + 1) * 2048], 0.0 if dbg == 0 else 1.0)
            P.memset('dve', accD[:, h_, j_ * 2048:(j_ + 1) * 2048], 0.0 if dbg == 0 else 2.0)
    hv0 = P.sb(pfx + "hv0", [128, 128]); hvb = P.sb(pfx + "hvb", [128, 128], BF16)
    P.dma('act', hv0.v, hv_d); P.copy('dve', hvb.v, hv0.v)
    Wq = P.sb(pfx + "Wq", [128, 8, 256], BF16); Wk = P.sb(pfx + "Wk", [128, 8, 256], BF16); Wv = P.sb(pfx + "Wv", [128, 8, 512], BF16)
    bT = [P.sb(pfx + "bT%d" % i, [128, 512]) for i in range(2)]
    xts = [P.sb(pfx + "xt%d" % i, [128, 1024]) for i in range(2)]
    xTb = [P.sb(pfx + "xTb%d" % i, [128, 8, 128], BF16) for i in range(2)]
    kT = [P.sb(pfx + "kT%d" % i, [128, 2, 128], BF16) for i in range(2)]
    Vt = [P.sb(pfx + "V%d" % i, [128, 512], BF16) for i in range(2)]
    qT = [P.sb(pfx + "qT%d" % i, [128, 2, 128], BF16) for i in range(2)]
    tmp = [P.sb(pfx + "tmp%d" % i, [128, 512]) for i in range(2)]
    PT = [[P.sb(pfx + "PT%d_%d" % (i, j), [128, 512], BF16) for j in range(2)] for i in range(2)]
    nps = PSRot(C, [0, 1, 2, 3, 4, 5, 6, 7])
    win = w_in_l.rearrange("(kc p) c -> p kc c", p=128)
    nb = 0
    for g, (_, d) in enumerate(ATT_PAT if dbg not in (1, 2, 4, 5, 6) else (ATT_PAT[:1] if dbg in (2, 4, 5, 6) else ())):
        load_w_cast(P, Wq.v, win[:, :, OFF['a_q'] + g * 256: OFF['a_q'] + (g + 1) * 256])
        load_w_cast(P, Wk.v, win[:, :, OFF['a_k'] + g * 256: OFF['a_k'] + (g + 1) * 256])
        load_w_cast(P, Wv.v, win[:, :, OFF['a_v'] + g * 512: OFF['a_v'] + (g + 1) * 512])
        P.dma('act', bT[0].v, bias_d[g, 0]); P.dma('act', bT[1].v, bias_d[g, 1])
        NB = NT // (128 * d)
        for r in range(d):
            for m in range(-1, NB):
                cur = nb % 2; prv = 1 - cur; nb += 1
                s0 = HALO + m * 128 * d + r
                xt = xts[cur]
                P.dma('sp', xt.v, xext_d[s0: s0 + 127 * d + 1: d, :])
                x_transpose(P, C, xt.v, [(xTb[cur].v, 'act')], [nps(), nps()])
                xb = xTb[cur]
                for c in range(2):
                    pz = nps()
                    for kc in range(8):
                        P.mm(pz[:, 0:128], Wk[:, kc, c * 128:(c + 1) * 128], xb[:, kc, :], start=(kc == 0), stop=(kc == 7))
                    P.copy('act', kT[cur][:, c, :], pz[:, 0:128])
                pz = nps()
                for kc in range(8):
                    P.mm(pz.v, xb[:, kc, :], Wv[:, kc, :], start=(kc == 0), stop=(kc == 7))
                P.copy('act', Vt[cur].v, pz.v)
                if m < 0 or dbg == 4:
                    continue
                for c in range(2):
                    pz = nps()
                    for kc in range(8):
                        P.mm(pz[:, 0:128], Wq[:, kc, c * 128:(c + 1) * 128], xb[:, kc, :], start=(kc == 0), stop=(kc == 7))
                    P.copy('act', qT[cur][:, c, :], pz[:, 0:128])
                for pc, kb in ((0, prv), (1, cur)):
                    psAB = [nps(), nps()]
                    for h in range(4):
                        psl = slice((h % 2) * 64, (h % 2) * 64 + 64); hp = h // 2
                        P.mm(psAB[h % 2][:, hp * 128:(hp + 1) * 128], kT[kb][psl, hp, :], qT[cur][psl, hp, :])
                    tm = tmp[pc]
                    for h in range(4):
                        hs = slice(h * 128, (h + 1) * 128); hp = h // 2
                        P.stt('dve', tm[:, hs], psAB[h % 2][:, hp * 128:(hp + 1) * 128], 0.125, bT[pc][:, hs], ALU.mult, ALU.add)
                    P.act(PT[cur][pc].v, tm.v, AF.Exp)
                if dbg in (5, 6):
                    continue
                pn = nps()
                for h in range(4):
                    hs = slice(h * 128, (h + 1) * 128)
                    P.mm(pn[:, hs], Vt[prv][:, hs], PT[cur][0][:, hs], start=True, stop=False)
                    P.mm(pn[:, hs], Vt[cur][:, hs], PT[cur][1][:, hs], start=False, stop=True)
                pd = nps()
                P.mm(pd.v, (hvb.v if m == 0 else C.onesb.v), PT[cur][0].v, start=True, stop=False)
                P.mm(pd.v, C.onesb.v, PT[cur][1].v, start=False, stop=True)
                t0 = m * 128 * d + r
                for h in range(4 if dbg != 3 else 0):
                    hs = slice(h * 128, (h + 1) * 128)
                    av = accN[:, h, t0: t0 + 127 * d + 1: d]
                    P.tt('dve', av, av, pn[:, hs], ALU.add)
                    dv = accD[:, h, t0: t0 + 127 * d + 1: d]
                    P.tt('dve', dv, dv, pd[:, hs], ALU.add)
    yo = [P.sb(pfx + "yo%d" % i, [128, 4, 512], BF16) for i in range(2)]
    rc = [P.sb(pfx + "rc%d" % i, [128, 4, 512]) for i in range(2)]
    for j in range(NT // 512):
        sl = slice(j * 512, (j + 1) * 512)
        for h in range(4):
            a_, b_ = rc[j % 2][:, h, :].ap, accD[:, h, sl].ap
            P.I('dve', (lambda a_, b_: (lambda e: e.reciprocal(a_, b_)))(a_, b_), w=[rc[j % 2]], r=[accD])
            P.tt('dve', yo[j % 2][:, h, :], accN[:, h, sl], rc[j % 2][:, h, :], ALU.mult)
        P.dma('sp', yT_out[:, :, sl], yo[j % 2].v)


NCORES = 8
NT_CORE = 4096
PRM_NAMES = ("ret_gn", "ml_conv_w", "ml_conv_b", "bdq", "bdk", "bdv", "ml_bi", "ml_bf", "ml_gn", "ml_skip")
PRM_SHAPES = dict(ret_gn=[512], ml_conv_w=[4, 512], ml_conv_b=[512], bdq=[4, 128, 128], bdk=[4, 128, 128], bdv=[4, 128, 128],
                  ml_bi=[4], ml_bf=[4], ml_gn=[512], ml_skip=[512])
CST_SHAPES = dict(aR=[128, 4], bR=[128, 4], eR=[128, 2], gR=[128, 2], mask=[128, 128])


def _scan_inputs(nc):
    def inp(n, sh):
        return nc.dram_tensor(n, sh, F32, kind="ExternalInput").ap()
    prm = {n: inp(n, PRM_SHAPES[n]) for n in PRM_NAMES}
    cst = {n: inp(n, CST_SHAPES[n]) for n in CST_SHAPES}
    return prm, cst


def build_A(NT=NT_CORE):
    nc = bass.Bass("TRN2", target_bir_lowering=False)
    with ExitStack() as es:
        P = Prog(nc, es)
        x_d = P.dram("x", [NT, D], F32, kind="ExternalInput")
        xprev = P.dram("xprev", [128, D], F32, kind="ExternalInput")
        w_in = nc.dram_tensor("w_in", [D, D_IN], F32, kind="ExternalInput").ap()
        prm, cst = _scan_inputs(nc)
        outs = [P.dram("oS", [128, 2, 128], F32, kind="ExternalOutput"), P.dram("oC", [128, 4, 129], F32, kind="ExternalOutput"),
                P.dram("oT", [128, 4], F32, kind="ExternalOutput")]
        C = make_ctx(P)
        pass_scan(P, C, NT, x_d, xprev, w_in, prm, cst, None, outs, mode="summary", pfx="s")
        P.wait_all('sp', outs)
        P.emit()
    return nc


def build_B(NT=NT_CORE, NE=NEXP):
    nc = bass.Bass("TRN2", target_bir_lowering=False)
    with ExitStack() as es:
        P = Prog(nc, es)

        def inp(n, sh):
            return nc.dram_tensor(n, sh, F32, kind="ExternalInput").ap()
        xext = P.dram("xext", [HALO + NT, D], F32, kind="ExternalInput")
        w_in = inp("w_in", [D, D_IN])
        prm, cst = _scan_inputs(nc)
        init = dict(Fret=inp("Fret", [3, 128, 2, 128]), Fml=inp("Fml", [3, 128, 4, 129]), totL=inp("totL", [3, 128, 4]),
                    sel=inp("sel", [128, 3]), nsel=inp("nsel", [128, 3]))
        abias = inp("abias", [3, 2, 128, 512]); hv = inp("hv", [128, 128])
        w_br = inp("w_branch", [3, MIXW, D]); w_out = inp("w_out", [D, D])
        ln1_g = inp("ln1_g", [D]); ln1_b = inp("ln1_b", [D]); ln2_g = inp("ln2_g", [D]); ln2_b = inp("ln2_b", [D])
        w_r = inp("w_router", [D, NE]); b_r = inp("b_router", [NE])
        w_g = inp("w_gate", [NE, D, D]); b_g = inp("b_gate", [NE, D])
        w_u = inp("w_up", [NE, D, D]); b_u = inp("b_up", [NE, D])
        w_d = inp("w_down", [NE, D, D]); b_d = inp("b_down", [NE, D])
        out = P.dram("out", [NT, D], F32, kind="ExternalOutput")
        yT = [P.dram("yT%d" % b, [128, 4, NT], BF16) for b in range(3)]
        x1s = P.dram("x1s", [NT, D], F32)
        C = make_ctx(P)
        x_own = Tile(P, xext.h[HALO:HALO + NT, :], "xown", "dram")
        x_own.lw, x_own.rd = xext.lw, xext.rd
        xprev = Tile(P, xext.h[HALO - 128:HALO, :], "xprv", "dram")
        xprev.lw, xprev.rd = xext.lw, xext.rd
        with P.scope():
            pass_attn(P, C, NT, xext, w_in, abias, hv, yT[2], pfx="a")
        with P.scope():
            pass_scan(P, C, NT, x_own, xprev, w_in, prm, cst, init, [yT[0], yT[1]], mode="full", pfx="s")
        with P.scope():
            pass_merge(P, C, NT, x_own, yT, w_in, w_br, w_out, ln1_g, ln1_b, x1s, pfx="m")
        NBLK = NT * 4 // 128 + NE
        Xs = P.dram("Xs", [NBLK * 128, D], BF16); Ys = P.dram("Ys", [NBLK * 128, D], F32)
        mc = {k: inp("mc_" + k, list(v.shape)) for k, v in host_consts_moe(NT, NE).items()}
        with P.scope():
            pass_moe2(P, C, NT, x1s, w_r, b_r, w_g, b_g, w_u, b_u, w_d, b_d, ln2_g, ln2_b, out, Xs, Ys, mc, NE=NE, pfx="f")
        P.wait_all('sp', [out])
        P.emit()
    return nc


def layer_params(inputs, l):
    g = lambda n: np.ascontiguousarray(np.asarray(inputs[n], dtype=np.float32)[l])
    prm = dict(ret_gn=g("ret_gn"), ml_conv_w=g("ml_conv_w"), ml_conv_b=g("ml_conv_b"),
               bdq=host_blockdiag(g("ml_wq")), bdk=host_blockdiag(g("ml_wk")), bdv=host_blockdiag(g("ml_wv")),
               ml_bi=g("ml_bi"), ml_bf=g("ml_bf"), ml_gn=g("ml_gn"), ml_skip=g("ml_skip"))
    big = dict(w_in=g("w_in"), w_branch=g("w_branch"), w_out=g("w_out"), ln1_g=g("ln1_g"), ln1_b=g("ln1_b"),
               ln2_g=g("ln2_g"), ln2_b=g("ln2_b"), w_router=g("w_router"), b_router=g("b_router"),
               w_gate=g("w_gate"), b_gate=g("b_gate"), w_up=g("w_up"), b_up=g("b_up"), w_down=g("w_down"), b_down=g("b_down"))
    return prm, big


def run_layer(ncA, ncB, xs, prm, big, cst, abias, QPB, NT):
    n = len(xs)
    zeros128 = np.zeros((128, D), np.float32)
    inA = []
    for c in range(n):
        q = c % QPB
        m = dict(prm); m.update(cst)
        m["w_in"] = big["w_in"]; m["x"] = xs[c]
        m["xprev"] = np.ascontiguousarray(xs[c - 1][-128:]) if q > 0 else zeros128
        inA.append(m)
    resA = run_bass_kernel_spmd(ncA, inA, core_ids=list(range(n))).results
    inB = []
    for c in range(n):
        q = c % QPB; b0 = c - q
        m = dict(prm); m.update(cst); m.update(big)
        halo = xs[c - 1][-HALO:] if q > 0 else np.zeros((HALO, D), np.float32)
        m["xext"] = np.ascontiguousarray(np.concatenate([halo, xs[c]], axis=0))
        Fret = np.zeros((3, 128, 2, 128), np.float32); Fml = np.zeros((3, 128, 4, 129), np.float32)
        totL = np.zeros((3, 128, 4), np.float32); sel = np.zeros((128, 3), np.float32)
        for qq in range(min(3, QPB)):
            Fret[qq] = resA[b0 + qq]["oS"]; Fml[qq] = resA[b0 + qq]["oC"]; totL[qq] = resA[b0 + qq]["oT"]
            if qq < q:
                sel[:, qq] = 1.0
        m.update(Fret=Fret, Fml=Fml, totL=totL, sel=sel, nsel=(1.0 - sel).astype(np.float32))
        m["abias"] = abias
        for k_, v_ in host_consts_moe(NT, big["w_router"].shape[1]).items():
            m["mc_" + k_] = v_
        m["hv"] = (np.ones((128, 128), np.float32) if q > 0 else np.zeros((128, 128), np.float32))
        inB.append(m)
    resB = run_bass_kernel_spmd(ncB, inB, core_ids=list(range(n))).results
    return [np.asarray(r["out"], dtype=np.float32) for r in resB]


def kernel(**inputs):
    x = np.asarray(inputs["x"], dtype=np.float32)
    B, S, _ = x.shape
    QPB = NCORES // B
    NT = S // QPB
    xs = [np.ascontiguousarray(x[c // QPB, (c % QPB) * NT:(c % QPB + 1) * NT]) for c in range(NCORES)]
    ncA = build_A(NT); ncB = build_B(NT)
    cst = host_consts_scan(NT); abias = host_consts_attn()
    L = np.asarray(inputs["w_in"]).shape[0]
    for l in range(L):
        prm, big = layer_params(inputs, l)
        xs = run_layer(ncA, ncB, xs, prm, big, cst, abias, QPB, NT)
    out = np.zeros((B, S, D), np.float32)
    for c in range(NCORES):
        out[c // QPB, (c % QPB) * NT:(c % QPB + 1) * NT] = xs[c]
    return out


BIGIDX = 4000000.0


def host_consts_moe(NT, NE=NEXP):
    NBLK = NT * 4 // 128 + NE
    p = np.arange(128, dtype=np.float32)
    d = dict(iota_p=p.reshape(128, 1).copy(),
             ustrict=(p[:, None] < p[None, :]).astype(np.float32),
             iotaJ=np.tile(np.arange(33, dtype=np.float32)[None, :], (128, 1)),
             iotaB=np.tile(np.arange(NBLK, dtype=np.float32)[None, :], (128, 1)))
    return d


def _breg(P, e, bound):
    if not hasattr(P, "_bregs"):
        P._bregs = {}
    if bound not in P._bregs:
        P._bregs[bound] = e.to_reg(int(bound))
    return P._bregs[bound]


def ind_dma(P, dst_v, src_tile, src_ap, idx_v, bound, scatter=False, extra_r=()):
    sb_tile = dst_v.tile
    key = P._tsem(sb_tile)
    d_ap, i_ap = dst_v.ap, idx_v.ap
    if not scatter:
        rt = [src_tile, idx_v.tile] + list(extra_r); wt = [sb_tile]

        def f(e):
            try:
                return e.indirect_dma_start(d_ap, None, src_ap, bass.IndirectOffsetOnAxis(i_ap, 0), bounds_check=_breg(P, e, bound), oob_is_err=False)
            except Exception:
                print("IND GATHER FAIL", d_ap, src_ap, i_ap, bound)
                raise
    else:
        rt = [sb_tile, idx_v.tile] + list(extra_r); wt = [src_tile]

        def f(e):
            try:
                return e.indirect_dma_start(src_ap, bass.IndirectOffsetOnAxis(i_ap, 0), d_ap, None, bounds_check=_breg(P, e, bound), oob_is_err=False)
            except Exception:
                print("IND SCATTER FAIL", d_ap, src_ap, i_ap, bound)
                raise
    waits = P._waits('pool', rt, wt)
    sb_tile.dcnt += 16
    P.stream['pool'].append((waits, f, (key, 16)))
    P._record((key, sb_tile.dcnt), rt, wt)
    P.ninst += 1


def pass_moe2(P, C, NT, x1_d, w_router, b_router, w_gate, b_gate, w_up, b_up, w_down, b_down, ln_g, ln_b, out_d,
              Xs, Ys, mc, NE=NEXP, pfx="f"):
    NTL = NT // 128
    NBLK = NT * 4 // 128 + NE
    NSLOT = NBLK * 128
    gB = P.sb(pfx + "gB", [128, 1024]); bB = P.sb(pfx + "bB", [128, 1024])
    bcast_rows(P, gB.v, ln_g); bcast_rows(P, bB.v, ln_b)
    wr = P.sb(pfx + "wr", [128, 8, NE], F32R); wr0 = P.sb(pfx + "wr0", [128, 8, NE])
    P.dma('act', wr0.v, w_router.rearrange("(kc p) e -> p kc e", p=128)); P.copy('dve', wr.v, wr0.v)
    brB = P.sb(pfx + "brB", [128, NE]); bcast_rows(P, brB.v, b_router)
    c1 = P.sb(pfx + "c1", [128, 1]); P.memset('dve', c1.v, 1.0)
    iop = P.sb(pfx + "iop", [128, 1]); P.dma('act', iop.v, mc["iota_p"])
    us0 = P.sb(pfx + "us0", [128, 128]); P.dma('act', us0.v, mc["ustrict"])
    usb = P.sb(pfx + "usb", [128, 128], BF16); P.copy('dve', usb.v, us0.v)
    ioJ = P.sb(pfx + "ioJ", [128, 33]); P.dma('act', ioJ.v, mc["iotaJ"])
    ioB = P.sb(pfx + "ioB", [128, NBLK]); P.dma('act', ioB.v, mc["iotaB"])
    lg_all = P.sb(pfx + "lg_all", [128, NTL, NE]); t8_all = P.sb(pfx + "t8_all", [128, NTL, 8])
    pall = P.sb(pfx + "pall", [128, NTL, NE]); pos_all = P.sb(pfx + "pos_all", [128, NTL, NE])
    slot_f = P.sb(pfx + "slot_f", [128, NTL, 4]); slot_i = P.sb(pfx + "slot_i", [128, NTL * 4], I32)
    pk_all = P.sb(pfx + "pk_all", [128, NTL, 4])
    carry = P.sb(pfx + "carry", [128, NE]); P.memset('dve', carry.v, 0.0)
    xts = [P.sb(pfx + "xt%d" % i, [128, 1024]) for i in range(2)]
    xT32 = P.sb(pfx + "xT32", [128, 8, 128], F32R)
    msk = P.sb(pfx + "msk", [128, NE]); mskb = P.sb(pfx + "mskb", [128, NE], BF16)
    ex = P.sb(pfx + "ex", [128, NE]); sm = P.sb(pfx + "sm", [128, 1]); nmx = P.sb(pfx + "nmx", [128, 1])
    nps = PSRot(C, [0, 1, 2, 3, 4, 5, 6, 7])
    for t in range(NTL):
        xt = xts[t % 2]
        P.dma('sp', xt.v, x1_d[t * 128:(t + 1) * 128, :])
        x_transpose(P, C, xt.v, [(xT32.v, 'dve')], [nps(), nps()])
        pl = nps()
        for kc in range(8):
            P.mm(pl[:, 0:NE], xT32[:, kc, :], wr[:, kc, :], start=(kc == 0), stop=(kc == 7))
        lg = lg_all[:, t, :]; t8 = t8_all[:, t, :]
        P.tt('dve', lg, pl[:, 0:NE], brB.v, ALU.add)
        a, b = t8.ap, lg.ap
        P.I('dve', (lambda a, b: (lambda e: e.max(out=a, in_=b)))(a, b), w=[t8_all], r=[lg_all])
        P.ts('dve', msk.v, lg, t8_all[:, t, 3:4], c1.v, op0=ALU.is_ge, op1=ALU.mult)
        P.ts('dve', nmx.v, t8_all[:, t, 0:1], -1.0, None, op0=ALU.mult)
        P.act(ex.v, lg, AF.Exp, bias=nmx.v)
        P.tt('dve', ex.v, ex.v, msk.v, ALU.mult)
        a2, b2 = sm.v.ap, ex.v.ap
        P.I('dve', (lambda a, b: (lambda e: e.reduce_sum(a, b, AX.X)))(a2, b2), w=[sm], r=[ex])
        a3 = sm.v.ap
        P.I('dve', (lambda a: (lambda e: e.reciprocal(a, a)))(a3), w=[sm], r=[sm])
        P.ts('dve', pall[:, t, :], ex.v, sm.v, c1.v, op0=ALU.mult, op1=ALU.mult)
        P.copy('dve', mskb.v, msk.v)
        pr = nps()
        P.mm(pr[:, 0:NE], usb.v, mskb.v)
        P.mm(pr[:, 64:64 + NE], C.onesb.v, mskb.v)
        P.tt('dve', pos_all[:, t, :], carry.v, pr[:, 0:NE], ALU.add)
        P.tt('dve', carry.v, carry.v, pr[:, 64:64 + NE], ALU.add)
    q = P.sb(pfx + "q", [128, NE]); nb = P.sb(pfx + "nb", [128, NE]); bend = P.sb(pfx + "bend", [128, NE])
    pstart = P.sb(pfx + "pstart", [128, NE]); t33 = P.sb(pfx + "t33", [128, 33])
    P.ts('dve', q.v, carry.v, 1.0 / 128.0, None, op0=ALU.mult)
    for e in range(NE):
        P.ts('dve', t33.v, ioJ.v, q[:, e:e + 1], c1.v, op0=ALU.is_lt, op1=ALU.mult)
        a_, b_ = nb[:, e:e + 1].ap, t33.v.ap
        P.I('dve', (lambda a, b: (lambda e_: e_.reduce_sum(a, b, AX.X)))(a_, b_), w=[nb], r=[t33])
    P.copy('dve', bend[:, 0:1], nb[:, 0:1])
    for e in range(1, NE):
        P.tt('dve', bend[:, e:e + 1], bend[:, e - 1:e], nb[:, e:e + 1], ALU.add)
    P.tt('dve', pstart.v, bend.v, nb.v, ALU.subtract)
    P.ts('dve', pstart.v, pstart.v, 128.0, None, op0=ALU.mult)
    Eall = P.sb(pfx + "Eall", [128, NBLK]); tB = P.sb(pfx + "tB", [128, NBLK]); sk = P.sb(pfx + "sk", [128, NBLK])
    P.memset('dve', Eall.v, 0.0)
    for e in range(NE):
        P.ts('dve', tB.v, ioB.v, bend[:, e:e + 1], c1.v, op0=ALU.is_ge, op1=ALU.mult)
        P.tt('dve', Eall.v, Eall.v, tB.v, ALU.add)
    P.ts('dve', Eall.v, Eall.v, float(NE - 1), None, op0=ALU.min)
    P.memset('dve', sk.v, 0.0)
    P.tt('dve', sk[:, 2:NBLK], Eall[:, 2:NBLK], Eall[:, 0:NBLK - 2], ALU.is_equal)
    P.ts('dve', sk.v, sk.v, BIGIDX, None, op0=ALU.mult)
    idxWf = P.sb(pfx + "idxWf", [128, NBLK]); idxW = P.sb(pfx + "idxW", [128, 8 * NBLK], I32)
    idxBf = P.sb(pfx + "idxBf", [128, NBLK]); idxB = P.sb(pfx + "idxB", [128, NBLK], I32)
    P.tt('dve', idxBf.v, Eall.v, sk.v, ALU.add)
    P.copy('dve', idxB.v, idxBf.v)
    P.ts('dve', idxWf.v, Eall.v, 1024.0, None, op0=ALU.mult)
    P.tt('dve', idxWf.v, idxWf.v, sk.v, ALU.add)
    P.ts('dve', idxWf.v, idxWf.v, iop.v, c1.v, op0=ALU.add, op1=ALU.mult)
    for kc in range(8):
        P.ts('dve', tB.v, idxWf.v, float(kc * 128), None, op0=ALU.add)
        P.copy('dve', idxW[:, kc * NBLK:(kc + 1) * NBLK], tB.v)
    xb16 = [P.sb(pfx + "xb16_%d" % i, [128, 1024], BF16) for i in range(2)]
    tA = P.sb(pfx + "tA", [128, NE]); oh = P.sb(pfx + "oh", [128, NE]); pr1 = P.sb(pfx + "pr1", [128, NE])
    Xs2 = Xs.h[:]
    for t in range(NTL):
        xt = xts[t % 2]; xb = xb16[t % 2]
        P.dma('sp', xt.v, x1_d[t * 128:(t + 1) * 128, :])
        P.copy('act', xb.v, xt.v)
        P.tt('dve', tA.v, pos_all[:, t, :], pstart.v, ALU.add)
        for k in range(4):
            P.ts('dve', oh.v, lg_all[:, t, :], t8_all[:, t, k:k + 1], c1.v, op0=ALU.is_equal, op1=ALU.mult)
            P.tt('dve', pr1.v, oh.v, tA.v, ALU.mult)
            a_, b_ = slot_f[:, t, k:k + 1].ap, pr1.v.ap
            P.I('dve', (lambda a, b: (lambda e_: e_.reduce_sum(a, b, AX.X)))(a_, b_), w=[slot_f], r=[pr1])
            P.tt('dve', pr1.v, oh.v, pall[:, t, :], ALU.mult)
            a_, b_ = pk_all[:, t, k:k + 1].ap, pr1.v.ap
            P.I('dve', (lambda a, b: (lambda e_: e_.reduce_sum(a, b, AX.X)))(a_, b_), w=[pk_all], r=[pr1])
        P.copy('dve', slot_i[:, t * 4:(t + 1) * 4], slot_f[:, t, :])
        for k in range(4):
            ind_dma(P, xb.v, Xs, Xs2, slot_i[:, t * 4 + k: t * 4 + k + 1], NSLOT - 1, scatter=True)
    with P.scope():
        inv128 = P.sb(pfx + 'inv128', [128, 128], BF16); P.memset('dve', inv128.v, 1.0 / 128.0)
        Wt = [[P.sb(pfx + "W%d_%d" % (j, i), [128, 8, 1024], BF16) for j in range(3)] for i in range(2)]
        Bt = [[P.sb(pfx + "B%d_%d" % (j, i), [128, 1024], BF16) for j in range(3)] for i in range(2)]
        wsrc = [w_gate.rearrange("e d f -> (e d) f"), w_up.rearrange("e d f -> (e d) f"), w_down.rearrange("e d f -> (e d) f")]
        bsrc = [b_gate, b_up, b_down]
        wtile = [Tile(P, None, pfx + "wsrc%d" % j, "dram") for j in range(3)]
        xblk = [P.sb(pfx + "xblk%d" % i, [128, 1024], BF16) for i in range(2)]
        xbT = [P.sb(pfx + "xbT%d" % i, [128, 8, 128], BF16) for i in range(2)]
        actT = [P.sb(pfx + "actT%d" % i, [128, 8, 128], BF16) for i in range(2)]
        gt = [P.sb(pfx + "g%d" % i, [128, 128]) for i in range(2)]
        s_t = [P.sb(pfx + "s%d" % i, [128, 128]) for i in range(2)]
        ut = [P.sb(pfx + "u%d" % i, [128, 128]) for i in range(2)]
        yb = [P.sb(pfx + "yb%d" % i, [128, 1024]) for i in range(2)]
        kk = 0
        for b in range(NBLK):
            cur = b % 2
            for j in range(3):
                for kc in range(8):
                    ind_dma(P, Wt[cur][j][:, kc, :], wtile[j], wsrc[j], idxW[:, kc * NBLK + b: kc * NBLK + b + 1], NE * 1024 - 1)
                ind_dma(P, Bt[cur][j].v, wtile[j], bsrc[j], idxB[:, b:b + 1], NE - 1)
            Wg, Wu, Wd = Wt[cur]; Bg, Bu, Bd = Bt[cur]
            xk = xblk[cur]
            P.dma('sp', xk.v, Xs[b * 128:(b + 1) * 128, :])
            xT_ = xbT[cur]
            for half in range(2):
                pz = nps(); pzb = pz.v.bitcast(BF16)
                for c in range(4):
                    kc = half * 4 + c
                    P.tr(pzb[:, c * 128:(c + 1) * 128], xk[:, kc * 128:(kc + 1) * 128], C.identb.v)
                P.copy('act', xT_[:, half * 4:(half + 1) * 4, :], pzb[:, 0:512].re("p (c t) -> p c t", c=4))
            aT = actT[cur]
            for fc in range(8):
                pgu = nps()
                fs = slice(fc * 128, (fc + 1) * 128)
                for kc in range(8):
                    P.mm(pgu[:, 0:128], Wg[:, kc, fs], xT_[:, kc, :], start=(kc == 0), stop=False)
                P.mm(pgu[:, 0:128], Bg[:, fs], inv128.v, start=False, stop=True)
                for kc in range(8):
                    P.mm(pgu[:, 128:256], Wu[:, kc, fs], xT_[:, kc, :], start=(kc == 0), stop=False)
                P.mm(pgu[:, 128:256], Bu[:, fs], inv128.v, start=False, stop=True)
                g = gt[kk % 2]; s = s_t[kk % 2]; u = ut[kk % 2]; kk += 1
                P.ts('dve', g.v, pgu[:, 0:128], 7.0, None, op0=ALU.min)
                P.act(s.v, g.v, AF.Sigmoid, scale=1.702)
                P.ts('dve', u.v, pgu[:, 128:256], 7.0, -7.0, op0=ALU.min, op1=ALU.max)
                P.tt('dve', g.v, g.v, s.v, ALU.mult)
                P.stt('dve', aT[:, fc, :], u.v, 1.0, g.v, ALU.add, ALU.mult)
            y = yb[cur]
            for hf in range(2):
                py = nps()
                hs = slice(hf * 512, (hf + 1) * 512)
                for fc in range(8):
                    P.mm(py.v, aT[:, fc, :], Wd[:, fc, hs], start=(fc == 0), stop=False)
                P.mm(py.v, inv128.v, Bd[:, hs], start=False, stop=True)
                P.copy('act', y[:, hs], py.v)
            P.dma('sp', Ys[b * 128:(b + 1) * 128, :], y.v)
    rk = [P.sb(pfx + "rk%d" % i, [128, 1024]) for i in range(4)]
    acA = P.sb(pfx + "acA", [128, 1024]); acB = P.sb(pfx + "acB", [128, 1024])
    st = P.sb(pfx + "st", [128, 2, 6]); mv = P.sb(pfx + "mv", [128, 2]); rstd = P.sb(pfx + "rstd", [128, 1])
    Ys2 = Ys.h[:]
    for t in range(NTL):
        xt = xts[t % 2]
        P.dma('sp', xt.v, x1_d[t * 128:(t + 1) * 128, :])
        for k in range(4):
            ind_dma(P, rk[k].v, Ys, Ys2, slot_i[:, t * 4 + k: t * 4 + k + 1], NSLOT - 1)
        P.ts('dve', acA.v, rk[0].v, pk_all[:, t, 0:1], c1.v, op0=ALU.mult, op1=ALU.mult)
        P.stt('dve', acB.v, rk[1].v, pk_all[:, t, 1:2], acA.v, ALU.mult, ALU.add)
        P.stt('dve', acA.v, rk[2].v, pk_all[:, t, 2:3], acB.v, ALU.mult, ALU.add)
        P.stt('dve', acB.v, rk[3].v, pk_all[:, t, 3:4], acA.v, ALU.mult, ALU.add)
        P.stt('dve', acA.v, xt.v, DN_ALPHA, acB.v, ALU.mult, ALU.add)
        layer_norm(P, acA.v, gB.v, bB.v, st.v, mv.v, rstd.v)
        P.dma('sp', out_d[t * 128:(t + 1) * 128, :], acA.v)
```

```python
import numpy as np
import concourse.bass as bass
import concourse.mybir as mybir
from concourse.bass_utils import run_bass_kernel_spmd
from contextlib import ExitStack

F32 = mybir.dt.float32
F32R = mybir.dt.float32r
BF16 = mybir.dt.bfloat16
I32 = mybir.dt.int32
U32 = mybir.dt.uint32
AF = mybir.ActivationFunctionType
ALU = mybir.AluOpType
AX = mybir.AxisListType

ENGS = ['pe', 'act', 'dve', 'pool', 'sp']


class Tile:
    def __init__(self, P, h, name, space='sb'):
        self.P = P
        self.h = h
        self.name = name
        self.space = space
        self.lw = {}
        self.rd = {}
        self.dsem = None
        self.dcnt = 0

    def __getitem__(self, k):
        return V(self, self.h[k])

    @property
    def v(self):
        return V(self, self.h[:])


class V:
    def __init__(self, tile, ap):
        self.tile = tile
        self.ap = ap

    def __getitem__(self, k):
        return V(self.tile, self.ap[k])

    def bitcast(self, dt):
        return V(self.tile, self.ap.bitcast(dt))

    def bc(self, shape):
        return V(self.tile, self.ap.to_broadcast(shape))

    def re(self, s, **kw):
        return V(self.tile, self.ap.rearrange(s, **kw))


def _ap(x):
    if isinstance(x, Tile):
        return x.h[:]
    return x.ap if isinstance(x, V) else x


class Prog:
    def __init__(self, nc, es, same_engine_sync=True):
        self.nc = nc
        self.es = es
        self.es_top = es
        self.all_tiles = []
        self.stream = {e: [] for e in ENGS}
        self.sems = {}
        self.cnt = {e: 0 for e in ENGS}
        self.known = {e: {} for e in ENGS}
        self.same = same_engine_sync
        self.nsem = 0
        for e in ['pe', 'act', 'dve', 'pool']:
            self.sems[e] = es.enter_context(nc.semaphore("s_" + e))
            self.nsem += 1
        self.ninst = 0
        self.nwait = 0

    def sb(self, name, shape, dt=F32):
        h = self.es.enter_context(self.nc.sbuf_tensor(name, list(shape), dt))
        return Tile(self, h, name)

    def ps(self, name, shape, dt=F32):
        h = self.es.enter_context(self.nc.psum_tensor(name, list(shape), dt))
        return Tile(self, h, name, 'ps')

    def dram(self, name, shape, dt=F32, kind="Internal"):
        h = self.nc.dram_tensor(name, list(shape), dt, kind=kind)
        return Tile(self, h.ap(), name, 'dram')

    def _tsem(self, t):
        if t.dsem is None:
            key = "d_" + t.name
            self.sems[key] = self.es_top.enter_context(self.nc.semaphore(key))
            self.nsem += 1
            t.dsem = key
            self.all_tiles.append(t)
        return t.dsem

    def _waits(self, eng, rt, wt):
        need = {}
        for t in rt:
            for s, v in t.lw.items():
                need[s] = max(need.get(s, 0), v)
        for t in wt:
            for s, v in t.lw.items():
                need[s] = max(need.get(s, 0), v)
            for s, v in t.rd.items():
                need[s] = max(need.get(s, 0), v)
        out = []
        kn = self.known[eng]
        for s, v in need.items():
            if s == eng and (eng == 'pe' or not self.same):
                continue
            if kn.get(s, 0) < v:
                kn[s] = v
                out.append((s, v))
        return out

    def _record(self, ev, rt, wt):
        s, v = ev
        for t in wt:
            t.lw[s] = max(t.lw.get(s, 0), v)
            t.rd = {}
        for t in rt:
            if t in wt:
                continue
            t.rd[s] = max(t.rd.get(s, 0), v)

    @staticmethod
    def _tiles(xs):
        out = []
        for x in xs:
            if x is None:
                continue
            t = x.tile if isinstance(x, V) else x
            if isinstance(t, Tile) and t not in out:
                out.append(t)
        return out

    def I(self, eng, fn, w=(), r=()):
        wt = self._tiles(w)
        rt = self._tiles(r)
        for t in rt:
            if t.space == 'ps' and t not in wt and eng != 'pe':
                wt.append(t)
        waits = self._waits(eng, rt, wt)
        self.cnt[eng] += 1
        ev = (eng, self.cnt[eng])
        self.stream[eng].append((waits, fn, (eng, 1)))
        self._record(ev, rt, wt)
        self.ninst += 1
        self.nwait += len(waits)

    def dma(self, q, out, in_, **kw):
        wt = self._tiles([out])
        rt = self._tiles([in_])
        owner = None
        for x in (out, in_):
            t_ = x.tile if isinstance(x, V) else (x if isinstance(x, Tile) else None)
            if t_ is not None and t_.space == 'sb':
                owner = t_
        if owner is None:
            for x in (out, in_):
                t_ = x.tile if isinstance(x, V) else (x if isinstance(x, Tile) else None)
                if t_ is not None and owner is None:
                    owner = t_
        key = self._tsem(owner)
        waits = self._waits(q, rt, wt)
        owner.dcnt += 16
        ev = (key, owner.dcnt)
        o, i = _ap(out), _ap(in_)
        self.stream[q].append((waits, lambda e: e.dma_start(out=o, in_=i, **kw), (key, 16)))
        self._record(ev, rt, wt)
        self.ninst += 1
        self.nwait += len(waits)
        return ev

    def wait_all(self, eng, tiles):
        ts = self._tiles(tiles)
        waits = self._waits(eng, ts, ts)
        self.stream[eng].append((waits, None, None))

    def mm(self, out, lhsT, rhs, start=True, stop=True, **kw):
        o, a, b = _ap(out), _ap(lhsT), _ap(rhs)
        self.I('pe', lambda e: e.matmul(o, a, b, start=start, stop=stop, **kw), w=[out], r=[lhsT, rhs])

    def tr(self, out, in_, ident):
        o, a, b = _ap(out), _ap(in_), _ap(ident)
        self.I('pe', lambda e: e.transpose(o, a, b), w=[out], r=[in_, ident])

    def act(self, out, in_, func, bias=None, scale=1.0, accum_out=None, eng='act'):
        o, a = _ap(out), _ap(in_)
        kw = {}
        if bias is not None:
            kw['bias'] = _ap(bias)
        if accum_out is not None:
            kw['accum_out'] = _ap(accum_out)
        sc = _ap(scale)
        self.I(eng, lambda e: e.activation(o, a, func, scale=sc, **kw),
               w=[out, accum_out], r=[in_, bias, scale if isinstance(scale, V) else None])

    def tt(self, eng, out, in0, in1, op):
        o, a, b = _ap(out), _ap(in0), _ap(in1)
        self.I(eng, lambda e: e.tensor_tensor(o, a, b, op), w=[out], r=[in0, in1])

    def ts(self, eng, out, in0, s1, s2=None, op0=ALU.mult, op1=None, accum_out=None):
        o, a = _ap(out), _ap(in0)
        x1, x2 = _ap(s1), _ap(s2)
        kw = {}
        if op1 is not None:
            kw['op1'] = op1
        if accum_out is not None:
            kw['accum_out'] = _ap(accum_out)
        self.I(eng, lambda e: e.tensor_scalar(o, a, x1, x2, op0, **kw), w=[out, accum_out],
               r=[in0, s1 if isinstance(s1, V) else None, s2 if isinstance(s2, V) else None])

    def stt(self, eng, out, in0, scalar, in1, op0, op1):
        o, a, b = _ap(out), _ap(in0), _ap(in1)
        s = _ap(scalar)
        self.I(eng, lambda e: e.scalar_tensor_tensor(o, a, s, b, op0, op1), w=[out],
               r=[in0, in1, scalar if isinstance(scalar, V) else None])

    def copy(self, eng, out, in_):
        o, a = _ap(out), _ap(in_)
        if eng == 'act':
            self.I(eng, lambda e: e.copy(o, a), w=[out], r=[in_])
        else:
            self.I(eng, lambda e: e.tensor_copy(o, a), w=[out], r=[in_])

    def memset(self, eng, out, val):
        o = _ap(out)
        self.I(eng, lambda e: e.memset(o, val), w=[out])

    def barrier(self):
        evs = {e: self.cnt[e] for e in ['pe', 'act', 'dve', 'pool'] if self.cnt[e] > 0}
        for t in self.all_tiles:
            if t.dcnt > 0:
                evs[t.dsem] = t.dcnt
        for eng in ENGS:
            kn = self.known[eng]
            waits = []
            for s_, v in evs.items():
                if kn.get(s_, 0) < v:
                    kn[s_] = v
                    waits.append((s_, v))
            if waits:
                self.stream[eng].append((waits, None, None))

    def scope(self):
        P = self

        class _S:
            def __enter__(self_):
                self_.old = P.es
                self_.st = ExitStack()
                self_.st.__enter__()
                P.es = self_.st
                return self_

            def __exit__(self_, *a):
                P.barrier()
                P.emit()
                P.es = self_.old
                self_.st.__exit__(None, None, None)
                return False
        return _S()

    def emit(self):
        nc = self.nc
        sems = self.sems
        with nc.Block() as block:
            def run(engobj, name):
                for waits, fn, inc in self.stream[name]:
                    for s, v in waits:
                        engobj.wait_ge(sems[s], v)
                    if fn is not None:
                        ins = fn(engobj)
                        ins.then_inc(sems[inc[0]], inc[1])

            @block.tensor
            def _(e):
                run(e, 'pe')

            @block.scalar
            def _(e):
                run(e, 'act')

            @block.vector
            def _(e):
                run(e, 'dve')

            @block.gpsimd
            def _(e):
                run(e, 'pool')

            @block.sync
            def _(e):
                run(e, 'sp')
        self.stream = {e: [] for e in ENGS}


D = 1024
MIXW = 512
NEXP = 32
DN_ALPHA = (2.0 * 2) ** 0.25
EPS = 1e-5
OFF = dict(r_q=0, r_k=256, r_v=512, r_g=1024, m_x=1536, m_i=2048, m_f=2052, m_o=2056,
           a_q=2568, a_k=3336, a_v=4104, gates=5640)
D_IN = 8712


class Ctx:
    pass


def make_ctx(P):
    C = Ctx()
    C.identf = P.sb("identf", [128, 128], F32)
    C.identb = P.sb("identb", [128, 128], BF16)
    C.onesb = P.sb("onesb", [128, 128], BF16)
    C.onesf = P.sb("onesf", [128, 128], F32)
    P.memset('pool', C.identf.v, 1.0)
    o = C.identf.v.ap
    P.I('pool', lambda e: e.affine_select(o, o, [[-1, 128]], ALU.is_equal, 0.0, base=0, channel_multiplier=1),
        w=[C.identf], r=[C.identf])
    P.copy('pool', C.identb.v, C.identf.v)
    P.memset('pool', C.onesb.v, 1.0)
    P.memset('pool', C.onesf.v, 1.0)
    C.ps = [P.ps("psb%d" % i, [128, 512], F32) for i in range(8)]
    return C


def load_w_cast(P, dst, src, q='pool'):
    cols = src.shape[-1]
    c0 = 0
    while c0 < cols:
        c1 = min(cols, c0 + 1024)
        P.dma(q, dst[:, :, c0:c1], src[:, :, c0:c1])
        c0 = c1


def bcast_rows(P, dst, src1d, q='act'):
    P.dma(q, dst, src1d.partition_broadcast(128))


def x_transpose(P, C, xt, outs, psl):
    for half in range(2):
        pt = psl[half]
        for c in range(4):
            k = half * 4 + c
            P.tr(pt[:, c * 128:(c + 1) * 128], xt[:, k * 128:(k + 1) * 128], C.identf.v)
        for (o, eng) in outs:
            P.copy(eng, o[:, half * 4:(half + 1) * 4, :], pt.v.re("p (c t) -> p c t", c=4))


def layer_norm(P, r, g_b, b_b, st, mv, rstd, eng2='pool'):
    for hf in range(2):
        a, b = st[:, hf, :].ap, r[:, hf * 512:(hf + 1) * 512].ap
        P.I('dve', (lambda a, b: (lambda e: e.bn_stats(a, b)))(a, b), w=[st], r=[r])
    a, b = mv.ap, st.ap
    P.I('dve', lambda e: e.bn_aggr(a, b), w=[mv], r=[st])
    P.ts('dve', rstd, mv[:, 1:2], EPS, None, op0=ALU.add)
    P.act(rstd, rstd, AF.Ln)
    P.act(rstd, rstd, AF.Exp, scale=-0.5)
    P.ts('dve', r, r, mv[:, 0:1], rstd, op0=ALU.subtract, op1=ALU.mult)
    P.tt(eng2, r, r, g_b, ALU.mult)
    P.tt(eng2, r, r, b_b, ALU.add)


def pass_merge(P, C, NT, x_d, yT_d, w_in_l, w_branch_l, w_out_l, ln_g, ln_b, x1_d, pfx="m"):
    wg = P.sb(pfx + "wg", [128, 8, 3072], BF16)
    wb = P.sb(pfx + "wb", [128, 12, 1024], BF16)
    wo = P.sb(pfx + "wo", [128, 8, 1024], BF16)
    load_w_cast(P, wg.v, w_in_l[:, OFF['gates']:D_IN].rearrange("(kc p) c -> p kc c", p=128))
    load_w_cast(P, wb.v, w_branch_l.rearrange("b (kc p) c -> p (b kc) c", p=128))
    load_w_cast(P, wo.v, w_out_l.rearrange("(kc p) c -> p kc c", p=128))
    gB = P.sb(pfx + "gB", [128, 1024]); bB = P.sb(pfx + "bB", [128, 1024])
    bcast_rows(P, gB.v, ln_g); bcast_rows(P, bB.v, ln_b)
    xts = [P.sb(pfx + "xt%d" % i, [128, 1024]) for i in range(2)]
    xTs = [P.sb(pfx + "xT%d" % i, [128, 8, 128], BF16) for i in range(2)]
    yTs = [[P.sb(pfx + "yT%d_%d" % (b, i), [128, 4, 128], BF16) for i in range(2)] for b in range(3)]
    mg = [P.sb(pfx + "mg%d" % i, [128, 1024]) for i in range(2)]
    mT = [P.sb(pfx + "mT%d" % i, [128, 8, 128], BF16) for i in range(2)]
    sg = [P.sb(pfx + "sg%d" % i, [128, 512]) for i in range(2)]
    tmp = [P.sb(pfx + "tmp%d" % i, [128, 512]) for i in range(2)]
    rr = [P.sb(pfx + "rr%d" % i, [128, 1024]) for i in range(2)]
    st = P.sb(pfx + "st", [128, 2, 6]); mv = P.sb(pfx + "mv", [128, 2]); rstd = P.sb(pfx + "rstd", [128, 1])
    k = 0
    for t in range(NT // 128):
        xt = xts[t % 2]; xT = xTs[t % 2]
        P.dma('sp', xt.v, x_d[t * 128:(t + 1) * 128, :])
        x_transpose(P, C, xt.v, [(xT.v, 'act')], [C.ps[0], C.ps[1]])
        for b in range(3):
            P.dma('act', yTs[b][t % 2].v, yT_d[b][:, :, t * 128:(t + 1) * 128])
        m = mg[t % 2]
        for b in range(3):
            for hf in range(2):
                pg = C.ps[2 + (k % 2)]; pb = C.ps[4 + (k % 2)]; s = sg[k % 2]; tm = tmp[k % 2]
                k += 1
                for kc in range(8):
                    P.mm(pg.v, xT[:, kc, :], wg[:, kc, b * 1024 + hf * 512: b * 1024 + (hf + 1) * 512],
                         start=(kc == 0), stop=(kc == 7))
                for kc in range(4):
                    P.mm(pb.v, yTs[b][t % 2][:, kc, :], wb[:, b * 4 + kc, hf * 512:(hf + 1) * 512],
                         start=(kc == 0), stop=(kc == 3))
                P.act(s.v, pg.v, AF.Sigmoid)
                msl = m[:, hf * 512:(hf + 1) * 512]
                if b == 0:
                    P.tt('dve', msl, s.v, pb.v, ALU.mult)
                else:
                    P.tt('dve', tm.v, s.v, pb.v, ALU.mult)
                    P.tt('pool', msl, msl, tm.v, ALU.add)
        x_transpose(P, C, m.v, [(mT[t % 2].v, 'act')], [C.ps[6], C.ps[7]])
        r = rr[t % 2]
        for hf in range(2):
            po = C.ps[2 + (k % 2)]
            k += 1
            for kc in range(8):
                P.mm(po.v, mT[t % 2][:, kc, :], wo[:, kc, hf * 512:(hf + 1) * 512], start=(kc == 0), stop=(kc == 7))
            P.stt('dve', r[:, hf * 512:(hf + 1) * 512], xt[:, hf * 512:(hf + 1) * 512], DN_ALPHA, po.v,
                  ALU.mult, ALU.add)
        layer_norm(P, r.v, gB.v, bB.v, st.v, mv.v, rstd.v)
        P.dma('sp', x1_d[t * 128:(t + 1) * 128, :], r.v)


def pass_moe(P, C, NT, x1_d, w_router, b_router, w_gate, b_gate, w_up, b_up, w_down, b_down, ln_g, ln_b, out_d,
             NE=NEXP, pfx="e", TGT=4, dbg=0):
    TG = TGT * 128
    gB = P.sb(pfx + "gB", [128, 1024]); bB = P.sb(pfx + "bB", [128, 1024])
    bcast_rows(P, gB.v, ln_g); bcast_rows(P, bB.v, ln_b)
    wr = P.sb(pfx + "wr", [128, 8, NE], F32R)
    wr0 = P.sb(pfx + "wr0", [128, 8, NE])
    P.dma('act', wr0.v, w_router.rearrange("(kc p) e -> p kc e", p=128))
    P.copy('dve', wr.v, wr0.v)
    brB = P.sb(pfx + "brB", [128, NE]); bcast_rows(P, brB.v, b_router)
    bgT = P.sb(pfx + "bgT", [128, NE, 8]); buT = P.sb(pfx + "buT", [128, NE, 8])
    bstage = P.sb(pfx + "bstage", [128, 128])
    if dbg in (3, 7):
        P.memset('dve', bgT.v, 0.0); P.memset('dve', buT.v, 0.0)
    for (dstT, src) in (((bgT, b_gate), (buT, b_up)) if dbg not in (3, 7) else ()):
        rows = NE * 8
        srcv = src.rearrange("e (fc p) -> (e fc) p", p=128)
        dv = dstT.v.re("p e fc -> p (e fc)")
        r0 = 0
        while r0 < rows:
            r1 = min(rows, r0 + 128)
            n = r1 - r0
            P.dma('act', bstage[0:n, :], srcv[r0:r1, :])
            pz = C.ps[7]
            P.tr(pz[:, 0:n], bstage[0:n, :], C.identf[0:n, 0:n])
            P.copy('dve', dv[:, r0:r1], pz[:, 0:n])
            r0 = r1
    bd = P.sb(pfx + "bd", [NE, 1024], F32R)
    bd0 = P.sb(pfx + "bd0", [NE, 1024])
    P.dma('act', bd0.v, b_down)
    P.copy('dve', bd.v, bd0.v)
    W = [[P.sb(pfx + "W%d_%d" % (j, i), [128, 8, 1024], BF16) for j in range(3)] for i in range(2)]
    xts = [P.sb(pfx + "xt%d" % i, [128, 1024]) for i in range(TGT)]
    xTg = P.sb(pfx + "xTg", [128, 8, TG], BF16)
    xT32 = P.sb(pfx + "xT32", [128, 8, 128], F32R)
    acc = P.sb(pfx + "acc", [128, TGT, 1024])
    pall = P.sb(pfx + "pall", [128, TGT, NE])
    actT = P.sb(pfx + "actT", [128, 8, TG], BF16)
    lg = P.sb(pfx + "lg", [128, NE]); t8 = P.sb(pfx + "t8", [128, 8]); msk = P.sb(pfx + "msk", [128, NE])
    ex = P.sb(pfx + "ex", [128, NE]); sm = P.sb(pfx + "sm", [128, 1]); nmx = P.sb(pfx + "nmx", [128, 1])
    pT = P.sb(pfx + "pT", [NE, 128], F32R)
    gt = [P.sb(pfx + "g%d" % i, [128, TG]) for i in range(2)]
    st_ = [P.sb(pfx + "s%d" % i, [128, TG]) for i in range(2)]
    ut = [P.sb(pfx + "u%d" % i, [128, TG]) for i in range(2)]
    st = P.sb(pfx + "st", [128, 2, 6]); mv = P.sb(pfx + "mv", [128, 2]); rstd = P.sb(pfx + "rstd", [128, 1])
    c1 = P.sb(pfx + "c1", [128, 1]); c7 = P.sb(pfx + "c7", [128, 1])
    P.memset('dve', c1.v, 1.0); P.memset('dve', c7.v, 7.0)
    rr = [P.sb(pfx + "rr%d" % i, [128, 1024]) for i in range(2)]
    tmpq = [P.sb(pfx + "tq%d" % i, [128, 512]) for i in range(2)]
    assert TG == 512
    if dbg == 5:
        P.wait_all('sp', [gB, bB, wr, brB, bd, bgT, buT])
        return
    wcnt = 0
    kk = 0
    for gi in range(NT // TG):
        for tt in range(TGT):
            t = gi * TGT + tt
            xt = xts[tt]
            P.dma('sp', xt.v, x1_d[t * 128:(t + 1) * 128, :])
            x_transpose(P, C, xt.v, [(xTg[:, :, tt * 128:(tt + 1) * 128], 'act')] + ([(xT32.v, 'dve')] if dbg != 3 else []), [C.ps[0], C.ps[1]])
            if dbg in (2, 3, 7, 8):
                P.memset('dve', acc[:, tt, :], 0.0)
                P.memset('dve', pall[:, tt, :], 0.25)
            else:
                pl = C.ps[6]
                for kc in range(8):
                    P.mm(pl[:, 0:NE], xT32[:, kc, :], wr[:, kc, :], start=(kc == 0), stop=(kc == 7))
                P.tt('dve', lg.v, pl[:, 0:NE], brB.v, ALU.add)
                a, b = t8.v.ap, lg.v.ap
                P.I('dve', (lambda a, b: (lambda e: e.max(out=a, in_=b)))(a, b), w=[t8], r=[lg])
                P.ts('dve', msk.v, lg.v, t8[:, 3:4], c1.v, op0=ALU.is_ge, op1=ALU.mult)
                P.ts('dve', nmx.v, t8[:, 0:1], -1.0, None, op0=ALU.mult)
                P.act(ex.v, lg.v, AF.Exp, bias=nmx.v)
                P.tt('dve', ex.v, ex.v, msk.v, ALU.mult)
                a2, b2 = sm.v.ap, ex.v.ap
                P.I('dve', (lambda a, b: (lambda e: e.reduce_sum(a, b, AX.X)))(a2, b2), w=[sm], r=[ex])
                a3 = sm.v.ap
                P.I('dve', (lambda a: (lambda e: e.reciprocal(a, a)))(a3), w=[sm], r=[sm])
                P.ts('dve', pall[:, tt, :], ex.v, sm.v, c1.v, op0=ALU.mult, op1=ALU.mult)
                P.tr(pl[0:NE, 128:256], pall[:, tt, :], C.identf.v)
                P.copy('dve', pT.v, pl[0:NE, 128:256])
                for hf in range(2):
                    pb = C.ps[7]
                    P.mm(pb.v, pT.v, bd[:, hf * 512:(hf + 1) * 512])
                    P.copy('dve', acc[:, tt, hf * 512:(hf + 1) * 512], pb.v)
        for e in range(NE if dbg not in (1, 3, 7, 8) else 0):
            Wg, Wu, Wd = W[wcnt % 2]
            wcnt += 1
            P.dma('pool', Wg.v, w_gate[e].rearrange("(kc p) f -> p kc f", p=128))
            P.dma('pool', Wu.v, w_up[e].rearrange("(kc p) f -> p kc f", p=128))
            P.dma('pool', Wd.v, w_down[e].rearrange("(kc p) f -> p kc f", p=128))
            for fc in range(8):
                pg = C.ps[(kk % 2) * 2]; pu = C.ps[(kk % 2) * 2 + 1]
                g = gt[kk % 2]; s = st_[kk % 2]; u = ut[kk % 2]
                kk += 1
                for kc in range(8):
                    P.mm(pg.v, Wg[:, kc, fc * 128:(fc + 1) * 128], xTg[:, kc, :], start=(kc == 0), stop=(kc == 7))
                for kc in range(8):
                    P.mm(pu.v, Wu[:, kc, fc * 128:(fc + 1) * 128], xTg[:, kc, :], start=(kc == 0), stop=(kc == 7))
                P.ts('dve', g.v, pg.v, bgT[:, e, fc:fc + 1], c7.v, op0=ALU.add, op1=ALU.min)
                P.act(s.v, g.v, AF.Sigmoid, scale=1.702)
                P.ts('dve', u.v, pu.v, buT[:, e, fc:fc + 1], c7.v, op0=ALU.add, op1=ALU.min)
                P.ts('dve', u.v, u.v, -7.0, 1.0, op0=ALU.max, op1=ALU.add)
                P.tt('dve', g.v, g.v, s.v, ALU.mult)
                P.tt('dve', actT[:, fc, :], g.v, u.v, ALU.mult)
            for tt in range(TGT):
                for hf in range(2):
                    py = C.ps[4 + (kk % 2)]
                    kk += 1
                    for fc in range(8):
                        P.mm(py.v, actT[:, fc, tt * 128:(tt + 1) * 128], Wd[:, fc, hf * 512:(hf + 1) * 512],
                             start=(fc == 0), stop=(fc == 7))
                    av = acc[:, tt, hf * 512:(hf + 1) * 512]
                    tq = tmpq[kk % 2]
                    P.ts('dve', tq.v, py.v, pall[:, tt, e:e + 1], c1.v, op0=ALU.mult, op1=ALU.mult)
                    P.tt('dve', av, av, tq.v, ALU.add)
        for tt in range(TGT):
            t = gi * TGT + tt
            r = rr[tt % 2].v
            P.stt('dve', r, xts[tt].v, DN_ALPHA, acc[:, tt, :], ALU.mult, ALU.add)
            layer_norm(P, r, gB.v, bB.v, st.v, mv.v, rstd.v)
            P.dma('sp', out_d[t * 128:(t + 1) * 128, :], r)


RET_GAMMA = [1.0 - 2.0 ** (-5.0 - h) for h in range(4)]


def host_consts_scan(NT):
    j = np.arange(128, dtype=np.float64)
    lg = np.log(np.array(RET_GAMMA, dtype=np.float64))
    aR = np.exp(lg[None, :] * (j[:, None] + 1.0)) * (64 ** -0.5)
    bR = np.exp(-lg[None, :] * (j[:, None] + 1.0))
    eR = np.zeros((128, 2)); gR = np.zeros((128, 2))
    for h in range(4):
        ps = (h % 2) * 64
        eR[ps:ps + 64, h // 2] = np.exp(lg[h] * 128.0)
        gR[ps:ps + 64, h // 2] = np.exp(lg[h] * float(NT))
    mask = (j[:, None] <= j[None, :]).astype(np.float64)
    return dict(aR=aR.astype(np.float32), bR=bR.astype(np.float32), eR=eR.astype(np.float32),
                gR=gR.astype(np.float32), mask=mask.astype(np.float32))


def host_blockdiag(w):
    out = np.zeros((4, 128, 128), dtype=np.float32)
    for h in range(4):
        for n in range(32):
            out[h, 4 * n:4 * n + 4, 4 * n:4 * n + 4] = w[32 * h + n]
    return out


class PSRot:
    def __init__(self, C, banks):
        self.C = C; self.banks = banks; self.i = 0

    def __call__(self):
        b = self.C.ps[self.banks[self.i % len(self.banks)]]
        self.i += 1
        return b


def small_T(P, C, dst, src2d, rows, stage, ps):
    P.dma('act', stage[0:rows, :], src2d)
    P.tr(ps[:, 0:rows], stage[0:rows, :], C.identf[0:rows, 0:rows])
    P.copy('dve', dst, ps[:, 0:rows])


def pass_scan(P, C, NT, x_d, xprev_d, w_in_l, prm, cst, init, outs, mode="full", pfx="s"):
    full = (mode == "full")
    NW = 2568
    W = P.sb(pfx + "W", [128, 8, NW], BF16)
    load_w_cast(P, W.v, w_in_l[:, 0:NW].rearrange("(kc p) c -> p kc c", p=128))
    BD = {}
    for nm in ("bdq", "bdk", "bdv"):
        BD[nm] = P.sb(pfx + nm, [128, 4, 128], BF16)
        P.dma('pool', BD[nm].v, prm[nm].rearrange("h i o -> i h o"))
    stage = P.sb(pfx + "stage", [128, 128])
    cwT = P.sb(pfx + "cwT", [128, 16]); cbT = P.sb(pfx + "cbT", [128, 4])
    small_T(P, C, cwT.v, prm["ml_conv_w"].rearrange("k (c p) -> (k c) p", p=128), 16, stage, C.ps[7])
    small_T(P, C, cbT.v, prm["ml_conv_b"].rearrange("(c p) -> c p", p=128), 4, stage, C.ps[7])
    biB = P.sb(pfx + "biB", [128, 4]); bfB = P.sb(pfx + "bfB", [128, 4])
    bcast_rows(P, biB.v, prm["ml_bi"]); bcast_rows(P, bfB.v, prm["ml_bf"])
    aR = P.sb(pfx + "aR", [128, 4]); bR = P.sb(pfx + "bR", [128, 4]); eR = P.sb(pfx + "eR", [128, 2]); gR = P.sb(pfx + "gR", [128, 2])
    for t_, n_ in ((aR, "aR"), (bR, "bR"), (eR, "eR"), (gR, "gR")):
        P.dma('act', t_.v, cst[n_])
    mask = P.sb(pfx + "mask", [128, 128]); P.dma('act', mask.v, cst["mask"])
    maskr = P.sb(pfx + "maskr", [128, 128], F32R); P.copy('dve', maskr.v, mask.v)
    onesr = P.sb(pfx + "onesr", [128, 128], F32R); P.copy('dve', onesr.v, C.onesf.v)
    c1 = P.sb(pfx + "c1", [128, 1]); P.memset('dve', c1.v, 1.0)
    if full:
        gnR = P.sb(pfx + "gnR", [128, 512]); gnM = P.sb(pfx + "gnM", [128, 512]); skM = P.sb(pfx + "skM", [128, 512])
        bcast_rows(P, gnR.v, prm["ret_gn"]); bcast_rows(P, gnM.v, prm["ml_gn"]); bcast_rows(P, skM.v, prm["ml_skip"])
    Sret = P.sb(pfx + "Sret", [128, 2, 128]); Sretb = P.sb(pfx + "Sretb", [128, 2, 128], BF16)
    Cml = P.sb(pfx + "Cml", [128, 4, 129]); Cmlb = P.sb(pfx + "Cmlb", [128, 4, 129], BF16)
    tmpS = P.sb(pfx + "tmpS", [128, 4, 129]); tmpS2 = P.sb(pfx + "tmpS2", [128, 4, 129])
    totacc = P.sb(pfx + "totacc", [128, 4])
    P.memset('dve', Sret.v, 0.0); P.memset('dve', Cml.v, 0.0); P.memset('dve', totacc.v, 0.0)
    if init is not None:
        sel = P.sb(pfx + "sel", [128, 3]); nsel = P.sb(pfx + "nsel", [128, 3])
        P.dma('act', sel.v, init["sel"]); P.dma('act', nsel.v, init["nsel"])
        Fr = P.sb(pfx + "Fr", [128, 2, 128]); Fm = P.sb(pfx + "Fm", [128, 4, 129]); tl = P.sb(pfx + "tl", [128, 4])
        Gm = P.sb(pfx + "Gm", [128, 4])
        for q in range(3):
            P.dma('act', Fr.v, init["Fret"][q]); P.dma('act', Fm.v, init["Fml"][q]); P.dma('act', tl.v, init["totL"][q])
            P.act(Gm.v, tl.v, AF.Exp, scale=-1.0)
            for hp in range(2):
                P.act(tmpS[:, hp, 0:128], Sret[:, hp, :], AF.Copy, scale=gR[:, hp:hp + 1])
                P.tt('dve', tmpS[:, hp, 0:128], tmpS[:, hp, 0:128], Fr[:, hp, :], ALU.add)
                P.act(tmpS[:, hp, 0:128], tmpS[:, hp, 0:128], AF.Copy, scale=sel[:, q:q + 1])
                P.act(tmpS2[:, hp, 0:128], Sret[:, hp, :], AF.Copy, scale=nsel[:, q:q + 1])
                P.tt('dve', Sret[:, hp, :], tmpS[:, hp, 0:128], tmpS2[:, hp, 0:128], ALU.add)
            for h in range(4):
                P.act(tmpS[:, h, :], Cml[:, h, :], AF.Copy, scale=Gm[:, h:h + 1])
                P.tt('dve', tmpS[:, h, :], tmpS[:, h, :], Fm[:, h, :], ALU.add)
                P.act(tmpS[:, h, :], tmpS[:, h, :], AF.Copy, scale=sel[:, q:q + 1])
                P.act(tmpS2[:, h, :], Cml[:, h, :], AF.Copy, scale=nsel[:, q:q + 1])
                P.tt('dve', Cml[:, h, :], tmpS[:, h, :], tmpS2[:, h, :], ALU.add)
    P.copy('dve', Sretb.v, Sret.v); P.copy('dve', Cmlb.v, Cml.v)
    SC = 512
    xts = [P.sb(pfx + "xt%d" % i, [128, 1024]) for i in range(2)]
    xT = P.sb(pfx + "xT", [128, 8, SC], BF16)
    rqT = P.sb(pfx + "rqT", [128, 2, SC], BF16); rkT = P.sb(pfx + "rkT", [128, 2, SC], BF16)
    mxT = P.sb(pfx + "mxT", [128, 4, 3 + SC]); mxb = P.sb(pfx + "mxb", [128, 4, SC], BF16)
    cva = P.sb(pfx + "cva", [128, SC]); cvb = P.sb(pfx + "cvb", [128, SC])
    mcT = P.sb(pfx + "mcT", [128, 4, SC], BF16)
    qmT = P.sb(pfx + "qmT", [128, 4, SC], BF16); kmT = P.sb(pfx + "kmT", [128, 4, SC], BF16)
    rk_tok = P.sb(pfx + "rk_tok", [128, 256], BF16); km_tok = P.sb(pfx + "km_tok", [128, 512], BF16)
    vpR = P.sb(pfx + "vpR", [128, 4, 128], BF16); vpM = P.sb(pfx + "vpM", [128, 4, 129], BF16)
    g8 = P.sb(pfx + "g8", [128, 8]); L1 = P.sb(pfx + "L1", [128, 4], F32R); e1 = P.sb(pfx + "e1", [128, 4])
    igt = P.sb(pfx + "igt", [128, 4]); aM = P.sb(pfx + "aM", [128, 4]); bM = P.sb(pfx + "bM", [128, 4]); eM = P.sb(pfx + "eM", [128, 4])
    tmp4 = P.sb(pfx + "tmp4", [128, 4])
    Pm = [P.sb(pfx + "Pm%d" % i, [128, 128], BF16) for i in range(2)]
    ot = [P.sb(pfx + "ot%d" % i, [128, 129]) for i in range(2)]
    hh = [P.sb(pfx + "hh%d" % i, [128, 128]) for i in range(2)]
    dn = P.sb(pfx + "dn", [128, 1]); st6 = P.sb(pfx + "st6", [128, 6]); mv = P.sb(pfx + "mv", [128, 2]); rs = P.sb(pfx + "rs", [128, 1])
    if full:
        yR = P.sb(pfx + "yR", [128, 512]); yM = P.sb(pfx + "yM", [128, 512])
        rg = P.sb(pfx + "rg", [128, 512]); mo = P.sb(pfx + "mo", [128, 512]); mct = P.sb(pfx + "mct", [128, 512])
        yTo = [P.sb(pfx + "yTo%d" % i, [128, 4, 128], BF16) for i in range(2)]
        ybf = P.sb(pfx + "ybf", [128, 512], BF16)
    nps = PSRot(C, [0, 1, 2, 3, 4, 5, 6, 7])
    P.dma('sp', xts[0].v, xprev_d)
    x_transpose(P, C, xts[0].v, [(xT[:, :, 0:128], 'act')], [nps(), nps()])
    for c in range(4):
        pz = nps()
        for kc in range(8):
            P.mm(pz[:, 0:128], W[:, kc, OFF['m_x'] + c * 128: OFF['m_x'] + (c + 1) * 128], xT[:, kc, 0:128],
                 start=(kc == 0), stop=(kc == 7))
        P.copy('dve', mxT[:, c, 0:3], pz[:, 125:128])
    lnscale = float(np.log(128 ** -0.5))
    for sc in range(NT // SC):
        for tt in range(4):
            t = sc * 4 + tt
            xt = xts[t % 2]
            P.dma('sp', xt.v, x_d[t * 128:(t + 1) * 128, :])
            x_transpose(P, C, xt.v, [(xT[:, :, tt * 128:(tt + 1) * 128], 'act')], [nps(), nps()])
        for (dst, off, nch, kind) in ((rqT, OFF['r_q'], 2, 'bf'), (rkT, OFF['r_k'], 2, 'bf'), (mxT, OFF['m_x'], 4, 'mx')):
            if not full and dst is rqT:
                continue
            for c in range(nch):
                pz = nps()
                for kc in range(8):
                    P.mm(pz.v, W[:, kc, off + c * 128: off + (c + 1) * 128], xT[:, kc, :], start=(kc == 0), stop=(kc == 7))
                if kind == 'bf':
                    P.copy('act', dst[:, c, :], pz.v)
                else:
                    P.copy('act', mxT[:, c, 3:3 + SC], pz.v)
                    P.copy('dve', mxb[:, c, :], pz.v)
        for c in range(4):
            P.ts('dve', cva.v, mxT[:, c, 3:3 + SC], cwT[:, 12 + c:13 + c], cbT[:, c:c + 1], op0=ALU.mult, op1=ALU.add)
            P.stt('dve', cvb.v, mxT[:, c, 2:2 + SC], cwT[:, 8 + c:9 + c], cva.v, ALU.mult, ALU.add)
            P.stt('dve', cva.v, mxT[:, c, 1:1 + SC], cwT[:, 4 + c:5 + c], cvb.v, ALU.mult, ALU.add)
            P.stt('dve', cvb.v, mxT[:, c, 0:SC], cwT[:, c:c + 1], cva.v, ALU.mult, ALU.add)
            P.act(mcT[:, c, :], cvb.v, AF.Silu)
            P.copy('dve', cva[:, 0:3], mxT[:, c, SC:SC + 3])
            P.copy('dve', mxT[:, c, 0:3], cva[:, 0:3])
        for (dst, bd) in ((qmT, BD["bdq"]), (kmT, BD["bdk"])):
            if not full and dst is qmT:
                continue
            for h in range(4):
                pz = nps()
                P.mm(pz.v, bd[:, h, :], mcT[:, h, :])
                P.copy('act', dst[:, h, :], pz.v)
        for tt in range(4):
            t = sc * 4 + tt
            ts_ = slice(tt * 128, (tt + 1) * 128)

            def tok_proj(off, n):
                pz = nps()
                for kc in range(8):
                    P.mm(pz[:, 0:n], xT[:, kc, ts_], W[:, kc, off:off + n], start=(kc == 0), stop=(kc == 7))
                return pz
            p_rk = tok_proj(OFF['r_k'], 256)
            P.copy('act', rk_tok.v, p_rk[:, 0:256])
            p_g8 = tok_proj(OFF['m_i'], 8)
            P.copy('dve', g8.v, p_g8[:, 0:8])
            P.tt('dve', igt.v, g8[:, 0:4], biB.v, ALU.add)
            P.tt('dve', tmp4.v, g8[:, 4:8], bfB.v, ALU.add)
            P.act(e1.v, tmp4.v, AF.Exp, scale=-1.0)
            P.ts('dve', e1.v, e1.v, 1.0, None, op0=ALU.add)
            P.act(L1.v, e1.v, AF.Ln)
            pc = nps()
            P.mm(pc[:, 0:4], maskr.v, L1.v)
            P.mm(pc[:, 8:12], onesr.v, L1.v)
            P.act(aM.v, pc[:, 0:4], AF.Exp, scale=-1.0, bias=lnscale)
            P.tt('dve', tmp4.v, igt.v, pc[:, 0:4], ALU.add)
            P.act(bM.v, tmp4.v, AF.Exp)
            P.act(eM.v, pc[:, 8:12], AF.Exp, scale=-1.0)
            P.tt('dve', totacc.v, totacc.v, pc[:, 8:12], ALU.add)
            p_rv = tok_proj(OFF['r_v'], 512)
            for h in range(4):
                P.act(vpR[:, h, :], p_rv[:, h * 128:(h + 1) * 128], AF.Copy, scale=bR[:, h:h + 1])
            p_vm = nps()
            for h in range(4):
                P.mm(p_vm[:, h * 128:(h + 1) * 128], mxb[:, h, ts_], BD["bdv"][:, h, :])
            for h in range(4):
                P.act(vpM[:, h, 0:128], p_vm[:, h * 128:(h + 1) * 128], AF.Copy, scale=bM[:, h:h + 1])
            P.copy('dve', vpM[:, :, 128:129], bM.v.re("p (h o) -> p h o", o=1))
            p_km = nps()
            for h in range(4):
                P.mm(p_km[:, h * 128:(h + 1) * 128], mcT[:, h, ts_], BD["bdk"][:, h, :])
            P.copy('act', km_tok.v, p_km.v)
            if full:
                p_rg = tok_proj(OFF['r_g'], 512)
                P.act(rg.v, p_rg.v, AF.Silu)
                p_mo = tok_proj(OFF['m_o'], 512)
                P.act(mo.v, p_mo.v, AF.Sigmoid)
                p_mc = nps()
                pmb = p_mc.v.bitcast(BF16)
                for c in range(4):
                    P.tr(pmb[:, c * 128:(c + 1) * 128], mcT[:, c, ts_], C.identb.v)
                P.tt('dve', mct.v, pmb[:, 0:512], skM.v, ALU.mult)
            kq = 0
            for h in range(4):
                psl = slice((h % 2) * 64, (h % 2) * 64 + 64); hp = h // 2
                if full:
                    p_st = nps()
                    P.mm(p_st[:, 0:128], rkT[psl, hp, ts_], rqT[psl, hp, ts_])
                    pm = Pm[kq % 2]; o = ot[kq % 2]; hx = hh[kq % 2]; kq += 1
                    P.tt('dve', pm.v, p_st[:, 0:128], mask.v, ALU.mult)
                    p_o = nps()
                    P.mm(p_o[:, 0:128], pm.v, vpR[:, h, :], start=True, stop=False)
                    P.mm(p_o[:, 0:128], rqT[psl, hp, ts_], Sretb[psl, hp, :], start=False, stop=True)
                    P.act(o[:, 0:128], p_o[:, 0:128], AF.Copy, scale=aR[:, h:h + 1])
                    head_norm(P, o[:, 0:128], hx.v, st6, mv, rs)
                    P.tt('dve', hx.v, hx.v, gnR[:, h * 128:(h + 1) * 128], ALU.mult)
                    P.tt('dve', yR[:, h * 128:(h + 1) * 128], hx.v, rg[:, h * 128:(h + 1) * 128], ALU.mult)
                p_kv = nps()
                P.mm(p_kv[:, 0:128], rk_tok[:, hp * 128:(hp + 1) * 128], vpR[:, h, :])
                P.tt('dve', tmpS[psl, hp, 0:128], Sret[psl, hp, :], p_kv[psl, 0:128], ALU.add)
                P.act(Sret[psl, hp, :], tmpS[psl, hp, 0:128], AF.Copy, scale=eR[psl, hp:hp + 1])
                P.copy('dve', Sretb[psl, hp, :], Sret[psl, hp, :])
            for h in range(4):
                if full:
                    p_st = nps()
                    P.mm(p_st[:, 0:128], kmT[:, h, ts_], qmT[:, h, ts_])
                    pm = Pm[kq % 2]; o = ot[kq % 2]; hx = hh[kq % 2]; kq += 1
                    P.tt('dve', pm.v, p_st[:, 0:128], mask.v, ALU.mult)
                    p_o = nps()
                    P.mm(p_o[:, 0:129], pm.v, vpM[:, h, :], start=True, stop=False)
                    P.mm(p_o[:, 0:129], qmT[:, h, ts_], Cmlb[:, h, :], start=False, stop=True)
                    P.act(o.v, p_o[:, 0:129], AF.Copy, scale=aM[:, h:h + 1])
                    P.act(dn.v, o[:, 128:129], AF.Abs)
                    P.ts('dve', dn.v, dn.v, 1.0, None, op0=ALU.max)
                    a_ = dn.v.ap
                    P.I('dve', (lambda a_: (lambda e: e.reciprocal(a_, a_)))(a_), w=[dn], r=[dn])
                    P.act(o[:, 0:128], o[:, 0:128], AF.Copy, scale=dn.v)
                    head_norm(P, o[:, 0:128], hx.v, st6, mv, rs)
                    P.tt('dve', hx.v, hx.v, gnM[:, h * 128:(h + 1) * 128], ALU.mult)
                    P.tt('dve', hx.v, hx.v, mct[:, h * 128:(h + 1) * 128], ALU.add)
                    P.tt('dve', yM[:, h * 128:(h + 1) * 128], hx.v, mo[:, h * 128:(h + 1) * 128], ALU.mult)
                p_kv = nps()
                P.mm(p_kv[:, 0:129], km_tok[:, h * 128:(h + 1) * 128], vpM[:, h, :])
                P.tt('dve', tmpS[:, h, :], Cml[:, h, :], p_kv[:, 0:129], ALU.add)
                P.act(Cml[:, h, :], tmpS[:, h, :], AF.Copy, scale=eM[:, h:h + 1])
                P.copy('dve', Cmlb[:, h, :], Cml[:, h, :])
            if full:
                for (ysrc, ydst) in ((yR, outs[0]), (yM, outs[1])):
                    P.copy('act', ybf.v, ysrc.v)
                    pz = nps(); pzb = pz.v.bitcast(BF16)
                    for c in range(4):
                        P.tr(pzb[:, c * 128:(c + 1) * 128], ybf[:, c * 128:(c + 1) * 128], C.identb.v)
                    yo = yTo[kq % 2]; kq += 1
                    P.copy('dve', yo.v, pzb[:, 0:512].re("p (c t) -> p c t", c=4))
                    P.dma('sp', ydst[:, :, t * 128:(t + 1) * 128], yo.v)
    if not full:
        P.dma('sp', outs[0].v, Sret.v)
        P.dma('sp', outs[1].v, Cml.v)
        P.dma('sp', outs[2].v, totacc.v)


def head_norm(P, src, dst, st6, mv, rs):
    a, b = st6.v.ap, src.ap
    P.I('dve', lambda e: e.bn_stats(a, b), w=[st6], r=[src])
    a2, b2 = mv.v.ap, st6.v.ap
    P.I('dve', lambda e: e.bn_aggr(a2, b2), w=[mv], r=[st6])
    P.ts('dve', rs.v, mv[:, 1:2], EPS, None, op0=ALU.add)
    P.act(rs.v, rs.v, AF.Ln)
    P.act(rs.v, rs.v, AF.Exp, scale=-0.5)
    P.ts('dve', dst, src, mv[:, 0:1], rs.v, op0=ALU.subtract, op1=ALU.mult)


ATT_PAT = ((128, 1), (512, 4), (2048, 16))
HALO = 2048


def host_consts_attn():
    slopes = np.exp2(-8.0 * np.arange(1, 13, dtype=np.float64) / 12.0).reshape(3, 4)
    s = np.arange(128)[:, None]; i = np.arange(128)[None, :]
    out = np.zeros((3, 2, 128, 4, 128), dtype=np.float32)
    for g, (win, d) in enumerate(ATT_PAT):
        for h in range(4):
            dcur = i - s
            b = np.where((dcur >= 0), -slopes[g, h] * d * dcur, -30000.0)
            out[g, 1, :, h, :] = b
            dprev = i + 128 - s
            b = np.where((dprev <= 128), -slopes[g, h] * d * dprev, -30000.0)
            out[g, 0, :, h, :] = b
    return out.reshape(3, 2, 128, 512)


def pass_attn(P, C, NT, xext_d, w_in_l, bias_d, hv_d, yT_out, pfx="a", dbg=0):
    accN = P.sb(pfx + "accN", [128, 4, NT]); accD = P.sb(pfx + "accD", [128, 4, NT])
    for h_ in range(4):
        for j_ in range(NT // 2048):
            P.memset('dve', accN[:, h_, j_ * 2048:(j_ + 1) * 2048], 0.0 if dbg == 0 else 1.0)
            P.memset('dve', accD[:, h_, j_ * 2048:(j_ + 1) * 2048], 0.0 if dbg == 0 else 2.0)
    hv0 = P.sb(pfx + "hv0", [128, 128]); hvb = P.sb(pfx + "hvb", [128, 128], BF16)
    P.dma('act', hv0.v, hv_d); P.copy('dve', hvb.v, hv0.v)
    Wq = P.sb(pfx + "Wq", [128, 8, 256], BF16); Wk = P.sb(pfx + "Wk", [128, 8, 256], BF16); Wv = P.sb(pfx + "Wv", [128, 8, 512], BF16)
    bT = [P.sb(pfx + "bT%d" % i, [128, 512]) for i in range(2)]
    xts = [P.sb(pfx + "xt%d" % i, [128, 1024]) for i in range(2)]
    xTb = [P.sb(pfx + "xTb%d" % i, [128, 8, 128], BF16) for i in range(2)]
    kT = [P.sb(pfx + "kT%d" % i, [128, 2, 128], BF16) for i in range(2)]
    Vt = [P.sb(pfx + "V%d" % i, [128, 512], BF16) for i in range(2)]
    qT = [P.sb(pfx + "qT%d" % i, [128, 2, 128], BF16) for i in range(2)]
    tmp = [P.sb(pfx + "tmp%d" % i, [128, 512]) for i in range(2)]
    PT = [[P.sb(pfx + "PT%d_%d" % (i, j), [128, 512], BF16) for j in range(2)] for i in range(2)]
    nps = PSRot(C, [0, 1, 2, 3, 4, 5, 6, 7])
    win = w_in_l.rearrange("(kc p) c -> p kc c", p=128)
    nb = 0
    for g, (_, d) in enumerate(ATT_PAT if dbg not in (1, 2, 4, 5, 6) else (ATT_PAT[:1] if dbg in (2, 4, 5, 6) else ())):
        load_w_cast(P, Wq.v, win[:, :, OFF['a_q'] + g * 256: OFF['a_q'] + (g + 1) * 256])
        load_w_cast(P, Wk.v, win[:, :, OFF['a_k'] + g * 256: OFF['a_k'] + (g + 1) * 256])
        load_w_cast(P, Wv.v, win[:, :, OFF['a_v'] + g * 512: OFF['a_v'] + (g + 1) * 512])
        P.dma('act', bT[0].v, bias_d[g, 0]); P.dma('act', bT[1].v, bias_d[g, 1])
        NB = NT // (128 * d)
        for r in range(d):
            for m in range(-1, NB):
                cur = nb % 2; prv = 1 - cur; nb += 1
                s0 = HALO + m * 128 * d + r
                xt = xts[cur]
                P.dma('sp', xt.v, xext_d[s0: s0 + 127 * d + 1: d, :])
                x_transpose(P, C, xt.v, [(xTb[cur].v, 'act')], [nps(), nps()])
                xb = xTb[cur]
                for c in range(2):
                    pz = nps()
                    for kc in range(8):
                        P.mm(pz[:, 0:128], Wk[:, kc, c * 128:(c + 1) * 128], xb[:, kc, :], start=(kc == 0), stop=(kc == 7))
                    P.copy('act', kT[cur][:, c, :], pz[:, 0:128])
                pz = nps()
                for kc in range(8):
                    P.mm(pz.v, xb[:, kc, :], Wv[:, kc, :], start=(kc == 0), stop=(kc == 7))
                P.copy('act', Vt[cur].v, pz.v)
                if m < 0 or dbg == 4:
                    continue
                for c in range(2):
                    pz = nps()
                    for kc in range(8):
                        P.mm(pz[:, 0:128], Wq[:, kc, c * 128:(c + 1) * 128], xb[:, kc, :], start=(kc == 0), stop=(kc == 7))
                    P.copy('act', qT[cur][:, c, :], pz[:, 0:128])
                for pc, kb in ((0, prv), (1, cur)):
                    psAB = [nps(), nps()]
                    for h in range(4):
                        psl = slice((h % 2) * 64, (h % 2) * 64 + 64); hp = h // 2
                        P.mm(psAB[h % 2][:, hp * 128:(hp + 1) * 128], kT[kb][psl, hp, :], qT[cur][psl, hp, :])
                    tm = tmp[pc]
                    for h in range(4):
                        hs = slice(h * 128, (h + 1) * 128); hp = h // 2
                        P.stt('dve', tm[:, hs], psAB[h % 2][:, hp * 128:(hp + 1) * 128], 0.125, bT[pc][:, hs], ALU.mult, ALU.add)
                    P.act(PT[cur][pc].v, tm.v, AF.Exp)
                if dbg in (5, 6):
                    continue
                pn = nps()
                for h in range(4):
                    hs = slice(h * 128, (h + 1) * 128)
                    P.mm(pn[:, hs], Vt[prv][:, hs], PT[cur][0][:, hs], start=True, stop=False)
                    P.mm(pn[:, hs], Vt[cur][:, hs], PT[cur][1][:, hs], start=False, stop=True)
                pd = nps()
                P.mm(pd.v, (hvb.v if m == 0 else C.onesb.v), PT[cur][0].v, start=True, stop=False)
                P.mm(pd.v, C.onesb.v, PT[cur][1].v, start=False, stop=True)
                t0 = m * 128 * d + r
                for h in range(4 if dbg != 3 else 0):
                    hs = slice(h * 128, (h + 1) * 128)
                    av = accN[:, h, t0: t0 + 127 * d + 1: d]
                    P.tt('dve', av, av, pn[:, hs], ALU.add)
                    dv = accD[:, h, t0: t0 + 127 * d + 1: d]
                    P.tt('dve', dv, dv, pd[:, hs], ALU.add)
    yo = [P.sb(pfx + "yo%d" % i, [128, 4, 512], BF16) for i in range(2)]
    rc = [P.sb(pfx + "rc%d" % i, [128, 4, 512]) for i in range(2)]
    for j in range(NT // 512):
        sl = slice(j * 512, (j + 1) * 512)
        for h in range(4):
            a_, b_ = rc[j % 2][:, h, :].ap, accD[:, h, sl].ap
            P.I('dve', (lambda a_, b_: (lambda e: e.reciprocal(a_, b_)))(a_, b_), w=[rc[j % 2]], r=[accD])
            P.tt('dve', yo[j % 2][:, h, :], accN[:, h, sl], rc[j % 2][:, h, :], ALU.mult)
        P.dma('sp', yT_out[:, :, sl], yo[j % 2].v)


NCORES = 8
NT_CORE = 4096
PRM_NAMES = ("ret_gn", "ml_conv_w", "ml_conv_b", "bdq", "bdk", "bdv", "ml_bi", "ml_bf", "ml_gn", "ml_skip")
PRM_SHAPES = dict(ret_gn=[512], ml_conv_w=[4, 512], ml_conv_b=[512], bdq=[4, 128, 128], bdk=[4, 128, 128], bdv=[4, 128, 128],
                  ml_bi=[4], ml_bf=[4], ml_gn=[512], ml_skip=[512])
CST_SHAPES = dict(aR=[128, 4], bR=[128, 4], eR=[128, 2], gR=[128, 2], mask=[128, 128])


def _scan_inputs(nc):
    def inp(n, sh):
        return nc.dram_tensor(n, sh, F32, kind="ExternalInput").ap()
    prm = {n: inp(n, PRM_SHAPES[n]) for n in PRM_NAMES}
    cst = {n: inp(n, CST_SHAPES[n]) for n in CST_SHAPES}
    return prm, cst


def build_A(NT=NT_CORE):
    nc = bass.Bass("TRN2", target_bir_lowering=False)
    with ExitStack() as es:
        P = Prog(nc, es)
        x_d = P.dram("x", [NT, D], F32, kind="ExternalInput")
        xprev = P.dram("xprev", [128, D], F32, kind="ExternalInput")
        w_in = nc.dram_tensor("w_in", [D, D_IN], F32, kind="ExternalInput").ap()
        prm, cst = _scan_inputs(nc)
        outs = [P.dram("oS", [128, 2, 128], F32, kind="ExternalOutput"), P.dram("oC", [128, 4, 129], F32, kind="ExternalOutput"),
                P.dram("oT", [128, 4], F32, kind="ExternalOutput")]
        C = make_ctx(P)
        pass_scan(P, C, NT, x_d, xprev, w_in, prm, cst, None, outs, mode="summary", pfx="s")
        P.wait_all('sp', outs)
        P.emit()
    return nc


def build_B(NT=NT_CORE, NE=NEXP):
    nc = bass.Bass("TRN2", target_bir_lowering=False)
    with ExitStack() as es:
        P = Prog(nc, es)

        def inp(n, sh):
            return nc.dram_tensor(n, sh, F32, kind="ExternalInput").ap()
        xext = P.dram("xext", [HALO + NT, D], F32, kind="ExternalInput")
        w_in = inp("w_in", [D, D_IN])
        prm, cst = _scan_inputs(nc)
        init = dict(Fret=inp("Fret", [3, 128, 2, 128]), Fml=inp("Fml", [3, 128, 4, 129]), totL=inp("totL", [3, 128, 4]),
                    sel=inp("sel", [128, 3]), nsel=inp("nsel", [128, 3]))
        abias = inp("abias", [3, 2, 128, 512]); hv = inp("hv", [128, 128])
        w_br = inp("w_branch", [3, MIXW, D]); w_out = inp("w_out", [D, D])
        ln1_g = inp("ln1_g", [D]); ln1_b = inp("ln1_b", [D]); ln2_g = inp("ln2_g", [D]); ln2_b = inp("ln2_b", [D])
        w_r = inp("w_router", [D, NE]); b_r = inp("b_router", [NE])
        w_g = inp("w_gate", [NE, D, D]); b_g = inp("b_gate", [NE, D])
        w_u = inp("w_up", [NE, D, D]); b_u = inp("b_up", [NE, D])
        w_d = inp("w_down", [NE, D, D]); b_d = inp("b_down", [NE, D])
        out = P.dram("out", [NT, D], F32, kind="ExternalOutput")
        yT = [P.dram("yT%d" % b, [128, 4, NT], BF16) for b in range(3)]
        x1s = P.dram("x1s", [NT, D], F32)
        C = make_ctx(P)
        x_own = Tile(P, xext.h[HALO:HALO + NT, :], "xown", "dram")
        x_own.lw, x_own.rd = xext.lw, xext.rd
        xprev = Tile(P, xext.h[HALO - 128:HALO, :], "xprv", "dram")
        xprev.lw, xprev.rd = xext.lw, xext.rd
        with P.scope():
            pass_attn(P, C, NT, xext, w_in, abias, hv, yT[2], pfx="a")
        with P.scope():
            pass_scan(P, C, NT, x_own, xprev, w_in, prm, cst, init, [yT[0], yT[1]], mode="full", pfx="s")
        with P.scope():
            pass_merge(P, C, NT, x_own, yT, w_in, w_br, w_out, ln1_g, ln1_b, x1s, pfx="m")
        NBLK = NT * 4 // 128 + NE
        Xs = P.dram("Xs", [NBLK * 128, D], BF16); Ys = P.dram("Ys", [NBLK * 128, D], F32)
        mc = {k: inp("mc_" + k, list(v.shape)) for k, v in host_consts_moe(NT, NE).items()}
        with P.scope():
            pass_moe2(P, C, NT, x1s, w_r, b_r, w_g, b_g, w_u, b_u, w_d, b_d, ln2_g, ln2_b, out, Xs, Ys, mc, NE=NE, pfx="f")
        P.wait_all('sp', [out])
        P.emit()
    return nc


def layer_params(inputs, l):
    g = lambda n: np.ascontiguousarray(np.asarray(inputs[n], dtype=np.float32)[l])
    prm = dict(ret_gn=g("ret_gn"), ml_conv_w=g("ml_conv_w"), ml_conv_b=g("ml_conv_b"),
               bdq=host_blockdiag(g("ml_wq")), bdk=host_blockdiag(g("ml_wk")), bdv=host_blockdiag(g("ml_wv")),
               ml_bi=g("ml_bi"), ml_bf=g("ml_bf"), ml_gn=g("ml_gn"), ml_skip=g("ml_skip"))
    big = dict(w_in=g("w_in"), w_branch=g("w_branch"), w_out=g("w_out"), ln1_g=g("ln1_g"), ln1_b=g("ln1_b"),
               ln2_g=g("ln2_g"), ln2_b=g("ln2_b"), w_router=g("w_router"), b_router=g("b_router"),
               w_gate=g("w_gate"), b_gate=g("b_gate"), w_up=g("w_up"), b_up=g("b_up"), w_down=g("w_down"), b_down=g("b_down"))
    return prm, big


def run_layer(ncA, ncB, xs, prm, big, cst, abias, QPB, NT):
    n = len(xs)
    zeros128 = np.zeros((128, D), np.float32)
    inA = []
    for c in range(n):
        q = c % QPB
        m = dict(prm); m.update(cst)
        m["w_in"] = big["w_in"]; m["x"] = xs[c]
        m["xprev"] = np.ascontiguousarray(xs[c - 1][-128:]) if q > 0 else zeros128
        inA.append(m)
    resA = run_bass_kernel_spmd(ncA, inA, core_ids=list(range(n))).results
    inB = []
    for c in range(n):
        q = c % QPB; b0 = c - q
        m = dict(prm); m.update(cst); m.update(big)
        halo = xs[c - 1][-HALO:] if q > 0 else np.zeros((HALO, D), np.float32)
        m["xext"] = np.ascontiguousarray(np.concatenate([halo, xs[c]], axis=0))
        Fret = np.zeros((3, 128, 2, 128), np.float32); Fml = np.zeros((3, 128, 4, 129), np.float32)
        totL = np.zeros((3, 128, 4), np.float32); sel = np.zeros((128, 3), np.float32)
        for qq in range(min(3, QPB)):
            Fret[qq] = resA[b0 + qq]["oS"]; Fml[qq] = resA[b0 + qq]["oC"]; totL[qq] = resA[b0 + qq]["oT"]
            if qq < q:
                sel[:, qq] = 1.0
        m.update(Fret=Fret, Fml=Fml, totL=totL, sel=sel, nsel=(1.0 - sel).astype(np.float32))
        m["abias"] = abias
        for k_, v_ in host_consts_moe(NT, big["w_router"].shape[1]).items():
            m["mc_" + k_] = v_
        m["hv"] = (np.ones((128, 128), np.float32) if q > 0 else np.zeros((128, 128), np.float32))
        inB.append(m)
    resB = run_bass_kernel_spmd(ncB, inB, core_ids=list(range(n))).results
    return [np.asarray(r["out"], dtype=np.float32) for r in resB]


def kernel(**inputs):
    x = np.asarray(inputs["x"], dtype=np.float32)
    B, S, _ = x.shape
    QPB = NCORES // B
    NT = S // QPB
    xs = [np.ascontiguousarray(x[c // QPB, (c % QPB) * NT:(c % QPB + 1) * NT]) for c in range(NCORES)]
    ncA = build_A(NT); ncB = build_B(NT)
    cst = host_consts_scan(NT); abias = host_consts_attn()
    L = np.asarray(inputs["w_in"]).shape[0]
    for l in range(L):
        prm, big = layer_params(inputs, l)
        xs = run_layer(ncA, ncB, xs, prm, big, cst, abias, QPB, NT)
    out = np.zeros((B, S, D), np.float32)
    for c in range(NCORES):
        out[c // QPB, (c % QPB) * NT:(c % QPB + 1) * NT] = xs[c]
    return out


BIGIDX = 4000000.0


def host_consts_moe(NT, NE=NEXP):
    NBLK = NT * 4 // 128 + NE
    p = np.arange(128, dtype=np.float32)
    d = dict(iota_p=p.reshape(128, 1).copy(),
             ustrict=(p[:, None] < p[None, :]).astype(np.float32),
             iotaJ=np.tile(np.arange(33, dtype=np.float32)[None, :], (128, 1)),
             iotaB=np.tile(np.arange(NBLK, dtype=np.float32)[None, :], (128, 1)))
    return d


def _breg(P, e, bound):
    if not hasattr(P, "_bregs"):
        P._bregs = {}
    if bound not in P._bregs:
        P._bregs[bound] = e.to_reg(int(bound))
    return P._bregs[bound]


def ind_dma(P, dst_v, src_tile, src_ap, idx_v, bound, scatter=False, extra_r=()):
    sb_tile = dst_v.tile
    key = P._tsem(sb_tile)
    d_ap, i_ap = dst_v.ap, idx_v.ap
    if not scatter:
        rt = [src_tile, idx_v.tile] + list(extra_r); wt = [sb_tile]

        def f(e):
            try:
                return e.indirect_dma_start(d_ap, None, src_ap, bass.IndirectOffsetOnAxis(i_ap, 0), bounds_check=_breg(P, e, bound), oob_is_err=False)
            except Exception:
                print("IND GATHER FAIL", d_ap, src_ap, i_ap, bound)
                raise
    else:
        rt = [sb_tile, idx_v.tile] + list(extra_r); wt = [src_tile]

        def f(e):
            try:
                return e.indirect_dma_start(src_ap, bass.IndirectOffsetOnAxis(i_ap, 0), d_ap, None, bounds_check=_breg(P, e, bound), oob_is_err=False)
            except Exception:
                print("IND SCATTER FAIL", d_ap, src_ap, i_ap, bound)
                raise
    waits = P._waits('pool', rt, wt)
    sb_tile.dcnt += 16
    P.stream['pool'].append((waits, f, (key, 16)))
    P._record((key, sb_tile.dcnt), rt, wt)
    P.ninst += 1


def pass_moe2(P, C, NT, x1_d, w_router, b_router, w_gate, b_gate, w_up, b_up, w_down, b_down, ln_g, ln_b, out_d,
              Xs, Ys, mc, NE=NEXP, pfx="f", dbg=0):
    NTL = NT // 128
    NBLK = NT * 4 // 128 + NE
    NSLOT = NBLK * 128
    gB = P.sb(pfx + "gB", [128, 1024]); bB = P.sb(pfx + "bB", [128, 1024])
    bcast_rows(P, gB.v, ln_g); bcast_rows(P, bB.v, ln_b)
    wr = P.sb(pfx + "wr", [128, 8, NE], F32R); wr0 = P.sb(pfx + "wr0", [128, 8, NE])
    P.dma('act', wr0.v, w_router.rearrange("(kc p) e -> p kc e", p=128)); P.copy('dve', wr.v, wr0.v)
    brB = P.sb(pfx + "brB", [128, NE]); bcast_rows(P, brB.v, b_router)
    c1 = P.sb(pfx + "c1", [128, 1]); P.memset('dve', c1.v, 1.0)
    iop = P.sb(pfx + "iop", [128, 1]); P.dma('act', iop.v, mc["iota_p"])
    us0 = P.sb(pfx + "us0", [128, 128]); P.dma('act', us0.v, mc["ustrict"])
    usb = P.sb(pfx + "usb", [128, 128], BF16); P.copy('dve', usb.v, us0.v)
    ioJ = P.sb(pfx + "ioJ", [128, 33]); P.dma('act', ioJ.v, mc["iotaJ"])
    ioB = P.sb(pfx + "ioB", [128, NBLK]); P.dma('act', ioB.v, mc["iotaB"])
    lg_all = P.sb(pfx + "lg_all", [128, NTL, NE]); t8_all = P.sb(pfx + "t8_all", [128, NTL, 8])
    pall = P.sb(pfx + "pall", [128, NTL, NE]); pos_all = P.sb(pfx + "pos_all", [128, NTL, NE])
    slot_f = P.sb(pfx + "slot_f", [128, NTL, 4]); slot_i = P.sb(pfx + "slot_i", [128, NTL * 4], I32)
    pk_all = P.sb(pfx + "pk_all", [128, NTL, 4])
    carry = P.sb(pfx + "carry", [128, NE]); P.memset('dve', carry.v, 0.0)
    xts = [P.sb(pfx + "xt%d" % i, [128, 1024]) for i in range(2)]
    xT32 = P.sb(pfx + "xT32", [128, 8, 128], F32R)
    msk = P.sb(pfx + "msk", [128, NE]); mskb = P.sb(pfx + "mskb", [128, NE], BF16)
    ex = P.sb(pfx + "ex", [128, NE]); sm = P.sb(pfx + "sm", [128, 1]); nmx = P.sb(pfx + "nmx", [128, 1])
    nps = PSRot(C, [0, 1, 2, 3, 4, 5, 6, 7])
    for t in range(NTL):
        xt = xts[t % 2]
        P.dma('sp', xt.v, x1_d[t * 128:(t + 1) * 128, :])
        x_transpose(P, C, xt.v, [(xT32.v, 'dve')], [nps(), nps()])
        pl = nps()
        for kc in range(8):
            P.mm(pl[:, 0:NE], xT32[:, kc, :], wr[:, kc, :], start=(kc == 0), stop=(kc == 7))
        lg = lg_all[:, t, :]; t8 = t8_all[:, t, :]
        P.tt('dve', lg, pl[:, 0:NE], brB.v, ALU.add)
        a, b = t8.ap, lg.ap
        P.I('dve', (lambda a, b: (lambda e: e.max(out=a, in_=b)))(a, b), w=[t8_all], r=[lg_all])
        P.ts('dve', msk.v, lg, t8_all[:, t, 3:4], c1.v, op0=ALU.is_ge, op1=ALU.mult)
        P.ts('dve', nmx.v, t8_all[:, t, 0:1], -1.0, None, op0=ALU.mult)
        P.act(ex.v, lg, AF.Exp, bias=nmx.v)
        P.tt('dve', ex.v, ex.v, msk.v, ALU.mult)
        a2, b2 = sm.v.ap, ex.v.ap
        P.I('dve', (lambda a, b: (lambda e: e.reduce_sum(a, b, AX.X)))(a2, b2), w=[sm], r=[ex])
        a3 = sm.v.ap
        P.I('dve', (lambda a: (lambda e: e.reciprocal(a, a)))(a3), w=[sm], r=[sm])
        P.ts('dve', pall[:, t, :], ex.v, sm.v, c1.v, op0=ALU.mult, op1=ALU.mult)
        P.copy('dve', mskb.v, msk.v)
        pr = nps()
        P.mm(pr[:, 0:NE], usb.v, mskb.v)
        P.mm(pr[:, 64:64 + NE], C.onesb.v, mskb.v)
        P.tt('dve', pos_all[:, t, :], carry.v, pr[:, 0:NE], ALU.add)
        P.tt('dve', carry.v, carry.v, pr[:, 64:64 + NE], ALU.add)
    q = P.sb(pfx + "q", [128, NE]); nb = P.sb(pfx + "nb", [128, NE]); bend = P.sb(pfx + "bend", [128, NE])
    pstart = P.sb(pfx + "pstart", [128, NE]); t33 = P.sb(pfx + "t33", [128, 33])
    P.ts('dve', q.v, carry.v, 1.0 / 128.0, None, op0=ALU.mult)
    for e in range(NE):
        P.ts('dve', t33.v, ioJ.v, q[:, e:e + 1], c1.v, op0=ALU.is_lt, op1=ALU.mult)
        a_, b_ = nb[:, e:e + 1].ap, t33.v.ap
        P.I('dve', (lambda a, b: (lambda e_: e_.reduce_sum(a, b, AX.X)))(a_, b_), w=[nb], r=[t33])
    P.copy('dve', bend[:, 0:1], nb[:, 0:1])
    for e in range(1, NE):
        P.tt('dve', bend[:, e:e + 1], bend[:, e - 1:e], nb[:, e:e + 1], ALU.add)
    P.tt('dve', pstart.v, bend.v, nb.v, ALU.subtract)
    P.ts('dve', pstart.v, pstart.v, 128.0, None, op0=ALU.mult)
    Eall = P.sb(pfx + "Eall", [128, NBLK]); tB = P.sb(pfx + "tB", [128, NBLK]); sk = P.sb(pfx + "sk", [128, NBLK])
    P.memset('dve', Eall.v, 0.0)
    for e in range(NE):
        P.ts('dve', tB.v, ioB.v, bend[:, e:e + 1], c1.v, op0=ALU.is_ge, op1=ALU.mult)
        P.tt('dve', Eall.v, Eall.v, tB.v, ALU.add)
    P.ts('dve', Eall.v, Eall.v, float(NE - 1), None, op0=ALU.min)
    P.memset('dve', sk.v, 0.0)
    P.tt('dve', sk[:, 2:NBLK], Eall[:, 2:NBLK], Eall[:, 0:NBLK - 2], ALU.is_equal)
    P.ts('dve', sk.v, sk.v, BIGIDX, None, op0=ALU.mult)
    idxWf = P.sb(pfx + "idxWf", [128, NBLK]); idxW = P.sb(pfx + "idxW", [128, 8 * NBLK], I32)
    idxBf = P.sb(pfx + "idxBf", [128, NBLK]); idxB = P.sb(pfx + "idxB", [128, NBLK], I32)
    P.tt('dve', idxBf.v, Eall.v, sk.v, ALU.add)
    P.copy('dve', idxB.v, idxBf.v)
    iop4 = P.sb(pfx + "iop4", [128, 1]); P.ts('dve', iop4.v, iop.v, 4.0, None, op0=ALU.mult)
    P.ts('dve', idxWf.v, Eall.v, 512.0, None, op0=ALU.mult)
    P.tt('dve', idxWf.v, idxWf.v, sk.v, ALU.add)
    P.ts('dve', idxWf.v, idxWf.v, iop4.v, c1.v, op0=ALU.add, op1=ALU.mult)
    for kq in range(4):
        P.ts('dve', tB.v, idxWf.v, float(kq), None, op0=ALU.add)
        P.copy('dve', idxW[:, kq * NBLK:(kq + 1) * NBLK], tB.v)
    OH = P.sb(pfx + "OH", [128, NBLK]); P.ts('dve', OH.v, Eall.v, iop.v, c1.v, op0=ALU.is_equal, op1=ALU.mult)
    if dbg == 2:
        return
    xb16 = [P.sb(pfx + "xb16_%d" % i, [128, 1024], BF16) for i in range(2)]
    tA = P.sb(pfx + "tA", [128, NE]); oh = P.sb(pfx + "oh", [128, NE]); pr1 = P.sb(pfx + "pr1", [128, NE])
    Xs2 = Xs.h[:]
    for t in range(NTL):
        xt = xts[t % 2]; xb = xb16[t % 2]
        P.dma('sp', xt.v, x1_d[t * 128:(t + 1) * 128, :])
        P.copy('act', xb.v, xt.v)
        P.tt('dve', tA.v, pos_all[:, t, :], pstart.v, ALU.add)
        for k in range(4):
            P.ts('dve', oh.v, lg_all[:, t, :], t8_all[:, t, k:k + 1], c1.v, op0=ALU.is_equal, op1=ALU.mult)
            P.tt('dve', pr1.v, oh.v, tA.v, ALU.mult)
            a_, b_ = slot_f[:, t, k:k + 1].ap, pr1.v.ap
            P.I('dve', (lambda a, b: (lambda e_: e_.reduce_sum(a, b, AX.X)))(a_, b_), w=[slot_f], r=[pr1])
            P.tt('dve', pr1.v, oh.v, pall[:, t, :], ALU.mult)
            a_, b_ = pk_all[:, t, k:k + 1].ap, pr1.v.ap
            P.I('dve', (lambda a, b: (lambda e_: e_.reduce_sum(a, b, AX.X)))(a_, b_), w=[pk_all], r=[pr1])
        P.copy('dve', slot_i[:, t * 4:(t + 1) * 4], slot_f[:, t, :])
        for k in range(4):
            ind_dma(P, xb.v, Xs, Xs2, slot_i[:, t * 4 + k: t * 4 + k + 1], NSLOT - 1, scatter=True)
    if dbg == 3:
        P.wait_all('sp', [Xs])
        return
    with P.scope():
        inv128 = P.sb(pfx + 'inv128', [128, 128], BF16); P.memset('dve', inv128.v, 1.0 / 128.0)
        Wq = [[[P.sb(pfx + "W%d_%d_%d" % (j, i, kq), [128, 2, 1024], BF16) for kq in range(4)] for j in range(3)] for i in range(2)]
        Wt = [[[Wq[i][j][kc // 2][:, kc % 2, :] for kc in range(8)] for j in range(3)] for i in range(2)]
        Ball = [P.sb(pfx + "Ball%d" % j, [NE, 1024], BF16) for j in range(3)]
        for j, bsrc_ in enumerate((b_gate, b_up, b_down)):
            P.dma('pool', Ball[j].v, bsrc_)
        OHb = [P.sb(pfx + "OHb%d" % i, [NE, 128], BF16) for i in range(2)]
        wsrc = [w_.rearrange("e (p kq r) f -> (e p kq) (r f)", p=128, kq=4, r=2) for w_ in (w_gate, w_up, w_down)]
        bsrc = [b_gate, b_up, b_down]
        wtile = [Tile(P, None, pfx + "wsrc%d" % j, "dram") for j in range(3)]
        xblk = [P.sb(pfx + "xblk%d" % i, [128, 1024], BF16) for i in range(2)]
        xbT = [P.sb(pfx + "xbT%d" % i, [128, 8, 128], BF16) for i in range(2)]
        actT = [P.sb(pfx + "actT%d" % i, [128, 8, 128], BF16) for i in range(2)]
        gt = [P.sb(pfx + "g%d" % i, [128, 512]) for i in range(2)]
        s_t = [P.sb(pfx + "s%d" % i, [128, 512]) for i in range(2)]
        ut = [P.sb(pfx + "u%d" % i, [128, 512]) for i in range(2)]
        atok = [P.sb(pfx + "atok%d" % i, [128, 1024], BF16) for i in range(2)]
        yb = [P.sb(pfx + "yb%d" % i, [128, 1024]) for i in range(2)]
        kk = 0
        for b in range(NBLK if dbg != 11 else 10):
            cur = b % 2
            for j in range(3):
                for kq in range(4):
                    ind_dma(P, Wq[cur][j][kq].v.re("p r f -> p (r f)"), wtile[j], wsrc[j],
                            idxW[:, kq * NBLK + b: kq * NBLK + b + 1], NE * 512 - 1)
            Wg, Wu, Wd = Wt[cur]
            P.copy('dve', OHb[cur].v, OH[0:NE, b:b + 1].bc([NE, 128]))
            xk = xblk[cur]
            if dbg != 9 or b < 2:
                P.dma('sp', xk.v, Xs[b * 128:(b + 1) * 128, :])
            xT_ = xbT[cur]
            for half in range(2):
                pz = nps(); pzb = pz.v.bitcast(BF16)
                for c in range(4):
                    kc = half * 4 + c
                    P.tr(pzb[:, c * 128:(c + 1) * 128], xk[:, kc:1024:8], C.identb.v)
                P.copy('act', xT_[:, half * 4:(half + 1) * 4, :], pzb[:, 0:512].re("p (c t) -> p c t", c=4))
            aT = actT[cur]
            at = atok[cur]
            for hf in range(2 if dbg not in (6, 10) else 0):
                hs = slice(hf * 512, (hf + 1) * 512)
                pg = nps()
                for kc in range(8):
                    P.mm(pg.v, xT_[:, kc, :], Wg[kc][:, hs], start=(kc == 0), stop=False)
                P.mm(pg.v, OHb[cur].v, Ball[0][:, hs], start=False, stop=True)
                pu = nps()
                for kc in range(8):
                    P.mm(pu.v, xT_[:, kc, :], Wu[kc][:, hs], start=(kc == 0), stop=False)
                P.mm(pu.v, OHb[cur].v, Ball[1][:, hs], start=False, stop=True)
                g = gt[kk % 2]; s_ = s_t[kk % 2]; u = ut[kk % 2]; kk += 1
                P.ts('dve', g.v, pg.v, 7.0, None, op0=ALU.min)
                P.act(s_.v, g.v, AF.Sigmoid, scale=1.702)
                P.ts('dve', u.v, pu.v, 7.0, -7.0, op0=ALU.min, op1=ALU.max)
                P.tt('dve', g.v, g.v, s_.v, ALU.mult)
                P.stt('dve', at[:, hs], u.v, 1.0, g.v, ALU.add, ALU.mult)
            for half in range(2 if dbg not in (6, 10) else 0):
                pz = nps(); pzb = pz.v.bitcast(BF16)
                for c in range(4):
                    fc = half * 4 + c
                    P.tr(pzb[:, c * 128:(c + 1) * 128], at[:, fc:1024:8], C.identb.v)
                P.copy('act', aT[:, half * 4:(half + 1) * 4, :], pzb[:, 0:512].re("p (c t) -> p c t", c=4))
            y = yb[cur]
            if dbg in (6, 10):
                P.memset('dve', y.v, 0.5)
            for hf in range(2 if dbg not in (6, 10) else 0):
                py = nps()
                hs = slice(hf * 512, (hf + 1) * 512)
                for fc in range(8):
                    P.mm(py.v, aT[:, fc, :], Wd[fc][:, hs], start=(fc == 0), stop=False)
                P.mm(py.v, OHb[cur].v, Ball[2][:, hs], start=False, stop=True)
                P.copy('act', y[:, hs], py.v)
            if dbg != 7:
                P.dma('sp', Ys[b * 128:(b + 1) * 128, :], y.v)
    if dbg == 4:
        return
    rk = [P.sb(pfx + "rk%d" % i, [128, 1024]) for i in range(4)]
    acA = P.sb(pfx + "acA", [128, 1024]); acB = P.sb(pfx + "acB", [128, 1024])
    st = P.sb(pfx + "st", [128, 2, 6]); mv = P.sb(pfx + "mv", [128, 2]); rstd = P.sb(pfx + "rstd", [128, 1])
    Ys2 = Ys.h[:]
    for t in range(NTL):
        xt = xts[t % 2]
        P.dma('sp', xt.v, x1_d[t * 128:(t + 1) * 128, :])
        for k in range(4):
            ind_dma(P, rk[k].v, Ys, Ys2, slot_i[:, t * 4 + k: t * 4 + k + 1], NSLOT - 1)
        P.ts('dve', acA.v, rk[0].v, pk_all[:, t, 0:1], c1.v, op0=ALU.mult, op1=ALU.mult)
        P.stt('dve', acB.v, rk[1].v, pk_all[:, t, 1:2], acA.v, ALU.mult, ALU.add)
        P.stt('dve', acA.v, rk[2].v, pk_all[:, t, 2:3], acB.v, ALU.mult, ALU.add)
        P.stt('dve', acB.v, rk[3].v, pk_all[:, t, 3:4], acA.v, ALU.mult, ALU.add)
        P.stt('dve', acA.v, xt.v, DN_ALPHA, acB.v, ALU.mult, ALU.add)
        layer_norm(P, acA.v, gB.v, bB.v, st.v, mv.v, rstd.v)
        P.dma('sp', out_d[t * 128:(t + 1) * 128, :], acA.v)
```

```python
import numpy as np
import concourse.bass as bass
import concourse.mybir as mybir
from concourse.bass_utils import run_bass_kernel_spmd
from contextlib import ExitStack

F32 = mybir.dt.float32
F32R = mybir.dt.float32r
BF16 = mybir.dt.bfloat16
I32 = mybir.dt.int32
U32 = mybir.dt.uint32
AF = mybir.ActivationFunctionType
ALU = mybir.AluOpType
AX = mybir.AxisListType

ENGS = ['pe', 'act', 'dve', 'pool', 'sp']


class Tile:
    def __init__(self, P, h, name, space='sb'):
        self.P = P
        self.h = h
        self.name = name
        self.space = space
        self.lw = {}
        self.rd = {}
        self.dsem = None
        self.dcnt = 0

    def __getitem__(self, k):
        return V(self, self.h[k])

    @property
    def v(self):
        return V(self, self.h[:])


class V:
    def __init__(self, tile, ap):
        self.tile = tile
        self.ap = ap

    def __getitem__(self, k):
        return V(self.tile, self.ap[k])

    def bitcast(self, dt):
        return V(self.tile, self.ap.bitcast(dt))

    def bc(self, shape):
        return V(self.tile, self.ap.to_broadcast(shape))

    def re(self, s, **kw):
        return V(self.tile, self.ap.rearrange(s, **kw))


def _ap(x):
    if isinstance(x, Tile):
        return x.h[:]
    return x.ap if isinstance(x, V) else x


class Prog:
    def __init__(self, nc, es, same_engine_sync=None):
        self.nc = nc
        self.es = es
        self.es_top = es
        self.all_tiles = []
        self.stream = {e: [] for e in ENGS}
        self.sems = {}
        self.cnt = {e: 0 for e in ENGS}
        self.known = {e: {} for e in ENGS}
        import os as _os
        self.same = (_os.environ.get('KSAME', '1') == '1') if same_engine_sync is None else same_engine_sync
        self.nsem = 0
        for e in ['pe', 'act', 'dve', 'pool']:
            self.sems[e] = es.enter_context(nc.semaphore("s_" + e))
            self.nsem += 1
        self.ninst = 0
        self.nwait = 0

    def sb(self, name, shape, dt=F32):
        h = self.es.enter_context(self.nc.sbuf_tensor(name, list(shape), dt))
        return Tile(self, h, name)

    def ps(self, name, shape, dt=F32):
        h = self.es.enter_context(self.nc.psum_tensor(name, list(shape), dt))
        return Tile(self, h, name, 'ps')

    def dram(self, name, shape, dt=F32, kind="Internal"):
        h = self.nc.dram_tensor(name, list(shape), dt, kind=kind)
        return Tile(self, h.ap(), name, 'dram')

    def _tsem(self, t):
        if t.dsem is None:
            key = "d_" + t.name
            self.sems[key] = self.es_top.enter_context(self.nc.semaphore(key))
            self.nsem += 1
            t.dsem = key
            self.all_tiles.append(t)
        return t.dsem

    def _waits(self, eng, rt, wt):
        need = {}
        for t in rt:
            for s, v in t.lw.items():
                need[s] = max(need.get(s, 0), v)
        for t in wt:
            for s, v in t.lw.items():
                need[s] = max(need.get(s, 0), v)
            for s, v in t.rd.items():
                need[s] = max(need.get(s, 0), v)
        out = []
        kn = self.known[eng]
        for s, v in need.items():
            if s == eng and (eng == 'pe' or not self.same):
                continue
            if kn.get(s, 0) < v:
                kn[s] = v
                out.append((s, v))
        return out

    def _record(self, ev, rt, wt):
        s, v = ev
        for t in wt:
            t.lw[s] = max(t.lw.get(s, 0), v)
            t.rd = {}
        for t in rt:
            if t in wt:
                continue
            t.rd[s] = max(t.rd.get(s, 0), v)

    @staticmethod
    def _tiles(xs):
        out = []
        for x in xs:
            if x is None:
                continue
            t = x.tile if isinstance(x, V) else x
            if isinstance(t, Tile) and t not in out:
                out.append(t)
        return out

    def I(self, eng, fn, w=(), r=()):
        wt = self._tiles(w)
        rt = self._tiles(r)
        for t in rt:
            if t.space == 'ps' and t not in wt and eng != 'pe':
                wt.append(t)
        waits = self._waits(eng, rt, wt)
        self.cnt[eng] += 1
        ev = (eng, self.cnt[eng])
        self.stream[eng].append((waits, fn, (eng, 1)))
        self._record(ev, rt, wt)
        self.ninst += 1
        self.nwait += len(waits)

    def dma(self, q, out, in_, **kw):
        wt = self._tiles([out])
        rt = self._tiles([in_])
        owner = None
        for x in (out, in_):
            t_ = x.tile if isinstance(x, V) else (x if isinstance(x, Tile) else None)
            if t_ is not None and t_.space == 'sb':
                owner = t_
        if owner is None:
            for x in (out, in_):
                t_ = x.tile if isinstance(x, V) else (x if isinstance(x, Tile) else None)
                if t_ is not None and owner is None:
                    owner = t_
        key = self._tsem(owner)
        waits = self._waits(q, rt, wt)
        owner.dcnt += 16
        ev = (key, owner.dcnt)
        o, i = _ap(out), _ap(in_)
        self.stream[q].append((waits, lambda e: e.dma_start(out=o, in_=i, **kw), (key, 16)))
        self._record(ev, rt, wt)
        self.ninst += 1
        self.nwait += len(waits)
        return ev

    def wait_all(self, eng, tiles):
        ts = self._tiles(tiles)
        waits = self._waits(eng, ts, ts)
        self.stream[eng].append((waits, None, None))

    def mm(self, out, lhsT, rhs, start=True, stop=True, **kw):
        o, a, b = _ap(out), _ap(lhsT), _ap(rhs)
        self.I('pe', lambda e: e.matmul(o, a, b, start=start, stop=stop, **kw), w=[out], r=[lhsT, rhs])

    def tr(self, out, in_, ident):
        o, a, b = _ap(out), _ap(in_), _ap(ident)
        self.I('pe', lambda e: e.transpose(o, a, b), w=[out], r=[in_, ident])

    def act(self, out, in_, func, bias=None, scale=1.0, accum_out=None, eng='act'):
        o, a = _ap(out), _ap(in_)
        kw = {}
        if bias is not None:
            kw['bias'] = _ap(bias)
        if accum_out is not None:
            kw['accum_out'] = _ap(accum_out)
        sc = _ap(scale)
        self.I(eng, lambda e: e.activation(o, a, func, scale=sc, **kw),
               w=[out, accum_out], r=[in_, bias, scale if isinstance(scale, V) else None])

    def tt(self, eng, out, in0, in1, op):
        o, a, b = _ap(out), _ap(in0), _ap(in1)
        self.I(eng, lambda e: e.tensor_tensor(o, a, b, op), w=[out], r=[in0, in1])

    def ts(self, eng, out, in0, s1, s2=None, op0=ALU.mult, op1=None, accum_out=None):
        o, a = _ap(out), _ap(in0)
        x1, x2 = _ap(s1), _ap(s2)
        kw = {}
        if op1 is not None:
            kw['op1'] = op1
        if accum_out is not None:
            kw['accum_out'] = _ap(accum_out)
        self.I(eng, lambda e: e.tensor_scalar(o, a, x1, x2, op0, **kw), w=[out, accum_out],
               r=[in0, s1 if isinstance(s1, V) else None, s2 if isinstance(s2, V) else None])

    def stt(self, eng, out, in0, scalar, in1, op0, op1):
        o, a, b = _ap(out), _ap(in0), _ap(in1)
        s = _ap(scalar)
        self.I(eng, lambda e: e.scalar_tensor_tensor(o, a, s, b, op0, op1), w=[out],
               r=[in0, in1, scalar if isinstance(scalar, V) else None])

    def copy(self, eng, out, in_):
        o, a = _ap(out), _ap(in_)
        if eng == 'act':
            self.I(eng, lambda e: e.copy(o, a), w=[out], r=[in_])
        else:
            self.I(eng, lambda e: e.tensor_copy(o, a), w=[out], r=[in_])

    def memset(self, eng, out, val):
        o = _ap(out)
        self.I(eng, lambda e: e.memset(o, val), w=[out])

    def barrier(self):
        evs = {e: self.cnt[e] for e in ['pe', 'act', 'dve', 'pool'] if self.cnt[e] > 0}
        for t in self.all_tiles:
            if t.dcnt > 0:
                evs[t.dsem] = t.dcnt
        for eng in ENGS:
            kn = self.known[eng]
            waits = []
            for s_, v in evs.items():
                if kn.get(s_, 0) < v:
                    kn[s_] = v
                    waits.append((s_, v))
            if waits:
                self.stream[eng].append((waits, None, None))

    def scope(self):
        P = self

        class _S:
            def __enter__(self_):
                self_.old = P.es
                self_.st = ExitStack()
                self_.st.__enter__()
                P.es = self_.st
                return self_

            def __exit__(self_, *a):
                P.barrier()
                P.emit()
                P.es = self_.old
                self_.st.__exit__(None, None, None)
                return False
        return _S()

    def emit(self):
        nc = self.nc
        sems = self.sems
        with nc.Block() as block:
            def run(engobj, name):
                for waits, fn, inc in self.stream[name]:
                    for s, v in waits:
                        engobj.wait_ge(sems[s], v)
                    if fn is not None:
                        ins = fn(engobj)
                        ins.then_inc(sems[inc[0]], inc[1])

            @block.tensor
            def _(e):
                run(e, 'pe')

            @block.scalar
            def _(e):
                run(e, 'act')

            @block.vector
            def _(e):
                run(e, 'dve')

            @block.gpsimd
            def _(e):
                run(e, 'pool')

            @block.sync
            def _(e):
                run(e, 'sp')
        self.stream = {e: [] for e in ENGS}


D = 1024
MIXW = 512
NEXP = 32
DN_ALPHA = (2.0 * 2) ** 0.25
EPS = 1e-5
OFF = dict(r_q=0, r_k=256, r_v=512, r_g=1024, m_x=1536, m_i=2048, m_f=2052, m_o=2056,
           a_q=2568, a_k=3336, a_v=4104, gates=5640)
D_IN = 8712


class Ctx:
    pass


def make_ctx(P):
    C = Ctx()
    C.identf = P.sb("identf", [128, 128], F32)
    C.identb = P.sb("identb", [128, 128], BF16)
    C.onesb = P.sb("onesb", [128, 128], BF16)
    C.onesf = P.sb("onesf", [128, 128], F32)
    P.memset('pool', C.identf.v, 1.0)
    o = C.identf.v.ap
    P.I('pool', lambda e: e.affine_select(o, o, [[-1, 128]], ALU.is_equal, 0.0, base=0, channel_multiplier=1),
        w=[C.identf], r=[C.identf])
    P.copy('pool', C.identb.v, C.identf.v)
    P.memset('pool', C.onesb.v, 1.0)
    P.memset('pool', C.onesf.v, 1.0)
    C.ps = [P.ps("psb%d" % i, [128, 512], F32) for i in range(8)]
    return C


def load_w_cast(P, dst, src, q='pool'):
    cols = src.shape[-1]
    c0 = 0
    while c0 < cols:
        c1 = min(cols, c0 + 1024)
        P.dma(q, dst[:, :, c0:c1], src[:, :, c0:c1])
        c0 = c1


def bcast_rows(P, dst, src1d, q='act'):
    P.dma(q, dst, src1d.partition_broadcast(128))


def x_transpose(P, C, xt, outs, psl):
    for half in range(2):
        pt = psl[half]
        for c in range(4):
            k = half * 4 + c
            P.tr(pt[:, c * 128:(c + 1) * 128], xt[:, k * 128:(k + 1) * 128], C.identf.v)
        for (o, eng) in outs:
            P.copy(eng, o[:, half * 4:(half + 1) * 4, :], pt.v.re("p (c t) -> p c t", c=4))


def layer_norm(P, r, g_b, b_b, st, mv, rstd, eng2='pool'):
    for hf in range(2):
        a, b = st[:, hf, :].ap, r[:, hf * 512:(hf + 1) * 512].ap
        P.I('dve', (lambda a, b: (lambda e: e.bn_stats(a, b)))(a, b), w=[st], r=[r])
    a, b = mv.ap, st.ap
    P.I('dve', lambda e: e.bn_aggr(a, b), w=[mv], r=[st])
    P.ts('dve', rstd, mv[:, 1:2], EPS, None, op0=ALU.add)
    P.act(rstd, rstd, AF.Ln)
    P.act(rstd, rstd, AF.Exp, scale=-0.5)
    P.ts('dve', r, r, mv[:, 0:1], rstd, op0=ALU.subtract, op1=ALU.mult)
    P.tt(eng2, r, r, g_b, ALU.mult)
    P.tt(eng2, r, r, b_b, ALU.add)


def pass_merge(P, C, NT, x_d, yT_d, w_in_l, w_branch_l, w_out_l, ln_g, ln_b, x1_d, pfx="m"):
    wg = P.sb(pfx + "wg", [128, 8, 3072], BF16)
    wb = P.sb(pfx + "wb", [128, 12, 1024], BF16)
    wo = P.sb(pfx + "wo", [128, 8, 1024], BF16)
    load_w_cast(P, wg.v, w_in_l[:, OFF['gates']:D_IN].rearrange("(kc p) c -> p kc c", p=128))
    load_w_cast(P, wb.v, w_branch_l.rearrange("b (kc p) c -> p (b kc) c", p=128))
    load_w_cast(P, wo.v, w_out_l.rearrange("(kc p) c -> p kc c", p=128))
    gB = P.sb(pfx + "gB", [128, 1024]); bB = P.sb(pfx + "bB", [128, 1024])
    bcast_rows(P, gB.v, ln_g); bcast_rows(P, bB.v, ln_b)
    xts = [P.sb(pfx + "xt%d" % i, [128, 1024]) for i in range(2)]
    xTs = [P.sb(pfx + "xT%d" % i, [128, 8, 128], BF16) for i in range(2)]
    yTs = [[P.sb(pfx + "yT%d_%d" % (b, i), [128, 4, 128], BF16) for i in range(2)] for b in range(3)]
    mg = [P.sb(pfx + "mg%d" % i, [128, 1024]) for i in range(2)]
    mT = [P.sb(pfx + "mT%d" % i, [128, 8, 128], BF16) for i in range(2)]
    sg = [P.sb(pfx + "sg%d" % i, [128, 512]) for i in range(2)]
    tmp = [P.sb(pfx + "tmp%d" % i, [128, 512]) for i in range(2)]
    rr = [P.sb(pfx + "rr%d" % i, [128, 1024]) for i in range(2)]
    st = P.sb(pfx + "st", [128, 2, 6]); mv = P.sb(pfx + "mv", [128, 2]); rstd = P.sb(pfx + "rstd", [128, 1])
    k = 0
    for t in range(NT // 128):
        xt = xts[t % 2]; xT = xTs[t % 2]
        P.dma('sp', xt.v, x_d[t * 128:(t + 1) * 128, :])
        x_transpose(P, C, xt.v, [(xT.v, 'act')], [C.ps[0], C.ps[1]])
        for b in range(3):
            P.dma('act', yTs[b][t % 2].v, yT_d[b][:, :, t * 128:(t + 1) * 128])
        m = mg[t % 2]
        for b in range(3):
            for hf in range(2):
                pg = C.ps[2 + (k % 2)]; pb = C.ps[4 + (k % 2)]; s = sg[k % 2]; tm = tmp[k % 2]
                k += 1
                for kc in range(8):
                    P.mm(pg.v, xT[:, kc, :], wg[:, kc, b * 1024 + hf * 512: b * 1024 + (hf + 1) * 512],
                         start=(kc == 0), stop=(kc == 7))
                for kc in range(4):
                    P.mm(pb.v, yTs[b][t % 2][:, kc, :], wb[:, b * 4 + kc, hf * 512:(hf + 1) * 512],
                         start=(kc == 0), stop=(kc == 3))
                P.act(s.v, pg.v, AF.Sigmoid)
                msl = m[:, hf * 512:(hf + 1) * 512]
                if b == 0:
                    P.tt('dve', msl, s.v, pb.v, ALU.mult)
                else:
                    P.tt('dve', tm.v, s.v, pb.v, ALU.mult)
                    P.tt('pool', msl, msl, tm.v, ALU.add)
        x_transpose(P, C, m.v, [(mT[t % 2].v, 'act')], [C.ps[6], C.ps[7]])
        r = rr[t % 2]
        for hf in range(2):
            po = C.ps[2 + (k % 2)]
            k += 1
            for kc in range(8):
                P.mm(po.v, mT[t % 2][:, kc, :], wo[:, kc, hf * 512:(hf + 1) * 512], start=(kc == 0), stop=(kc == 7))
            P.stt('dve', r[:, hf * 512:(hf + 1) * 512], xt[:, hf * 512:(hf + 1) * 512], DN_ALPHA, po.v,
                  ALU.mult, ALU.add)
        layer_norm(P, r.v, gB.v, bB.v, st.v, mv.v, rstd.v)
        P.dma('sp', x1_d[t * 128:(t + 1) * 128, :], r.v)


def pass_moe(P, C, NT, x1_d, w_router, b_router, w_gate, b_gate, w_up, b_up, w_down, b_down, ln_g, ln_b, out_d,
             NE=NEXP, pfx="e", TGT=4, dbg=0):
    TG = TGT * 128
    gB = P.sb(pfx + "gB", [128, 1024]); bB = P.sb(pfx + "bB", [128, 1024])
    bcast_rows(P, gB.v, ln_g); bcast_rows(P, bB.v, ln_b)
    wr = P.sb(pfx + "wr", [128, 8, NE], F32R)
    wr0 = P.sb(pfx + "wr0", [128, 8, NE])
    P.dma('act', wr0.v, w_router.rearrange("(kc p) e -> p kc e", p=128))
    P.copy('dve', wr.v, wr0.v)
    brB = P.sb(pfx + "brB", [128, NE]); bcast_rows(P, brB.v, b_router)
    bgT = P.sb(pfx + "bgT", [128, NE, 8]); buT = P.sb(pfx + "buT", [128, NE, 8])
    bstage = P.sb(pfx + "bstage", [128, 128])
    if dbg in (3, 7):
        P.memset('dve', bgT.v, 0.0); P.memset('dve', buT.v, 0.0)
    for (dstT, src) in (((bgT, b_gate), (buT, b_up)) if dbg not in (3, 7) else ()):
        rows = NE * 8
        srcv = src.rearrange("e (fc p) -> (e fc) p", p=128)
        dv = dstT.v.re("p e fc -> p (e fc)")
        r0 = 0
        while r0 < rows:
            r1 = min(rows, r0 + 128)
            n = r1 - r0
            P.dma('act', bstage[0:n, :], srcv[r0:r1, :])
            pz = C.ps[7]
            P.tr(pz[:, 0:n], bstage[0:n, :], C.identf[0:n, 0:n])
            P.copy('dve', dv[:, r0:r1], pz[:, 0:n])
            r0 = r1
    bd = P.sb(pfx + "bd", [NE, 1024], F32R)
    bd0 = P.sb(pfx + "bd0", [NE, 1024])
    P.dma('act', bd0.v, b_down)
    P.copy('dve', bd.v, bd0.v)
    W = [[P.sb(pfx + "W%d_%d" % (j, i), [128, 8, 1024], BF16) for j in range(3)] for i in range(2)]
    xts = [P.sb(pfx + "xt%d" % i, [128, 1024]) for i in range(TGT)]
    xTg = P.sb(pfx + "xTg", [128, 8, TG], BF16)
    xT32 = P.sb(pfx + "xT32", [128, 8, 128], F32R)
    acc = P.sb(pfx + "acc", [128, TGT, 1024])
    pall = P.sb(pfx + "pall", [128, TGT, NE])
    actT = P.sb(pfx + "actT", [128, 8, TG], BF16)
    lg = P.sb(pfx + "lg", [128, NE]); t8 = P.sb(pfx + "t8", [128, 8]); msk = P.sb(pfx + "msk", [128, NE])
    ex = P.sb(pfx + "ex", [128, NE]); sm = P.sb(pfx + "sm", [128, 1]); nmx = P.sb(pfx + "nmx", [128, 1])
    pT = P.sb(pfx + "pT", [NE, 128], F32R)
    gt = [P.sb(pfx + "g%d" % i, [128, TG]) for i in range(2)]
    st_ = [P.sb(pfx + "s%d" % i, [128, TG]) for i in range(2)]
    ut = [P.sb(pfx + "u%d" % i, [128, TG]) for i in range(2)]
    st = P.sb(pfx + "st", [128, 2, 6]); mv = P.sb(pfx + "mv", [128, 2]); rstd = P.sb(pfx + "rstd", [128, 1])
    c1 = P.sb(pfx + "c1", [128, 1]); c7 = P.sb(pfx + "c7", [128, 1])
    P.memset('dve', c1.v, 1.0); P.memset('dve', c7.v, 7.0)
    rr = [P.sb(pfx + "rr%d" % i, [128, 1024]) for i in range(2)]
    tmpq = [P.sb(pfx + "tq%d" % i, [128, 512]) for i in range(2)]
    assert TG == 512
    if dbg == 5:
        P.wait_all('sp', [gB, bB, wr, brB, bd, bgT, buT])
        return
    wcnt = 0
    kk = 0
    for gi in range(NT // TG):
        for tt in range(TGT):
            t = gi * TGT + tt
            xt = xts[tt]
            P.dma('sp', xt.v, x1_d[t * 128:(t + 1) * 128, :])
            x_transpose(P, C, xt.v, [(xTg[:, :, tt * 128:(tt + 1) * 128], 'act')] + ([(xT32.v, 'dve')] if dbg != 3 else []), [C.ps[0], C.ps[1]])
            if dbg in (2, 3, 7, 8):
                P.memset('dve', acc[:, tt, :], 0.0)
                P.memset('dve', pall[:, tt, :], 0.25)
            else:
                pl = C.ps[6]
                for kc in range(8):
                    P.mm(pl[:, 0:NE], xT32[:, kc, :], wr[:, kc, :], start=(kc == 0), stop=(kc == 7))
                P.tt('dve', lg.v, pl[:, 0:NE], brB.v, ALU.add)
                a, b = t8.v.ap, lg.v.ap
                P.I('dve', (lambda a, b: (lambda e: e.max(out=a, in_=b)))(a, b), w=[t8], r=[lg])
                P.ts('dve', msk.v, lg.v, t8[:, 3:4], c1.v, op0=ALU.is_ge, op1=ALU.mult)
                P.ts('dve', nmx.v, t8[:, 0:1], -1.0, None, op0=ALU.mult)
                P.act(ex.v, lg.v, AF.Exp, bias=nmx.v)
                P.tt('dve', ex.v, ex.v, msk.v, ALU.mult)
                a2, b2 = sm.v.ap, ex.v.ap
                P.I('dve', (lambda a, b: (lambda e: e.reduce_sum(a, b, AX.X)))(a2, b2), w=[sm], r=[ex])
                a3 = sm.v.ap
                P.I('dve', (lambda a: (lambda e: e.reciprocal(a, a)))(a3), w=[sm], r=[sm])
                P.ts('dve', pall[:, tt, :], ex.v, sm.v, c1.v, op0=ALU.mult, op1=ALU.mult)
                P.tr(pl[0:NE, 128:256], pall[:, tt, :], C.identf.v)
                P.copy('dve', pT.v, pl[0:NE, 128:256])
                for hf in range(2):
                    pb = C.ps[7]
                    P.mm(pb.v, pT.v, bd[:, hf * 512:(hf + 1) * 512])
                    P.copy('dve', acc[:, tt, hf * 512:(hf + 1) * 512], pb.v)
        for e in range(NE if dbg not in (1, 3, 7, 8) else 0):
            Wg, Wu, Wd = W[wcnt % 2]
            wcnt += 1
            P.dma('pool', Wg.v, w_gate[e].rearrange("(kc p) f -> p kc f", p=128))
            P.dma('pool', Wu.v, w_up[e].rearrange("(kc p) f -> p kc f", p=128))
            P.dma('pool', Wd.v, w_down[e].rearrange("(kc p) f -> p kc f", p=128))
            for fc in range(8):
                pg = C.ps[(kk % 2) * 2]; pu = C.ps[(kk % 2) * 2 + 1]
                g = gt[kk % 2]; s = st_[kk % 2]; u = ut[kk % 2]
                kk += 1
                for kc in range(8):
                    P.mm(pg.v, Wg[:, kc, fc * 128:(fc + 1) * 128], xTg[:, kc, :], start=(kc == 0), stop=(kc == 7))
                for kc in range(8):
                    P.mm(pu.v, Wu[:, kc, fc * 128:(fc + 1) * 128], xTg[:, kc, :], start=(kc == 0), stop=(kc == 7))
                P.ts('dve', g.v, pg.v, bgT[:, e, fc:fc + 1], c7.v, op0=ALU.add, op1=ALU.min)
                P.act(s.v, g.v, AF.Sigmoid, scale=1.702)
                P.ts('dve', u.v, pu.v, buT[:, e, fc:fc + 1], c7.v, op0=ALU.add, op1=ALU.min)
                P.ts('dve', u.v, u.v, -7.0, 1.0, op0=ALU.max, op1=ALU.add)
                P.tt('dve', g.v, g.v, s.v, ALU.mult)
                P.tt('dve', actT[:, fc, :], g.v, u.v, ALU.mult)
            for tt in range(TGT):
                for hf in range(2):
                    py = C.ps[4 + (kk % 2)]
                    kk += 1
                    for fc in range(8):
                        P.mm(py.v, actT[:, fc, tt * 128:(tt + 1) * 128], Wd[:, fc, hf * 512:(hf + 1) * 512],
                             start=(fc == 0), stop=(fc == 7))
                    av = acc[:, tt, hf * 512:(hf + 1) * 512]
                    tq = tmpq[kk % 2]
                    P.ts('dve', tq.v, py.v, pall[:, tt, e:e + 1], c1.v, op0=ALU.mult, op1=ALU.mult)
                    P.tt('dve', av, av, tq.v, ALU.add)
        for tt in range(TGT):
            t = gi * TGT + tt
            r = rr[tt % 2].v
            P.stt('dve', r, xts[tt].v, DN_ALPHA, acc[:, tt, :], ALU.mult, ALU.add)
            layer_norm(P, r, gB.v, bB.v, st.v, mv.v, rstd.v)
            P.dma('sp', out_d[t * 128:(t + 1) * 128, :], r)


RET_GAMMA = [1.0 - 2.0 ** (-5.0 - h) for h in range(4)]


def host_consts_scan(NT):
    j = np.arange(128, dtype=np.float64)
    lg = np.log(np.array(RET_GAMMA, dtype=np.float64))
    aR = np.exp(lg[None, :] * (j[:, None] + 1.0)) * (64 ** -0.5)
    bR = np.exp(-lg[None, :] * (j[:, None] + 1.0))
    eR = np.zeros((128, 2)); gR = np.zeros((128, 2))
    for h in range(4):
        ps = (h % 2) * 64
        eR[ps:ps + 64, h // 2] = np.exp(lg[h] * 128.0)
        gR[ps:ps + 64, h // 2] = np.exp(lg[h] * float(NT))
    mask = (j[:, None] <= j[None, :]).astype(np.float64)
    return dict(aR=aR.astype(np.float32), bR=bR.astype(np.float32), eR=eR.astype(np.float32),
                gR=gR.astype(np.float32), mask=mask.astype(np.float32))


def host_blockdiag(w):
    out = np.zeros((4, 128, 128), dtype=np.float32)
    for h in range(4):
        for n in range(32):
            out[h, 4 * n:4 * n + 4, 4 * n:4 * n + 4] = w[32 * h + n]
    return out


class PSRot:
    def __init__(self, C, banks):
        self.C = C; self.banks = banks; self.i = 0

    def __call__(self):
        b = self.C.ps[self.banks[self.i % len(self.banks)]]
        self.i += 1
        return b


def small_T(P, C, dst, src2d, rows, stage, ps):
    P.dma('act', stage[0:rows, :], src2d)
    P.tr(ps[:, 0:rows], stage[0:rows, :], C.identf[0:rows, 0:rows])
    P.copy('dve', dst, ps[:, 0:rows])


def pass_scan(P, C, NT, x_d, xprev_d, w_in_l, prm, cst, init, outs, mode="full", pfx="s"):
    full = (mode == "full")
    NW = 2568
    W = P.sb(pfx + "W", [128, 8, NW], BF16)
    load_w_cast(P, W.v, w_in_l[:, 0:NW].rearrange("(kc p) c -> p kc c", p=128))
    BD = {}
    for nm in ("bdq", "bdk", "bdv"):
        BD[nm] = P.sb(pfx + nm, [128, 4, 128], BF16)
        P.dma('pool', BD[nm].v, prm[nm].rearrange("h i o -> i h o"))
    stage = P.sb(pfx + "stage", [128, 128])
    cwT = P.sb(pfx + "cwT", [128, 16]); cbT = P.sb(pfx + "cbT", [128, 4])
    small_T(P, C, cwT.v, prm["ml_conv_w"].rearrange("k (c p) -> (k c) p", p=128), 16, stage, C.ps[7])
    small_T(P, C, cbT.v, prm["ml_conv_b"].rearrange("(c p) -> c p", p=128), 4, stage, C.ps[7])
    biB = P.sb(pfx + "biB", [128, 4]); bfB = P.sb(pfx + "bfB", [128, 4])
    bcast_rows(P, biB.v, prm["ml_bi"]); bcast_rows(P, bfB.v, prm["ml_bf"])
    aR = P.sb(pfx + "aR", [128, 4]); bR = P.sb(pfx + "bR", [128, 4]); eR = P.sb(pfx + "eR", [128, 2]); gR = P.sb(pfx + "gR", [128, 2])
    for t_, n_ in ((aR, "aR"), (bR, "bR"), (eR, "eR"), (gR, "gR")):
        P.dma('act', t_.v, cst[n_])
    mask = P.sb(pfx + "mask", [128, 128]); P.dma('act', mask.v, cst["mask"])
    maskr = P.sb(pfx + "maskr", [128, 128], F32R); P.copy('dve', maskr.v, mask.v)
    onesr = P.sb(pfx + "onesr", [128, 128], F32R); P.copy('dve', onesr.v, C.onesf.v)
    c1 = P.sb(pfx + "c1", [128, 1]); P.memset('dve', c1.v, 1.0)
    if full:
        gnR = P.sb(pfx + "gnR", [128, 512]); gnM = P.sb(pfx + "gnM", [128, 512]); skM = P.sb(pfx + "skM", [128, 512])
        bcast_rows(P, gnR.v, prm["ret_gn"]); bcast_rows(P, gnM.v, prm["ml_gn"]); bcast_rows(P, skM.v, prm["ml_skip"])
    Sret = P.sb(pfx + "Sret", [128, 2, 128]); Sretb = P.sb(pfx + "Sretb", [128, 2, 128], BF16)
    Cml = P.sb(pfx + "Cml", [128, 4, 129]); Cmlb = P.sb(pfx + "Cmlb", [128, 4, 129], BF16)
    tmpS = P.sb(pfx + "tmpS", [128, 4, 129]); tmpS2 = P.sb(pfx + "tmpS2", [128, 4, 129])
    totacc = P.sb(pfx + "totacc", [128, 4])
    P.memset('dve', Sret.v, 0.0); P.memset('dve', Cml.v, 0.0); P.memset('dve', totacc.v, 0.0)
    if init is not None:
        sel = P.sb(pfx + "sel", [128, 3]); nsel = P.sb(pfx + "nsel", [128, 3])
        P.dma('act', sel.v, init["sel"]); P.dma('act', nsel.v, init["nsel"])
        Fr = P.sb(pfx + "Fr", [128, 2, 128]); Fm = P.sb(pfx + "Fm", [128, 4, 129]); tl = P.sb(pfx + "tl", [128, 4])
        Gm = P.sb(pfx + "Gm", [128, 4])
        for q in range(3):
            P.dma('act', Fr.v, init["Fret"][q]); P.dma('act', Fm.v, init["Fml"][q]); P.dma('act', tl.v, init["totL"][q])
            P.act(Gm.v, tl.v, AF.Exp, scale=-1.0)
            for hp in range(2):
                P.act(tmpS[:, hp, 0:128], Sret[:, hp, :], AF.Copy, scale=gR[:, hp:hp + 1])
                P.tt('dve', tmpS[:, hp, 0:128], tmpS[:, hp, 0:128], Fr[:, hp, :], ALU.add)
                P.act(tmpS[:, hp, 0:128], tmpS[:, hp, 0:128], AF.Copy, scale=sel[:, q:q + 1])
                P.act(tmpS2[:, hp, 0:128], Sret[:, hp, :], AF.Copy, scale=nsel[:, q:q + 1])
                P.tt('dve', Sret[:, hp, :], tmpS[:, hp, 0:128], tmpS2[:, hp, 0:128], ALU.add)
            for h in range(4):
                P.act(tmpS[:, h, :], Cml[:, h, :], AF.Copy, scale=Gm[:, h:h + 1])
                P.tt('dve', tmpS[:, h, :], tmpS[:, h, :], Fm[:, h, :], ALU.add)
                P.act(tmpS[:, h, :], tmpS[:, h, :], AF.Copy, scale=sel[:, q:q + 1])
                P.act(tmpS2[:, h, :], Cml[:, h, :], AF.Copy, scale=nsel[:, q:q + 1])
                P.tt('dve', Cml[:, h, :], tmpS[:, h, :], tmpS2[:, h, :], ALU.add)
    P.copy('dve', Sretb.v, Sret.v); P.copy('dve', Cmlb.v, Cml.v)
    SC = 512
    xts = [P.sb(pfx + "xt%d" % i, [128, 1024]) for i in range(2)]
    xT = P.sb(pfx + "xT", [128, 8, SC], BF16)
    rqT = P.sb(pfx + "rqT", [128, 2, SC], BF16); rkT = P.sb(pfx + "rkT", [128, 2, SC], BF16)
    mxT = P.sb(pfx + "mxT", [128, 4, 3 + SC]); mxb = P.sb(pfx + "mxb", [128, 4, SC], BF16)
    cva = P.sb(pfx + "cva", [128, SC]); cvb = P.sb(pfx + "cvb", [128, SC])
    mcT = P.sb(pfx + "mcT", [128, 4, SC], BF16)
    qmT = P.sb(pfx + "qmT", [128, 4, SC], BF16); kmT = P.sb(pfx + "kmT", [128, 4, SC], BF16)
    rk_tok = P.sb(pfx + "rk_tok", [128, 256], BF16); km_tok = P.sb(pfx + "km_tok", [128, 512], BF16)
    vpR = P.sb(pfx + "vpR", [128, 4, 128], BF16); vpM = P.sb(pfx + "vpM", [128, 4, 129], BF16)
    g8 = P.sb(pfx + "g8", [128, 8]); L1 = P.sb(pfx + "L1", [128, 4], F32R); e1 = P.sb(pfx + "e1", [128, 4])
    igt = P.sb(pfx + "igt", [128, 4]); aM = P.sb(pfx + "aM", [128, 4]); bM = P.sb(pfx + "bM", [128, 4]); eM = P.sb(pfx + "eM", [128, 4])
    tmp4 = P.sb(pfx + "tmp4", [128, 4])
    Pm = [P.sb(pfx + "Pm%d" % i, [128, 128], BF16) for i in range(2)]
    ot = [P.sb(pfx + "ot%d" % i, [128, 129]) for i in range(2)]
    hh = [P.sb(pfx + "hh%d" % i, [128, 128]) for i in range(2)]
    dn = P.sb(pfx + "dn", [128, 1]); st6 = P.sb(pfx + "st6", [128, 6]); mv = P.sb(pfx + "mv", [128, 2]); rs = P.sb(pfx + "rs", [128, 1])
    if full:
        yR = P.sb(pfx + "yR", [128, 512]); yM = P.sb(pfx + "yM", [128, 512])
        rg = P.sb(pfx + "rg", [128, 512]); mo = P.sb(pfx + "mo", [128, 512]); mct = P.sb(pfx + "mct", [128, 512])
        yTo = [P.sb(pfx + "yTo%d" % i, [128, 4, 128], BF16) for i in range(2)]
        ybf = P.sb(pfx + "ybf", [128, 512], BF16)
    nps = PSRot(C, [0, 1, 2, 3, 4, 5, 6, 7])
    P.dma('sp', xts[0].v, xprev_d)
    x_transpose(P, C, xts[0].v, [(xT[:, :, 0:128], 'act')], [nps(), nps()])
    for c in range(4):
        pz = nps()
        for kc in range(8):
            P.mm(pz[:, 0:128], W[:, kc, OFF['m_x'] + c * 128: OFF['m_x'] + (c + 1) * 128], xT[:, kc, 0:128],
                 start=(kc == 0), stop=(kc == 7))
        P.copy('dve', mxT[:, c, 0:3], pz[:, 125:128])
    lnscale = float(np.log(128 ** -0.5))
    for sc in range(NT // SC):
        for tt in range(4):
            t = sc * 4 + tt
            xt = xts[t % 2]
            P.dma('sp', xt.v, x_d[t * 128:(t + 1) * 128, :])
            x_transpose(P, C, xt.v, [(xT[:, :, tt * 128:(tt + 1) * 128], 'act')], [nps(), nps()])
        for (dst, off, nch, kind) in ((rqT, OFF['r_q'], 2, 'bf'), (rkT, OFF['r_k'], 2, 'bf'), (mxT, OFF['m_x'], 4, 'mx')):
            if not full and dst is rqT:
                continue
            for c in range(nch):
                pz = nps()
                for kc in range(8):
                    P.mm(pz.v, W[:, kc, off + c * 128: off + (c + 1) * 128], xT[:, kc, :], start=(kc == 0), stop=(kc == 7))
                if kind == 'bf':
                    P.copy('act', dst[:, c, :], pz.v)
                else:
                    P.copy('act', mxT[:, c, 3:3 + SC], pz.v)
                    P.copy('dve', mxb[:, c, :], pz.v)
        for c in range(4):
            P.ts('dve', cva.v, mxT[:, c, 3:3 + SC], cwT[:, 12 + c:13 + c], cbT[:, c:c + 1], op0=ALU.mult, op1=ALU.add)
            P.stt('dve', cvb.v, mxT[:, c, 2:2 + SC], cwT[:, 8 + c:9 + c], cva.v, ALU.mult, ALU.add)
            P.stt('dve', cva.v, mxT[:, c, 1:1 + SC], cwT[:, 4 + c:5 + c], cvb.v, ALU.mult, ALU.add)
            P.stt('dve', cvb.v, mxT[:, c, 0:SC], cwT[:, c:c + 1], cva.v, ALU.mult, ALU.add)
            P.act(mcT[:, c, :], cvb.v, AF.Silu)
            P.copy('dve', cva[:, 0:3], mxT[:, c, SC:SC + 3])
            P.copy('dve', mxT[:, c, 0:3], cva[:, 0:3])
        for (dst, bd) in ((qmT, BD["bdq"]), (kmT, BD["bdk"])):
            if not full and dst is qmT:
                continue
            for h in range(4):
                pz = nps()
                P.mm(pz.v, bd[:, h, :], mcT[:, h, :])
                P.copy('act', dst[:, h, :], pz.v)
        for tt in range(4):
            t = sc * 4 + tt
            ts_ = slice(tt * 128, (tt + 1) * 128)

            def tok_proj(off, n):
                pz = nps()
                for kc in range(8):
                    P.mm(pz[:, 0:n], xT[:, kc, ts_], W[:, kc, off:off + n], start=(kc == 0), stop=(kc == 7))
                return pz
            p_rk = tok_proj(OFF['r_k'], 256)
            P.copy('act', rk_tok.v, p_rk[:, 0:256])
            p_g8 = tok_proj(OFF['m_i'], 8)
            P.copy('dve', g8.v, p_g8[:, 0:8])
            P.tt('dve', igt.v, g8[:, 0:4], biB.v, ALU.add)
            P.tt('dve', tmp4.v, g8[:, 4:8], bfB.v, ALU.add)
            P.act(e1.v, tmp4.v, AF.Exp, scale=-1.0)
            P.ts('dve', e1.v, e1.v, 1.0, None, op0=ALU.add)
            P.act(L1.v, e1.v, AF.Ln)
            pc = nps()
            P.mm(pc[:, 0:4], maskr.v, L1.v)
            P.mm(pc[:, 8:12], onesr.v, L1.v)
            P.act(aM.v, pc[:, 0:4], AF.Exp, scale=-1.0, bias=lnscale)
            P.tt('dve', tmp4.v, igt.v, pc[:, 0:4], ALU.add)
            P.act(bM.v, tmp4.v, AF.Exp)
            P.act(eM.v, pc[:, 8:12], AF.Exp, scale=-1.0)
            P.tt('dve', totacc.v, totacc.v, pc[:, 8:12], ALU.add)
            p_rv = tok_proj(OFF['r_v'], 512)
            for h in range(4):
                P.act(vpR[:, h, :], p_rv[:, h * 128:(h + 1) * 128], AF.Copy, scale=bR[:, h:h + 1])
            p_vm = nps()
            for h in range(4):
                P.mm(p_vm[:, h * 128:(h + 1) * 128], mxb[:, h, ts_], BD["bdv"][:, h, :])
            for h in range(4):
                P.act(vpM[:, h, 0:128], p_vm[:, h * 128:(h + 1) * 128], AF.Copy, scale=bM[:, h:h + 1])
            P.copy('dve', vpM[:, :, 128:129], bM.v.re("p (h o) -> p h o", o=1))
            p_km = nps()
            for h in range(4):
                P.mm(p_km[:, h * 128:(h + 1) * 128], mcT[:, h, ts_], BD["bdk"][:, h, :])
            P.copy('act', km_tok.v, p_km.v)
            if full:
                p_rg = tok_proj(OFF['r_g'], 512)
                P.act(rg.v, p_rg.v, AF.Silu)
                p_mo = tok_proj(OFF['m_o'], 512)
                P.act(mo.v, p_mo.v, AF.Sigmoid)
                p_mc = nps()
                pmb = p_mc.v.bitcast(BF16)
                for c in range(4):
                    P.tr(pmb[:, c * 128:(c + 1) * 128], mcT[:, c, ts_], C.identb.v)
                P.tt('dve', mct.v, pmb[:, 0:512], skM.v, ALU.mult)
            kq = 0
            for h in range(4):
                psl = slice((h % 2) * 64, (h % 2) * 64 + 64); hp = h // 2
                if full:
                    p_st = nps()
                    P.mm(p_st[:, 0:128], rkT[psl, hp, ts_], rqT[psl, hp, ts_])
                    pm = Pm[kq % 2]; o = ot[kq % 2]; hx = hh[kq % 2]; kq += 1
                    P.tt('dve', pm.v, p_st[:, 0:128], mask.v, ALU.mult)
                    p_o = nps()
                    P.mm(p_o[:, 0:128], pm.v, vpR[:, h, :], start=True, stop=False)
                    P.mm(p_o[:, 0:128], rqT[psl, hp, ts_], Sretb[psl, hp, :], start=False, stop=True)
                    P.act(o[:, 0:128], p_o[:, 0:128], AF.Copy, scale=aR[:, h:h + 1])
                    head_norm(P, o[:, 0:128], hx.v, st6, mv, rs)
                    P.tt('dve', hx.v, hx.v, gnR[:, h * 128:(h + 1) * 128], ALU.mult)
                    P.tt('dve', yR[:, h * 128:(h + 1) * 128], hx.v, rg[:, h * 128:(h + 1) * 128], ALU.mult)
                p_kv = nps()
                P.mm(p_kv[:, 0:128], rk_tok[:, hp * 128:(hp + 1) * 128], vpR[:, h, :])
                P.tt('dve', tmpS[psl, hp, 0:128], Sret[psl, hp, :], p_kv[psl, 0:128], ALU.add)
                P.act(Sret[psl, hp, :], tmpS[psl, hp, 0:128], AF.Copy, scale=eR[psl, hp:hp + 1])
                P.copy('dve', Sretb[psl, hp, :], Sret[psl, hp, :])
            for h in range(4):
                if full:
                    p_st = nps()
                    P.mm(p_st[:, 0:128], kmT[:, h, ts_], qmT[:, h, ts_])
                    pm = Pm[kq % 2]; o = ot[kq % 2]; hx = hh[kq % 2]; kq += 1
                    P.tt('dve', pm.v, p_st[:, 0:128], mask.v, ALU.mult)
                    p_o = nps()
                    P.mm(p_o[:, 0:129], pm.v, vpM[:, h, :], start=True, stop=False)
                    P.mm(p_o[:, 0:129], qmT[:, h, ts_], Cmlb[:, h, :], start=False, stop=True)
                    P.act(o.v, p_o[:, 0:129], AF.Copy, scale=aM[:, h:h + 1])
                    P.act(dn.v, o[:, 128:129], AF.Abs)
                    P.ts('dve', dn.v, dn.v, 1.0, None, op0=ALU.max)
                    a_ = dn.v.ap
                    P.I('dve', (lambda a_: (lambda e: e.reciprocal(a_, a_)))(a_), w=[dn], r=[dn])
                    P.act(o[:, 0:128], o[:, 0:128], AF.Copy, scale=dn.v)
                    head_norm(P, o[:, 0:128], hx.v, st6, mv, rs)
                    P.tt('dve', hx.v, hx.v, gnM[:, h * 128:(h + 1) * 128], ALU.mult)
                    P.tt('dve', hx.v, hx.v, mct[:, h * 128:(h + 1) * 128], ALU.add)
                    P.tt('dve', yM[:, h * 128:(h + 1) * 128], hx.v, mo[:, h * 128:(h + 1) * 128], ALU.mult)
                p_kv = nps()
                P.mm(p_kv[:, 0:129], km_tok[:, h * 128:(h + 1) * 128], vpM[:, h, :])
                P.tt('dve', tmpS[:, h, :], Cml[:, h, :], p_kv[:, 0:129], ALU.add)
                P.act(Cml[:, h, :], tmpS[:, h, :], AF.Copy, scale=eM[:, h:h + 1])
                P.copy('dve', Cmlb[:, h, :], Cml[:, h, :])
            if full:
                for (ysrc, ydst) in ((yR, outs[0]), (yM, outs[1])):
                    P.copy('act', ybf.v, ysrc.v)
                    pz = nps(); pzb = pz.v.bitcast(BF16)
                    for c in range(4):
                        P.tr(pzb[:, c * 128:(c + 1) * 128], ybf[:, c * 128:(c + 1) * 128], C.identb.v)
                    yo = yTo[kq % 2]; kq += 1
                    P.copy('dve', yo.v, pzb[:, 0:512].re("p (c t) -> p c t", c=4))
                    P.dma('sp', ydst[:, :, t * 128:(t + 1) * 128], yo.v)
    if not full:
        P.dma('sp', outs[0].v, Sret.v)
        P.dma('sp', outs[1].v, Cml.v)
        P.dma('sp', outs[2].v, totacc.v)


def head_norm(P, src, dst, st6, mv, rs):
    a, b = st6.v.ap, src.ap
    P.I('dve', lambda e: e.bn_stats(a, b), w=[st6], r=[src])
    a2, b2 = mv.v.ap, st6.v.ap
    P.I('dve', lambda e: e.bn_aggr(a2, b2), w=[mv], r=[st6])
    P.ts('dve', rs.v, mv[:, 1:2], EPS, None, op0=ALU.add)
    P.act(rs.v, rs.v, AF.Ln)
    P.act(rs.v, rs.v, AF.Exp, scale=-0.5)
    P.ts('dve', dst, src, mv[:, 0:1], rs.v, op0=ALU.subtract, op1=ALU.mult)


ATT_PAT = ((128, 1), (512, 4), (2048, 16))
HALO = 2048


def host_consts_attn():
    slopes = np.exp2(-8.0 * np.arange(1, 13, dtype=np.float64) / 12.0).reshape(3, 4)
    s = np.arange(128)[:, None]; i = np.arange(128)[None, :]
    out = np.zeros((3, 2, 128, 4, 128), dtype=np.float32)
    for g, (win, d) in enumerate(ATT_PAT):
        for h in range(4):
            dcur = i - s
            b = np.where((dcur >= 0), -slopes[g, h] * d * dcur, -30000.0)
            out[g, 1, :, h, :] = b
            dprev = i + 128 - s
            b = np.where((dprev <= 128), -slopes[g, h] * d * dprev, -30000.0)
            out[g, 0, :, h, :] = b
    return out.reshape(3, 2, 128, 512)


def pass_attn(P, C, NT, xext_d, w_in_l, bias_d, hv_d, yT_out, pfx="a", dbg=0):
    accN = P.sb(pfx + "accN", [128, 4, NT]); accD = P.sb(pfx + "accD", [128, 4, NT])
    for h_ in range(4):
        for j_ in range(NT // 2048):
            P.memset('dve', accN[:, h_, j_ * 2048:(j_ + 1) * 2048], 0.0 if dbg == 0 else 1.0)
            P.memset('dve', accD[:, h_, j_ * 2048:(j_ + 1) * 2048], 0.0 if dbg == 0 else 2.0)
    hv0 = P.sb(pfx + "hv0", [128, 128]); hvb = P.sb(pfx + "hvb", [128, 128], BF16)
    P.dma('act', hv0.v, hv_d); P.copy('dve', hvb.v, hv0.v)
    Wq = P.sb(pfx + "Wq", [128, 8, 256], BF16); Wk = P.sb(pfx + "Wk", [128, 8, 256], BF16); Wv = P.sb(pfx + "Wv", [128, 8, 512], BF16)
    bT = [P.sb(pfx + "bT%d" % i, [128, 512]) for i in range(2)]
    xts = [P.sb(pfx + "xt%d" % i, [128, 1024]) for i in range(2)]
    xTb = [P.sb(pfx + "xTb%d" % i, [128, 8, 128], BF16) for i in range(2)]
    kT = [P.sb(pfx + "kT%d" % i, [128, 2, 128], BF16) for i in range(2)]
    Vt = [P.sb(pfx + "V%d" % i, [128, 512], BF16) for i in range(2)]
    qT = [P.sb(pfx + "qT%d" % i, [128, 2, 128], BF16) for i in range(2)]
    tmp = [P.sb(pfx + "tmp%d" % i, [128, 512]) for i in range(2)]
    PT = [[P.sb(pfx + "PT%d_%d" % (i, j), [128, 512], BF16) for j in range(2)] for i in range(2)]
    nps = PSRot(C, [0, 1, 2, 3, 4, 5, 6, 7])
    win = w_in_l.rearrange("(kc p) c -> p kc c", p=128)
    nb = 0
    for g, (_, d) in enumerate(ATT_PAT if dbg not in (1, 2, 4, 5, 6) else (ATT_PAT[:1] if dbg in (2, 4, 5, 6) else ())):
        load_w_cast(P, Wq.v, win[:, :, OFF['a_q'] + g * 256: OFF['a_q'] + (g + 1) * 256])
        load_w_cast(P, Wk.v, win[:, :, OFF['a_k'] + g * 256: OFF['a_k'] + (g + 1) * 256])
        load_w_cast(P, Wv.v, win[:, :, OFF['a_v'] + g * 512: OFF['a_v'] + (g + 1) * 512])
        P.dma('act', bT[0].v, bias_d[g, 0]); P.dma('act', bT[1].v, bias_d[g, 1])
        NB = NT // (128 * d)
        for r in range(d):
            for m in range(-1, NB):
                cur = nb % 2; prv = 1 - cur; nb += 1
                s0 = HALO + m * 128 * d + r
                xt = xts[cur]
                P.dma('sp', xt.v, xext_d[s0: s0 + 127 * d + 1: d, :])
                x_transpose(P, C, xt.v, [(xTb[cur].v, 'act')], [nps(), nps()])
                xb = xTb[cur]
                for c in range(2):
                    pz = nps()
                    for kc in range(8):
                        P.mm(pz[:, 0:128], Wk[:, kc, c * 128:(c + 1) * 128], xb[:, kc, :], start=(kc == 0), stop=(kc == 7))
                    P.copy('act', kT[cur][:, c, :], pz[:, 0:128])
                pz = nps()
                for kc in range(8):
                    P.mm(pz.v, xb[:, kc, :], Wv[:, kc, :], start=(kc == 0), stop=(kc == 7))
                P.copy('act', Vt[cur].v, pz.v)
                if m < 0 or dbg == 4:
                    continue
                for c in range(2):
                    pz = nps()
                    for kc in range(8):
                        P.mm(pz[:, 0:128], Wq[:, kc, c * 128:(c + 1) * 128], xb[:, kc, :], start=(kc == 0), stop=(kc == 7))
                    P.copy('act', qT[cur][:, c, :], pz[:, 0:128])
                for pc, kb in ((0, prv), (1, cur)):
                    psAB = [nps(), nps()]
                    for h in range(4):
                        psl = slice((h % 2) * 64, (h % 2) * 64 + 64); hp = h // 2
                        P.mm(psAB[h % 2][:, hp * 128:(hp + 1) * 128], kT[kb][psl, hp, :], qT[cur][psl, hp, :])
                    tm = tmp[pc]
                    for h in range(4):
                        hs = slice(h * 128, (h + 1) * 128); hp = h // 2
                        P.stt('dve', tm[:, hs], psAB[h % 2][:, hp * 128:(hp + 1) * 128], 0.125, bT[pc][:, hs], ALU.mult, ALU.add)
                    P.act(PT[cur][pc].v, tm.v, AF.Exp)
                if dbg in (5, 6):
                    continue
                pn = nps()
                for h in range(4):
                    hs = slice(h * 128, (h + 1) * 128)
                    P.mm(pn[:, hs], Vt[prv][:, hs], PT[cur][0][:, hs], start=True, stop=False)
                    P.mm(pn[:, hs], Vt[cur][:, hs], PT[cur][1][:, hs], start=False, stop=True)
                pd = nps()
                P.mm(pd.v, (hvb.v if m == 0 else C.onesb.v), PT[cur][0].v, start=True, stop=False)
                P.mm(pd.v, C.onesb.v, PT[cur][1].v, start=False, stop=True)
                t0 = m * 128 * d + r
                for h in range(4 if dbg != 3 else 0):
                    hs = slice(h * 128, (h + 1) * 128)
                    av = accN[:, h, t0: t0 + 127 * d + 1: d]
                    P.tt('dve', av, av, pn[:, hs], ALU.add)
                    dv = accD[:, h, t0: t0 + 127 * d + 1: d]
                    P.tt('dve', dv, dv, pd[:, hs], ALU.add)
    yo = [P.sb(pfx + "yo%d" % i, [128, 4, 512], BF16) for i in range(2)]
    rc = [P.sb(pfx + "rc%d" % i, [128, 4, 512]) for i in range(2)]
    for j in range(NT // 512):
        sl = slice(j * 512, (j + 1) * 512)
        for h in range(4):
            a_, b_ = rc[j % 2][:, h, :].ap, accD[:, h, sl].ap
            P.I('dve', (lambda a_, b_: (lambda e: e.reciprocal(a_, b_)))(a_, b_), w=[rc[j % 2]], r=[accD])
            P.tt('dve', yo[j % 2][:, h, :], accN[:, h, sl], rc[j % 2][:, h, :], ALU.mult)
        P.dma('sp', yT_out[:, :, sl], yo[j % 2].v)


NCORES = 8
NT_CORE = 4096
PRM_NAMES = ("ret_gn", "ml_conv_w", "ml_conv_b", "bdq", "bdk", "bdv", "ml_bi", "ml_bf", "ml_gn", "ml_skip")
PRM_SHAPES = dict(ret_gn=[512], ml_conv_w=[4, 512], ml_conv_b=[512], bdq=[4, 128, 128], bdk=[4, 128, 128], bdv=[4, 128, 128],
                  ml_bi=[4], ml_bf=[4], ml_gn=[512], ml_skip=[512])
CST_SHAPES = dict(aR=[128, 4], bR=[128, 4], eR=[128, 2], gR=[128, 2], mask=[128, 128])


def _scan_inputs(nc):
    def inp(n, sh):
        return nc.dram_tensor(n, sh, F32, kind="ExternalInput").ap()
    prm = {n: inp(n, PRM_SHAPES[n]) for n in PRM_NAMES}
    cst = {n: inp(n, CST_SHAPES[n]) for n in CST_SHAPES}
    return prm, cst


def build_A(NT=NT_CORE):
    nc = bass.Bass("TRN2", target_bir_lowering=False)
    with ExitStack() as es:
        P = Prog(nc, es)
        x_d = P.dram("x", [NT, D], F32, kind="ExternalInput")
        xprev = P.dram("xprev", [128, D], F32, kind="ExternalInput")
        w_in = nc.dram_tensor("w_in", [D, D_IN], F32, kind="ExternalInput").ap()
        prm, cst = _scan_inputs(nc)
        outs = [P.dram("oS", [128, 2, 128], F32, kind="ExternalOutput"), P.dram("oC", [128, 4, 129], F32, kind="ExternalOutput"),
                P.dram("oT", [128, 4], F32, kind="ExternalOutput")]
        C = make_ctx(P)
        pass_scan(P, C, NT, x_d, xprev, w_in, prm, cst, None, outs, mode="summary", pfx="s")
        P.wait_all('sp', outs)
        P.emit()
    return nc


def build_B(NT=NT_CORE, NE=NEXP):
    nc = bass.Bass("TRN2", target_bir_lowering=False)
    with ExitStack() as es:
        P = Prog(nc, es)

        def inp(n, sh):
            return nc.dram_tensor(n, sh, F32, kind="ExternalInput").ap()
        xext = P.dram("xext", [HALO + NT, D], F32, kind="ExternalInput")
        w_in = inp("w_in", [D, D_IN])
        prm, cst = _scan_inputs(nc)
        init = dict(Fret=inp("Fret", [3, 128, 2, 128]), Fml=inp("Fml", [3, 128, 4, 129]), totL=inp("totL", [3, 128, 4]),
                    sel=inp("sel", [128, 3]), nsel=inp("nsel", [128, 3]))
        abias = inp("abias", [3, 2, 128, 512]); hv = inp("hv", [128, 128])
        w_br = inp("w_branch", [3, MIXW, D]); w_out = inp("w_out", [D, D])
        ln1_g = inp("ln1_g", [D]); ln1_b = inp("ln1_b", [D]); ln2_g = inp("ln2_g", [D]); ln2_b = inp("ln2_b", [D])
        w_r = inp("w_router", [D, NE]); b_r = inp("b_router", [NE])
        w_g = inp("w_gate", [NE, D, D]); b_g = inp("b_gate", [NE, D])
        w_u = inp("w_up", [NE, D, D]); b_u = inp("b_up", [NE, D])
        w_d = inp("w_down", [NE, D, D]); b_d = inp("b_down", [NE, D])
        out = P.dram("out", [NT, D], F32, kind="ExternalOutput")
        yT = [P.dram("yT%d" % b, [128, 4, NT], BF16) for b in range(3)]
        x1s = P.dram("x1s", [NT, D], F32)
        C = make_ctx(P)
        x_own = Tile(P, xext.h[HALO:HALO + NT, :], "xown", "dram")
        x_own.lw, x_own.rd = xext.lw, xext.rd
        xprev = Tile(P, xext.h[HALO - 128:HALO, :], "xprv", "dram")
        xprev.lw, xprev.rd = xext.lw, xext.rd
        with P.scope():
            pass_attn(P, C, NT, xext, w_in, abias, hv, yT[2], pfx="a")
        with P.scope():
            pass_scan(P, C, NT, x_own, xprev, w_in, prm, cst, init, [yT[0], yT[1]], mode="full", pfx="s")
        with P.scope():
            pass_merge(P, C, NT, x_own, yT, w_in, w_br, w_out, ln1_g, ln1_b, x1s, pfx="m")
        NBLK = NT * 4 // 128 + NE
        Xs = P.dram("Xs", [NBLK * 128, D], BF16); Ys = P.dram("Ys", [NBLK * 128, D], F32)
        mc = {k: inp("mc_" + k, list(v.shape)) for k, v in host_consts_moe(NT, NE).items()}
        with P.scope():
            pass_moe2(P, C, NT, x1s, w_r, b_r, w_g, b_g, w_u, b_u, w_d, b_d, ln2_g, ln2_b, out, Xs, Ys, mc, NE=NE, pfx="f")
        P.wait_all('sp', [out])
        P.emit()
    return nc


def layer_params(inputs, l):
    g = lambda n: np.ascontiguousarray(np.asarray(inputs[n], dtype=np.float32)[l])
    prm = dict(ret_gn=g("ret_gn"), ml_conv_w=g("ml_conv_w"), ml_conv_b=g("ml_conv_b"),
               bdq=host_blockdiag(g("ml_wq")), bdk=host_blockdiag(g("ml_wk")), bdv=host_blockdiag(g("ml_wv")),
               ml_bi=g("ml_bi"), ml_bf=g("ml_bf"), ml_gn=g("ml_gn"), ml_skip=g("ml_skip"))
    big = dict(w_in=g("w_in"), w_branch=g("w_branch"), w_out=g("w_out"), ln1_g=g("ln1_g"), ln1_b=g("ln1_b"),
               ln2_g=g("ln2_g"), ln2_b=g("ln2_b"), w_router=g("w_router"), b_router=g("b_router"),
               w_gate=g("w_gate"), b_gate=g("b_gate"), w_up=g("w_up"), b_up=g("b_up"), w_down=g("w_down"), b_down=g("b_down"))
    return prm, big


def run_layer(ncA, ncB, xs, prm, big, cst, abias, QPB, NT):
    n = len(xs)
    zeros128 = np.zeros((128, D), np.float32)
    inA = []
    for c in range(n):
        q = c % QPB
        m = dict(prm); m.update(cst)
        m["w_in"] = big["w_in"]; m["x"] = xs[c]
        m["xprev"] = np.ascontiguousarray(xs[c - 1][-128:]) if q > 0 else zeros128
        inA.append(m)
    resA = run_bass_kernel_spmd(ncA, inA, core_ids=list(range(n))).results
    inB = []
    for c in range(n):
        q = c % QPB; b0 = c - q
        m = dict(prm); m.update(cst); m.update(big)
        halo = xs[c - 1][-HALO:] if q > 0 else np.zeros((HALO, D), np.float32)
        m["xext"] = np.ascontiguousarray(np.concatenate([halo, xs[c]], axis=0))
        Fret = np.zeros((3, 128, 2, 128), np.float32); Fml = np.zeros((3, 128, 4, 129), np.float32)
        totL = np.zeros((3, 128, 4), np.float32); sel = np.zeros((128, 3), np.float32)
        for qq in range(min(3, QPB)):
            Fret[qq] = resA[b0 + qq]["oS"]; Fml[qq] = resA[b0 + qq]["oC"]; totL[qq] = resA[b0 + qq]["oT"]
            if qq < q:
                sel[:, qq] = 1.0
        m.update(Fret=Fret, Fml=Fml, totL=totL, sel=sel, nsel=(1.0 - sel).astype(np.float32))
        m["abias"] = abias
        for k_, v_ in host_consts_moe(NT, big["w_router"].shape[1]).items():
            m["mc_" + k_] = v_
        m["hv"] = (np.ones((128, 128), np.float32) if q > 0 else np.zeros((128, 128), np.float32))
        inB.append(m)
    resB = run_bass_kernel_spmd(ncB, inB, core_ids=list(range(n))).results
    return [np.asarray(r["out"], dtype=np.float32) for r in resB]


def kernel(**inputs):
    x = np.asarray(inputs["x"], dtype=np.float32)
    B, S, _ = x.shape
    QPB = NCORES // B
    NT = S // QPB
    xs = [np.ascontiguousarray(x[c // QPB, (c % QPB) * NT:(c % QPB + 1) * NT]) for c in range(NCORES)]
    ncA = build_A(NT); ncB = build_B(NT)
    cst = host_consts_scan(NT); abias = host_consts_attn()
    L = np.asarray(inputs["w_in"]).shape[0]
    for l in range(L):
        prm, big = layer_params(inputs, l)
        xs = run_layer(ncA, ncB, xs, prm, big, cst, abias, QPB, NT)
    out = np.zeros((B, S, D), np.float32)
    for c in range(NCORES):
        out[c // QPB, (c % QPB) * NT:(c % QPB + 1) * NT] = xs[c]
    return out


BIGIDX = 4000000.0


def host_consts_moe(NT, NE=NEXP):
    NBLK = NT * 4 // 128 + NE
    p = np.arange(128, dtype=np.float32)
    d = dict(iota_p=p.reshape(128, 1).copy(),
             ustrict=(p[:, None] < p[None, :]).astype(np.float32),
             iotaJ=np.tile(np.arange(33, dtype=np.float32)[None, :], (128, 1)),
             iotaB=np.tile(np.arange(NBLK, dtype=np.float32)[None, :], (128, 1)))
    return d


def _breg(P, e, bound):
    if not hasattr(P, "_bregs"):
        P._bregs = {}
    if bound not in P._bregs:
        P._bregs[bound] = e.to_reg(int(bound))
    return P._bregs[bound]


def ind_dma(P, dst_v, src_tile, src_ap, idx_v, bound, scatter=False, extra_r=()):
    sb_tile = dst_v.tile
    key = P._tsem(sb_tile)
    d_ap, i_ap = dst_v.ap, idx_v.ap
    if not scatter:
        rt = [src_tile, idx_v.tile] + list(extra_r); wt = [sb_tile]

        def f(e):
            try:
                return e.indirect_dma_start(d_ap, None, src_ap, bass.IndirectOffsetOnAxis(i_ap, 0), bounds_check=_breg(P, e, bound), oob_is_err=False)
            except Exception:
                print("IND GATHER FAIL", d_ap, src_ap, i_ap, bound)
                raise
    else:
        rt = [sb_tile, idx_v.tile] + list(extra_r); wt = [src_tile]

        def f(e):
            try:
                return e.indirect_dma_start(src_ap, bass.IndirectOffsetOnAxis(i_ap, 0), d_ap, None, bounds_check=_breg(P, e, bound), oob_is_err=False)
            except Exception:
                print("IND SCATTER FAIL", d_ap, src_ap, i_ap, bound)
                raise
    waits = P._waits('pool', rt, wt)
    sb_tile.dcnt += 16
    P.stream['pool'].append((waits, f, (key, 16)))
    P._record((key, sb_tile.dcnt), rt, wt)
    P.ninst += 1


def pass_moe2(P, C, NT, x1_d, w_router, b_router, w_gate, b_gate, w_up, b_up, w_down, b_down, ln_g, ln_b, out_d,
              Xs, Ys, mc, NE=NEXP, pfx="f", dbg=0):
    NTL = NT // 128
    NBLK = NT * 4 // 128 + NE
    NSLOT = NBLK * 128
    gB = P.sb(pfx + "gB", [128, 1024]); bB = P.sb(pfx + "bB", [128, 1024])
    bcast_rows(P, gB.v, ln_g); bcast_rows(P, bB.v, ln_b)
    wr = P.sb(pfx + "wr", [128, 8, NE], F32R); wr0 = P.sb(pfx + "wr0", [128, 8, NE])
    P.dma('act', wr0.v, w_router.rearrange("(kc p) e -> p kc e", p=128)); P.copy('dve', wr.v, wr0.v)
    brB = P.sb(pfx + "brB", [128, NE]); bcast_rows(P, brB.v, b_router)
    c1 = P.sb(pfx + "c1", [128, 1]); P.memset('dve', c1.v, 1.0)
    iop = P.sb(pfx + "iop", [128, 1]); P.dma('act', iop.v, mc["iota_p"])
    us0 = P.sb(pfx + "us0", [128, 128]); P.dma('act', us0.v, mc["ustrict"])
    usb = P.sb(pfx + "usb", [128, 128], BF16); P.copy('dve', usb.v, us0.v)
    ioJ = P.sb(pfx + "ioJ", [128, 33]); P.dma('act', ioJ.v, mc["iotaJ"])
    ioB = P.sb(pfx + "ioB", [128, NBLK]); P.dma('act', ioB.v, mc["iotaB"])
    lg_all = P.sb(pfx + "lg_all", [128, NTL, NE]); t8_all = P.sb(pfx + "t8_all", [128, NTL, 8])
    pall = P.sb(pfx + "pall", [128, NTL, NE]); pos_all = P.sb(pfx + "pos_all", [128, NTL, NE])
    slot_f = P.sb(pfx + "slot_f", [128, NTL, 4]); slot_i = P.sb(pfx + "slot_i", [128, NTL * 4], I32)
    pk_all = P.sb(pfx + "pk_all", [128, NTL, 4])
    carry = P.sb(pfx + "carry", [128, NE]); P.memset('dve', carry.v, 0.0)
    xts = [P.sb(pfx + "xt%d" % i, [128, 1024]) for i in range(2)]
    xT32 = P.sb(pfx + "xT32", [128, 8, 128], F32R)
    msk = P.sb(pfx + "msk", [128, NE]); mskb = P.sb(pfx + "mskb", [128, NE], BF16)
    ex = P.sb(pfx + "ex", [128, NE]); sm = P.sb(pfx + "sm", [128, 1]); nmx = P.sb(pfx + "nmx", [128, 1])
    nps = PSRot(C, [0, 1, 2, 3, 4, 5, 6, 7])
    for t in range(NTL):
        xt = xts[t % 2]
        P.dma('sp', xt.v, x1_d[t * 128:(t + 1) * 128, :])
        x_transpose(P, C, xt.v, [(xT32.v, 'dve')], [nps(), nps()])
        pl = nps()
        for kc in range(8):
            P.mm(pl[:, 0:NE], xT32[:, kc, :], wr[:, kc, :], start=(kc == 0), stop=(kc == 7))
        lg = lg_all[:, t, :]; t8 = t8_all[:, t, :]
        P.tt('dve', lg, pl[:, 0:NE], brB.v, ALU.add)
        a, b = t8.ap, lg.ap
        P.I('dve', (lambda a, b: (lambda e: e.max(out=a, in_=b)))(a, b), w=[t8_all], r=[lg_all])
        P.ts('dve', msk.v, lg, t8_all[:, t, 3:4], c1.v, op0=ALU.is_ge, op1=ALU.mult)
        P.ts('dve', nmx.v, t8_all[:, t, 0:1], -1.0, None, op0=ALU.mult)
        P.act(ex.v, lg, AF.Exp, bias=nmx.v)
        P.tt('dve', ex.v, ex.v, msk.v, ALU.mult)
        a2, b2 = sm.v.ap, ex.v.ap
        P.I('dve', (lambda a, b: (lambda e: e.reduce_sum(a, b, AX.X)))(a2, b2), w=[sm], r=[ex])
        a3 = sm.v.ap
        P.I('dve', (lambda a: (lambda e: e.reciprocal(a, a)))(a3), w=[sm], r=[sm])
        P.ts('dve', pall[:, t, :], ex.v, sm.v, c1.v, op0=ALU.mult, op1=ALU.mult)
        P.copy('dve', mskb.v, msk.v)
        pr = nps()
        P.mm(pr[:, 0:NE], usb.v, mskb.v)
        P.mm(pr[:, 64:64 + NE], C.onesb.v, mskb.v)
        P.tt('dve', pos_all[:, t, :], carry.v, pr[:, 0:NE], ALU.add)
        P.tt('dve', carry.v, carry.v, pr[:, 64:64 + NE], ALU.add)
    q = P.sb(pfx + "q", [128, NE]); nb = P.sb(pfx + "nb", [128, NE]); bend = P.sb(pfx + "bend", [128, NE])
    pstart = P.sb(pfx + "pstart", [128, NE]); t33 = P.sb(pfx + "t33", [128, 33])
    P.ts('dve', q.v, carry.v, 1.0 / 128.0, None, op0=ALU.mult)
    for e in range(NE):
        P.ts('dve', t33.v, ioJ.v, q[:, e:e + 1], c1.v, op0=ALU.is_lt, op1=ALU.mult)
        a_, b_ = nb[:, e:e + 1].ap, t33.v.ap
        P.I('dve', (lambda a, b: (lambda e_: e_.reduce_sum(a, b, AX.X)))(a_, b_), w=[nb], r=[t33])
    P.copy('dve', bend[:, 0:1], nb[:, 0:1])
    for e in range(1, NE):
        P.tt('dve', bend[:, e:e + 1], bend[:, e - 1:e], nb[:, e:e + 1], ALU.add)
    P.tt('dve', pstart.v, bend.v, nb.v, ALU.subtract)
    P.ts('dve', pstart.v, pstart.v, 128.0, None, op0=ALU.mult)
    Eall = P.sb(pfx + "Eall", [128, NBLK]); tB = P.sb(pfx + "tB", [128, NBLK]); sk = P.sb(pfx + "sk", [128, NBLK])
    P.memset('dve', Eall.v, 0.0)
    for e in range(NE):
        P.ts('dve', tB.v, ioB.v, bend[:, e:e + 1], c1.v, op0=ALU.is_ge, op1=ALU.mult)
        P.tt('dve', Eall.v, Eall.v, tB.v, ALU.add)
    P.ts('dve', Eall.v, Eall.v, float(NE - 1), None, op0=ALU.min)
    P.memset('dve', sk.v, 0.0)
    P.tt('dve', sk[:, 2:NBLK], Eall[:, 2:NBLK], Eall[:, 0:NBLK - 2], ALU.is_equal)
    P.ts('dve', sk.v, sk.v, BIGIDX, None, op0=ALU.mult)
    idxWf = P.sb(pfx + "idxWf", [128, NBLK]); idxW = P.sb(pfx + "idxW", [128, 8 * NBLK], I32)
    idxBf = P.sb(pfx + "idxBf", [128, NBLK]); idxB = P.sb(pfx + "idxB", [128, NBLK], I32)
    P.tt('dve', idxBf.v, Eall.v, sk.v, ALU.add)
    P.copy('dve', idxB.v, idxBf.v)
    iop4 = P.sb(pfx + "iop4", [128, 1]); P.ts('dve', iop4.v, iop.v, 4.0, None, op0=ALU.mult)
    P.ts('dve', idxWf.v, Eall.v, 512.0, None, op0=ALU.mult)
    P.tt('dve', idxWf.v, idxWf.v, sk.v, ALU.add)
    P.ts('dve', idxWf.v, idxWf.v, iop4.v, c1.v, op0=ALU.add, op1=ALU.mult)
    for kq in range(4):
        P.ts('dve', tB.v, idxWf.v, float(kq), None, op0=ALU.add)
        P.copy('dve', idxW[:, kq * NBLK:(kq + 1) * NBLK], tB.v)
    OH = P.sb(pfx + "OH", [128, NBLK]); P.ts('dve', OH.v, Eall.v, iop.v, c1.v, op0=ALU.is_equal, op1=ALU.mult)
    if dbg == 2:
        return
    xb16 = [P.sb(pfx + "xb16_%d" % i, [128, 1024], BF16) for i in range(2)]
    tA = P.sb(pfx + "tA", [128, NE]); oh = P.sb(pfx + "oh", [128, NE]); pr1 = P.sb(pfx + "pr1", [128, NE])
    Xs2 = Xs.h[:]
    for t in range(NTL):
        xt = xts[t % 2]; xb = xb16[t % 2]
        P.dma('sp', xt.v, x1_d[t * 128:(t + 1) * 128, :])
        P.copy('act', xb.v, xt.v)
        P.tt('dve', tA.v, pos_all[:, t, :], pstart.v, ALU.add)
        for k in range(4):
            P.ts('dve', oh.v, lg_all[:, t, :], t8_all[:, t, k:k + 1], c1.v, op0=ALU.is_equal, op1=ALU.mult)
            P.tt('dve', pr1.v, oh.v, tA.v, ALU.mult)
            a_, b_ = slot_f[:, t, k:k + 1].ap, pr1.v.ap
            P.I('dve', (lambda a, b: (lambda e_: e_.reduce_sum(a, b, AX.X)))(a_, b_), w=[slot_f], r=[pr1])
            P.tt('dve', pr1.v, oh.v, pall[:, t, :], ALU.mult)
            a_, b_ = pk_all[:, t, k:k + 1].ap, pr1.v.ap
            P.I('dve', (lambda a, b: (lambda e_: e_.reduce_sum(a, b, AX.X)))(a_, b_), w=[pk_all], r=[pr1])
        P.copy('dve', slot_i[:, t * 4:(t + 1) * 4], slot_f[:, t, :])
        for k in range(4):
            ind_dma(P, xb.v, Xs, Xs2, slot_i[:, t * 4 + k: t * 4 + k + 1], NSLOT - 1, scatter=True)
    if dbg == 3:
        P.wait_all('sp', [Xs])
        return
    with P.scope():
        inv128 = P.sb(pfx + 'inv128', [128, 128], BF16); P.memset('dve', inv128.v, 1.0 / 128.0)
        Wq = [[[P.sb(pfx + "W%d_%d_%d" % (j, i, kq), [128, 2, 1024], BF16) for kq in range(4)] for j in range(3)] for i in range(2)]
        Wt = [[[Wq[i][j][kc // 2][:, kc % 2, :] for kc in range(8)] for j in range(3)] for i in range(2)]
        Ball = [P.sb(pfx + "Ball%d" % j, [NE, 1024], BF16) for j in range(3)]
        for j, bsrc_ in enumerate((b_gate, b_up, b_down)):
            P.dma('pool', Ball[j].v, bsrc_)
        OHb = [P.sb(pfx + "OHb%d" % i, [NE, 128], BF16) for i in range(2)]
        wsrc = [w_.rearrange("e (p kq r) f -> (e p kq) (r f)", p=128, kq=4, r=2) for w_ in (w_gate, w_up, w_down)]
        bsrc = [b_gate, b_up, b_down]
        wtile = [Tile(P, None, pfx + "wsrc%d" % j, "dram") for j in range(3)]
        xblk = [P.sb(pfx + "xblk%d" % i, [128, 1024], BF16) for i in range(2)]
        xbT = [P.sb(pfx + "xbT%d" % i, [128, 8, 128], BF16) for i in range(2)]
        actT = [P.sb(pfx + "actT%d" % i, [128, 8, 128], BF16) for i in range(2)]
        gt = [P.sb(pfx + "g%d" % i, [128, 512]) for i in range(2)]
        s_t = [P.sb(pfx + "s%d" % i, [128, 512]) for i in range(2)]
        ut = [P.sb(pfx + "u%d" % i, [128, 512]) for i in range(2)]
        atok = [P.sb(pfx + "atok%d" % i, [128, 1024], BF16) for i in range(2)]
        yb = [P.sb(pfx + "yb%d" % i, [128, 1024]) for i in range(2)]
        kk = 0
        for b in range(NBLK if dbg != 11 else 10):
            cur = b % 2
            for j in range(3):
                for kq in range(4):
                    ind_dma(P, Wq[cur][j][kq].v.re("p r f -> p (r f)"), wtile[j], wsrc[j],
                            idxW[:, kq * NBLK + b: kq * NBLK + b + 1], NE * 512 - 1)
            Wg, Wu, Wd = Wt[cur]
            P.copy('dve', OHb[cur].v, OH[0:NE, b:b + 1].bc([NE, 128]))
            xk = xblk[cur]
            if dbg != 9 or b < 2:
                P.dma('sp', xk.v, Xs[b * 128:(b + 1) * 128, :])
            xT_ = xbT[cur]
            for half in range(2):
                pz = nps(); pzb = pz.v.bitcast(BF16)
                for c in range(4):
                    kc = half * 4 + c
                    P.tr(pzb[:, c * 128:(c + 1) * 128], xk[:, kc:1024:8], C.identb.v)
                P.copy('act', xT_[:, half * 4:(half + 1) * 4, :], pzb[:, 0:512].re("p (c t) -> p c t", c=4))
            aT = actT[cur]
            at = atok[cur]
            for hf in range(2 if dbg not in (6, 10) else 0):
                hs = slice(hf * 512, (hf + 1) * 512)
                pg = nps()
                for kc in range(8):
                    P.mm(pg.v, xT_[:, kc, :], Wg[kc][:, hs], start=(kc == 0), stop=False)
                P.mm(pg.v, OHb[cur].v, Ball[0][:, hs], start=False, stop=True)
                pu = nps()
                for kc in range(8):
                    P.mm(pu.v, xT_[:, kc, :], Wu[kc][:, hs], start=(kc == 0), stop=False)
                P.mm(pu.v, OHb[cur].v, Ball[1][:, hs], start=False, stop=True)
                g = gt[kk % 2]; s_ = s_t[kk % 2]; u = ut[kk % 2]; kk += 1
                P.ts('dve', g.v, pg.v, 7.0, None, op0=ALU.min)
                P.act(s_.v, g.v, AF.Sigmoid, scale=1.702)
                P.ts('dve', u.v, pu.v, 7.0, -7.0, op0=ALU.min, op1=ALU.max)
                P.tt('dve', g.v, g.v, s_.v, ALU.mult)
                P.stt('dve', at[:, hs], u.v, 1.0, g.v, ALU.add, ALU.mult)
            for half in range(2 if dbg not in (6, 10) else 0):
                pz = nps(); pzb = pz.v.bitcast(BF16)
                for c in range(4):
                    fc = half * 4 + c
                    P.tr(pzb[:, c * 128:(c + 1) * 128], at[:, fc:1024:8], C.identb.v)
                P.copy('act', aT[:, half * 4:(half + 1) * 4, :], pzb[:, 0:512].re("p (c t) -> p c t", c=4))
            y = yb[cur]
            if dbg in (6, 10):
                P.memset('dve', y.v, 0.5)
            for hf in range(2 if dbg not in (6, 10) else 0):
                py = nps()
                hs = slice(hf * 512, (hf + 1) * 512)
                for fc in range(8):
                    P.mm(py.v, aT[:, fc, :], Wd[fc][:, hs], start=(fc == 0), stop=False)
                P.mm(py.v, OHb[cur].v, Ball[2][:, hs], start=False, stop=True)
                P.copy('act', y[:, hs], py.v)
            if dbg != 7:
                P.dma('sp', Ys[b * 128:(b + 1) * 128, :], y.v)
    if dbg == 4:
        return
    rk = [P.sb(pfx + "rk%d" % i, [128, 1024]) for i in range(4)]
    acA = P.sb(pfx + "acA", [128, 1024]); acB = P.sb(pfx + "acB", [128, 1024])
    st = P.sb(pfx + "st", [128, 2, 6]); mv = P.sb(pfx + "mv", [128, 2]); rstd = P.sb(pfx + "rstd", [128, 1])
    Ys2 = Ys.h[:]
    for t in range(NTL):
        xt = xts[t % 2]
        P.dma('sp', xt.v, x1_d[t * 128:(t + 1) * 128, :])
        for k in range(4):
            ind_dma(P, rk[k].v, Ys, Ys2, slot_i[:, t * 4 + k: t * 4 + k + 1], NSLOT - 1)
        P.ts('dve', acA.v, rk[0].v, pk_all[:, t, 0:1], c1.v, op0=ALU.mult, op1=ALU.mult)
        P.stt('dve', acB.v, rk[1].v, pk_all[:, t, 1:2], acA.v, ALU.mult, ALU.add)
        P.stt('dve', acA.v, rk[2].v, pk_all[:, t, 2:3], acB.v, ALU.mult, ALU.add)
        P.stt('dve', acB.v, rk[3].v, pk_all[:, t, 3:4], acA.v, ALU.mult, ALU.add)
        P.stt('dve', acA.v, xt.v, DN_ALPHA, acB.v, ALU.mult, ALU.add)
        layer_norm(P, acA.v, gB.v, bB.v, st.v, mv.v, rstd.v)
        P.dma('sp', out_d[t * 128:(t + 1) * 128, :], acA.v)
```

```python
import numpy as np
import concourse.bass as bass
import concourse.mybir as mybir
from concourse.bass_utils import run_bass_kernel_spmd
from contextlib import ExitStack

F32 = mybir.dt.float32
F32R = mybir.dt.float32r
BF16 = mybir.dt.bfloat16
I32 = mybir.dt.int32
U32 = mybir.dt.uint32
AF = mybir.ActivationFunctionType
ALU = mybir.AluOpType
AX = mybir.AxisListType

ENGS = ['pe', 'act', 'dve', 'pool', 'sp']


class Tile:
    def __init__(self, P, h, name, space='sb'):
        self.P = P
        self.h = h
        self.name = name
        self.space = space
        self.lw = {}
        self.rd = {}
        self.dsem = None
        self.dcnt = 0

    def __getitem__(self, k):
        return V(self, self.h[k])

    @property
    def v(self):
        return V(self, self.h[:])


class V:
    def __init__(self, tile, ap):
        self.tile = tile
        self.ap = ap

    def __getitem__(self, k):
        return V(self.tile, self.ap[k])

    def bitcast(self, dt):
        return V(self.tile, self.ap.bitcast(dt))

    def bc(self, shape):
        return V(self.tile, self.ap.to_broadcast(shape))

    def re(self, s, **kw):
        return V(self.tile, self.ap.rearrange(s, **kw))


def _ap(x):
    if isinstance(x, Tile):
        return x.h[:]
    return x.ap if isinstance(x, V) else x


class Prog:
    def __init__(self, nc, es, same_engine_sync=None):
        self.nc = nc
        self.es = es
        self.es_top = es
        self.all_tiles = []
        self.stream = {e: [] for e in ENGS}
        self.sems = {}
        self.cnt = {e: 0 for e in ENGS}
        self.known = {e: {} for e in ENGS}
        import os as _os
        self.same = (_os.environ.get('KSAME', '1') == '1') if same_engine_sync is None else same_engine_sync
        self.nsem = 0
        for e in ['pe', 'act', 'dve', 'pool']:
            self.sems[e] = es.enter_context(nc.semaphore("s_" + e))
            self.nsem += 1
        self.ninst = 0
        self.nwait = 0

    def sb(self, name, shape, dt=F32):
        h = self.es.enter_context(self.nc.sbuf_tensor(name, list(shape), dt))
        return Tile(self, h, name)

    def ps(self, name, shape, dt=F32):
        h = self.es.enter_context(self.nc.psum_tensor(name, list(shape), dt))
        return Tile(self, h, name, 'ps')

    def dram(self, name, shape, dt=F32, kind="Internal"):
        h = self.nc.dram_tensor(name, list(shape), dt, kind=kind)
        return Tile(self, h.ap(), name, 'dram')

    def _tsem(self, t):
        if t.dsem is None:
            key = "d_" + t.name
            self.sems[key] = self.es_top.enter_context(self.nc.semaphore(key))
            self.nsem += 1
            t.dsem = key
            self.all_tiles.append(t)
        return t.dsem

    def _waits(self, eng, rt, wt):
        need = {}
        for t in rt:
            for s, v in t.lw.items():
                need[s] = max(need.get(s, 0), v)
        for t in wt:
            for s, v in t.lw.items():
                need[s] = max(need.get(s, 0), v)
            for s, v in t.rd.items():
                need[s] = max(need.get(s, 0), v)
        out = []
        kn = self.known[eng]
        for s, v in need.items():
            if s == eng and (eng == 'pe' or not self.same):
                continue
            if kn.get(s, 0) < v:
                kn[s] = v
                out.append((s, v))
        return out

    def _record(self, ev, rt, wt):
        s, v = ev
        for t in wt:
            t.lw[s] = max(t.lw.get(s, 0), v)
            t.rd = {}
        for t in rt:
            if t in wt:
                continue
            t.rd[s] = max(t.rd.get(s, 0), v)

    @staticmethod
    def _tiles(xs):
        out = []
        for x in xs:
            if x is None:
                continue
            t = x.tile if isinstance(x, V) else x
            if isinstance(t, Tile) and t not in out:
                out.append(t)
        return out

    def I(self, eng, fn, w=(), r=()):
        wt = self._tiles(w)
        rt = self._tiles(r)
        for t in rt:
            if t.space == 'ps' and t not in wt and eng != 'pe':
                wt.append(t)
        waits = self._waits(eng, rt, wt)
        self.cnt[eng] += 1
        ev = (eng, self.cnt[eng])
        self.stream[eng].append((waits, fn, (eng, 1)))
        self._record(ev, rt, wt)
        self.ninst += 1
        self.nwait += len(waits)

    def dma(self, q, out, in_, **kw):
        wt = self._tiles([out])
        rt = self._tiles([in_])
        owner = None
        for x in (out, in_):
            t_ = x.tile if isinstance(x, V) else (x if isinstance(x, Tile) else None)
            if t_ is not None and t_.space == 'sb':
                owner = t_
        if owner is None:
            for x in (out, in_):
                t_ = x.tile if isinstance(x, V) else (x if isinstance(x, Tile) else None)
                if t_ is not None and owner is None:
                    owner = t_
        key = self._tsem(owner)
        waits = self._waits(q, rt, wt)
        owner.dcnt += 16
        ev = (key, owner.dcnt)
        o, i = _ap(out), _ap(in_)
        self.stream[q].append((waits, lambda e: e.dma_start(out=o, in_=i, **kw), (key, 16)))
        self._record(ev, rt, wt)
        self.ninst += 1
        self.nwait += len(waits)
        return ev

    def wait_all(self, eng, tiles):
        ts = self._tiles(tiles)
        waits = self._waits(eng, ts, ts)
        self.stream[eng].append((waits, None, None))

    def mm(self, out, lhsT, rhs, start=True, stop=True, **kw):
        o, a, b = _ap(out), _ap(lhsT), _ap(rhs)
        self.I('pe', lambda e: e.matmul(o, a, b, start=start, stop=stop, **kw), w=[out], r=[lhsT, rhs])

    def tr(self, out, in_, ident):
        o, a, b = _ap(out), _ap(in_), _ap(ident)
        self.I('pe', lambda e: e.transpose(o, a, b), w=[out], r=[in_, ident])

    def act(self, out, in_, func, bias=None, scale=1.0, accum_out=None, eng='act'):
        o, a = _ap(out), _ap(in_)
        kw = {}
        if bias is not None:
            kw['bias'] = _ap(bias)
        if accum_out is not None:
            kw['accum_out'] = _ap(accum_out)
        sc = _ap(scale)
        self.I(eng, lambda e: e.activation(o, a, func, scale=sc, **kw),
               w=[out, accum_out], r=[in_, bias, scale if isinstance(scale, V) else None])

    def tt(self, eng, out, in0, in1, op):
        o, a, b = _ap(out), _ap(in0), _ap(in1)
        self.I(eng, lambda e: e.tensor_tensor(o, a, b, op), w=[out], r=[in0, in1])

    def ts(self, eng, out, in0, s1, s2=None, op0=ALU.mult, op1=None, accum_out=None):
        o, a = _ap(out), _ap(in0)
        x1, x2 = _ap(s1), _ap(s2)
        kw = {}
        if op1 is not None:
            kw['op1'] = op1
        if accum_out is not None:
            kw['accum_out'] = _ap(accum_out)
        self.I(eng, lambda e: e.tensor_scalar(o, a, x1, x2, op0, **kw), w=[out, accum_out],
               r=[in0, s1 if isinstance(s1, V) else None, s2 if isinstance(s2, V) else None])

    def stt(self, eng, out, in0, scalar, in1, op0, op1):
        o, a, b = _ap(out), _ap(in0), _ap(in1)
        s = _ap(scalar)
        self.I(eng, lambda e: e.scalar_tensor_tensor(o, a, s, b, op0, op1), w=[out],
               r=[in0, in1, scalar if isinstance(scalar, V) else None])

    def copy(self, eng, out, in_):
        o, a = _ap(out), _ap(in_)
        if eng == 'act':
            self.I(eng, lambda e: e.copy(o, a), w=[out], r=[in_])
        else:
            self.I(eng, lambda e: e.tensor_copy(o, a), w=[out], r=[in_])

    def memset(self, eng, out, val):
        o = _ap(out)
        self.I(eng, lambda e: e.memset(o, val), w=[out])

    def barrier(self):
        evs = {e: self.cnt[e] for e in ['pe', 'act', 'dve', 'pool'] if self.cnt[e] > 0}
        for t in self.all_tiles:
            if t.dcnt > 0:
                evs[t.dsem] = t.dcnt
        for eng in ENGS:
            kn = self.known[eng]
            waits = []
            for s_, v in evs.items():
                if kn.get(s_, 0) < v:
                    kn[s_] = v
                    waits.append((s_, v))
            if waits:
                self.stream[eng].append((waits, None, None))

    def scope(self):
        P = self

        class _S:
            def __enter__(self_):
                self_.old = P.es
                self_.st = ExitStack()
                self_.st.__enter__()
                P.es = self_.st
                return self_

            def __exit__(self_, *a):
                P.barrier()
                P.emit()
                P.es = self_.old
                self_.st.__exit__(None, None, None)
                return False
        return _S()

    def emit(self):
        nc = self.nc
        sems = self.sems
        with nc.Block() as block:
            def run(engobj, name):
                for waits, fn, inc in self.stream[name]:
                    for s, v in waits:
                        engobj.wait_ge(sems[s], v)
                    if fn is not None:
                        ins = fn(engobj)
                        ins.then_inc(sems[inc[0]], inc[1])

            @block.tensor
            def _(e):
                run(e, 'pe')

            @block.scalar
            def _(e):
                run(e, 'act')

            @block.vector
            def _(e):
                run(e, 'dve')

            @block.gpsimd
            def _(e):
                run(e, 'pool')

            @block.sync
            def _(e):
                run(e, 'sp')
        self.stream = {e: [] for e in ENGS}


D = 1024
MIXW = 512
NEXP = 32
DN_ALPHA = (2.0 * 2) ** 0.25
EPS = 1e-5
OFF = dict(r_q=0, r_k=256, r_v=512, r_g=1024, m_x=1536, m_i=2048, m_f=2052, m_o=2056,
           a_q=2568, a_k=3336, a_v=4104, gates=5640)
D_IN = 8712


class Ctx:
    pass


def make_ctx(P):
    C = Ctx()
    C.identf = P.sb("identf", [128, 128], F32)
    C.identb = P.sb("identb", [128, 128], BF16)
    C.onesb = P.sb("onesb", [128, 128], BF16)
    C.onesf = P.sb("onesf", [128, 128], F32)
    P.memset('pool', C.identf.v, 1.0)
    o = C.identf.v.ap
    P.I('pool', lambda e: e.affine_select(o, o, [[-1, 128]], ALU.is_equal, 0.0, base=0, channel_multiplier=1),
        w=[C.identf], r=[C.identf])
    P.copy('pool', C.identb.v, C.identf.v)
    P.memset('pool', C.onesb.v, 1.0)
    P.memset('pool', C.onesf.v, 1.0)
    C.ps = [P.ps("psb%d" % i, [128, 512], F32) for i in range(8)]
    return C


def load_w_cast(P, dst, src, q='pool'):
    cols = src.shape[-1]
    c0 = 0
    while c0 < cols:
        c1 = min(cols, c0 + 1024)
        P.dma(q, dst[:, :, c0:c1], src[:, :, c0:c1])
        c0 = c1


def bcast_rows(P, dst, src1d, q='act'):
    P.dma(q, dst, src1d.partition_broadcast(128))


def x_transpose(P, C, xt, outs, psl):
    for half in range(2):
        pt = psl[half]
        for c in range(4):
            k = half * 4 + c
            P.tr(pt[:, c * 128:(c + 1) * 128], xt[:, k * 128:(k + 1) * 128], C.identf.v)
        for (o, eng) in outs:
            P.copy(eng, o[:, half * 4:(half + 1) * 4, :], pt.v.re("p (c t) -> p c t", c=4))


def layer_norm(P, r, g_b, b_b, st, mv, rstd, eng2='pool'):
    for hf in range(2):
        a, b = st[:, hf, :].ap, r[:, hf * 512:(hf + 1) * 512].ap
        P.I('dve', (lambda a, b: (lambda e: e.bn_stats(a, b)))(a, b), w=[st], r=[r])
    a, b = mv.ap, st.ap
    P.I('dve', lambda e: e.bn_aggr(a, b), w=[mv], r=[st])
    P.ts('dve', rstd, mv[:, 1:2], EPS, None, op0=ALU.add)
    P.act(rstd, rstd, AF.Ln)
    P.act(rstd, rstd, AF.Exp, scale=-0.5)
    P.ts('dve', r, r, mv[:, 0:1], rstd, op0=ALU.subtract, op1=ALU.mult)
    P.tt(eng2, r, r, g_b, ALU.mult)
    P.tt(eng2, r, r, b_b, ALU.add)


def pass_merge(P, C, NT, x_d, yT_d, w_in_l, w_branch_l, w_out_l, ln_g, ln_b, x1_d, pfx="m"):
    wg = P.sb(pfx + "wg", [128, 8, 3072], BF16)
    wb = P.sb(pfx + "wb", [128, 12, 1024], BF16)
    wo = P.sb(pfx + "wo", [128, 8, 1024], BF16)
    load_w_cast(P, wg.v, w_in_l[:, OFF['gates']:D_IN].rearrange("(kc p) c -> p kc c", p=128))
    load_w_cast(P, wb.v, w_branch_l.rearrange("b (kc p) c -> p (b kc) c", p=128))
    load_w_cast(P, wo.v, w_out_l.rearrange("(kc p) c -> p kc c", p=128))
    gB = P.sb(pfx + "gB", [128, 1024]); bB = P.sb(pfx + "bB", [128, 1024])
    bcast_rows(P, gB.v, ln_g); bcast_rows(P, bB.v, ln_b)
    xts = [P.sb(pfx + "xt%d" % i, [128, 1024]) for i in range(2)]
    xTs = [P.sb(pfx + "xT%d" % i, [128, 8, 128], BF16) for i in range(2)]
    yTs = [[P.sb(pfx + "yT%d_%d" % (b, i), [128, 4, 128], BF16) for i in range(2)] for b in range(3)]
    mg = [P.sb(pfx + "mg%d" % i, [128, 1024]) for i in range(2)]
    mT = [P.sb(pfx + "mT%d" % i, [128, 8, 128], BF16) for i in range(2)]
    sg = [P.sb(pfx + "sg%d" % i, [128, 512]) for i in range(2)]
    tmp = [P.sb(pfx + "tmp%d" % i, [128, 512]) for i in range(2)]
    rr = [P.sb(pfx + "rr%d" % i, [128, 1024]) for i in range(2)]
    st = P.sb(pfx + "st", [128, 2, 6]); mv = P.sb(pfx + "mv", [128, 2]); rstd = P.sb(pfx + "rstd", [128, 1])
    k = 0
    for t in range(NT // 128):
        xt = xts[t % 2]; xT = xTs[t % 2]
        P.dma('sp', xt.v, x_d[t * 128:(t + 1) * 128, :])
        x_transpose(P, C, xt.v, [(xT.v, 'act')], [C.ps[0], C.ps[1]])
        for b in range(3):
            P.dma('act', yTs[b][t % 2].v, yT_d[b][:, :, t * 128:(t + 1) * 128])
        m = mg[t % 2]
        for b in range(3):
            for hf in range(2):
                pg = C.ps[2 + (k % 2)]; pb = C.ps[4 + (k % 2)]; s = sg[k % 2]; tm = tmp[k % 2]
                k += 1
                for kc in range(8):
                    P.mm(pg.v, xT[:, kc, :], wg[:, kc, b * 1024 + hf * 512: b * 1024 + (hf + 1) * 512],
                         start=(kc == 0), stop=(kc == 7))
                for kc in range(4):
                    P.mm(pb.v, yTs[b][t % 2][:, kc, :], wb[:, b * 4 + kc, hf * 512:(hf + 1) * 512],
                         start=(kc == 0), stop=(kc == 3))
                P.act(s.v, pg.v, AF.Sigmoid)
                msl = m[:, hf * 512:(hf + 1) * 512]
                if b == 0:
                    P.tt('dve', msl, s.v, pb.v, ALU.mult)
                else:
                    P.tt('dve', tm.v, s.v, pb.v, ALU.mult)
                    P.tt('pool', msl, msl, tm.v, ALU.add)
        x_transpose(P, C, m.v, [(mT[t % 2].v, 'act')], [C.ps[6], C.ps[7]])
        r = rr[t % 2]
        for hf in range(2):
            po = C.ps[2 + (k % 2)]
            k += 1
            for kc in range(8):
                P.mm(po.v, mT[t % 2][:, kc, :], wo[:, kc, hf * 512:(hf + 1) * 512], start=(kc == 0), stop=(kc == 7))
            P.stt('dve', r[:, hf * 512:(hf + 1) * 512], xt[:, hf * 512:(hf + 1) * 512], DN_ALPHA, po.v,
                  ALU.mult, ALU.add)
        layer_norm(P, r.v, gB.v, bB.v, st.v, mv.v, rstd.v)
        P.dma('sp', x1_d[t * 128:(t + 1) * 128, :], r.v)


def pass_moe(P, C, NT, x1_d, w_router, b_router, w_gate, b_gate, w_up, b_up, w_down, b_down, ln_g, ln_b, out_d,
             NE=NEXP, pfx="e", TGT=4, dbg=0):
    TG = TGT * 128
    gB = P.sb(pfx + "gB", [128, 1024]); bB = P.sb(pfx + "bB", [128, 1024])
    bcast_rows(P, gB.v, ln_g); bcast_rows(P, bB.v, ln_b)
    wr = P.sb(pfx + "wr", [128, 8, NE], F32R)
    wr0 = P.sb(pfx + "wr0", [128, 8, NE])
    P.dma('act', wr0.v, w_router.rearrange("(kc p) e -> p kc e", p=128))
    P.copy('dve', wr.v, wr0.v)
    brB = P.sb(pfx + "brB", [128, NE]); bcast_rows(P, brB.v, b_router)
    bgT = P.sb(pfx + "bgT", [128, NE, 8]); buT = P.sb(pfx + "buT", [128, NE, 8])
    bstage = P.sb(pfx + "bstage", [128, 128])
    if dbg in (3, 7):
        P.memset('dve', bgT.v, 0.0); P.memset('dve', buT.v, 0.0)
    for (dstT, src) in (((bgT, b_gate), (buT, b_up)) if dbg not in (3, 7) else ()):
        rows = NE * 8
        srcv = src.rearrange("e (fc p) -> (e fc) p", p=128)
        dv = dstT.v.re("p e fc -> p (e fc)")
        r0 = 0
        while r0 < rows:
            r1 = min(rows, r0 + 128)
            n = r1 - r0
            P.dma('act', bstage[0:n, :], srcv[r0:r1, :])
            pz = C.ps[7]
            P.tr(pz[:, 0:n], bstage[0:n, :], C.identf[0:n, 0:n])
            P.copy('dve', dv[:, r0:r1], pz[:, 0:n])
            r0 = r1
    bd = P.sb(pfx + "bd", [NE, 1024], F32R)
    bd0 = P.sb(pfx + "bd0", [NE, 1024])
    P.dma('act', bd0.v, b_down)
    P.copy('dve', bd.v, bd0.v)
    W = [[P.sb(pfx + "W%d_%d" % (j, i), [128, 8, 1024], BF16) for j in range(3)] for i in range(2)]
    xts = [P.sb(pfx + "xt%d" % i, [128, 1024]) for i in range(TGT)]
    xTg = P.sb(pfx + "xTg", [128, 8, TG], BF16)
    xT32 = P.sb(pfx + "xT32", [128, 8, 128], F32R)
    acc = P.sb(pfx + "acc", [128, TGT, 1024])
    pall = P.sb(pfx + "pall", [128, TGT, NE])
    actT = P.sb(pfx + "actT", [128, 8, TG], BF16)
    lg = P.sb(pfx + "lg", [128, NE]); t8 = P.sb(pfx + "t8", [128, 8]); msk = P.sb(pfx + "msk", [128, NE])
    ex = P.sb(pfx + "ex", [128, NE]); sm = P.sb(pfx + "sm", [128, 1]); nmx = P.sb(pfx + "nmx", [128, 1])
    pT = P.sb(pfx + "pT", [NE, 128], F32R)
    gt = [P.sb(pfx + "g%d" % i, [128, TG]) for i in range(2)]
    st_ = [P.sb(pfx + "s%d" % i, [128, TG]) for i in range(2)]
    ut = [P.sb(pfx + "u%d" % i, [128, TG]) for i in range(2)]
    st = P.sb(pfx + "st", [128, 2, 6]); mv = P.sb(pfx + "mv", [128, 2]); rstd = P.sb(pfx + "rstd", [128, 1])
    c1 = P.sb(pfx + "c1", [128, 1]); c7 = P.sb(pfx + "c7", [128, 1])
    P.memset('dve', c1.v, 1.0); P.memset('dve', c7.v, 7.0)
    rr = [P.sb(pfx + "rr%d" % i, [128, 1024]) for i in range(2)]
    tmpq = [P.sb(pfx + "tq%d" % i, [128, 512]) for i in range(2)]
    assert TG == 512
    if dbg == 5:
        P.wait_all('sp', [gB, bB, wr, brB, bd, bgT, buT])
        return
    wcnt = 0
    kk = 0
    for gi in range(NT // TG):
        for tt in range(TGT):
            t = gi * TGT + tt
            xt = xts[tt]
            P.dma('sp', xt.v, x1_d[t * 128:(t + 1) * 128, :])
            x_transpose(P, C, xt.v, [(xTg[:, :, tt * 128:(tt + 1) * 128], 'act')] + ([(xT32.v, 'dve')] if dbg != 3 else []), [C.ps[0], C.ps[1]])
            if dbg in (2, 3, 7, 8):
                P.memset('dve', acc[:, tt, :], 0.0)
                P.memset('dve', pall[:, tt, :], 0.25)
            else:
                pl = C.ps[6]
                for kc in range(8):
                    P.mm(pl[:, 0:NE], xT32[:, kc, :], wr[:, kc, :], start=(kc == 0), stop=(kc == 7))
                P.tt('dve', lg.v, pl[:, 0:NE], brB.v, ALU.add)
                a, b = t8.v.ap, lg.v.ap
                P.I('dve', (lambda a, b: (lambda e: e.max(out=a, in_=b)))(a, b), w=[t8], r=[lg])
                P.ts('dve', msk.v, lg.v, t8[:, 3:4], c1.v, op0=ALU.is_ge, op1=ALU.mult)
                P.ts('dve', nmx.v, t8[:, 0:1], -1.0, None, op0=ALU.mult)
                P.act(ex.v, lg.v, AF.Exp, bias=nmx.v)
                P.tt('dve', ex.v, ex.v, msk.v, ALU.mult)
                a2, b2 = sm.v.ap, ex.v.ap
                P.I('dve', (lambda a, b: (lambda e: e.reduce_sum(a, b, AX.X)))(a2, b2), w=[sm], r=[ex])
                a3 = sm.v.ap
                P.I('dve', (lambda a: (lambda e: e.reciprocal(a, a)))(a3), w=[sm], r=[sm])
                P.ts('dve', pall[:, tt, :], ex.v, sm.v, c1.v, op0=ALU.mult, op1=ALU.mult)
                P.tr(pl[0:NE, 128:256], pall[:, tt, :], C.identf.v)
                P.copy('dve', pT.v, pl[0:NE, 128:256])
                for hf in range(2):
                    pb = C.ps[7]
                    P.mm(pb.v, pT.v, bd[:, hf * 512:(hf + 1) * 512])
                    P.copy('dve', acc[:, tt, hf * 512:(hf + 1) * 512], pb.v)
        for e in range(NE if dbg not in (1, 3, 7, 8) else 0):
            Wg, Wu, Wd = W[wcnt % 2]
            wcnt += 1
            P.dma('pool', Wg.v, w_gate[e].rearrange("(kc p) f -> p kc f", p=128))
            P.dma('pool', Wu.v, w_up[e].rearrange("(kc p) f -> p kc f", p=128))
            P.dma('pool', Wd.v, w_down[e].rearrange("(kc p) f -> p kc f", p=128))
            for fc in range(8):
                pg = C.ps[(kk % 2) * 2]; pu = C.ps[(kk % 2) * 2 + 1]
                g = gt[kk % 2]; s = st_[kk % 2]; u = ut[kk % 2]
                kk += 1
                for kc in range(8):
                    P.mm(pg.v, Wg[:, kc, fc * 128:(fc + 1) * 128], xTg[:, kc, :], start=(kc == 0), stop=(kc == 7))
                for kc in range(8):
                    P.mm(pu.v, Wu[:, kc, fc * 128:(fc + 1) * 128], xTg[:, kc, :], start=(kc == 0), stop=(kc == 7))
                P.ts('dve', g.v, pg.v, bgT[:, e, fc:fc + 1], c7.v, op0=ALU.add, op1=ALU.min)
                P.act(s.v, g.v, AF.Sigmoid, scale=1.702)
                P.ts('dve', u.v, pu.v, buT[:, e, fc:fc + 1], c7.v, op0=ALU.add, op1=ALU.min)
                P.ts('dve', u.v, u.v, -7.0, 1.0, op0=ALU.max, op1=ALU.add)
                P.tt('dve', g.v, g.v, s.v, ALU.mult)
                P.tt('dve', actT[:, fc, :], g.v, u.v, ALU.mult)
            for tt in range(TGT):
                for hf in range(2):
                    py = C.ps[4 + (kk % 2)]
                    kk += 1
                    for fc in range(8):
                        P.mm(py.v, actT[:, fc, tt * 128:(tt + 1) * 128], Wd[:, fc, hf * 512:(hf + 1) * 512],
                             start=(fc == 0), stop=(fc == 7))
                    av = acc[:, tt, hf * 512:(hf + 1) * 512]
                    tq = tmpq[kk % 2]
                    P.ts('dve', tq.v, py.v, pall[:, tt, e:e + 1], c1.v, op0=ALU.mult, op1=ALU.mult)
                    P.tt('dve', av, av, tq.v, ALU.add)
        for tt in range(TGT):
            t = gi * TGT + tt
            r = rr[tt % 2].v
            P.stt('dve', r, xts[tt].v, DN_ALPHA, acc[:, tt, :], ALU.mult, ALU.add)
            layer_norm(P, r, gB.v, bB.v, st.v, mv.v, rstd.v)
            P.dma('sp', out_d[t * 128:(t + 1) * 128, :], r)


RET_GAMMA = [1.0 - 2.0 ** (-5.0 - h) for h in range(4)]


def host_consts_scan(NT):
    j = np.arange(128, dtype=np.float64)
    lg = np.log(np.array(RET_GAMMA, dtype=np.float64))
    aR = np.exp(lg[None, :] * (j[:, None] + 1.0)) * (64 ** -0.5)
    bR = np.exp(-lg[None, :] * (j[:, None] + 1.0))
    eR = np.zeros((128, 2)); gR = np.zeros((128, 2))
    for h in range(4):
        ps = (h % 2) * 64
        eR[ps:ps + 64, h // 2] = np.exp(lg[h] * 128.0)
        gR[ps:ps + 64, h // 2] = np.exp(lg[h] * float(NT))
    mask = (j[:, None] <= j[None, :]).astype(np.float64)
    return dict(aR=aR.astype(np.float32), bR=bR.astype(np.float32), eR=eR.astype(np.float32),
                gR=gR.astype(np.float32), mask=mask.astype(np.float32))


def host_blockdiag(w):
    out = np.zeros((4, 128, 128), dtype=np.float32)
    for h in range(4):
        for n in range(32):
            out[h, 4 * n:4 * n + 4, 4 * n:4 * n + 4] = w[32 * h + n]
    return out


class PSRot:
    def __init__(self, C, banks):
        self.C = C; self.banks = banks; self.i = 0

    def __call__(self):
        b = self.C.ps[self.banks[self.i % len(self.banks)]]
        self.i += 1
        return b


def small_T(P, C, dst, src2d, rows, stage, ps):
    P.dma('act', stage[0:rows, :], src2d)
    P.tr(ps[:, 0:rows], stage[0:rows, :], C.identf[0:rows, 0:rows])
    P.copy('dve', dst, ps[:, 0:rows])


def pass_scan(P, C, NT, x_d, xprev_d, w_in_l, prm, cst, init, outs, mode="full", pfx="s"):
    full = (mode == "full")
    NW = 2568
    W = P.sb(pfx + "W", [128, 8, NW], BF16)
    load_w_cast(P, W.v, w_in_l[:, 0:NW].rearrange("(kc p) c -> p kc c", p=128))
    BD = {}
    for nm in ("bdq", "bdk", "bdv"):
        BD[nm] = P.sb(pfx + nm, [128, 4, 128], BF16)
        P.dma('pool', BD[nm].v, prm[nm].rearrange("h i o -> i h o"))
    stage = P.sb(pfx + "stage", [128, 128])
    cwT = P.sb(pfx + "cwT", [128, 16]); cbT = P.sb(pfx + "cbT", [128, 4])
    small_T(P, C, cwT.v, prm["ml_conv_w"].rearrange("k (c p) -> (k c) p", p=128), 16, stage, C.ps[7])
    small_T(P, C, cbT.v, prm["ml_conv_b"].rearrange("(c p) -> c p", p=128), 4, stage, C.ps[7])
    biB = P.sb(pfx + "biB", [128, 4]); bfB = P.sb(pfx + "bfB", [128, 4])
    bcast_rows(P, biB.v, prm["ml_bi"]); bcast_rows(P, bfB.v, prm["ml_bf"])
    aR = P.sb(pfx + "aR", [128, 4]); bR = P.sb(pfx + "bR", [128, 4]); eR = P.sb(pfx + "eR", [128, 2]); gR = P.sb(pfx + "gR", [128, 2])
    for t_, n_ in ((aR, "aR"), (bR, "bR"), (eR, "eR"), (gR, "gR")):
        P.dma('act', t_.v, cst[n_])
    mask = P.sb(pfx + "mask", [128, 128]); P.dma('act', mask.v, cst["mask"])
    maskr = P.sb(pfx + "maskr", [128, 128], F32R); P.copy('dve', maskr.v, mask.v)
    onesr = P.sb(pfx + "onesr", [128, 128], F32R); P.copy('dve', onesr.v, C.onesf.v)
    c1 = P.sb(pfx + "c1", [128, 1]); P.memset('dve', c1.v, 1.0)
    if full:
        gnR = P.sb(pfx + "gnR", [128, 512]); gnM = P.sb(pfx + "gnM", [128, 512]); skM = P.sb(pfx + "skM", [128, 512])
        bcast_rows(P, gnR.v, prm["ret_gn"]); bcast_rows(P, gnM.v, prm["ml_gn"]); bcast_rows(P, skM.v, prm["ml_skip"])
    Sret = P.sb(pfx + "Sret", [128, 2, 128]); Sretb = P.sb(pfx + "Sretb", [128, 2, 128], BF16)
    Cml = P.sb(pfx + "Cml", [128, 4, 129]); Cmlb = P.sb(pfx + "Cmlb", [128, 4, 129], BF16)
    tmpS = P.sb(pfx + "tmpS", [128, 4, 129]); tmpS2 = P.sb(pfx + "tmpS2", [128, 4, 129])
    totacc = P.sb(pfx + "totacc", [128, 4])
    P.memset('dve', Sret.v, 0.0); P.memset('dve', Cml.v, 0.0); P.memset('dve', totacc.v, 0.0)
    if init is not None:
        sel = P.sb(pfx + "sel", [128, 3]); nsel = P.sb(pfx + "nsel", [128, 3])
        P.dma('act', sel.v, init["sel"]); P.dma('act', nsel.v, init["nsel"])
        Fr = P.sb(pfx + "Fr", [128, 2, 128]); Fm = P.sb(pfx + "Fm", [128, 4, 129]); tl = P.sb(pfx + "tl", [128, 4])
        Gm = P.sb(pfx + "Gm", [128, 4])
        for q in range(3):
            P.dma('act', Fr.v, init["Fret"][q]); P.dma('act', Fm.v, init["Fml"][q]); P.dma('act', tl.v, init["totL"][q])
            P.act(Gm.v, tl.v, AF.Exp, scale=-1.0)
            for hp in range(2):
                P.act(tmpS[:, hp, 0:128], Sret[:, hp, :], AF.Copy, scale=gR[:, hp:hp + 1])
                P.tt('dve', tmpS[:, hp, 0:128], tmpS[:, hp, 0:128], Fr[:, hp, :], ALU.add)
                P.act(tmpS[:, hp, 0:128], tmpS[:, hp, 0:128], AF.Copy, scale=sel[:, q:q + 1])
                P.act(tmpS2[:, hp, 0:128], Sret[:, hp, :], AF.Copy, scale=nsel[:, q:q + 1])
                P.tt('dve', Sret[:, hp, :], tmpS[:, hp, 0:128], tmpS2[:, hp, 0:128], ALU.add)
            for h in range(4):
                P.act(tmpS[:, h, :], Cml[:, h, :], AF.Copy, scale=Gm[:, h:h + 1])
                P.tt('dve', tmpS[:, h, :], tmpS[:, h, :], Fm[:, h, :], ALU.add)
                P.act(tmpS[:, h, :], tmpS[:, h, :], AF.Copy, scale=sel[:, q:q + 1])
                P.act(tmpS2[:, h, :], Cml[:, h, :], AF.Copy, scale=nsel[:, q:q + 1])
                P.tt('dve', Cml[:, h, :], tmpS[:, h, :], tmpS2[:, h, :], ALU.add)
    P.copy('dve', Sretb.v, Sret.v); P.copy('dve', Cmlb.v, Cml.v)
    SC = 512
    xts = [P.sb(pfx + "xt%d" % i, [128, 1024]) for i in range(2)]
    xT = P.sb(pfx + "xT", [128, 8, SC], BF16)
    rqT = P.sb(pfx + "rqT", [128, 2, SC], BF16); rkT = P.sb(pfx + "rkT", [128, 2, SC], BF16)
    mxT = P.sb(pfx + "mxT", [128, 4, 3 + SC]); mxb = P.sb(pfx + "mxb", [128, 4, SC], BF16)
    cva = P.sb(pfx + "cva", [128, SC]); cvb = P.sb(pfx + "cvb", [128, SC])
    mcT = P.sb(pfx + "mcT", [128, 4, SC], BF16)
    qmT = P.sb(pfx + "qmT", [128, 4, SC], BF16); kmT = P.sb(pfx + "kmT", [128, 4, SC], BF16)
    rk_tok = P.sb(pfx + "rk_tok", [128, 256], BF16); km_tok = P.sb(pfx + "km_tok", [128, 512], BF16)
    vpR = P.sb(pfx + "vpR", [128, 4, 128], BF16); vpM = P.sb(pfx + "vpM", [128, 4, 129], BF16)
    g8 = P.sb(pfx + "g8", [128, 8]); L1 = P.sb(pfx + "L1", [128, 4], F32R); e1 = P.sb(pfx + "e1", [128, 4])
    igt = P.sb(pfx + "igt", [128, 4]); aM = P.sb(pfx + "aM", [128, 4]); bM = P.sb(pfx + "bM", [128, 4]); eM = P.sb(pfx + "eM", [128, 4])
    tmp4 = P.sb(pfx + "tmp4", [128, 4])
    Pm = [P.sb(pfx + "Pm%d" % i, [128, 128], BF16) for i in range(2)]
    ot = [P.sb(pfx + "ot%d" % i, [128, 129]) for i in range(2)]
    hh = [P.sb(pfx + "hh%d" % i, [128, 128]) for i in range(2)]
    dn = P.sb(pfx + "dn", [128, 1]); st6 = P.sb(pfx + "st6", [128, 6]); mv = P.sb(pfx + "mv", [128, 2]); rs = P.sb(pfx + "rs", [128, 1])
    if full:
        yR = P.sb(pfx + "yR", [128, 512]); yM = P.sb(pfx + "yM", [128, 512])
        rg = P.sb(pfx + "rg", [128, 512]); mo = P.sb(pfx + "mo", [128, 512]); mct = P.sb(pfx + "mct", [128, 512])
        yTo = [P.sb(pfx + "yTo%d" % i, [128, 4, 128], BF16) for i in range(2)]
        ybf = P.sb(pfx + "ybf", [128, 512], BF16)
    nps = PSRot(C, [0, 1, 2, 3, 4, 5, 6, 7])
    P.dma('sp', xts[0].v, xprev_d)
    x_transpose(P, C, xts[0].v, [(xT[:, :, 0:128], 'act')], [nps(), nps()])
    for c in range(4):
        pz = nps()
        for kc in range(8):
            P.mm(pz[:, 0:128], W[:, kc, OFF['m_x'] + c * 128: OFF['m_x'] + (c + 1) * 128], xT[:, kc, 0:128],
                 start=(kc == 0), stop=(kc == 7))
        P.copy('dve', mxT[:, c, 0:3], pz[:, 125:128])
    lnscale = float(np.log(128 ** -0.5))
    for sc in range(NT // SC):
        for tt in range(4):
            t = sc * 4 + tt
            xt = xts[t % 2]
            P.dma('sp', xt.v, x_d[t * 128:(t + 1) * 128, :])
            x_transpose(P, C, xt.v, [(xT[:, :, tt * 128:(tt + 1) * 128], 'act')], [nps(), nps()])
        for (dst, off, nch, kind) in ((rqT, OFF['r_q'], 2, 'bf'), (rkT, OFF['r_k'], 2, 'bf'), (mxT, OFF['m_x'], 4, 'mx')):
            if not full and (dst is rqT or dst is rkT):
                continue
            for c in range(nch):
                pz = nps()
                for kc in range(8):
                    P.mm(pz.v, W[:, kc, off + c * 128: off + (c + 1) * 128], xT[:, kc, :], start=(kc == 0), stop=(kc == 7))
                if kind == 'bf':
                    P.copy('act', dst[:, c, :], pz.v)
                else:
                    P.copy('act', mxT[:, c, 3:3 + SC], pz.v)
                    P.copy('dve', mxb[:, c, :], pz.v)
        for c in range(4):
            P.ts('dve', cva.v, mxT[:, c, 3:3 + SC], cwT[:, 12 + c:13 + c], cbT[:, c:c + 1], op0=ALU.mult, op1=ALU.add)
            P.stt('dve', cvb.v, mxT[:, c, 2:2 + SC], cwT[:, 8 + c:9 + c], cva.v, ALU.mult, ALU.add)
            P.stt('dve', cva.v, mxT[:, c, 1:1 + SC], cwT[:, 4 + c:5 + c], cvb.v, ALU.mult, ALU.add)
            P.stt('dve', cvb.v, mxT[:, c, 0:SC], cwT[:, c:c + 1], cva.v, ALU.mult, ALU.add)
            P.act(mcT[:, c, :], cvb.v, AF.Silu)
            P.copy('dve', cva[:, 0:3], mxT[:, c, SC:SC + 3])
            P.copy('dve', mxT[:, c, 0:3], cva[:, 0:3])
        for (dst, bd) in ((qmT, BD["bdq"]), (kmT, BD["bdk"])):
            if not full:
                continue
            for h in range(4):
                pz = nps()
                P.mm(pz.v, bd[:, h, :], mcT[:, h, :])
                P.copy('act', dst[:, h, :], pz.v)
        for tt in range(4):
            t = sc * 4 + tt
            ts_ = slice(tt * 128, (tt + 1) * 128)

            def tok_proj(off, n):
                pz = nps()
                for kc in range(8):
                    P.mm(pz[:, 0:n], xT[:, kc, ts_], W[:, kc, off:off + n], start=(kc == 0), stop=(kc == 7))
                return pz
            p_rk = tok_proj(OFF['r_k'], 256)
            P.copy('act', rk_tok.v, p_rk[:, 0:256])
            p_g8 = tok_proj(OFF['m_i'], 8)
            P.copy('dve', g8.v, p_g8[:, 0:8])
            P.tt('dve', igt.v, g8[:, 0:4], biB.v, ALU.add)
            P.tt('dve', tmp4.v, g8[:, 4:8], bfB.v, ALU.add)
            P.act(e1.v, tmp4.v, AF.Exp, scale=-1.0)
            P.ts('dve', e1.v, e1.v, 1.0, None, op0=ALU.add)
            P.act(L1.v, e1.v, AF.Ln)
            pc = nps()
            P.mm(pc[:, 0:4], maskr.v, L1.v)
            P.mm(pc[:, 8:12], onesr.v, L1.v)
            P.act(aM.v, pc[:, 0:4], AF.Exp, scale=-1.0, bias=lnscale)
            P.tt('dve', tmp4.v, igt.v, pc[:, 0:4], ALU.add)
            P.act(bM.v, tmp4.v, AF.Exp)
            P.act(eM.v, pc[:, 8:12], AF.Exp, scale=-1.0)
            P.tt('dve', totacc.v, totacc.v, pc[:, 8:12], ALU.add)
            p_rv = tok_proj(OFF['r_v'], 512)
            for h in range(4):
                P.act(vpR[:, h, :], p_rv[:, h * 128:(h + 1) * 128], AF.Copy, scale=bR[:, h:h + 1])
            p_vm = nps()
            for h in range(4):
                P.mm(p_vm[:, h * 128:(h + 1) * 128], mxb[:, h, ts_], BD["bdv"][:, h, :])
            for h in range(4):
                P.act(vpM[:, h, 0:128], p_vm[:, h * 128:(h + 1) * 128], AF.Copy, scale=bM[:, h:h + 1])
            P.copy('dve', vpM[:, :, 128:129], bM.v.re("p (h o) -> p h o", o=1))
            p_km = nps()
            for h in range(4):
                P.mm(p_km[:, h * 128:(h + 1) * 128], mcT[:, h, ts_], BD["bdk"][:, h, :])
            P.copy('act', km_tok.v, p_km.v)
            if full:
                p_rg = tok_proj(OFF['r_g'], 512)
                P.act(rg.v, p_rg.v, AF.Silu)
                p_mo = tok_proj(OFF['m_o'], 512)
                P.act(mo.v, p_mo.v, AF.Sigmoid)
                p_mc = nps()
                pmb = p_mc.v.bitcast(BF16)
                for c in range(4):
                    P.tr(pmb[:, c * 128:(c + 1) * 128], mcT[:, c, ts_], C.identb.v)
                P.tt('dve', mct.v, pmb[:, 0:512], skM.v, ALU.mult)
            kq = 0
            for h in range(4):
                psl = slice((h % 2) * 64, (h % 2) * 64 + 64); hp = h // 2
                if full:
                    p_st = nps()
                    P.mm(p_st[:, 0:128], rkT[psl, hp, ts_], rqT[psl, hp, ts_])
                    pm = Pm[kq % 2]; o = ot[kq % 2]; hx = hh[kq % 2]; kq += 1
                    P.tt('dve', pm.v, p_st[:, 0:128], mask.v, ALU.mult)
                    p_o = nps()
                    P.mm(p_o[:, 0:128], pm.v, vpR[:, h, :], start=True, stop=False)
                    P.mm(p_o[:, 0:128], rqT[psl, hp, ts_], Sretb[psl, hp, :], start=False, stop=True)
                    P.act(o[:, 0:128], p_o[:, 0:128], AF.Copy, scale=aR[:, h:h + 1])
                    head_norm(P, o[:, 0:128], hx.v, st6, mv, rs)
                    P.tt('pool', hx.v, hx.v, gnR[:, h * 128:(h + 1) * 128], ALU.mult)
                    P.tt('pool', yR[:, h * 128:(h + 1) * 128], hx.v, rg[:, h * 128:(h + 1) * 128], ALU.mult)
                p_kv = nps()
                P.mm(p_kv[:, 0:128], rk_tok[:, hp * 128:(hp + 1) * 128], vpR[:, h, :])
                P.tt('dve', tmpS[psl, hp, 0:128], Sret[psl, hp, :], p_kv[psl, 0:128], ALU.add)
                P.act(Sret[psl, hp, :], tmpS[psl, hp, 0:128], AF.Copy, scale=eR[psl, hp:hp + 1])
                P.copy('dve', Sretb[psl, hp, :], Sret[psl, hp, :])
            for h in range(4):
                if full:
                    p_st = nps()
                    P.mm(p_st[:, 0:128], kmT[:, h, ts_], qmT[:, h, ts_])
                    pm = Pm[kq % 2]; o = ot[kq % 2]; hx = hh[kq % 2]; kq += 1
                    P.tt('dve', pm.v, p_st[:, 0:128], mask.v, ALU.mult)
                    p_o = nps()
                    P.mm(p_o[:, 0:129], pm.v, vpM[:, h, :], start=True, stop=False)
                    P.mm(p_o[:, 0:129], qmT[:, h, ts_], Cmlb[:, h, :], start=False, stop=True)
                    P.act(o.v, p_o[:, 0:129], AF.Copy, scale=aM[:, h:h + 1])
                    P.act(dn.v, o[:, 128:129], AF.Abs)
                    P.ts('dve', dn.v, dn.v, 1.0, None, op0=ALU.max)
                    a_ = dn.v.ap
                    P.I('dve', (lambda a_: (lambda e: e.reciprocal(a_, a_)))(a_), w=[dn], r=[dn])
                    P.act(o[:, 0:128], o[:, 0:128], AF.Copy, scale=dn.v)
                    head_norm(P, o[:, 0:128], hx.v, st6, mv, rs)
                    P.tt('pool', hx.v, hx.v, gnM[:, h * 128:(h + 1) * 128], ALU.mult)
                    P.tt('pool', hx.v, hx.v, mct[:, h * 128:(h + 1) * 128], ALU.add)
                    P.tt('pool', yM[:, h * 128:(h + 1) * 128], hx.v, mo[:, h * 128:(h + 1) * 128], ALU.mult)
                p_kv = nps()
                P.mm(p_kv[:, 0:129], km_tok[:, h * 128:(h + 1) * 128], vpM[:, h, :])
                P.tt('dve', tmpS[:, h, :], Cml[:, h, :], p_kv[:, 0:129], ALU.add)
                P.act(Cml[:, h, :], tmpS[:, h, :], AF.Copy, scale=eM[:, h:h + 1])
                P.copy('dve', Cmlb[:, h, :], Cml[:, h, :])
            if full:
                for (ysrc, ydst) in ((yR, outs[0]), (yM, outs[1])):
                    P.copy('act', ybf.v, ysrc.v)
                    pz = nps(); pzb = pz.v.bitcast(BF16)
                    for c in range(4):
                        P.tr(pzb[:, c * 128:(c + 1) * 128], ybf[:, c * 128:(c + 1) * 128], C.identb.v)
                    yo = yTo[kq % 2]; kq += 1
                    P.copy('dve', yo.v, pzb[:, 0:512].re("p (c t) -> p c t", c=4))
                    P.dma('sp', ydst[:, :, t * 128:(t + 1) * 128], yo.v)
    if not full:
        P.dma('sp', outs[0].v, Sret.v)
        P.dma('sp', outs[1].v, Cml.v)
        P.dma('sp', outs[2].v, totacc.v)


def head_norm(P, src, dst, st6, mv, rs):
    a, b = st6.v.ap, src.ap
    P.I('dve', lambda e: e.bn_stats(a, b), w=[st6], r=[src])
    a2, b2 = mv.v.ap, st6.v.ap
    P.I('dve', lambda e: e.bn_aggr(a2, b2), w=[mv], r=[st6])
    P.ts('dve', rs.v, mv[:, 1:2], EPS, None, op0=ALU.add)
    P.act(rs.v, rs.v, AF.Ln)
    P.act(rs.v, rs.v, AF.Exp, scale=-0.5)
    P.ts('dve', dst, src, mv[:, 0:1], rs.v, op0=ALU.subtract, op1=ALU.mult)


ATT_PAT = ((128, 1), (512, 4), (2048, 16))
HALO = 2048


def host_consts_attn():
    slopes = np.exp2(-8.0 * np.arange(1, 13, dtype=np.float64) / 12.0).reshape(3, 4)
    s = np.arange(128)[:, None]; i = np.arange(128)[None, :]
    out = np.zeros((3, 2, 128, 4, 128), dtype=np.float32)
    for g, (win, d) in enumerate(ATT_PAT):
        for h in range(4):
            dcur = i - s
            b = np.where((dcur >= 0), -slopes[g, h] * d * dcur, -30000.0)
            out[g, 1, :, h, :] = b
            dprev = i + 128 - s
            b = np.where((dprev <= 128), -slopes[g, h] * d * dprev, -30000.0)
            out[g, 0, :, h, :] = b
    return out.reshape(3, 2, 128, 512)


def pass_attn(P, C, NT, xext_d, w_in_l, bias_d, hv_d, yT_out, pfx="a", dbg=0):
    accN = P.sb(pfx + "accN", [128, 4, NT]); accD = P.sb(pfx + "accD", [128, 4, NT])
    for h_ in range(4):
        for j_ in range(NT // 2048):
            P.memset('dve', accN[:, h_, j_ * 2048:(j_ + 1) * 2048], 0.0 if dbg == 0 else 1.0)
            P.memset('dve', accD[:, h_, j_ * 2048:(j_ + 1) * 2048], 0.0 if dbg == 0 else 2.0)
    hv0 = P.sb(pfx + "hv0", [128, 128]); hvb = P.sb(pfx + "hvb", [128, 128], BF16)
    P.dma('act', hv0.v, hv_d); P.copy('dve', hvb.v, hv0.v)
    Wq = P.sb(pfx + "Wq", [128, 8, 256], BF16); Wk = P.sb(pfx + "Wk", [128, 8, 256], BF16); Wv = P.sb(pfx + "Wv", [128, 8, 512], BF16)
    bT = [P.sb(pfx + "bT%d" % i, [128, 512]) for i in range(2)]
    xts = [P.sb(pfx + "xt%d" % i, [128, 1024]) for i in range(2)]
    xTb = [P.sb(pfx + "xTb%d" % i, [128, 8, 128], BF16) for i in range(2)]
    kT = [P.sb(pfx + "kT%d" % i, [128, 2, 128], BF16) for i in range(2)]
    Vt = [P.sb(pfx + "V%d" % i, [128, 512], BF16) for i in range(2)]
    qT = [P.sb(pfx + "qT%d" % i, [128, 2, 128], BF16) for i in range(2)]
    tmp = [P.sb(pfx + "tmp%d" % i, [128, 512]) for i in range(2)]
    PT = [[P.sb(pfx + "PT%d_%d" % (i, j), [128, 512], BF16) for j in range(2)] for i in range(2)]
    nps = PSRot(C, [0, 1, 2, 3, 4, 5, 6, 7])
    win = w_in_l.rearrange("(kc p) c -> p kc c", p=128)
    nb = 0
    for g, (_, d) in enumerate(ATT_PAT if dbg not in (1, 2, 4, 5, 6) else (ATT_PAT[:1] if dbg in (2, 4, 5, 6) else ())):
        load_w_cast(P, Wq.v, win[:, :, OFF['a_q'] + g * 256: OFF['a_q'] + (g + 1) * 256])
        load_w_cast(P, Wk.v, win[:, :, OFF['a_k'] + g * 256: OFF['a_k'] + (g + 1) * 256])
        load_w_cast(P, Wv.v, win[:, :, OFF['a_v'] + g * 512: OFF['a_v'] + (g + 1) * 512])
        P.dma('act', bT[0].v, bias_d[g, 0]); P.dma('act', bT[1].v, bias_d[g, 1])
        NB = NT // (128 * d)
        for r in range(d):
            for m in range(-1, NB):
                cur = nb % 2; prv = 1 - cur; nb += 1
                s0 = HALO + m * 128 * d + r
                xt = xts[cur]
                P.dma('sp', xt.v, xext_d[s0: s0 + 127 * d + 1: d, :])
                x_transpose(P, C, xt.v, [(xTb[cur].v, 'act')], [nps(), nps()])
                xb = xTb[cur]
                for c in range(2):
                    pz = nps()
                    for kc in range(8):
                        P.mm(pz[:, 0:128], Wk[:, kc, c * 128:(c + 1) * 128], xb[:, kc, :], start=(kc == 0), stop=(kc == 7))
                    P.copy('act', kT[cur][:, c, :], pz[:, 0:128])
                pz = nps()
                for kc in range(8):
                    P.mm(pz.v, xb[:, kc, :], Wv[:, kc, :], start=(kc == 0), stop=(kc == 7))
                P.copy('act', Vt[cur].v, pz.v)
                if m < 0 or dbg == 4:
                    continue
                for c in range(2):
                    pz = nps()
                    for kc in range(8):
                        P.mm(pz[:, 0:128], Wq[:, kc, c * 128:(c + 1) * 128], xb[:, kc, :], start=(kc == 0), stop=(kc == 7))
                    P.copy('act', qT[cur][:, c, :], pz[:, 0:128])
                for pc, kb in ((0, prv), (1, cur)):
                    psAB = [nps(), nps()]
                    for h in range(4):
                        psl = slice((h % 2) * 64, (h % 2) * 64 + 64); hp = h // 2
                        P.mm(psAB[h % 2][:, hp * 128:(hp + 1) * 128], kT[kb][psl, hp, :], qT[cur][psl, hp, :])
                    tm = tmp[pc]
                    for h in range(4):
                        hs = slice(h * 128, (h + 1) * 128); hp = h // 2
                        P.stt('dve', tm[:, hs], psAB[h % 2][:, hp * 128:(hp + 1) * 128], 0.125, bT[pc][:, hs], ALU.mult, ALU.add)
                    P.act(PT[cur][pc].v, tm.v, AF.Exp)
                if dbg in (5, 6):
                    continue
                pn = nps()
                for h in range(4):
                    hs = slice(h * 128, (h + 1) * 128)
                    P.mm(pn[:, hs], Vt[prv][:, hs], PT[cur][0][:, hs], start=True, stop=False)
                    P.mm(pn[:, hs], Vt[cur][:, hs], PT[cur][1][:, hs], start=False, stop=True)
                pd = nps()
                P.mm(pd.v, (hvb.v if m == 0 else C.onesb.v), PT[cur][0].v, start=True, stop=False)
                P.mm(pd.v, C.onesb.v, PT[cur][1].v, start=False, stop=True)
                t0 = m * 128 * d + r
                for h in range(4 if dbg != 3 else 0):
                    hs = slice(h * 128, (h + 1) * 128)
                    av = accN[:, h, t0: t0 + 127 * d + 1: d]
                    P.tt('dve', av, av, pn[:, hs], ALU.add)
                    dv = accD[:, h, t0: t0 + 127 * d + 1: d]
                    P.tt('dve', dv, dv, pd[:, hs], ALU.add)
    yo = [P.sb(pfx + "yo%d" % i, [128, 4, 512], BF16) for i in range(2)]
    rc = [P.sb(pfx + "rc%d" % i, [128, 4, 512]) for i in range(2)]
    for j in range(NT // 512):
        sl = slice(j * 512, (j + 1) * 512)
        for h in range(4):
            a_, b_ = rc[j % 2][:, h, :].ap, accD[:, h, sl].ap
            P.I('dve', (lambda a_, b_: (lambda e: e.reciprocal(a_, b_)))(a_, b_), w=[rc[j % 2]], r=[accD])
            P.tt('dve', yo[j % 2][:, h, :], accN[:, h, sl], rc[j % 2][:, h, :], ALU.mult)
        P.dma('sp', yT_out[:, :, sl], yo[j % 2].v)


NCORES = 8
NT_CORE = 4096
PRM_NAMES = ("ret_gn", "ml_conv_w", "ml_conv_b", "bdq", "bdk", "bdv", "ml_bi", "ml_bf", "ml_gn", "ml_skip")
PRM_SHAPES = dict(ret_gn=[512], ml_conv_w=[4, 512], ml_conv_b=[512], bdq=[4, 128, 128], bdk=[4, 128, 128], bdv=[4, 128, 128],
                  ml_bi=[4], ml_bf=[4], ml_gn=[512], ml_skip=[512])
CST_SHAPES = dict(aR=[128, 4], bR=[128, 4], eR=[128, 2], gR=[128, 2], mask=[128, 128])


def _scan_inputs(nc):
    def inp(n, sh):
        return nc.dram_tensor(n, sh, F32, kind="ExternalInput").ap()
    prm = {n: inp(n, PRM_SHAPES[n]) for n in PRM_NAMES}
    cst = {n: inp(n, CST_SHAPES[n]) for n in CST_SHAPES}
    return prm, cst


def build_A(NT=NT_CORE):
    nc = bass.Bass("TRN2", target_bir_lowering=False)
    with ExitStack() as es:
        P = Prog(nc, es)
        x_d = P.dram("x", [NT, D], F32, kind="ExternalInput")
        xprev = P.dram("xprev", [128, D], F32, kind="ExternalInput")
        w_in = nc.dram_tensor("w_in", [D, D_IN], F32, kind="ExternalInput").ap()
        prm, cst = _scan_inputs(nc)
        outs = [P.dram("oS", [128, 2, 128], F32, kind="ExternalOutput"), P.dram("oC", [128, 4, 129], F32, kind="ExternalOutput"),
                P.dram("oT", [128, 4], F32, kind="ExternalOutput")]
        C = make_ctx(P)
        pass_scan(P, C, NT, x_d, xprev, w_in, prm, cst, None, outs, mode="summary", pfx="s")
        P.wait_all('sp', outs)
        P.emit()
    return nc


def build_B(NT=NT_CORE, NE=NEXP):
    nc = bass.Bass("TRN2", target_bir_lowering=False)
    with ExitStack() as es:
        P = Prog(nc, es)

        def inp(n, sh):
            return nc.dram_tensor(n, sh, F32, kind="ExternalInput").ap()
        xext = P.dram("xext", [HALO + NT, D], F32, kind="ExternalInput")
        w_in = inp("w_in", [D, D_IN])
        prm, cst = _scan_inputs(nc)
        init = dict(Fret=inp("Fret", [3, 128, 2, 128]), Fml=inp("Fml", [3, 128, 4, 129]), totL=inp("totL", [3, 128, 4]),
                    sel=inp("sel", [128, 3]), nsel=inp("nsel", [128, 3]))
        abias = inp("abias", [3, 2, 128, 512]); hv = inp("hv", [128, 128])
        w_br = inp("w_branch", [3, MIXW, D]); w_out = inp("w_out", [D, D])
        ln1_g = inp("ln1_g", [D]); ln1_b = inp("ln1_b", [D]); ln2_g = inp("ln2_g", [D]); ln2_b = inp("ln2_b", [D])
        w_r = inp("w_router", [D, NE]); b_r = inp("b_router", [NE])
        w_g = inp("w_gate", [NE, D, D]); b_g = inp("b_gate", [NE, D])
        w_u = inp("w_up", [NE, D, D]); b_u = inp("b_up", [NE, D])
        w_d = inp("w_down", [NE, D, D]); b_d = inp("b_down", [NE, D])
        out = P.dram("out", [NT, D], F32, kind="ExternalOutput")
        yT = [P.dram("yT%d" % b, [128, 4, NT], BF16) for b in range(3)]
        x1s = P.dram("x1s", [NT, D], F32)
        C = make_ctx(P)
        x_own = Tile(P, xext.h[HALO:HALO + NT, :], "xown", "dram")
        x_own.lw, x_own.rd = xext.lw, xext.rd
        xprev = Tile(P, xext.h[HALO - 128:HALO, :], "xprv", "dram")
        xprev.lw, xprev.rd = xext.lw, xext.rd
        with P.scope():
            pass_attn(P, C, NT, xext, w_in, abias, hv, yT[2], pfx="a")
        with P.scope():
            pass_scan(P, C, NT, x_own, xprev, w_in, prm, cst, init, [yT[0], yT[1]], mode="full", pfx="s")
        with P.scope():
            pass_merge(P, C, NT, x_own, yT, w_in, w_br, w_out, ln1_g, ln1_b, x1s, pfx="m")
        NBLK = NT * 4 // 128 + NE
        Xs = P.dram("Xs", [NBLK * 128, D], BF16); Ys = P.dram("Ys", [NBLK * 128, D], F32)
        mc = {k: inp("mc_" + k, list(v.shape)) for k, v in host_consts_moe(NT, NE).items()}
        with P.scope():
            pass_moe2(P, C, NT, x1s, w_r, b_r, w_g, b_g, w_u, b_u, w_d, b_d, ln2_g, ln2_b, out, Xs, Ys, mc, NE=NE, pfx="f")
        P.wait_all('sp', [out])
        P.emit()
    return nc


def layer_params(inputs, l):
    g = lambda n: np.ascontiguousarray(np.asarray(inputs[n], dtype=np.float32)[l])
    prm = dict(ret_gn=g("ret_gn"), ml_conv_w=g("ml_conv_w"), ml_conv_b=g("ml_conv_b"),
               bdq=host_blockdiag(g("ml_wq")), bdk=host_blockdiag(g("ml_wk")), bdv=host_blockdiag(g("ml_wv")),
               ml_bi=g("ml_bi"), ml_bf=g("ml_bf"), ml_gn=g("ml_gn"), ml_skip=g("ml_skip"))
    big = dict(w_in=g("w_in"), w_branch=g("w_branch"), w_out=g("w_out"), ln1_g=g("ln1_g"), ln1_b=g("ln1_b"),
               ln2_g=g("ln2_g"), ln2_b=g("ln2_b"), w_router=g("w_router"), b_router=g("b_router"),
               w_gate=g("w_gate"), b_gate=g("b_gate"), w_up=g("w_up"), b_up=g("b_up"), w_down=g("w_down"), b_down=g("b_down"))
    return prm, big


def run_layer(ncA, ncB, xs, prm, big, cst, abias, QPB, NT):
    n = len(xs)
    zeros128 = np.zeros((128, D), np.float32)
    inA = []
    for c in range(n):
        q = c % QPB
        m = dict(prm); m.update(cst)
        m["w_in"] = big["w_in"]; m["x"] = xs[c]
        m["xprev"] = np.ascontiguousarray(xs[c - 1][-128:]) if q > 0 else zeros128
        inA.append(m)
    resA = run_bass_kernel_spmd(ncA, inA, core_ids=list(range(n))).results
    inB = []
    for c in range(n):
        q = c % QPB; b0 = c - q
        m = dict(prm); m.update(cst); m.update(big)
        halo = xs[c - 1][-HALO:] if q > 0 else np.zeros((HALO, D), np.float32)
        m["xext"] = np.ascontiguousarray(np.concatenate([halo, xs[c]], axis=0))
        Fret = np.zeros((3, 128, 2, 128), np.float32); Fml = np.zeros((3, 128, 4, 129), np.float32)
        totL = np.zeros((3, 128, 4), np.float32); sel = np.zeros((128, 3), np.float32)
        for qq in range(min(3, QPB)):
            Fret[qq] = resA[b0 + qq]["oS"]; Fml[qq] = resA[b0 + qq]["oC"]; totL[qq] = resA[b0 + qq]["oT"]
            if qq < q:
                sel[:, qq] = 1.0
        m.update(Fret=Fret, Fml=Fml, totL=totL, sel=sel, nsel=(1.0 - sel).astype(np.float32))
        m["abias"] = abias
        for k_, v_ in host_consts_moe(NT, big["w_router"].shape[1]).items():
            m["mc_" + k_] = v_
        m["hv"] = (np.ones((128, 128), np.float32) if q > 0 else np.zeros((128, 128), np.float32))
        inB.append(m)
    resB = run_bass_kernel_spmd(ncB, inB, core_ids=list(range(n))).results
    return [np.asarray(r["out"], dtype=np.float32) for r in resB]


def kernel(**inputs):
    x = np.asarray(inputs["x"], dtype=np.float32)
    B, S, _ = x.shape
    QPB = NCORES // B
    NT = S // QPB
    xs = [np.ascontiguousarray(x[c // QPB, (c % QPB) * NT:(c % QPB + 1) * NT]) for c in range(NCORES)]
    ncA = build_A(NT); ncB = build_B(NT)
    cst = host_consts_scan(NT); abias = host_consts_attn()
    L = np.asarray(inputs["w_in"]).shape[0]
    for l in range(L):
        prm, big = layer_params(inputs, l)
        xs = run_layer(ncA, ncB, xs, prm, big, cst, abias, QPB, NT)
    out = np.zeros((B, S, D), np.float32)
    for c in range(NCORES):
        out[c // QPB, (c % QPB) * NT:(c % QPB + 1) * NT] = xs[c]
    return out


BIGIDX = 4000000.0


def host_consts_moe(NT, NE=NEXP):
    NBLK = NT * 4 // 128 + NE
    p = np.arange(128, dtype=np.float32)
    d = dict(iota_p=p.reshape(128, 1).copy(),
             ustrict=(p[:, None] < p[None, :]).astype(np.float32),
             iotaJ=np.tile(np.arange(33, dtype=np.float32)[None, :], (128, 1)),
             iotaB=np.tile(np.arange(NBLK, dtype=np.float32)[None, :], (128, 1)))
    return d


def _breg(P, e, bound):
    if not hasattr(P, "_bregs"):
        P._bregs = {}
    if bound not in P._bregs:
        P._bregs[bound] = e.to_reg(int(bound))
    return P._bregs[bound]


def ind_dma(P, dst_v, src_tile, src_ap, idx_v, bound, scatter=False, extra_r=()):
    sb_tile = dst_v.tile
    key = P._tsem(sb_tile)
    d_ap, i_ap = dst_v.ap, idx_v.ap
    if not scatter:
        rt = [src_tile, idx_v.tile] + list(extra_r); wt = [sb_tile]

        def f(e):
            try:
                return e.indirect_dma_start(d_ap, None, src_ap, bass.IndirectOffsetOnAxis(i_ap, 0), bounds_check=_breg(P, e, bound), oob_is_err=False)
            except Exception:
                print("IND GATHER FAIL", d_ap, src_ap, i_ap, bound)
                raise
    else:
        rt = [sb_tile, idx_v.tile] + list(extra_r); wt = [src_tile]

        def f(e):
            try:
                return e.indirect_dma_start(src_ap, bass.IndirectOffsetOnAxis(i_ap, 0), d_ap, None, bounds_check=_breg(P, e, bound), oob_is_err=False)
            except Exception:
                print("IND SCATTER FAIL", d_ap, src_ap, i_ap, bound)
                raise
    waits = P._waits('pool', rt, wt)
    sb_tile.dcnt += 16
    P.stream['pool'].append((waits, f, (key, 16)))
    P._record((key, sb_tile.dcnt), rt, wt)
    P.ninst += 1


def pass_moe2(P, C, NT, x1_d, w_router, b_router, w_gate, b_gate, w_up, b_up, w_down, b_down, ln_g, ln_b, out_d,
              Xs, Ys, mc, NE=NEXP, pfx="f", dbg=0):
    NTL = NT // 128
    NBLK = NT * 4 // 128 + NE
    NSLOT = NBLK * 128
    gB = P.sb(pfx + "gB", [128, 1024]); bB = P.sb(pfx + "bB", [128, 1024])
    bcast_rows(P, gB.v, ln_g); bcast_rows(P, bB.v, ln_b)
    wr = P.sb(pfx + "wr", [128, 8, NE], F32R); wr0 = P.sb(pfx + "wr0", [128, 8, NE])
    P.dma('act', wr0.v, w_router.rearrange("(kc p) e -> p kc e", p=128)); P.copy('dve', wr.v, wr0.v)
    brB = P.sb(pfx + "brB", [128, NE]); bcast_rows(P, brB.v, b_router)
    c1 = P.sb(pfx + "c1", [128, 1]); P.memset('dve', c1.v, 1.0)
    iop = P.sb(pfx + "iop", [128, 1]); P.dma('act', iop.v, mc["iota_p"])
    us0 = P.sb(pfx + "us0", [128, 128]); P.dma('act', us0.v, mc["ustrict"])
    usb = P.sb(pfx + "usb", [128, 128], BF16); P.copy('dve', usb.v, us0.v)
    ioJ = P.sb(pfx + "ioJ", [128, 33]); P.dma('act', ioJ.v, mc["iotaJ"])
    ioB = P.sb(pfx + "ioB", [128, NBLK]); P.dma('act', ioB.v, mc["iotaB"])
    lg_all = P.sb(pfx + "lg_all", [128, NTL, NE]); t8_all = P.sb(pfx + "t8_all", [128, NTL, 8])
    pall = P.sb(pfx + "pall", [128, NTL, NE]); pos_all = P.sb(pfx + "pos_all", [128, NTL, NE])
    slot_f = P.sb(pfx + "slot_f", [128, NTL, 4]); slot_i = P.sb(pfx + "slot_i", [128, NTL * 4], I32)
    pk_all = P.sb(pfx + "pk_all", [128, NTL, 4])
    carry = P.sb(pfx + "carry", [128, NE]); P.memset('dve', carry.v, 0.0)
    xts = [P.sb(pfx + "xt%d" % i, [128, 1024]) for i in range(2)]
    xT32 = P.sb(pfx + "xT32", [128, 8, 128], F32R)
    msk = P.sb(pfx + "msk", [128, NE]); mskb = P.sb(pfx + "mskb", [128, NE], BF16)
    ex = P.sb(pfx + "ex", [128, NE]); sm = P.sb(pfx + "sm", [128, 1]); nmx = P.sb(pfx + "nmx", [128, 1])
    nps = PSRot(C, [0, 1, 2, 3, 4, 5, 6, 7])
    for t in range(NTL):
        xt = xts[t % 2]
        P.dma('sp', xt.v, x1_d[t * 128:(t + 1) * 128, :])
        x_transpose(P, C, xt.v, [(xT32.v, 'dve')], [nps(), nps()])
        pl = nps()
        for kc in range(8):
            P.mm(pl[:, 0:NE], xT32[:, kc, :], wr[:, kc, :], start=(kc == 0), stop=(kc == 7))
        lg = lg_all[:, t, :]; t8 = t8_all[:, t, :]
        P.tt('dve', lg, pl[:, 0:NE], brB.v, ALU.add)
        a, b = t8.ap, lg.ap
        P.I('dve', (lambda a, b: (lambda e: e.max(out=a, in_=b)))(a, b), w=[t8_all], r=[lg_all])
        P.ts('dve', msk.v, lg, t8_all[:, t, 3:4], c1.v, op0=ALU.is_ge, op1=ALU.mult)
        P.ts('dve', nmx.v, t8_all[:, t, 0:1], -1.0, None, op0=ALU.mult)
        P.act(ex.v, lg, AF.Exp, bias=nmx.v)
        P.tt('dve', ex.v, ex.v, msk.v, ALU.mult)
        a2, b2 = sm.v.ap, ex.v.ap
        P.I('dve', (lambda a, b: (lambda e: e.reduce_sum(a, b, AX.X)))(a2, b2), w=[sm], r=[ex])
        a3 = sm.v.ap
        P.I('dve', (lambda a: (lambda e: e.reciprocal(a, a)))(a3), w=[sm], r=[sm])
        P.ts('dve', pall[:, t, :], ex.v, sm.v, c1.v, op0=ALU.mult, op1=ALU.mult)
        P.copy('dve', mskb.v, msk.v)
        pr = nps()
        P.mm(pr[:, 0:NE], usb.v, mskb.v)
        P.mm(pr[:, 64:64 + NE], C.onesb.v, mskb.v)
        P.tt('dve', pos_all[:, t, :], carry.v, pr[:, 0:NE], ALU.add)
        P.tt('dve', carry.v, carry.v, pr[:, 64:64 + NE], ALU.add)
    q = P.sb(pfx + "q", [128, NE]); nb = P.sb(pfx + "nb", [128, NE]); bend = P.sb(pfx + "bend", [128, NE])
    pstart = P.sb(pfx + "pstart", [128, NE]); t33 = P.sb(pfx + "t33", [128, 33])
    P.ts('dve', q.v, carry.v, 1.0 / 128.0, None, op0=ALU.mult)
    for e in range(NE):
        P.ts('dve', t33.v, ioJ.v, q[:, e:e + 1], c1.v, op0=ALU.is_lt, op1=ALU.mult)
        a_, b_ = nb[:, e:e + 1].ap, t33.v.ap
        P.I('dve', (lambda a, b: (lambda e_: e_.reduce_sum(a, b, AX.X)))(a_, b_), w=[nb], r=[t33])
    P.copy('dve', bend[:, 0:1], nb[:, 0:1])
    for e in range(1, NE):
        P.tt('dve', bend[:, e:e + 1], bend[:, e - 1:e], nb[:, e:e + 1], ALU.add)
    P.tt('dve', pstart.v, bend.v, nb.v, ALU.subtract)
    P.ts('dve', pstart.v, pstart.v, 128.0, None, op0=ALU.mult)
    Eall = P.sb(pfx + "Eall", [128, NBLK]); tB = P.sb(pfx + "tB", [128, NBLK]); sk = P.sb(pfx + "sk", [128, NBLK])
    P.memset('dve', Eall.v, 0.0)
    for e in range(NE):
        P.ts('dve', tB.v, ioB.v, bend[:, e:e + 1], c1.v, op0=ALU.is_ge, op1=ALU.mult)
        P.tt('dve', Eall.v, Eall.v, tB.v, ALU.add)
    P.ts('dve', Eall.v, Eall.v, float(NE - 1), None, op0=ALU.min)
    P.memset('dve', sk.v, 0.0)
    P.tt('dve', sk[:, 2:NBLK], Eall[:, 2:NBLK], Eall[:, 0:NBLK - 2], ALU.is_equal)
    P.ts('dve', sk.v, sk.v, BIGIDX, None, op0=ALU.mult)
    idxWf = P.sb(pfx + "idxWf", [128, NBLK]); idxW = P.sb(pfx + "idxW", [128, 8 * NBLK], I32)
    idxBf = P.sb(pfx + "idxBf", [128, NBLK]); idxB = P.sb(pfx + "idxB", [128, NBLK], I32)
    P.tt('dve', idxBf.v, Eall.v, sk.v, ALU.add)
    P.copy('dve', idxB.v, idxBf.v)
    iop4 = P.sb(pfx + "iop4", [128, 1]); P.ts('dve', iop4.v, iop.v, 4.0, None, op0=ALU.mult)
    P.ts('dve', idxWf.v, Eall.v, 512.0, None, op0=ALU.mult)
    P.tt('dve', idxWf.v, idxWf.v, sk.v, ALU.add)
    P.ts('dve', idxWf.v, idxWf.v, iop4.v, c1.v, op0=ALU.add, op1=ALU.mult)
    for kq in range(4):
        P.ts('dve', tB.v, idxWf.v, float(kq), None, op0=ALU.add)
        P.copy('dve', idxW[:, kq * NBLK:(kq + 1) * NBLK], tB.v)
    OH = P.sb(pfx + "OH", [128, NBLK]); P.ts('dve', OH.v, Eall.v, iop.v, c1.v, op0=ALU.is_equal, op1=ALU.mult)
    if dbg == 2:
        return
    xb16 = [P.sb(pfx + "xb16_%d" % i, [128, 1024], BF16) for i in range(2)]
    tA = P.sb(pfx + "tA", [128, NE]); oh = P.sb(pfx + "oh", [128, NE]); pr1 = P.sb(pfx + "pr1", [128, NE])
    Xs2 = Xs.h[:]
    for t in range(NTL):
        xt = xts[t % 2]; xb = xb16[t % 2]
        P.dma('sp', xt.v, x1_d[t * 128:(t + 1) * 128, :])
        P.copy('act', xb.v, xt.v)
        P.tt('dve', tA.v, pos_all[:, t, :], pstart.v, ALU.add)
        for k in range(4):
            P.ts('dve', oh.v, lg_all[:, t, :], t8_all[:, t, k:k + 1], c1.v, op0=ALU.is_equal, op1=ALU.mult)
            P.tt('dve', pr1.v, oh.v, tA.v, ALU.mult)
            a_, b_ = slot_f[:, t, k:k + 1].ap, pr1.v.ap
            P.I('dve', (lambda a, b: (lambda e_: e_.reduce_sum(a, b, AX.X)))(a_, b_), w=[slot_f], r=[pr1])
            P.tt('dve', pr1.v, oh.v, pall[:, t, :], ALU.mult)
            a_, b_ = pk_all[:, t, k:k + 1].ap, pr1.v.ap
            P.I('dve', (lambda a, b: (lambda e_: e_.reduce_sum(a, b, AX.X)))(a_, b_), w=[pk_all], r=[pr1])
        P.copy('dve', slot_i[:, t * 4:(t + 1) * 4], slot_f[:, t, :])
        for k in range(4):
            ind_dma(P, xb.v, Xs, Xs2, slot_i[:, t * 4 + k: t * 4 + k + 1], NSLOT - 1, scatter=True)
    if dbg == 3:
        P.wait_all('sp', [Xs])
        return
    with P.scope():
        inv128 = P.sb(pfx + 'inv128', [128, 128], BF16); P.memset('dve', inv128.v, 1.0 / 128.0)
        Wq = [[[P.sb(pfx + "W%d_%d_%d" % (j, i, kq), [128, 2, 1024], BF16) for kq in range(4)] for j in range(3)] for i in range(2)]
        Wt = [[[Wq[i][j][kc // 2][:, kc % 2, :] for kc in range(8)] for j in range(3)] for i in range(2)]
        Ball = [P.sb(pfx + "Ball%d" % j, [NE, 1024], BF16) for j in range(3)]
        for j, bsrc_ in enumerate((b_gate, b_up, b_down)):
            P.dma('pool', Ball[j].v, bsrc_)
        OHb = [P.sb(pfx + "OHb%d" % i, [NE, 128], BF16) for i in range(2)]
        wsrc = [w_.rearrange("e (p kq r) f -> (e p kq) (r f)", p=128, kq=4, r=2) for w_ in (w_gate, w_up, w_down)]
        bsrc = [b_gate, b_up, b_down]
        wtile = [Tile(P, None, pfx + "wsrc%d" % j, "dram") for j in range(3)]
        xblk = [P.sb(pfx + "xblk%d" % i, [128, 1024], BF16) for i in range(2)]
        xbT = [P.sb(pfx + "xbT%d" % i, [128, 8, 128], BF16) for i in range(2)]
        actT = [P.sb(pfx + "actT%d" % i, [128, 8, 128], BF16) for i in range(2)]
        gt = [P.sb(pfx + "g%d" % i, [128, 512]) for i in range(2)]
        s_t = [P.sb(pfx + "s%d" % i, [128, 512]) for i in range(2)]
        ut = [P.sb(pfx + "u%d" % i, [128, 512]) for i in range(2)]
        atok = [P.sb(pfx + "atok%d" % i, [128, 1024], BF16) for i in range(2)]
        yb = [P.sb(pfx + "yb%d" % i, [128, 1024]) for i in range(2)]
        kk = 0
        for b in range(NBLK if dbg != 11 else 10):
            cur = b % 2
            for j in range(3):
                for kq in range(4):
                    ind_dma(P, Wq[cur][j][kq].v.re("p r f -> p (r f)"), wtile[j], wsrc[j],
                            idxW[:, kq * NBLK + b: kq * NBLK + b + 1], NE * 512 - 1)
            Wg, Wu, Wd = Wt[cur]
            P.copy('dve', OHb[cur].v, OH[0:NE, b:b + 1].bc([NE, 128]))
            xk = xblk[cur]
            if dbg != 9 or b < 2:
                P.dma('sp', xk.v, Xs[b * 128:(b + 1) * 128, :])
            xT_ = xbT[cur]
            for half in range(2):
                pz = nps(); pzb = pz.v.bitcast(BF16)
                for c in range(4):
                    kc = half * 4 + c
                    P.tr(pzb[:, c * 128:(c + 1) * 128], xk[:, kc:1024:8], C.identb.v)
                P.copy('act', xT_[:, half * 4:(half + 1) * 4, :], pzb[:, 0:512].re("p (c t) -> p c t", c=4))
            aT = actT[cur]
            at = atok[cur]
            for hf in range(2 if dbg not in (6, 10) else 0):
                hs = slice(hf * 512, (hf + 1) * 512)
                pg = nps()
                for kc in range(8):
                    P.mm(pg.v, xT_[:, kc, :], Wg[kc][:, hs], start=(kc == 0), stop=False)
                P.mm(pg.v, OHb[cur].v, Ball[0][:, hs], start=False, stop=True)
                pu = nps()
                for kc in range(8):
                    P.mm(pu.v, xT_[:, kc, :], Wu[kc][:, hs], start=(kc == 0), stop=False)
                P.mm(pu.v, OHb[cur].v, Ball[1][:, hs], start=False, stop=True)
                g = gt[kk % 2]; s_ = s_t[kk % 2]; u = ut[kk % 2]; kk += 1
                P.ts('dve', g.v, pg.v, 7.0, None, op0=ALU.min)
                P.act(s_.v, g.v, AF.Sigmoid, scale=1.702)
                P.ts('dve', u.v, pu.v, 7.0, -7.0, op0=ALU.min, op1=ALU.max)
                P.tt('dve', g.v, g.v, s_.v, ALU.mult)
                P.stt('dve', at[:, hs], u.v, 1.0, g.v, ALU.add, ALU.mult)
            for half in range(2 if dbg not in (6, 10) else 0):
                pz = nps(); pzb = pz.v.bitcast(BF16)
                for c in range(4):
                    fc = half * 4 + c
                    P.tr(pzb[:, c * 128:(c + 1) * 128], at[:, fc:1024:8], C.identb.v)
                P.copy('act', aT[:, half * 4:(half + 1) * 4, :], pzb[:, 0:512].re("p (c t) -> p c t", c=4))
            y = yb[cur]
            if dbg in (6, 10):
                P.memset('dve', y.v, 0.5)
            for hf in range(2 if dbg not in (6, 10) else 0):
                py = nps()
                hs = slice(hf * 512, (hf + 1) * 512)
                for fc in range(8):
                    P.mm(py.v, aT[:, fc, :], Wd[fc][:, hs], start=(fc == 0), stop=False)
                P.mm(py.v, OHb[cur].v, Ball[2][:, hs], start=False, stop=True)
                P.copy('act', y[:, hs], py.v)
            if dbg != 7:
                P.dma('sp', Ys[b * 128:(b + 1) * 128, :], y.v)
    if dbg == 4:
        return
    rk = [P.sb(pfx + "rk%d" % i, [128, 1024]) for i in range(4)]
    acA = P.sb(pfx + "acA", [128, 1024]); acB = P.sb(pfx + "acB", [128, 1024])
    st = P.sb(pfx + "st", [128, 2, 6]); mv = P.sb(pfx + "mv", [128, 2]); rstd = P.sb(pfx + "rstd", [128, 1])
    Ys2 = Ys.h[:]
    for t in range(NTL):
        xt = xts[t % 2]
        P.dma('sp', xt.v, x1_d[t * 128:(t + 1) * 128, :])
        for k in range(4):
            ind_dma(P, rk[k].v, Ys, Ys2, slot_i[:, t * 4 + k: t * 4 + k + 1], NSLOT - 1)
        P.ts('dve', acA.v, rk[0].v, pk_all[:, t, 0:1], c1.v, op0=ALU.mult, op1=ALU.mult)
        P.stt('dve', acB.v, rk[1].v, pk_all[:, t, 1:2], acA.v, ALU.mult, ALU.add)
        P.stt('dve', acA.v, rk[2].v, pk_all[:, t, 2:3], acB.v, ALU.mult, ALU.add)
        P.stt('dve', acB.v, rk[3].v, pk_all[:, t, 3:4], acA.v, ALU.mult, ALU.add)
        P.stt('dve', acA.v, xt.v, DN_ALPHA, acB.v, ALU.mult, ALU.add)
        layer_norm(P, acA.v, gB.v, bB.v, st.v, mv.v, rstd.v)
        P.dma('sp', out_d[t * 128:(t + 1) * 128, :], acA.v)
```

```python
import numpy as np
import concourse.bass as bass
import concourse.mybir as mybir
from concourse.bass_utils import run_bass_kernel_spmd
from contextlib import ExitStack

F32 = mybir.dt.float32
F32R = mybir.dt.float32r
BF16 = mybir.dt.bfloat16
I32 = mybir.dt.int32
U32 = mybir.dt.uint32
AF = mybir.ActivationFunctionType
ALU = mybir.AluOpType
AX = mybir.AxisListType

ENGS = ['pe', 'act', 'dve', 'pool', 'sp']


class Tile:
    def __init__(self, P, h, name, space='sb'):
        self.P = P
        self.h = h
        self.name = name
        self.space = space
        self.lw = {}
        self.rd = {}
        self.dsem = None
        self.dcnt = 0

    def __getitem__(self, k):
        return V(self, self.h[k])

    @property
    def v(self):
        return V(self, self.h[:])


class V:
    def __init__(self, tile, ap):
        self.tile = tile
        self.ap = ap

    def __getitem__(self, k):
        return V(self.tile, self.ap[k])

    def bitcast(self, dt):
        return V(self.tile, self.ap.bitcast(dt))

    def bc(self, shape):
        return V(self.tile, self.ap.to_broadcast(shape))

    def re(self, s, **kw):
        return V(self.tile, self.ap.rearrange(s, **kw))


def _ap(x):
    if isinstance(x, Tile):
        return x.h[:]
    return x.ap if isinstance(x, V) else x


class Prog:
    def __init__(self, nc, es, same_engine_sync=None):
        self.nc = nc
        self.es = es
        self.es_top = es
        self.all_tiles = []
        self.stream = {e: [] for e in ENGS}
        self.sems = {}
        self.cnt = {e: 0 for e in ENGS}
        self.known = {e: {} for e in ENGS}
        import os as _os
        self.same = (_os.environ.get('KSAME', '1') == '1') if same_engine_sync is None else same_engine_sync
        self.nsem = 0
        for e in ['pe', 'act', 'dve', 'pool']:
            self.sems[e] = es.enter_context(nc.semaphore("s_" + e))
            self.nsem += 1
        self.ninst = 0
        self.nwait = 0

    def sb(self, name, shape, dt=F32):
        h = self.es.enter_context(self.nc.sbuf_tensor(name, list(shape), dt))
        return Tile(self, h, name)

    def ps(self, name, shape, dt=F32):
        h = self.es.enter_context(self.nc.psum_tensor(name, list(shape), dt))
        return Tile(self, h, name, 'ps')

    def dram(self, name, shape, dt=F32, kind="Internal"):
        h = self.nc.dram_tensor(name, list(shape), dt, kind=kind)
        return Tile(self, h.ap(), name, 'dram')

    def _tsem(self, t):
        if t.dsem is None:
            key = "d_" + t.name
            self.sems[key] = self.es_top.enter_context(self.nc.semaphore(key))
            self.nsem += 1
            t.dsem = key
            self.all_tiles.append(t)
        return t.dsem

    def _waits(self, eng, rt, wt):
        need = {}
        for t in rt:
            for s, v in t.lw.items():
                need[s] = max(need.get(s, 0), v)
        for t in wt:
            for s, v in t.lw.items():
                need[s] = max(need.get(s, 0), v)
            for s, v in t.rd.items():
                need[s] = max(need.get(s, 0), v)
        out = []
        kn = self.known[eng]
        for s, v in need.items():
            if s == eng and (eng == 'pe' or not self.same):
                continue
            if kn.get(s, 0) < v:
                kn[s] = v
                out.append((s, v))
        return out

    def _record(self, ev, rt, wt):
        s, v = ev
        for t in wt:
            t.lw[s] = max(t.lw.get(s, 0), v)
            t.rd = {}
        for t in rt:
            if t in wt:
                continue
            t.rd[s] = max(t.rd.get(s, 0), v)

    @staticmethod
    def _tiles(xs):
        out = []
        for x in xs:
            if x is None:
                continue
            t = x.tile if isinstance(x, V) else x
            if isinstance(t, Tile) and t not in out:
                out.append(t)
        return out

    def I(self, eng, fn, w=(), r=()):
        wt = self._tiles(w)
        rt = self._tiles(r)
        for t in rt:
            if t.space == 'ps' and t not in wt and eng != 'pe':
                wt.append(t)
        waits = self._waits(eng, rt, wt)
        self.cnt[eng] += 1
        ev = (eng, self.cnt[eng])
        self.stream[eng].append((waits, fn, (eng, 1)))
        self._record(ev, rt, wt)
        self.ninst += 1
        self.nwait += len(waits)

    def dma(self, q, out, in_, **kw):
        wt = self._tiles([out])
        rt = self._tiles([in_])
        owner = None
        for x in (out, in_):
            t_ = x.tile if isinstance(x, V) else (x if isinstance(x, Tile) else None)
            if t_ is not None and t_.space == 'sb':
                owner = t_
        if owner is None:
            for x in (out, in_):
                t_ = x.tile if isinstance(x, V) else (x if isinstance(x, Tile) else None)
                if t_ is not None and owner is None:
                    owner = t_
        key = self._tsem(owner)
        waits = self._waits(q, rt, wt)
        owner.dcnt += 16
        ev = (key, owner.dcnt)
        o, i = _ap(out), _ap(in_)
        self.stream[q].append((waits, lambda e: e.dma_start(out=o, in_=i, **kw), (key, 16)))
        self._record(ev, rt, wt)
        self.ninst += 1
        self.nwait += len(waits)
        return ev

    def wait_all(self, eng, tiles):
        ts = self._tiles(tiles)
        waits = self._waits(eng, ts, ts)
        self.stream[eng].append((waits, None, None))

    def mm(self, out, lhsT, rhs, start=True, stop=True, **kw):
        o, a, b = _ap(out), _ap(lhsT), _ap(rhs)
        self.I('pe', lambda e: e.matmul(o, a, b, start=start, stop=stop, **kw), w=[out], r=[lhsT, rhs])

    def tr(self, out, in_, ident):
        o, a, b = _ap(out), _ap(in_), _ap(ident)
        self.I('pe', lambda e: e.transpose(o, a, b), w=[out], r=[in_, ident])

    def act(self, out, in_, func, bias=None, scale=1.0, accum_out=None, eng='act'):
        o, a = _ap(out), _ap(in_)
        kw = {}
        if bias is not None:
            kw['bias'] = _ap(bias)
        if accum_out is not None:
            kw['accum_out'] = _ap(accum_out)
        sc = _ap(scale)
        self.I(eng, lambda e: e.activation(o, a, func, scale=sc, **kw),
               w=[out, accum_out], r=[in_, bias, scale if isinstance(scale, V) else None])

    def tt(self, eng, out, in0, in1, op):
        o, a, b = _ap(out), _ap(in0), _ap(in1)
        self.I(eng, lambda e: e.tensor_tensor(o, a, b, op), w=[out], r=[in0, in1])

    def ts(self, eng, out, in0, s1, s2=None, op0=ALU.mult, op1=None, accum_out=None):
        o, a = _ap(out), _ap(in0)
        x1, x2 = _ap(s1), _ap(s2)
        kw = {}
        if op1 is not None:
            kw['op1'] = op1
        if accum_out is not None:
            kw['accum_out'] = _ap(accum_out)
        self.I(eng, lambda e: e.tensor_scalar(o, a, x1, x2, op0, **kw), w=[out, accum_out],
               r=[in0, s1 if isinstance(s1, V) else None, s2 if isinstance(s2, V) else None])

    def stt(self, eng, out, in0, scalar, in1, op0, op1):
        o, a, b = _ap(out), _ap(in0), _ap(in1)
        s = _ap(scalar)
        self.I(eng, lambda e: e.scalar_tensor_tensor(o, a, s, b, op0, op1), w=[out],
               r=[in0, in1, scalar if isinstance(scalar, V) else None])

    def copy(self, eng, out, in_):
        o, a = _ap(out), _ap(in_)
        if eng == 'act':
            self.I(eng, lambda e: e.copy(o, a), w=[out], r=[in_])
        else:
            self.I(eng, lambda e: e.tensor_copy(o, a), w=[out], r=[in_])

    def memset(self, eng, out, val):
        o = _ap(out)
        self.I(eng, lambda e: e.memset(o, val), w=[out])

    def barrier(self):
        evs = {e: self.cnt[e] for e in ['pe', 'act', 'dve', 'pool'] if self.cnt[e] > 0}
        for t in self.all_tiles:
            if t.dcnt > 0:
                evs[t.dsem] = t.dcnt
        for eng in ENGS:
            kn = self.known[eng]
            waits = []
            for s_, v in evs.items():
                if kn.get(s_, 0) < v:
                    kn[s_] = v
                    waits.append((s_, v))
            if waits:
                self.stream[eng].append((waits, None, None))

    def scope(self):
        P = self

        class _S:
            def __enter__(self_):
                self_.old = P.es
                self_.st = ExitStack()
                self_.st.__enter__()
                P.es = self_.st
                return self_

            def __exit__(self_, *a):
                P.barrier()
                P.emit()
                P.es = self_.old
                self_.st.__exit__(None, None, None)
                return False
        return _S()

    def emit(self):
        nc = self.nc
        sems = self.sems
        with nc.Block() as block:
            def run(engobj, name):
                for waits, fn, inc in self.stream[name]:
                    for s, v in waits:
                        engobj.wait_ge(sems[s], v)
                    if fn is not None:
                        ins = fn(engobj)
                        ins.then_inc(sems[inc[0]], inc[1])

            @block.tensor
            def _(e):
                run(e, 'pe')

            @block.scalar
            def _(e):
                run(e, 'act')

            @block.vector
            def _(e):
                run(e, 'dve')

            @block.gpsimd
            def _(e):
                run(e, 'pool')

            @block.sync
            def _(e):
                run(e, 'sp')
        self.stream = {e: [] for e in ENGS}


D = 1024
MIXW = 512
NEXP = 32
DN_ALPHA = (2.0 * 2) ** 0.25
EPS = 1e-5
OFF = dict(r_q=0, r_k=256, r_v=512, r_g=1024, m_x=1536, m_i=2048, m_f=2052, m_o=2056,
           a_q=2568, a_k=3336, a_v=4104, gates=5640)
D_IN = 8712


class Ctx:
    pass


def make_ctx(P):
    C = Ctx()
    C.identf = P.sb("identf", [128, 128], F32)
    C.identb = P.sb("identb", [128, 128], BF16)
    C.onesb = P.sb("onesb", [128, 128], BF16)
    C.onesf = P.sb("onesf", [128, 128], F32)
    P.memset('pool', C.identf.v, 1.0)
    o = C.identf.v.ap
    P.I('pool', lambda e: e.affine_select(o, o, [[-1, 128]], ALU.is_equal, 0.0, base=0, channel_multiplier=1),
        w=[C.identf], r=[C.identf])
    P.copy('pool', C.identb.v, C.identf.v)
    P.memset('pool', C.onesb.v, 1.0)
    P.memset('pool', C.onesf.v, 1.0)
    C.ps = [P.ps("psb%d" % i, [128, 512], F32) for i in range(8)]
    return C


def load_w_cast(P, dst, src, q='pool'):
    cols = src.shape[-1]
    c0 = 0
    while c0 < cols:
        c1 = min(cols, c0 + 1024)
        P.dma(q, dst[:, :, c0:c1], src[:, :, c0:c1])
        c0 = c1


def bcast_rows(P, dst, src1d, q='act'):
    P.dma(q, dst, src1d.partition_broadcast(128))


def x_transpose(P, C, xt, outs, psl):
    for half in range(2):
        pt = psl[half]
        for c in range(4):
            k = half * 4 + c
            P.tr(pt[:, c * 128:(c + 1) * 128], xt[:, k * 128:(k + 1) * 128], C.identf.v)
        for (o, eng) in outs:
            P.copy(eng, o[:, half * 4:(half + 1) * 4, :], pt.v.re("p (c t) -> p c t", c=4))


def layer_norm(P, r, g_b, b_b, st, mv, rstd, eng2='pool'):
    for hf in range(2):
        a, b = st[:, hf, :].ap, r[:, hf * 512:(hf + 1) * 512].ap
        P.I('dve', (lambda a, b: (lambda e: e.bn_stats(a, b)))(a, b), w=[st], r=[r])
    a, b = mv.ap, st.ap
    P.I('dve', lambda e: e.bn_aggr(a, b), w=[mv], r=[st])
    P.ts('dve', rstd, mv[:, 1:2], EPS, None, op0=ALU.add)
    P.act(rstd, rstd, AF.Ln)
    P.act(rstd, rstd, AF.Exp, scale=-0.5)
    P.ts('dve', r, r, mv[:, 0:1], rstd, op0=ALU.subtract, op1=ALU.mult)
    P.tt(eng2, r, r, g_b, ALU.mult)
    P.tt(eng2, r, r, b_b, ALU.add)


def pass_merge(P, C, NT, x_d, yT_d, w_in_l, w_branch_l, w_out_l, ln_g, ln_b, x1_d, pfx="m"):
    wg = P.sb(pfx + "wg", [128, 8, 3072], BF16)
    wb = P.sb(pfx + "wb", [128, 12, 1024], BF16)
    wo = P.sb(pfx + "wo", [128, 8, 1024], BF16)
    load_w_cast(P, wg.v, w_in_l[:, OFF['gates']:D_IN].rearrange("(kc p) c -> p kc c", p=128))
    load_w_cast(P, wb.v, w_branch_l.rearrange("b (kc p) c -> p (b kc) c", p=128))
    load_w_cast(P, wo.v, w_out_l.rearrange("(kc p) c -> p kc c", p=128))
    gB = P.sb(pfx + "gB", [128, 1024]); bB = P.sb(pfx + "bB", [128, 1024])
    bcast_rows(P, gB.v, ln_g); bcast_rows(P, bB.v, ln_b)
    xts = [P.sb(pfx + "xt%d" % i, [128, 1024]) for i in range(2)]
    xTs = [P.sb(pfx + "xT%d" % i, [128, 8, 128], BF16) for i in range(2)]
    yTs = [[P.sb(pfx + "yT%d_%d" % (b, i), [128, 4, 128], BF16) for i in range(2)] for b in range(3)]
    mg = [P.sb(pfx + "mg%d" % i, [128, 1024]) for i in range(2)]
    mT = [P.sb(pfx + "mT%d" % i, [128, 8, 128], BF16) for i in range(2)]
    sg = [P.sb(pfx + "sg%d" % i, [128, 512]) for i in range(2)]
    tmp = [P.sb(pfx + "tmp%d" % i, [128, 512]) for i in range(2)]
    rr = [P.sb(pfx + "rr%d" % i, [128, 1024]) for i in range(2)]
    st = P.sb(pfx + "st", [128, 2, 6]); mv = P.sb(pfx + "mv", [128, 2]); rstd = P.sb(pfx + "rstd", [128, 1])
    k = 0
    for t in range(NT // 128):
        xt = xts[t % 2]; xT = xTs[t % 2]
        P.dma('sp', xt.v, x_d[t * 128:(t + 1) * 128, :])
        x_transpose(P, C, xt.v, [(xT.v, 'act')], [C.ps[0], C.ps[1]])
        for b in range(3):
            P.dma('act', yTs[b][t % 2].v, yT_d[b][:, :, t * 128:(t + 1) * 128])
        m = mg[t % 2]
        for b in range(3):
            for hf in range(2):
                pg = C.ps[2 + (k % 2)]; pb = C.ps[4 + (k % 2)]; s = sg[k % 2]; tm = tmp[k % 2]
                k += 1
                for kc in range(8):
                    P.mm(pg.v, xT[:, kc, :], wg[:, kc, b * 1024 + hf * 512: b * 1024 + (hf + 1) * 512],
                         start=(kc == 0), stop=(kc == 7))
                for kc in range(4):
                    P.mm(pb.v, yTs[b][t % 2][:, kc, :], wb[:, b * 4 + kc, hf * 512:(hf + 1) * 512],
                         start=(kc == 0), stop=(kc == 3))
                P.act(s.v, pg.v, AF.Sigmoid)
                msl = m[:, hf * 512:(hf + 1) * 512]
                if b == 0:
                    P.tt('dve', msl, s.v, pb.v, ALU.mult)
                else:
                    P.tt('dve', tm.v, s.v, pb.v, ALU.mult)
                    P.tt('pool', msl, msl, tm.v, ALU.add)
        x_transpose(P, C, m.v, [(mT[t % 2].v, 'act')], [C.ps[6], C.ps[7]])
        r = rr[t % 2]
        for hf in range(2):
            po = C.ps[2 + (k % 2)]
            k += 1
            for kc in range(8):
                P.mm(po.v, mT[t % 2][:, kc, :], wo[:, kc, hf * 512:(hf + 1) * 512], start=(kc == 0), stop=(kc == 7))
            P.stt('dve', r[:, hf * 512:(hf + 1) * 512], xt[:, hf * 512:(hf + 1) * 512], DN_ALPHA, po.v,
                  ALU.mult, ALU.add)
        layer_norm(P, r.v, gB.v, bB.v, st.v, mv.v, rstd.v)
        P.dma('sp', x1_d[t * 128:(t + 1) * 128, :], r.v)


def pass_moe(P, C, NT, x1_d, w_router, b_router, w_gate, b_gate, w_up, b_up, w_down, b_down, ln_g, ln_b, out_d,
             NE=NEXP, pfx="e", TGT=4, dbg=0):
    TG = TGT * 128
    gB = P.sb(pfx + "gB", [128, 1024]); bB = P.sb(pfx + "bB", [128, 1024])
    bcast_rows(P, gB.v, ln_g); bcast_rows(P, bB.v, ln_b)
    wr = P.sb(pfx + "wr", [128, 8, NE], F32R)
    wr0 = P.sb(pfx + "wr0", [128, 8, NE])
    P.dma('act', wr0.v, w_router.rearrange("(kc p) e -> p kc e", p=128))
    P.copy('dve', wr.v, wr0.v)
    brB = P.sb(pfx + "brB", [128, NE]); bcast_rows(P, brB.v, b_router)
    bgT = P.sb(pfx + "bgT", [128, NE, 8]); buT = P.sb(pfx + "buT", [128, NE, 8])
    bstage = P.sb(pfx + "bstage", [128, 128])
    if dbg in (3, 7):
        P.memset('dve', bgT.v, 0.0); P.memset('dve', buT.v, 0.0)
    for (dstT, src) in (((bgT, b_gate), (buT, b_up)) if dbg not in (3, 7) else ()):
        rows = NE * 8
        srcv = src.rearrange("e (fc p) -> (e fc) p", p=128)
        dv = dstT.v.re("p e fc -> p (e fc)")
        r0 = 0
        while r0 < rows:
            r1 = min(rows, r0 + 128)
            n = r1 - r0
            P.dma('act', bstage[0:n, :], srcv[r0:r1, :])
            pz = C.ps[7]
            P.tr(pz[:, 0:n], bstage[0:n, :], C.identf[0:n, 0:n])
            P.copy('dve', dv[:, r0:r1], pz[:, 0:n])
            r0 = r1
    bd = P.sb(pfx + "bd", [NE, 1024], F32R)
    bd0 = P.sb(pfx + "bd0", [NE, 1024])
    P.dma('act', bd0.v, b_down)
    P.copy('dve', bd.v, bd0.v)
    W = [[P.sb(pfx + "W%d_%d" % (j, i), [128, 8, 1024], BF16) for j in range(3)] for i in range(2)]
    xts = [P.sb(pfx + "xt%d" % i, [128, 1024]) for i in range(TGT)]
    xTg = P.sb(pfx + "xTg", [128, 8, TG], BF16)
    xT32 = P.sb(pfx + "xT32", [128, 8, 128], F32R)
    acc = P.sb(pfx + "acc", [128, TGT, 1024])
    pall = P.sb(pfx + "pall", [128, TGT, NE])
    actT = P.sb(pfx + "actT", [128, 8, TG], BF16)
    lg = P.sb(pfx + "lg", [128, NE]); t8 = P.sb(pfx + "t8", [128, 8]); msk = P.sb(pfx + "msk", [128, NE])
    ex = P.sb(pfx + "ex", [128, NE]); sm = P.sb(pfx + "sm", [128, 1]); nmx = P.sb(pfx + "nmx", [128, 1])
    pT = P.sb(pfx + "pT", [NE, 128], F32R)
    gt = [P.sb(pfx + "g%d" % i, [128, TG]) for i in range(2)]
    st_ = [P.sb(pfx + "s%d" % i, [128, TG]) for i in range(2)]
    ut = [P.sb(pfx + "u%d" % i, [128, TG]) for i in range(2)]
    st = P.sb(pfx + "st", [128, 2, 6]); mv = P.sb(pfx + "mv", [128, 2]); rstd = P.sb(pfx + "rstd", [128, 1])
    c1 = P.sb(pfx + "c1", [128, 1]); c7 = P.sb(pfx + "c7", [128, 1])
    P.memset('dve', c1.v, 1.0); P.memset('dve', c7.v, 7.0)
    rr = [P.sb(pfx + "rr%d" % i, [128, 1024]) for i in range(2)]
    tmpq = [P.sb(pfx + "tq%d" % i, [128, 512]) for i in range(2)]
    assert TG == 512
    if dbg == 5:
        P.wait_all('sp', [gB, bB, wr, brB, bd, bgT, buT])
        return
    wcnt = 0
    kk = 0
    for gi in range(NT // TG):
        for tt in range(TGT):
            t = gi * TGT + tt
            xt = xts[tt]
            P.dma('sp', xt.v, x1_d[t * 128:(t + 1) * 128, :])
            x_transpose(P, C, xt.v, [(xTg[:, :, tt * 128:(tt + 1) * 128], 'act')] + ([(xT32.v, 'dve')] if dbg != 3 else []), [C.ps[0], C.ps[1]])
            if dbg in (2, 3, 7, 8):
                P.memset('dve', acc[:, tt, :], 0.0)
                P.memset('dve', pall[:, tt, :], 0.25)
            else:
                pl = C.ps[6]
                for kc in range(8):
                    P.mm(pl[:, 0:NE], xT32[:, kc, :], wr[:, kc, :], start=(kc == 0), stop=(kc == 7))
                P.tt('dve', lg.v, pl[:, 0:NE], brB.v, ALU.add)
                a, b = t8.v.ap, lg.v.ap
                P.I('dve', (lambda a, b: (lambda e: e.max(out=a, in_=b)))(a, b), w=[t8], r=[lg])
                P.ts('dve', msk.v, lg.v, t8[:, 3:4], c1.v, op0=ALU.is_ge, op1=ALU.mult)
                P.ts('dve', nmx.v, t8[:, 0:1], -1.0, None, op0=ALU.mult)
                P.act(ex.v, lg.v, AF.Exp, bias=nmx.v)
                P.tt('dve', ex.v, ex.v, msk.v, ALU.mult)
                a2, b2 = sm.v.ap, ex.v.ap
                P.I('dve', (lambda a, b: (lambda e: e.reduce_sum(a, b, AX.X)))(a2, b2), w=[sm], r=[ex])
                a3 = sm.v.ap
                P.I('dve', (lambda a: (lambda e: e.reciprocal(a, a)))(a3), w=[sm], r=[sm])
                P.ts('dve', pall[:, tt, :], ex.v, sm.v, c1.v, op0=ALU.mult, op1=ALU.mult)
                P.tr(pl[0:NE, 128:256], pall[:, tt, :], C.identf.v)
                P.copy('dve', pT.v, pl[0:NE, 128:256])
                for hf in range(2):
                    pb = C.ps[7]
                    P.mm(pb.v, pT.v, bd[:, hf * 512:(hf + 1) * 512])
                    P.copy('dve', acc[:, tt, hf * 512:(hf + 1) * 512], pb.v)
        for e in range(NE if dbg not in (1, 3, 7, 8) else 0):
            Wg, Wu, Wd = W[wcnt % 2]
            wcnt += 1
            P.dma('pool', Wg.v, w_gate[e].rearrange("(kc p) f -> p kc f", p=128))
            P.dma('pool', Wu.v, w_up[e].rearrange("(kc p) f -> p kc f", p=128))
            P.dma('pool', Wd.v, w_down[e].rearrange("(kc p) f -> p kc f", p=128))
            for fc in range(8):
                pg = C.ps[(kk % 2) * 2]; pu = C.ps[(kk % 2) * 2 + 1]
                g = gt[kk % 2]; s = st_[kk % 2]; u = ut[kk % 2]
                kk += 1
                for kc in range(8):
                    P.mm(pg.v, Wg[:, kc, fc * 128:(fc + 1) * 128], xTg[:, kc, :], start=(kc == 0), stop=(kc == 7))
                for kc in range(8):
                    P.mm(pu.v, Wu[:, kc, fc * 128:(fc + 1) * 128], xTg[:, kc, :], start=(kc == 0), stop=(kc == 7))
                P.ts('dve', g.v, pg.v, bgT[:, e, fc:fc + 1], c7.v, op0=ALU.add, op1=ALU.min)
                P.act(s.v, g.v, AF.Sigmoid, scale=1.702)
                P.ts('dve', u.v, pu.v, buT[:, e, fc:fc + 1], c7.v, op0=ALU.add, op1=ALU.min)
                P.ts('dve', u.v, u.v, -7.0, 1.0, op0=ALU.max, op1=ALU.add)
                P.tt('dve', g.v, g.v, s.v, ALU.mult)
                P.tt('dve', actT[:, fc, :], g.v, u.v, ALU.mult)
            for tt in range(TGT):
                for hf in range(2):
                    py = C.ps[4 + (kk % 2)]
                    kk += 1
                    for fc in range(8):
                        P.mm(py.v, actT[:, fc, tt * 128:(tt + 1) * 128], Wd[:, fc, hf * 512:(hf + 1) * 512],
                             start=(fc == 0), stop=(fc == 7))
                    av = acc[:, tt, hf * 512:(hf + 1) * 512]
                    tq = tmpq[kk % 2]
                    P.ts('dve', tq.v, py.v, pall[:, tt, e:e + 1], c1.v, op0=ALU.mult, op1=ALU.mult)
                    P.tt('dve', av, av, tq.v, ALU.add)
        for tt in range(TGT):
            t = gi * TGT + tt
            r = rr[tt % 2].v
            P.stt('dve', r, xts[tt].v, DN_ALPHA, acc[:, tt, :], ALU.mult, ALU.add)
            layer_norm(P, r, gB.v, bB.v, st.v, mv.v, rstd.v)
            P.dma('sp', out_d[t * 128:(t + 1) * 128, :], r)


RET_GAMMA = [1.0 - 2.0 ** (-5.0 - h) for h in range(4)]


def host_consts_scan(NT):
    j = np.arange(128, dtype=np.float64)
    lg = np.log(np.array(RET_GAMMA, dtype=np.float64))
    aR = np.exp(lg[None, :] * (j[:, None] + 1.0)) * (64 ** -0.5)
    bR = np.exp(-lg[None, :] * (j[:, None] + 1.0))
    eR = np.zeros((128, 2)); gR = np.zeros((128, 2))
    for h in range(4):
        ps = (h % 2) * 64
        eR[ps:ps + 64, h // 2] = np.exp(lg[h] * 128.0)
        gR[ps:ps + 64, h // 2] = np.exp(lg[h] * float(NT))
    mask = (j[:, None] <= j[None, :]).astype(np.float64)
    return dict(aR=aR.astype(np.float32), bR=bR.astype(np.float32), eR=eR.astype(np.float32),
                gR=gR.astype(np.float32), mask=mask.astype(np.float32))


def host_blockdiag(w):
    out = np.zeros((4, 128, 128), dtype=np.float32)
    for h in range(4):
        for n in range(32):
            out[h, 4 * n:4 * n + 4, 4 * n:4 * n + 4] = w[32 * h + n]
    return out


class PSRot:
    def __init__(self, C, banks):
        self.C = C; self.banks = banks; self.i = 0

    def __call__(self):
        b = self.C.ps[self.banks[self.i % len(self.banks)]]
        self.i += 1
        return b


def small_T(P, C, dst, src2d, rows, stage, ps):
    P.dma('act', stage[0:rows, :], src2d)
    P.tr(ps[:, 0:rows], stage[0:rows, :], C.identf[0:rows, 0:rows])
    P.copy('dve', dst, ps[:, 0:rows])


def pass_scan(P, C, NT, x_d, xprev_d, w_in_l, prm, cst, init, outs, mode="full", pfx="s"):
    full = (mode == "full")
    NW = 2568
    W = P.sb(pfx + "W", [128, 8, NW], BF16)
    load_w_cast(P, W.v, w_in_l[:, 0:NW].rearrange("(kc p) c -> p kc c", p=128))
    BD = {}
    for nm in ("bdq", "bdk", "bdv"):
        BD[nm] = P.sb(pfx + nm, [128, 4, 128], BF16)
        P.dma('pool', BD[nm].v, prm[nm].rearrange("h i o -> i h o"))
    stage = P.sb(pfx + "stage", [128, 128])
    cwT = P.sb(pfx + "cwT", [128, 16]); cbT = P.sb(pfx + "cbT", [128, 4])
    small_T(P, C, cwT.v, prm["ml_conv_w"].rearrange("k (c p) -> (k c) p", p=128), 16, stage, C.ps[7])
    small_T(P, C, cbT.v, prm["ml_conv_b"].rearrange("(c p) -> c p", p=128), 4, stage, C.ps[7])
    biB = P.sb(pfx + "biB", [128, 4]); bfB = P.sb(pfx + "bfB", [128, 4])
    bcast_rows(P, biB.v, prm["ml_bi"]); bcast_rows(P, bfB.v, prm["ml_bf"])
    aR = P.sb(pfx + "aR", [128, 4]); bR = P.sb(pfx + "bR", [128, 4]); eR = P.sb(pfx + "eR", [128, 2]); gR = P.sb(pfx + "gR", [128, 2])
    for t_, n_ in ((aR, "aR"), (bR, "bR"), (eR, "eR"), (gR, "gR")):
        P.dma('act', t_.v, cst[n_])
    mask = P.sb(pfx + "mask", [128, 128]); P.dma('act', mask.v, cst["mask"])
    maskr = P.sb(pfx + "maskr", [128, 128], F32R); P.copy('dve', maskr.v, mask.v)
    onesr = P.sb(pfx + "onesr", [128, 128], F32R); P.copy('dve', onesr.v, C.onesf.v)
    c1 = P.sb(pfx + "c1", [128, 1]); P.memset('dve', c1.v, 1.0)
    if full:
        gnR = P.sb(pfx + "gnR", [128, 512]); gnM = P.sb(pfx + "gnM", [128, 512]); skM = P.sb(pfx + "skM", [128, 512])
        bcast_rows(P, gnR.v, prm["ret_gn"]); bcast_rows(P, gnM.v, prm["ml_gn"]); bcast_rows(P, skM.v, prm["ml_skip"])
    Sret = P.sb(pfx + "Sret", [128, 2, 128]); Sretb = P.sb(pfx + "Sretb", [128, 2, 128], BF16)
    Cml = P.sb(pfx + "Cml", [128, 4, 129]); Cmlb = P.sb(pfx + "Cmlb", [128, 4, 129], BF16)
    tmpS = P.sb(pfx + "tmpS", [128, 4, 129]); tmpS2 = P.sb(pfx + "tmpS2", [128, 4, 129])
    totacc = P.sb(pfx + "totacc", [128, 4])
    P.memset('dve', Sret.v, 0.0); P.memset('dve', Cml.v, 0.0); P.memset('dve', totacc.v, 0.0)
    if init is not None:
        sel = P.sb(pfx + "sel", [128, 3]); nsel = P.sb(pfx + "nsel", [128, 3])
        P.dma('act', sel.v, init["sel"]); P.dma('act', nsel.v, init["nsel"])
        Fr = P.sb(pfx + "Fr", [128, 2, 128]); Fm = P.sb(pfx + "Fm", [128, 4, 129]); tl = P.sb(pfx + "tl", [128, 4])
        Gm = P.sb(pfx + "Gm", [128, 4])
        for q in range(3):
            P.dma('act', Fr.v, init["Fret"][q]); P.dma('act', Fm.v, init["Fml"][q]); P.dma('act', tl.v, init["totL"][q])
            P.act(Gm.v, tl.v, AF.Exp, scale=-1.0)
            for hp in range(2):
                P.act(tmpS[:, hp, 0:128], Sret[:, hp, :], AF.Copy, scale=gR[:, hp:hp + 1])
                P.tt('dve', tmpS[:, hp, 0:128], tmpS[:, hp, 0:128], Fr[:, hp, :], ALU.add)
                P.act(tmpS[:, hp, 0:128], tmpS[:, hp, 0:128], AF.Copy, scale=sel[:, q:q + 1])
                P.act(tmpS2[:, hp, 0:128], Sret[:, hp, :], AF.Copy, scale=nsel[:, q:q + 1])
                P.tt('dve', Sret[:, hp, :], tmpS[:, hp, 0:128], tmpS2[:, hp, 0:128], ALU.add)
            for h in range(4):
                P.act(tmpS[:, h, :], Cml[:, h, :], AF.Copy, scale=Gm[:, h:h + 1])
                P.tt('dve', tmpS[:, h, :], tmpS[:, h, :], Fm[:, h, :], ALU.add)
                P.act(tmpS[:, h, :], tmpS[:, h, :], AF.Copy, scale=sel[:, q:q + 1])
                P.act(tmpS2[:, h, :], Cml[:, h, :], AF.Copy, scale=nsel[:, q:q + 1])
                P.tt('dve', Cml[:, h, :], tmpS[:, h, :], tmpS2[:, h, :], ALU.add)
    P.copy('dve', Sretb.v, Sret.v); P.copy('dve', Cmlb.v, Cml.v)
    SC = 512
    xts = [P.sb(pfx + "xt%d" % i, [128, 1024]) for i in range(2)]
    xT = P.sb(pfx + "xT", [128, 8, SC], BF16)
    rqT = P.sb(pfx + "rqT", [128, 2, SC], BF16); rkT = P.sb(pfx + "rkT", [128, 2, SC], BF16)
    mxT = P.sb(pfx + "mxT", [128, 4, 3 + SC]); mxb = P.sb(pfx + "mxb", [128, 4, SC], BF16)
    cva = P.sb(pfx + "cva", [128, SC]); cvb = P.sb(pfx + "cvb", [128, SC])
    mcT = P.sb(pfx + "mcT", [128, 4, SC], BF16)
    qmT = P.sb(pfx + "qmT", [128, 4, SC], BF16); kmT = P.sb(pfx + "kmT", [128, 4, SC], BF16)
    rk_tok = P.sb(pfx + "rk_tok", [128, 256], BF16); km_tok = P.sb(pfx + "km_tok", [128, 512], BF16)
    vpR = P.sb(pfx + "vpR", [128, 4, 128], BF16); vpM = P.sb(pfx + "vpM", [128, 4, 129], BF16)
    g8 = P.sb(pfx + "g8", [128, 8]); L1 = P.sb(pfx + "L1", [128, 4], F32R); e1 = P.sb(pfx + "e1", [128, 4])
    igt = P.sb(pfx + "igt", [128, 4]); aM = P.sb(pfx + "aM", [128, 4]); bM = P.sb(pfx + "bM", [128, 4]); eM = P.sb(pfx + "eM", [128, 4])
    tmp4 = P.sb(pfx + "tmp4", [128, 4])
    Pm = [P.sb(pfx + "Pm%d" % i, [128, 128], BF16) for i in range(2)]
    ot = [P.sb(pfx + "ot%d" % i, [128, 129]) for i in range(2)]
    hh = [P.sb(pfx + "hh%d" % i, [128, 128]) for i in range(2)]
    dn = P.sb(pfx + "dn", [128, 1]); st6 = P.sb(pfx + "st6", [128, 6]); mv = P.sb(pfx + "mv", [128, 2]); rs = P.sb(pfx + "rs", [128, 1])
    if full:
        yR = P.sb(pfx + "yR", [128, 512]); yM = P.sb(pfx + "yM", [128, 512])
        rg = P.sb(pfx + "rg", [128, 512]); mo = P.sb(pfx + "mo", [128, 512]); mct = P.sb(pfx + "mct", [128, 512])
        yTo = [P.sb(pfx + "yTo%d" % i, [128, 4, 128], BF16) for i in range(2)]
        ybf = P.sb(pfx + "ybf", [128, 512], BF16)
    nps = PSRot(C, [0, 1, 2, 3, 4, 5, 6, 7])
    P.dma('sp', xts[0].v, xprev_d)
    x_transpose(P, C, xts[0].v, [(xT[:, :, 0:128], 'act')], [nps(), nps()])
    for c in range(4):
        pz = nps()
        for kc in range(8):
            P.mm(pz[:, 0:128], W[:, kc, OFF['m_x'] + c * 128: OFF['m_x'] + (c + 1) * 128], xT[:, kc, 0:128],
                 start=(kc == 0), stop=(kc == 7))
        P.copy('dve', mxT[:, c, 0:3], pz[:, 125:128])
    lnscale = float(np.log(128 ** -0.5))
    for sc in range(NT // SC):
        for tt in range(4):
            t = sc * 4 + tt
            xt = xts[t % 2]
            P.dma('sp', xt.v, x_d[t * 128:(t + 1) * 128, :])
            x_transpose(P, C, xt.v, [(xT[:, :, tt * 128:(tt + 1) * 128], 'act')], [nps(), nps()])
        for (dst, off, nch, kind) in ((rqT, OFF['r_q'], 2, 'bf'), (rkT, OFF['r_k'], 2, 'bf'), (mxT, OFF['m_x'], 4, 'mx')):
            if not full and (dst is rqT or dst is rkT):
                continue
            for c in range(nch):
                pz = nps()
                for kc in range(8):
                    P.mm(pz.v, W[:, kc, off + c * 128: off + (c + 1) * 128], xT[:, kc, :], start=(kc == 0), stop=(kc == 7))
                if kind == 'bf':
                    P.copy('act', dst[:, c, :], pz.v)
                else:
                    P.copy('act', mxT[:, c, 3:3 + SC], pz.v)
                    P.copy('dve', mxb[:, c, :], pz.v)
        for c in range(4):
            P.ts('dve', cva.v, mxT[:, c, 3:3 + SC], cwT[:, 12 + c:13 + c], cbT[:, c:c + 1], op0=ALU.mult, op1=ALU.add)
            P.stt('dve', cvb.v, mxT[:, c, 2:2 + SC], cwT[:, 8 + c:9 + c], cva.v, ALU.mult, ALU.add)
            P.stt('dve', cva.v, mxT[:, c, 1:1 + SC], cwT[:, 4 + c:5 + c], cvb.v, ALU.mult, ALU.add)
            P.stt('dve', cvb.v, mxT[:, c, 0:SC], cwT[:, c:c + 1], cva.v, ALU.mult, ALU.add)
            P.act(mcT[:, c, :], cvb.v, AF.Silu)
            P.copy('dve', cva[:, 0:3], mxT[:, c, SC:SC + 3])
            P.copy('dve', mxT[:, c, 0:3], cva[:, 0:3])
        for (dst, bd) in ((qmT, BD["bdq"]), (kmT, BD["bdk"])):
            if not full:
                continue
            for h in range(4):
                pz = nps()
                P.mm(pz.v, bd[:, h, :], mcT[:, h, :])
                P.copy('act', dst[:, h, :], pz.v)
        for tt in range(4):
            t = sc * 4 + tt
            ts_ = slice(tt * 128, (tt + 1) * 128)

            def tok_proj(off, n):
                pz = nps()
                for kc in range(8):
                    P.mm(pz[:, 0:n], xT[:, kc, ts_], W[:, kc, off:off + n], start=(kc == 0), stop=(kc == 7))
                return pz
            p_rk = tok_proj(OFF['r_k'], 256)
            P.copy('act', rk_tok.v, p_rk[:, 0:256])
            p_g8 = tok_proj(OFF['m_i'], 8)
            P.copy('dve', g8.v, p_g8[:, 0:8])
            P.tt('dve', igt.v, g8[:, 0:4], biB.v, ALU.add)
            P.tt('dve', tmp4.v, g8[:, 4:8], bfB.v, ALU.add)
            P.act(e1.v, tmp4.v, AF.Exp, scale=-1.0)
            P.ts('dve', e1.v, e1.v, 1.0, None, op0=ALU.add)
            P.act(L1.v, e1.v, AF.Ln)
            pc = nps()
            P.mm(pc[:, 0:4], maskr.v, L1.v)
            P.mm(pc[:, 8:12], onesr.v, L1.v)
            P.act(aM.v, pc[:, 0:4], AF.Exp, scale=-1.0, bias=lnscale)
            P.tt('dve', tmp4.v, igt.v, pc[:, 0:4], ALU.add)
            P.act(bM.v, tmp4.v, AF.Exp)
            P.act(eM.v, pc[:, 8:12], AF.Exp, scale=-1.0)
            P.tt('dve', totacc.v, totacc.v, pc[:, 8:12], ALU.add)
            p_rv = tok_proj(OFF['r_v'], 512)
            for h in range(4):
                P.act(vpR[:, h, :], p_rv[:, h * 128:(h + 1) * 128], AF.Copy, scale=bR[:, h:h + 1])
            p_vm = nps()
            for h in range(4):
                P.mm(p_vm[:, h * 128:(h + 1) * 128], mxb[:, h, ts_], BD["bdv"][:, h, :])
            for h in range(4):
                P.act(vpM[:, h, 0:128], p_vm[:, h * 128:(h + 1) * 128], AF.Copy, scale=bM[:, h:h + 1])
            P.copy('dve', vpM[:, :, 128:129], bM.v.re("p (h o) -> p h o", o=1))
            p_km = nps()
            for h in range(4):
                P.mm(p_km[:, h * 128:(h + 1) * 128], mcT[:, h, ts_], BD["bdk"][:, h, :])
            P.copy('act', km_tok.v, p_km.v)
            if full:
                p_rg = tok_proj(OFF['r_g'], 512)
                P.act(rg.v, p_rg.v, AF.Silu)
                p_mo = tok_proj(OFF['m_o'], 512)
                P.act(mo.v, p_mo.v, AF.Sigmoid)
                p_mc = nps()
                pmb = p_mc.v.bitcast(BF16)
                for c in range(4):
                    P.tr(pmb[:, c * 128:(c + 1) * 128], mcT[:, c, ts_], C.identb.v)
                P.tt('dve', mct.v, pmb[:, 0:512], skM.v, ALU.mult)
            kq = 0
            for h in range(4):
                psl = slice((h % 2) * 64, (h % 2) * 64 + 64); hp = h // 2
                if full:
                    p_st = nps()
                    P.mm(p_st[:, 0:128], rkT[psl, hp, ts_], rqT[psl, hp, ts_])
                    pm = Pm[kq % 2]; o = ot[kq % 2]; hx = hh[kq % 2]; kq += 1
                    P.tt('dve', pm.v, p_st[:, 0:128], mask.v, ALU.mult)
                    p_o = nps()
                    P.mm(p_o[:, 0:128], pm.v, vpR[:, h, :], start=True, stop=False)
                    P.mm(p_o[:, 0:128], rqT[psl, hp, ts_], Sretb[psl, hp, :], start=False, stop=True)
                    P.act(o[:, 0:128], p_o[:, 0:128], AF.Copy, scale=aR[:, h:h + 1])
                    head_norm(P, o[:, 0:128], hx.v, st6, mv, rs)
                    P.tt('pool', hx.v, hx.v, gnR[:, h * 128:(h + 1) * 128], ALU.mult)
                    P.tt('pool', yR[:, h * 128:(h + 1) * 128], hx.v, rg[:, h * 128:(h + 1) * 128], ALU.mult)
                p_kv = nps()
                P.mm(p_kv[:, 0:128], rk_tok[:, hp * 128:(hp + 1) * 128], vpR[:, h, :])
                P.tt('dve', tmpS[psl, hp, 0:128], Sret[psl, hp, :], p_kv[psl, 0:128], ALU.add)
                P.act(Sret[psl, hp, :], tmpS[psl, hp, 0:128], AF.Copy, scale=eR[psl, hp:hp + 1])
                P.copy('dve', Sretb[psl, hp, :], Sret[psl, hp, :])
            for h in range(4):
                if full:
                    p_st = nps()
                    P.mm(p_st[:, 0:128], kmT[:, h, ts_], qmT[:, h, ts_])
                    pm = Pm[kq % 2]; o = ot[kq % 2]; hx = hh[kq % 2]; kq += 1
                    P.tt('dve', pm.v, p_st[:, 0:128], mask.v, ALU.mult)
                    p_o = nps()
                    P.mm(p_o[:, 0:129], pm.v, vpM[:, h, :], start=True, stop=False)
                    P.mm(p_o[:, 0:129], qmT[:, h, ts_], Cmlb[:, h, :], start=False, stop=True)
                    P.act(o.v, p_o[:, 0:129], AF.Copy, scale=aM[:, h:h + 1])
                    P.act(dn.v, o[:, 128:129], AF.Abs)
                    P.ts('dve', dn.v, dn.v, 1.0, None, op0=ALU.max)
                    a_ = dn.v.ap
                    P.I('dve', (lambda a_: (lambda e: e.reciprocal(a_, a_)))(a_), w=[dn], r=[dn])
                    P.act(o[:, 0:128], o[:, 0:128], AF.Copy, scale=dn.v)
                    head_norm(P, o[:, 0:128], hx.v, st6, mv, rs)
                    P.tt('pool', hx.v, hx.v, gnM[:, h * 128:(h + 1) * 128], ALU.mult)
                    P.tt('pool', hx.v, hx.v, mct[:, h * 128:(h + 1) * 128], ALU.add)
                    P.tt('pool', yM[:, h * 128:(h + 1) * 128], hx.v, mo[:, h * 128:(h + 1) * 128], ALU.mult)
                p_kv = nps()
                P.mm(p_kv[:, 0:129], km_tok[:, h * 128:(h + 1) * 128], vpM[:, h, :])
                P.tt('dve', tmpS[:, h, :], Cml[:, h, :], p_kv[:, 0:129], ALU.add)
                P.act(Cml[:, h, :], tmpS[:, h, :], AF.Copy, scale=eM[:, h:h + 1])
                P.copy('dve', Cmlb[:, h, :], Cml[:, h, :])
            if full:
                for (ysrc, ydst) in ((yR, outs[0]), (yM, outs[1])):
                    P.copy('act', ybf.v, ysrc.v)
                    pz = nps(); pzb = pz.v.bitcast(BF16)
                    for c in range(4):
                        P.tr(pzb[:, c * 128:(c + 1) * 128], ybf[:, c * 128:(c + 1) * 128], C.identb.v)
                    yo = yTo[kq % 2]; kq += 1
                    P.copy('dve', yo.v, pzb[:, 0:512].re("p (c t) -> p c t", c=4))
                    P.dma('sp', ydst[:, :, t * 128:(t + 1) * 128], yo.v)
    if not full:
        P.dma('sp', outs[0].v, Sret.v)
        P.dma('sp', outs[1].v, Cml.v)
        P.dma('sp', outs[2].v, totacc.v)


def head_norm(P, src, dst, st6, mv, rs):
    a, b = st6.v.ap, src.ap
    P.I('dve', lambda e: e.bn_stats(a, b), w=[st6], r=[src])
    a2, b2 = mv.v.ap, st6.v.ap
    P.I('dve', lambda e: e.bn_aggr(a2, b2), w=[mv], r=[st6])
    P.ts('dve', rs.v, mv[:, 1:2], EPS, None, op0=ALU.add)
    P.act(rs.v, rs.v, AF.Ln)
    P.act(rs.v, rs.v, AF.Exp, scale=-0.5)
    P.ts('dve', dst, src, mv[:, 0:1], rs.v, op0=ALU.subtract, op1=ALU.mult)


ATT_PAT = ((128, 1), (512, 4), (2048, 16))
HALO = 2048


def host_consts_attn():
    slopes = np.exp2(-8.0 * np.arange(1, 13, dtype=np.float64) / 12.0).reshape(3, 4)
    s = np.arange(128)[:, None]; i = np.arange(128)[None, :]
    out = np.zeros((3, 2, 128, 4, 128), dtype=np.float32)
    for g, (win, d) in enumerate(ATT_PAT):
        for h in range(4):
            dcur = i - s
            b = np.where((dcur >= 0), -slopes[g, h] * d * dcur, -30000.0)
            out[g, 1, :, h, :] = b
            dprev = i + 128 - s
            b = np.where((dprev <= 128), -slopes[g, h] * d * dprev, -30000.0)
            out[g, 0, :, h, :] = b
    return out.reshape(3, 2, 128, 512)


def pass_attn(P, C, NT, xext_d, w_in_l, bias_d, hv_d, yT_out, pfx="a", dbg=0):
    accN = P.sb(pfx + "accN", [128, 4, NT]); accD = P.sb(pfx + "accD", [128, 4, NT])
    for h_ in range(4):
        for j_ in range(NT // 2048):
            P.memset('dve', accN[:, h_, j_ * 2048:(j_ + 1) * 2048], 0.0 if dbg == 0 else 1.0)
            P.memset('dve', accD[:, h_, j_ * 2048:(j_ + 1) * 2048], 0.0 if dbg == 0 else 2.0)
    hv0 = P.sb(pfx + "hv0", [128, 128]); hvb = P.sb(pfx + "hvb", [128, 128], BF16)
    P.dma('act', hv0.v, hv_d); P.copy('dve', hvb.v, hv0.v)
    Wq = P.sb(pfx + "Wq", [128, 8, 256], BF16); Wk = P.sb(pfx + "Wk", [128, 8, 256], BF16); Wv = P.sb(pfx + "Wv", [128, 8, 512], BF16)
    bT = [P.sb(pfx + "bT%d" % i, [128, 512]) for i in range(2)]
    xts = [P.sb(pfx + "xt%d" % i, [128, 1024]) for i in range(2)]
    xTb = [P.sb(pfx + "xTb%d" % i, [128, 8, 128], BF16) for i in range(2)]
    kT = [P.sb(pfx + "kT%d" % i, [128, 2, 128], BF16) for i in range(2)]
    Vt = [P.sb(pfx + "V%d" % i, [128, 512], BF16) for i in range(2)]
    qT = [P.sb(pfx + "qT%d" % i, [128, 2, 128], BF16) for i in range(2)]
    tmp = [P.sb(pfx + "tmp%d" % i, [128, 512]) for i in range(2)]
    PT = [[P.sb(pfx + "PT%d_%d" % (i, j), [128, 512], BF16) for j in range(2)] for i in range(2)]
    nps = PSRot(C, [0, 1, 2, 3, 4, 5, 6, 7])
    win = w_in_l.rearrange("(kc p) c -> p kc c", p=128)
    nb = 0
    for g, (_, d) in enumerate(ATT_PAT if dbg not in (1, 2, 4, 5, 6) else (ATT_PAT[:1] if dbg in (2, 4, 5, 6) else ())):
        load_w_cast(P, Wq.v, win[:, :, OFF['a_q'] + g * 256: OFF['a_q'] + (g + 1) * 256])
        load_w_cast(P, Wk.v, win[:, :, OFF['a_k'] + g * 256: OFF['a_k'] + (g + 1) * 256])
        load_w_cast(P, Wv.v, win[:, :, OFF['a_v'] + g * 512: OFF['a_v'] + (g + 1) * 512])
        P.dma('act', bT[0].v, bias_d[g, 0]); P.dma('act', bT[1].v, bias_d[g, 1])
        NB = NT // (128 * d)
        for r in range(d):
            for m in range(-1, NB):
                cur = nb % 2; prv = 1 - cur; nb += 1
                s0 = HALO + m * 128 * d + r
                xt = xts[cur]
                P.dma('sp', xt.v, xext_d[s0: s0 + 127 * d + 1: d, :])
                x_transpose(P, C, xt.v, [(xTb[cur].v, 'act')], [nps(), nps()])
                xb = xTb[cur]
                for c in range(2):
                    pz = nps()
                    for kc in range(8):
                        P.mm(pz[:, 0:128], Wk[:, kc, c * 128:(c + 1) * 128], xb[:, kc, :], start=(kc == 0), stop=(kc == 7))
                    P.copy('act', kT[cur][:, c, :], pz[:, 0:128])
                pz = nps()
                for kc in range(8):
                    P.mm(pz.v, xb[:, kc, :], Wv[:, kc, :], start=(kc == 0), stop=(kc == 7))
                P.copy('act', Vt[cur].v, pz.v)
                if m < 0 or dbg == 4:
                    continue
                for c in range(2):
                    pz = nps()
                    for kc in range(8):
                        P.mm(pz[:, 0:128], Wq[:, kc, c * 128:(c + 1) * 128], xb[:, kc, :], start=(kc == 0), stop=(kc == 7))
                    P.copy('act', qT[cur][:, c, :], pz[:, 0:128])
                for pc, kb in ((0, prv), (1, cur)):
                    psAB = [nps(), nps()]
                    for h in range(4):
                        psl = slice((h % 2) * 64, (h % 2) * 64 + 64); hp = h // 2
                        P.mm(psAB[h % 2][:, hp * 128:(hp + 1) * 128], kT[kb][psl, hp, :], qT[cur][psl, hp, :])
                    tm = tmp[pc]
                    for h in range(4):
                        hs = slice(h * 128, (h + 1) * 128); hp = h // 2
                        P.stt('dve', tm[:, hs], psAB[h % 2][:, hp * 128:(hp + 1) * 128], 0.125, bT[pc][:, hs], ALU.mult, ALU.add)
                    P.act(PT[cur][pc].v, tm.v, AF.Exp)
                if dbg in (5, 6):
                    continue
                pn = nps()
                for h in range(4):
                    hs = slice(h * 128, (h + 1) * 128)
                    P.mm(pn[:, hs], Vt[prv][:, hs], PT[cur][0][:, hs], start=True, stop=False)
                    P.mm(pn[:, hs], Vt[cur][:, hs], PT[cur][1][:, hs], start=False, stop=True)
                pd = nps()
                P.mm(pd.v, (hvb.v if m == 0 else C.onesb.v), PT[cur][0].v, start=True, stop=False)
                P.mm(pd.v, C.onesb.v, PT[cur][1].v, start=False, stop=True)
                t0 = m * 128 * d + r
                for h in range(4 if dbg != 3 else 0):
                    hs = slice(h * 128, (h + 1) * 128)
                    av = accN[:, h, t0: t0 + 127 * d + 1: d]
                    P.tt('dve', av, av, pn[:, hs], ALU.add)
                    dv = accD[:, h, t0: t0 + 127 * d + 1: d]
                    P.tt('dve', dv, dv, pd[:, hs], ALU.add)
    yo = [P.sb(pfx + "yo%d" % i, [128, 4, 512], BF16) for i in range(2)]
    rc = [P.sb(pfx + "rc%d" % i, [128, 4, 512]) for i in range(2)]
    for j in range(NT // 512):
        sl = slice(j * 512, (j + 1) * 512)
        for h in range(4):
            a_, b_ = rc[j % 2][:, h, :].ap, accD[:, h, sl].ap
            P.I('dve', (lambda a_, b_: (lambda e: e.reciprocal(a_, b_)))(a_, b_), w=[rc[j % 2]], r=[accD])
            P.tt('dve', yo[j % 2][:, h, :], accN[:, h, sl], rc[j % 2][:, h, :], ALU.mult)
        P.dma('sp', yT_out[:, :, sl], yo[j % 2].v)


NCORES = 8
NT_CORE = 4096
PRM_NAMES = ("ret_gn", "ml_conv_w", "ml_conv_b", "bdq", "bdk", "bdv", "ml_bi", "ml_bf", "ml_gn", "ml_skip")
PRM_SHAPES = dict(ret_gn=[512], ml_conv_w=[4, 512], ml_conv_b=[512], bdq=[4, 128, 128], bdk=[4, 128, 128], bdv=[4, 128, 128],
                  ml_bi=[4], ml_bf=[4], ml_gn=[512], ml_skip=[512])
CST_SHAPES = dict(aR=[128, 4], bR=[128, 4], eR=[128, 2], gR=[128, 2], mask=[128, 128])


def _scan_inputs(nc):
    def inp(n, sh):
        return nc.dram_tensor(n, sh, F32, kind="ExternalInput").ap()
    prm = {n: inp(n, PRM_SHAPES[n]) for n in PRM_NAMES}
    cst = {n: inp(n, CST_SHAPES[n]) for n in CST_SHAPES}
    return prm, cst


def build_A(NT=NT_CORE):
    nc = bass.Bass("TRN2", target_bir_lowering=False)
    with ExitStack() as es:
        P = Prog(nc, es)
        x_d = P.dram("x", [NT, D], F32, kind="ExternalInput")
        xprev = P.dram("xprev", [128, D], F32, kind="ExternalInput")
        w_in = nc.dram_tensor("w_in", [D, D_IN], F32, kind="ExternalInput").ap()
        prm, cst = _scan_inputs(nc)
        outs = [P.dram("oS", [128, 2, 128], F32, kind="ExternalOutput"), P.dram("oC", [128, 4, 129], F32, kind="ExternalOutput"),
                P.dram("oT", [128, 4], F32, kind="ExternalOutput")]
        C = make_ctx(P)
        pass_scan(P, C, NT, x_d, xprev, w_in, prm, cst, None, outs, mode="summary", pfx="s")
        P.wait_all('sp', outs)
        P.emit()
    return nc


def build_B(NT=NT_CORE, NE=NEXP):
    nc = bass.Bass("TRN2", target_bir_lowering=False)
    with ExitStack() as es:
        P = Prog(nc, es)

        def inp(n, sh):
            return nc.dram_tensor(n, sh, F32, kind="ExternalInput").ap()
        xext = P.dram("xext", [HALO + NT, D], F32, kind="ExternalInput")
        w_in = inp("w_in", [D, D_IN])
        prm, cst = _scan_inputs(nc)
        init = dict(Fret=inp("Fret", [3, 128, 2, 128]), Fml=inp("Fml", [3, 128, 4, 129]), totL=inp("totL", [3, 128, 4]),
                    sel=inp("sel", [128, 3]), nsel=inp("nsel", [128, 3]))
        abias = inp("abias", [3, 2, 128, 512]); hv = inp("hv", [128, 128])
        w_br = inp("w_branch", [3, MIXW, D]); w_out = inp("w_out", [D, D])
        ln1_g = inp("ln1_g", [D]); ln1_b = inp("ln1_b", [D]); ln2_g = inp("ln2_g", [D]); ln2_b = inp("ln2_b", [D])
        w_r = inp("w_router", [D, NE]); b_r = inp("b_router", [NE])
        w_g = inp("w_gate", [NE, D, D]); b_g = inp("b_gate", [NE, D])
        w_u = inp("w_up", [NE, D, D]); b_u = inp("b_up", [NE, D])
        w_d = inp("w_down", [NE, D, D]); b_d = inp("b_down", [NE, D])
        out = P.dram("out", [NT, D], F32, kind="ExternalOutput")
        yT = [P.dram("yT%d" % b, [128, 4, NT], BF16) for b in range(3)]
        x1s = P.dram("x1s", [NT, D], F32)
        C = make_ctx(P)
        x_own = Tile(P, xext.h[HALO:HALO + NT, :], "xown", "dram")
        x_own.lw, x_own.rd = xext.lw, xext.rd
        xprev = Tile(P, xext.h[HALO - 128:HALO, :], "xprv", "dram")
        xprev.lw, xprev.rd = xext.lw, xext.rd
        with P.scope():
            pass_attn(P, C, NT, xext, w_in, abias, hv, yT[2], pfx="a")
        with P.scope():
            pass_scan(P, C, NT, x_own, xprev, w_in, prm, cst, init, [yT[0], yT[1]], mode="full", pfx="s")
        with P.scope():
            pass_merge(P, C, NT, x_own, yT, w_in, w_br, w_out, ln1_g, ln1_b, x1s, pfx="m")
        NBLK = NT * 4 // 128 + NE
        Xs = P.dram("Xs", [NBLK * 128, D], BF16); Ys = P.dram("Ys", [NBLK * 128, D], F32)
        mc = {k: inp("mc_" + k, list(v.shape)) for k, v in host_consts_moe(NT, NE).items()}
        with P.scope():
            pass_moe2(P, C, NT, x1s, w_r, b_r, w_g, b_g, w_u, b_u, w_d, b_d, ln2_g, ln2_b, out, Xs, Ys, mc, NE=NE, pfx="f")
        P.wait_all('sp', [out])
        P.emit()
    return nc


def layer_params(inputs, l):
    g = lambda n: np.ascontiguousarray(np.asarray(inputs[n], dtype=np.float32)[l])
    prm = dict(ret_gn=g("ret_gn"), ml_conv_w=g("ml_conv_w"), ml_conv_b=g("ml_conv_b"),
               bdq=host_blockdiag(g("ml_wq")), bdk=host_blockdiag(g("ml_wk")), bdv=host_blockdiag(g("ml_wv")),
               ml_bi=g("ml_bi"), ml_bf=g("ml_bf"), ml_gn=g("ml_gn"), ml_skip=g("ml_skip"))
    big = dict(w_in=g("w_in"), w_branch=g("w_branch"), w_out=g("w_out"), ln1_g=g("ln1_g"), ln1_b=g("ln1_b"),
               ln2_g=g("ln2_g"), ln2_b=g("ln2_b"), w_router=g("w_router"), b_router=g("b_router"),
               w_gate=g("w_gate"), b_gate=g("b_gate"), w_up=g("w_up"), b_up=g("b_up"), w_down=g("w_down"), b_down=g("b_down"))
    return prm, big


def run_layer(ncA, ncB, xs, prm, big, cst, abias, QPB, NT):
    n = len(xs)
    zeros128 = np.zeros((128, D), np.float32)
    inA = []
    for c in range(n):
        q = c % QPB
        m = dict(prm); m.update(cst)
        m["w_in"] = big["w_in"]; m["x"] = xs[c]
        m["xprev"] = np.ascontiguousarray(xs[c - 1][-128:]) if q > 0 else zeros128
        inA.append(m)
    resA = run_bass_kernel_spmd(ncA, inA, core_ids=list(range(n))).results
    inB = []
    for c in range(n):
        q = c % QPB; b0 = c - q
        m = dict(prm); m.update(cst); m.update(big)
        halo = xs[c - 1][-HALO:] if q > 0 else np.zeros((HALO, D), np.float32)
        m["xext"] = np.ascontiguousarray(np.concatenate([halo, xs[c]], axis=0))
        Fret = np.zeros((3, 128, 2, 128), np.float32); Fml = np.zeros((3, 128, 4, 129), np.float32)
        totL = np.zeros((3, 128, 4), np.float32); sel = np.zeros((128, 3), np.float32)
        for qq in range(min(3, QPB)):
            Fret[qq] = resA[b0 + qq]["oS"]; Fml[qq] = resA[b0 + qq]["oC"]; totL[qq] = resA[b0 + qq]["oT"]
            if qq < q:
                sel[:, qq] = 1.0
        m.update(Fret=Fret, Fml=Fml, totL=totL, sel=sel, nsel=(1.0 - sel).astype(np.float32))
        m["abias"] = abias
        for k_, v_ in host_consts_moe(NT, big["w_router"].shape[1]).items():
            m["mc_" + k_] = v_
        m["hv"] = (np.ones((128, 128), np.float32) if q > 0 else np.zeros((128, 128), np.float32))
        inB.append(m)
    resB = run_bass_kernel_spmd(ncB, inB, core_ids=list(range(n))).results
    return [np.asarray(r["out"], dtype=np.float32) for r in resB]


def kernel(**inputs):
    x = np.asarray(inputs["x"], dtype=np.float32)
    B, S, _ = x.shape
    QPB = NCORES // B
    NT = S // QPB
    xs = [np.ascontiguousarray(x[c // QPB, (c % QPB) * NT:(c % QPB + 1) * NT]) for c in range(NCORES)]
    ncA = build_A(NT); ncB = build_B(NT)
    cst = host_consts_scan(NT); abias = host_consts_attn()
    L = np.asarray(inputs["w_in"]).shape[0]
    for l in range(L):
        prm, big = layer_params(inputs, l)
        xs = run_layer(ncA, ncB, xs, prm, big, cst, abias, QPB, NT)
    out = np.zeros((B, S, D), np.float32)
    for c in range(NCORES):
        out[c // QPB, (c % QPB) * NT:(c % QPB + 1) * NT] = xs[c]
    return out


BIGIDX = 4000000.0


def host_consts_moe(NT, NE=NEXP):
    NBLK = NT * 4 // 128 + NE
    p = np.arange(128, dtype=np.float32)
    d = dict(iota_p=p.reshape(128, 1).copy(),
             ustrict=(p[:, None] < p[None, :]).astype(np.float32),
             iotaJ=np.tile(np.arange(33, dtype=np.float32)[None, :], (128, 1)),
             iotaB=np.tile(np.arange(NBLK, dtype=np.float32)[None, :], (128, 1)))
    return d


def _breg(P, e, bound):
    if not hasattr(P, "_bregs"):
        P._bregs = {}
    if bound not in P._bregs:
        P._bregs[bound] = e.to_reg(int(bound))
    return P._bregs[bound]


def ind_dma(P, dst_v, src_tile, src_ap, idx_v, bound, scatter=False, extra_r=()):
    sb_tile = dst_v.tile
    key = P._tsem(sb_tile)
    d_ap, i_ap = dst_v.ap, idx_v.ap
    if not scatter:
        rt = [src_tile, idx_v.tile] + list(extra_r); wt = [sb_tile]

        def f(e):
            try:
                return e.indirect_dma_start(d_ap, None, src_ap, bass.IndirectOffsetOnAxis(i_ap, 0), bounds_check=_breg(P, e, bound), oob_is_err=False)
            except Exception:
                print("IND GATHER FAIL", d_ap, src_ap, i_ap, bound)
                raise
    else:
        rt = [sb_tile, idx_v.tile] + list(extra_r); wt = [src_tile]

        def f(e):
            try:
                return e.indirect_dma_start(src_ap, bass.IndirectOffsetOnAxis(i_ap, 0), d_ap, None, bounds_check=_breg(P, e, bound), oob_is_err=False)
            except Exception:
                print("IND SCATTER FAIL", d_ap, src_ap, i_ap, bound)
                raise
    waits = P._waits('pool', rt, wt)
    sb_tile.dcnt += 16
    P.stream['pool'].append((waits, f, (key, 16)))
    P._record((key, sb_tile.dcnt), rt, wt)
    P.ninst += 1


def pass_moe2(P, C, NT, x1_d, w_router, b_router, w_gate, b_gate, w_up, b_up, w_down, b_down, ln_g, ln_b, out_d,
              Xs, Ys, mc, NE=NEXP, pfx="f", dbg=0):
    NTL = NT // 128
    NBLK = NT * 4 // 128 + NE
    NSLOT = NBLK * 128
    gB = P.sb(pfx + "gB", [128, 1024]); bB = P.sb(pfx + "bB", [128, 1024])
    bcast_rows(P, gB.v, ln_g); bcast_rows(P, bB.v, ln_b)
    wr = P.sb(pfx + "wr", [128, 8, NE], F32R); wr0 = P.sb(pfx + "wr0", [128, 8, NE])
    P.dma('act', wr0.v, w_router.rearrange("(kc p) e -> p kc e", p=128)); P.copy('dve', wr.v, wr0.v)
    brB = P.sb(pfx + "brB", [128, NE]); bcast_rows(P, brB.v, b_router)
    c1 = P.sb(pfx + "c1", [128, 1]); P.memset('dve', c1.v, 1.0)
    iop = P.sb(pfx + "iop", [128, 1]); P.dma('act', iop.v, mc["iota_p"])
    us0 = P.sb(pfx + "us0", [128, 128]); P.dma('act', us0.v, mc["ustrict"])
    usb = P.sb(pfx + "usb", [128, 128], BF16); P.copy('dve', usb.v, us0.v)
    ioJ = P.sb(pfx + "ioJ", [128, 33]); P.dma('act', ioJ.v, mc["iotaJ"])
    ioB = P.sb(pfx + "ioB", [128, NBLK]); P.dma('act', ioB.v, mc["iotaB"])
    lg_all = P.sb(pfx + "lg_all", [128, NTL, NE]); t8_all = P.sb(pfx + "t8_all", [128, NTL, 8])
    pall = P.sb(pfx + "pall", [128, NTL, NE]); pos_all = P.sb(pfx + "pos_all", [128, NTL, NE])
    slot_f = P.sb(pfx + "slot_f", [128, NTL, 4]); slot_i = P.sb(pfx + "slot_i", [128, NTL * 4], I32)
    pk_all = P.sb(pfx + "pk_all", [128, NTL, 4])
    carry = P.sb(pfx + "carry", [128, NE]); P.memset('dve', carry.v, 0.0)
    xts = [P.sb(pfx + "xt%d" % i, [128, 1024]) for i in range(2)]
    xT32 = P.sb(pfx + "xT32", [128, 8, 128], F32R)
    msk = P.sb(pfx + "msk", [128, NE]); mskb = P.sb(pfx + "mskb", [128, NE], BF16)
    ex = P.sb(pfx + "ex", [128, NE]); sm = P.sb(pfx + "sm", [128, 1]); nmx = P.sb(pfx + "nmx", [128, 1])
    nps = PSRot(C, [0, 1, 2, 3, 4, 5, 6, 7])
    for t in range(NTL):
        xt = xts[t % 2]
        P.dma('sp', xt.v, x1_d[t * 128:(t + 1) * 128, :])
        x_transpose(P, C, xt.v, [(xT32.v, 'dve')], [nps(), nps()])
        pl = nps()
        for kc in range(8):
            P.mm(pl[:, 0:NE], xT32[:, kc, :], wr[:, kc, :], start=(kc == 0), stop=(kc == 7))
        lg = lg_all[:, t, :]; t8 = t8_all[:, t, :]
        P.tt('dve', lg, pl[:, 0:NE], brB.v, ALU.add)
        a, b = t8.ap, lg.ap
        P.I('dve', (lambda a, b: (lambda e: e.max(out=a, in_=b)))(a, b), w=[t8_all], r=[lg_all])
        P.ts('dve', msk.v, lg, t8_all[:, t, 3:4], c1.v, op0=ALU.is_ge, op1=ALU.mult)
        P.ts('dve', nmx.v, t8_all[:, t, 0:1], -1.0, None, op0=ALU.mult)
        P.act(ex.v, lg, AF.Exp, bias=nmx.v)
        P.tt('dve', ex.v, ex.v, msk.v, ALU.mult)
        a2, b2 = sm.v.ap, ex.v.ap
        P.I('dve', (lambda a, b: (lambda e: e.reduce_sum(a, b, AX.X)))(a2, b2), w=[sm], r=[ex])
        a3 = sm.v.ap
        P.I('dve', (lambda a: (lambda e: e.reciprocal(a, a)))(a3), w=[sm], r=[sm])
        P.ts('dve', pall[:, t, :], ex.v, sm.v, c1.v, op0=ALU.mult, op1=ALU.mult)
        P.copy('dve', mskb.v, msk.v)
        pr = nps()
        P.mm(pr[:, 0:NE], usb.v, mskb.v)
        P.mm(pr[:, 64:64 + NE], C.onesb.v, mskb.v)
        P.tt('dve', pos_all[:, t, :], carry.v, pr[:, 0:NE], ALU.add)
        P.tt('dve', carry.v, carry.v, pr[:, 64:64 + NE], ALU.add)
    q = P.sb(pfx + "q", [128, NE]); nb = P.sb(pfx + "nb", [128, NE]); bend = P.sb(pfx + "bend", [128, NE])
    pstart = P.sb(pfx + "pstart", [128, NE]); t33 = P.sb(pfx + "t33", [128, 33])
    P.ts('dve', q.v, carry.v, 1.0 / 128.0, None, op0=ALU.mult)
    for e in range(NE):
        P.ts('dve', t33.v, ioJ.v, q[:, e:e + 1], c1.v, op0=ALU.is_lt, op1=ALU.mult)
        a_, b_ = nb[:, e:e + 1].ap, t33.v.ap
        P.I('dve', (lambda a, b: (lambda e_: e_.reduce_sum(a, b, AX.X)))(a_, b_), w=[nb], r=[t33])
    P.copy('dve', bend[:, 0:1], nb[:, 0:1])
    for e in range(1, NE):
        P.tt('dve', bend[:, e:e + 1], bend[:, e - 1:e], nb[:, e:e + 1], ALU.add)
    P.tt('dve', pstart.v, bend.v, nb.v, ALU.subtract)
    P.ts('dve', pstart.v, pstart.v, 128.0, None, op0=ALU.mult)
    Eall = P.sb(pfx + "Eall", [128, NBLK]); tB = P.sb(pfx + "tB", [128, NBLK]); sk = P.sb(pfx + "sk", [128, NBLK])
    P.memset('dve', Eall.v, 0.0)
    for e in range(NE):
        P.ts('dve', tB.v, ioB.v, bend[:, e:e + 1], c1.v, op0=ALU.is_ge, op1=ALU.mult)
        P.tt('dve', Eall.v, Eall.v, tB.v, ALU.add)
    P.ts('dve', Eall.v, Eall.v, float(NE - 1), None, op0=ALU.min)
    P.memset('dve', sk.v, 0.0)
    P.tt('dve', sk[:, 2:NBLK], Eall[:, 2:NBLK], Eall[:, 0:NBLK - 2], ALU.is_equal)
    P.ts('dve', sk.v, sk.v, BIGIDX, None, op0=ALU.mult)
    idxWf = P.sb(pfx + "idxWf", [128, NBLK]); idxW = P.sb(pfx + "idxW", [128, 8 * NBLK], I32)
    idxBf = P.sb(pfx + "idxBf", [128, NBLK]); idxB = P.sb(pfx + "idxB", [128, NBLK], I32)
    P.tt('dve', idxBf.v, Eall.v, sk.v, ALU.add)
    P.copy('dve', idxB.v, idxBf.v)
    iop4 = P.sb(pfx + "iop4", [128, 1]); P.ts('dve', iop4.v, iop.v, 4.0, None, op0=ALU.mult)
    P.ts('dve', idxWf.v, Eall.v, 512.0, None, op0=ALU.mult)
    P.tt('dve', idxWf.v, idxWf.v, sk.v, ALU.add)
    P.ts('dve', idxWf.v, idxWf.v, iop4.v, c1.v, op0=ALU.add, op1=ALU.mult)
    for kq in range(4):
        P.ts('dve', tB.v, idxWf.v, float(kq), None, op0=ALU.add)
        P.copy('dve', idxW[:, kq * NBLK:(kq + 1) * NBLK], tB.v)
    OH = P.sb(pfx + "OH", [128, NBLK]); P.ts('dve', OH.v, Eall.v, iop.v, c1.v, op0=ALU.is_equal, op1=ALU.mult)
    if dbg == 2:
        return
    xb16 = [P.sb(pfx + "xb16_%d" % i, [128, 1024], BF16) for i in range(2)]
    tA = P.sb(pfx + "tA", [128, NE]); oh = P.sb(pfx + "oh", [128, NE]); pr1 = P.sb(pfx + "pr1", [128, NE])
    Xs2 = Xs.h[:]
    for t in range(NTL):
        xt = xts[t % 2]; xb = xb16[t % 2]
        P.dma('sp', xt.v, x1_d[t * 128:(t + 1) * 128, :])
        P.copy('act', xb.v, xt.v)
        P.tt('dve', tA.v, pos_all[:, t, :], pstart.v, ALU.add)
        for k in range(4):
            P.ts('dve', oh.v, lg_all[:, t, :], t8_all[:, t, k:k + 1], c1.v, op0=ALU.is_equal, op1=ALU.mult)
            P.tt('dve', pr1.v, oh.v, tA.v, ALU.mult)
            a_, b_ = slot_f[:, t, k:k + 1].ap, pr1.v.ap
            P.I('dve', (lambda a, b: (lambda e_: e_.reduce_sum(a, b, AX.X)))(a_, b_), w=[slot_f], r=[pr1])
            P.tt('dve', pr1.v, oh.v, pall[:, t, :], ALU.mult)
            a_, b_ = pk_all[:, t, k:k + 1].ap, pr1.v.ap
            P.I('dve', (lambda a, b: (lambda e_: e_.reduce_sum(a, b, AX.X)))(a_, b_), w=[pk_all], r=[pr1])
        P.copy('dve', slot_i[:, t * 4:(t + 1) * 4], slot_f[:, t, :])
        for k in range(4):
            ind_dma(P, xb.v, Xs, Xs2, slot_i[:, t * 4 + k: t * 4 + k + 1], NSLOT - 1, scatter=True)
    if dbg == 3:
        P.wait_all('sp', [Xs])
        return
    with P.scope():
        inv128 = P.sb(pfx + 'inv128', [128, 128], BF16); P.memset('dve', inv128.v, 1.0 / 128.0)
        Wq = [[[P.sb(pfx + "W%d_%d_%d" % (j, i, kq), [128, 2, 1024], BF16) for kq in range(4)] for j in range(3)] for i in range(2)]
        Wt = [[[Wq[i][j][kc // 2][:, kc % 2, :] for kc in range(8)] for j in range(3)] for i in range(2)]
        Ball = [P.sb(pfx + "Ball%d" % j, [NE, 1024], BF16) for j in range(3)]
        for j, bsrc_ in enumerate((b_gate, b_up, b_down)):
            P.dma('pool', Ball[j].v, bsrc_)
        OHb = [P.sb(pfx + "OHb%d" % i, [NE, 128], BF16) for i in range(2)]
        wsrc = [w_.rearrange("e (p kq r) f -> (e p kq) (r f)", p=128, kq=4, r=2) for w_ in (w_gate, w_up, w_down)]
        bsrc = [b_gate, b_up, b_down]
        wtile = [Tile(P, None, pfx + "wsrc%d" % j, "dram") for j in range(3)]
        xblk = [P.sb(pfx + "xblk%d" % i, [128, 1024], BF16) for i in range(2)]
        xbT = [P.sb(pfx + "xbT%d" % i, [128, 8, 128], BF16) for i in range(2)]
        actT = [P.sb(pfx + "actT%d" % i, [128, 8, 128], BF16) for i in range(2)]
        gt = [P.sb(pfx + "g%d" % i, [128, 512]) for i in range(2)]
        s_t = [P.sb(pfx + "s%d" % i, [128, 512]) for i in range(2)]
        ut = [P.sb(pfx + "u%d" % i, [128, 512]) for i in range(2)]
        atok = [P.sb(pfx + "atok%d" % i, [128, 1024], BF16) for i in range(2)]
        yb = [P.sb(pfx + "yb%d" % i, [128, 1024]) for i in range(2)]
        kkc = [0]

        def stageXg(b):
            cur = b % 2
            for j in range(3):
                for kq in range(4):
                    ind_dma(P, Wq[cur][j][kq].v.re("p r f -> p (r f)"), wtile[j], wsrc[j],
                            idxW[:, kq * NBLK + b: kq * NBLK + b + 1], NE * 512 - 1)
            Wg, Wu, Wd = Wt[cur]
            P.copy('dve', OHb[cur].v, OH[0:NE, b:b + 1].bc([NE, 128]))

        def stageXx(b):
            cur = b % 2
            xk = xblk[cur]
            if dbg != 9 or b < 2:
                P.dma('sp', xk.v, Xs[b * 128:(b + 1) * 128, :])
            xT_ = xbT[cur]
            for half in range(2):
                pz = nps(); pzb = pz.v.bitcast(BF16)
                for c in range(4):
                    kc = half * 4 + c
                    P.tr(pzb[:, c * 128:(c + 1) * 128], xk[:, kc:1024:8], C.identb.v)
                P.copy('act', xT_[:, half * 4:(half + 1) * 4, :], pzb[:, 0:512].re("p (c t) -> p c t", c=4))

        def stageG(b):
            cur = b % 2
            Wg, Wu, Wd = Wt[cur]
            xT_ = xbT[cur]
            aT = actT[cur]
            at = atok[cur]
            for hf in range(2 if dbg not in (6, 10) else 0):
                hs = slice(hf * 512, (hf + 1) * 512)
                pg = nps()
                for kc in range(8):
                    P.mm(pg.v, xT_[:, kc, :], Wg[kc][:, hs], start=(kc == 0), stop=False)
                P.mm(pg.v, OHb[cur].v, Ball[0][:, hs], start=False, stop=True)
                pu = nps()
                for kc in range(8):
                    P.mm(pu.v, xT_[:, kc, :], Wu[kc][:, hs], start=(kc == 0), stop=False)
                P.mm(pu.v, OHb[cur].v, Ball[1][:, hs], start=False, stop=True)
                g = gt[kkc[0] % 2]; s_ = s_t[kkc[0] % 2]; u = ut[kkc[0] % 2]; kkc[0] += 1
                P.ts('dve', g.v, pg.v, 7.0, None, op0=ALU.min)
                P.act(s_.v, g.v, AF.Sigmoid, scale=1.702)
                P.ts('dve', u.v, pu.v, 7.0, -7.0, op0=ALU.min, op1=ALU.max)
                P.tt('dve', g.v, g.v, s_.v, ALU.mult)
                P.stt('dve', at[:, hs], u.v, 1.0, g.v, ALU.add, ALU.mult)

        def stageT(b):
            cur = b % 2
            Wg, Wu, Wd = Wt[cur]
            aT = actT[cur]
            at = atok[cur]
            for half in range(2 if dbg not in (6, 10) else 0):
                pz = nps(); pzb = pz.v.bitcast(BF16)
                for c in range(4):
                    fc = half * 4 + c
                    P.tr(pzb[:, c * 128:(c + 1) * 128], at[:, fc:1024:8], C.identb.v)
                P.copy('act', aT[:, half * 4:(half + 1) * 4, :], pzb[:, 0:512].re("p (c t) -> p c t", c=4))

        def stageDn(b):
            cur = b % 2
            Wg, Wu, Wd = Wt[cur]
            aT = actT[cur]
            y = yb[cur]
            if dbg in (6, 10):
                P.memset('dve', y.v, 0.5)
            for hf in range(2 if dbg not in (6, 10) else 0):
                py = nps()
                hs = slice(hf * 512, (hf + 1) * 512)
                for fc in range(8):
                    P.mm(py.v, aT[:, fc, :], Wd[fc][:, hs], start=(fc == 0), stop=False)
                P.mm(py.v, OHb[cur].v, Ball[2][:, hs], start=False, stop=True)
                P.copy('act', y[:, hs], py.v)
            if dbg != 7:
                P.dma('sp', Ys[b * 128:(b + 1) * 128, :], y.v)
        nblk_ = NBLK if dbg != 11 else 10
        stageXx(0)
        for b in range(nblk_ + 1):
            if b < nblk_:
                stageXg(b)
                stageG(b)
            if b >= 1:
                stageT(b - 1)
            if b + 1 < nblk_:
                stageXx(b + 1)
            if b >= 1:
                stageDn(b - 1)
    if dbg == 4:
        return
    rk = [P.sb(pfx + "rk%d" % i, [128, 1024]) for i in range(4)]
    acA = P.sb(pfx + "acA", [128, 1024]); acB = P.sb(pfx + "acB", [128, 1024])
    st = P.sb(pfx + "st", [128, 2, 6]); mv = P.sb(pfx + "mv", [128, 2]); rstd = P.sb(pfx + "rstd", [128, 1])
    Ys2 = Ys.h[:]
    for t in range(NTL):
        xt = xts[t % 2]
        P.dma('sp', xt.v, x1_d[t * 128:(t + 1) * 128, :])
        for k in range(4):
            ind_dma(P, rk[k].v, Ys, Ys2, slot_i[:, t * 4 + k: t * 4 + k + 1], NSLOT - 1)
        P.ts('dve', acA.v, rk[0].v, pk_all[:, t, 0:1], c1.v, op0=ALU.mult, op1=ALU.mult)
        P.stt('dve', acB.v, rk[1].v, pk_all[:, t, 1:2], acA.v, ALU.mult, ALU.add)
        P.stt('dve', acA.v, rk[2].v, pk_all[:, t, 2:3], acB.v, ALU.mult, ALU.add)
        P.stt('dve', acB.v, rk[3].v, pk_all[:, t, 3:4], acA.v, ALU.mult, ALU.add)
        P.stt('dve', acA.v, xt.v, DN_ALPHA, acB.v, ALU.mult, ALU.add)
        layer_norm(P, acA.v, gB.v, bB.v, st.v, mv.v, rstd.v)
        P.dma('sp', out_d[t * 128:(t + 1) * 128, :], acA.v)
```

```python
import numpy as np
import concourse.bass as bass
import concourse.mybir as mybir
from concourse.bass_utils import run_bass_kernel_spmd
from contextlib import ExitStack

F32 = mybir.dt.float32
F32R = mybir.dt.float32r
BF16 = mybir.dt.bfloat16
I32 = mybir.dt.int32
U32 = mybir.dt.uint32
AF = mybir.ActivationFunctionType
ALU = mybir.AluOpType
AX = mybir.AxisListType

ENGS = ['pe', 'act', 'dve', 'pool', 'sp']


class Tile:
    def __init__(self, P, h, name, space='sb'):
        self.P = P
        self.h = h
        self.name = name
        self.space = space
        self.lw = {}
        self.rd = {}
        self.dsem = None
        self.dcnt = 0

    def __getitem__(self, k):
        return V(self, self.h[k])

    @property
    def v(self):
        return V(self, self.h[:])


class V:
    def __init__(self, tile, ap):
        self.tile = tile
        self.ap = ap

    def __getitem__(self, k):
        return V(self.tile, self.ap[k])

    def bitcast(self, dt):
        return V(self.tile, self.ap.bitcast(dt))

    def bc(self, shape):
        return V(self.tile, self.ap.to_broadcast(shape))

    def re(self, s, **kw):
        return V(self.tile, self.ap.rearrange(s, **kw))


def _ap(x):
    if isinstance(x, Tile):
        return x.h[:]
    return x.ap if isinstance(x, V) else x


class Prog:
    def __init__(self, nc, es, same_engine_sync=None):
        self.nc = nc
        self.es = es
        self.es_top = es
        self.all_tiles = []
        self.stream = {e: [] for e in ENGS}
        self.sems = {}
        self.cnt = {e: 0 for e in ENGS}
        self.known = {e: {} for e in ENGS}
        import os as _os
        self.same = (_os.environ.get('KSAME', '1') == '1') if same_engine_sync is None else same_engine_sync
        self.nsem = 0
        for e in ['pe', 'act', 'dve', 'pool']:
            self.sems[e] = es.enter_context(nc.semaphore("s_" + e))
            self.nsem += 1
        self.ninst = 0
        self.nwait = 0

    def sb(self, name, shape, dt=F32):
        h = self.es.enter_context(self.nc.sbuf_tensor(name, list(shape), dt))
        return Tile(self, h, name)

    def ps(self, name, shape, dt=F32):
        h = self.es.enter_context(self.nc.psum_tensor(name, list(shape), dt))
        return Tile(self, h, name, 'ps')

    def dram(self, name, shape, dt=F32, kind="Internal"):
        h = self.nc.dram_tensor(name, list(shape), dt, kind=kind)
        return Tile(self, h.ap(), name, 'dram')

    def _tsem(self, t):
        if t.dsem is None:
            key = "d_" + t.name
            self.sems[key] = self.es_top.enter_context(self.nc.semaphore(key))
            self.nsem += 1
            t.dsem = key
            self.all_tiles.append(t)
        return t.dsem

    def _waits(self, eng, rt, wt):
        need = {}
        for t in rt:
            for s, v in t.lw.items():
                need[s] = max(need.get(s, 0), v)
        for t in wt:
            for s, v in t.lw.items():
                need[s] = max(need.get(s, 0), v)
            for s, v in t.rd.items():
                need[s] = max(need.get(s, 0), v)
        out = []
        kn = self.known[eng]
        for s, v in need.items():
            if s == eng and (eng == 'pe' or not self.same):
                continue
            if kn.get(s, 0) < v:
                kn[s] = v
                out.append((s, v))
        return out

    def _record(self, ev, rt, wt):
        s, v = ev
        for t in wt:
            t.lw[s] = max(t.lw.get(s, 0), v)
            t.rd = {}
        for t in rt:
            if t in wt:
                continue
            t.rd[s] = max(t.rd.get(s, 0), v)

    @staticmethod
    def _tiles(xs):
        out = []
        for x in xs:
            if x is None:
                continue
            t = x.tile if isinstance(x, V) else x
            if isinstance(t, Tile) and t not in out:
                out.append(t)
        return out

    def I(self, eng, fn, w=(), r=()):
        wt = self._tiles(w)
        rt = self._tiles(r)
        for t in rt:
            if t.space == 'ps' and t not in wt and eng != 'pe':
                wt.append(t)
        waits = self._waits(eng, rt, wt)
        self.cnt[eng] += 1
        ev = (eng, self.cnt[eng])
        self.stream[eng].append((waits, fn, (eng, 1)))
        self._record(ev, rt, wt)
        self.ninst += 1
        self.nwait += len(waits)

    def dma(self, q, out, in_, **kw):
        wt = self._tiles([out])
        rt = self._tiles([in_])
        owner = None
        for x in (out, in_):
            t_ = x.tile if isinstance(x, V) else (x if isinstance(x, Tile) else None)
            if t_ is not None and t_.space == 'sb':
                owner = t_
        if owner is None:
            for x in (out, in_):
                t_ = x.tile if isinstance(x, V) else (x if isinstance(x, Tile) else None)
                if t_ is not None and owner is None:
                    owner = t_
        key = self._tsem(owner)
        waits = self._waits(q, rt, wt)
        owner.dcnt += 16
        ev = (key, owner.dcnt)
        o, i = _ap(out), _ap(in_)
        self.stream[q].append((waits, lambda e: e.dma_start(out=o, in_=i, **kw), (key, 16)))
        self._record(ev, rt, wt)
        self.ninst += 1
        self.nwait += len(waits)
        return ev

    def wait_all(self, eng, tiles):
        ts = self._tiles(tiles)
        waits = self._waits(eng, ts, ts)
        self.stream[eng].append((waits, None, None))

    def mm(self, out, lhsT, rhs, start=True, stop=True, **kw):
        o, a, b = _ap(out), _ap(lhsT), _ap(rhs)
        self.I('pe', lambda e: e.matmul(o, a, b, start=start, stop=stop, **kw), w=[out], r=[lhsT, rhs])

    def tr(self, out, in_, ident):
        o, a, b = _ap(out), _ap(in_), _ap(ident)
        self.I('pe', lambda e: e.transpose(o, a, b), w=[out], r=[in_, ident])

    def act(self, out, in_, func, bias=None, scale=1.0, accum_out=None, eng='act'):
        o, a = _ap(out), _ap(in_)
        kw = {}
        if bias is not None:
            kw['bias'] = _ap(bias)
        if accum_out is not None:
            kw['accum_out'] = _ap(accum_out)
        sc = _ap(scale)
        self.I(eng, lambda e: e.activation(o, a, func, scale=sc, **kw),
               w=[out, accum_out], r=[in_, bias, scale if isinstance(scale, V) else None])

    def tt(self, eng, out, in0, in1, op):
        o, a, b = _ap(out), _ap(in0), _ap(in1)
        self.I(eng, lambda e: e.tensor_tensor(o, a, b, op), w=[out], r=[in0, in1])

    def ts(self, eng, out, in0, s1, s2=None, op0=ALU.mult, op1=None, accum_out=None):
        o, a = _ap(out), _ap(in0)
        x1, x2 = _ap(s1), _ap(s2)
        kw = {}
        if op1 is not None:
            kw['op1'] = op1
        if accum_out is not None:
            kw['accum_out'] = _ap(accum_out)
        self.I(eng, lambda e: e.tensor_scalar(o, a, x1, x2, op0, **kw), w=[out, accum_out],
               r=[in0, s1 if isinstance(s1, V) else None, s2 if isinstance(s2, V) else None])

    def stt(self, eng, out, in0, scalar, in1, op0, op1):
        o, a, b = _ap(out), _ap(in0), _ap(in1)
        s = _ap(scalar)
        self.I(eng, lambda e: e.scalar_tensor_tensor(o, a, s, b, op0, op1), w=[out],
               r=[in0, in1, scalar if isinstance(scalar, V) else None])

    def copy(self, eng, out, in_):
        o, a = _ap(out), _ap(in_)
        if eng == 'act':
            self.I(eng, lambda e: e.copy(o, a), w=[out], r=[in_])
        else:
            self.I(eng, lambda e: e.tensor_copy(o, a), w=[out], r=[in_])

    def memset(self, eng, out, val):
        o = _ap(out)
        self.I(eng, lambda e: e.memset(o, val), w=[out])

    def barrier(self):
        evs = {e: self.cnt[e] for e in ['pe', 'act', 'dve', 'pool'] if self.cnt[e] > 0}
        for t in self.all_tiles:
            if t.dcnt > 0:
                evs[t.dsem] = t.dcnt
        for eng in ENGS:
            kn = self.known[eng]
            waits = []
            for s_, v in evs.items():
                if kn.get(s_, 0) < v:
                    kn[s_] = v
                    waits.append((s_, v))
            if waits:
                self.stream[eng].append((waits, None, None))

    def scope(self):
        P = self

        class _S:
            def __enter__(self_):
                self_.old = P.es
                self_.st = ExitStack()
                self_.st.__enter__()
                P.es = self_.st
                return self_

            def __exit__(self_, *a):
                P.barrier()
                P.emit()
                P.es = self_.old
                self_.st.__exit__(None, None, None)
                return False
        return _S()

    def emit(self):
        nc = self.nc
        sems = self.sems
        with nc.Block() as block:
            def run(engobj, name):
                for waits, fn, inc in self.stream[name]:
                    for s, v in waits:
                        engobj.wait_ge(sems[s], v)
                    if fn is not None:
                        ins = fn(engobj)
                        ins.then_inc(sems[inc[0]], inc[1])

            @block.tensor
            def _(e):
                run(e, 'pe')

            @block.scalar
            def _(e):
                run(e, 'act')

            @block.vector
            def _(e):
                run(e, 'dve')

            @block.gpsimd
            def _(e):
                run(e, 'pool')

            @block.sync
            def _(e):
                run(e, 'sp')
        self.stream = {e: [] for e in ENGS}


D = 1024
MIXW = 512
NEXP = 32
DN_ALPHA = (2.0 * 2) ** 0.25
EPS = 1e-5
OFF = dict(r_q=0, r_k=256, r_v=512, r_g=1024, m_x=1536, m_i=2048, m_f=2052, m_o=2056,
           a_q=2568, a_k=3336, a_v=4104, gates=5640)
D_IN = 8712


class Ctx:
    pass


def make_ctx(P):
    C = Ctx()
    C.identf = P.sb("identf", [128, 128], F32)
    C.identb = P.sb("identb", [128, 128], BF16)
    C.onesb = P.sb("onesb", [128, 128], BF16)
    C.onesf = P.sb("onesf", [128, 128], F32)
    P.memset('pool', C.identf.v, 1.0)
    o = C.identf.v.ap
    P.I('pool', lambda e: e.affine_select(o, o, [[-1, 128]], ALU.is_equal, 0.0, base=0, channel_multiplier=1),
        w=[C.identf], r=[C.identf])
    P.copy('pool', C.identb.v, C.identf.v)
    P.memset('pool', C.onesb.v, 1.0)
    P.memset('pool', C.onesf.v, 1.0)
    C.ps = [P.ps("psb%d" % i, [128, 512], F32) for i in range(8)]
    return C


def load_w_cast(P, dst, src, q='pool'):
    cols = src.shape[-1]
    c0 = 0
    while c0 < cols:
        c1 = min(cols, c0 + 1024)
        P.dma(q, dst[:, :, c0:c1], src[:, :, c0:c1])
        c0 = c1


def bcast_rows(P, dst, src1d, q='act'):
    P.dma(q, dst, src1d.partition_broadcast(128))


def x_transpose(P, C, xt, outs, psl):
    for half in range(2):
        pt = psl[half]
        for c in range(4):
            k = half * 4 + c
            P.tr(pt[:, c * 128:(c + 1) * 128], xt[:, k * 128:(k + 1) * 128], C.identf.v)
        for (o, eng) in outs:
            P.copy(eng, o[:, half * 4:(half + 1) * 4, :], pt.v.re("p (c t) -> p c t", c=4))


def layer_norm(P, r, g_b, b_b, st, mv, rstd, eng2='pool'):
    for hf in range(2):
        a, b = st[:, hf, :].ap, r[:, hf * 512:(hf + 1) * 512].ap
        P.I('dve', (lambda a, b: (lambda e: e.bn_stats(a, b)))(a, b), w=[st], r=[r])
    a, b = mv.ap, st.ap
    P.I('dve', lambda e: e.bn_aggr(a, b), w=[mv], r=[st])
    P.ts('dve', rstd, mv[:, 1:2], EPS, None, op0=ALU.add)
    P.act(rstd, rstd, AF.Ln)
    P.act(rstd, rstd, AF.Exp, scale=-0.5)
    P.ts('dve', r, r, mv[:, 0:1], rstd, op0=ALU.subtract, op1=ALU.mult)
    P.tt(eng2, r, r, g_b, ALU.mult)
    P.tt(eng2, r, r, b_b, ALU.add)


def pass_merge(P, C, NT, x_d, yT_d, w_in_l, w_branch_l, w_out_l, ln_g, ln_b, x1_d, pfx="m"):
    wg = P.sb(pfx + "wg", [128, 8, 3072], BF16)
    wb = P.sb(pfx + "wb", [128, 12, 1024], BF16)
    wo = P.sb(pfx + "wo", [128, 8, 1024], BF16)
    load_w_cast(P, wg.v, w_in_l[:, OFF['gates']:D_IN].rearrange("(kc p) c -> p kc c", p=128))
    load_w_cast(P, wb.v, w_branch_l.rearrange("b (kc p) c -> p (b kc) c", p=128))
    load_w_cast(P, wo.v, w_out_l.rearrange("(kc p) c -> p kc c", p=128))
    gB = P.sb(pfx + "gB", [128, 1024]); bB = P.sb(pfx + "bB", [128, 1024])
    bcast_rows(P, gB.v, ln_g); bcast_rows(P, bB.v, ln_b)
    xts = [P.sb(pfx + "xt%d" % i, [128, 1024]) for i in range(2)]
    xTs = [P.sb(pfx + "xT%d" % i, [128, 8, 128], BF16) for i in range(2)]
    yTs = [[P.sb(pfx + "yT%d_%d" % (b, i), [128, 4, 128], BF16) for i in range(2)] for b in range(3)]
    mg = [P.sb(pfx + "mg%d" % i, [128, 1024]) for i in range(2)]
    mT = [P.sb(pfx + "mT%d" % i, [128, 8, 128], BF16) for i in range(2)]
    sg = [P.sb(pfx + "sg%d" % i, [128, 512]) for i in range(2)]
    tmp = [P.sb(pfx + "tmp%d" % i, [128, 512]) for i in range(2)]
    rr = [P.sb(pfx + "rr%d" % i, [128, 1024]) for i in range(2)]
    st = P.sb(pfx + "st", [128, 2, 6]); mv = P.sb(pfx + "mv", [128, 2]); rstd = P.sb(pfx + "rstd", [128, 1])
    kc_ = [0]

    def S1(t):
        xt = xts[t % 2]; xT = xTs[t % 2]
        P.dma('sp', xt.v, x_d[t * 128:(t + 1) * 128, :])
        x_transpose(P, C, xt.v, [(xT.v, 'act')], [C.ps[0], C.ps[1]])
        for b in range(3):
            P.dma('act', yTs[b][t % 2].v, yT_d[b][:, :, t * 128:(t + 1) * 128])
        m = mg[t % 2]
        for b in range(3):
            for hf in range(2):
                pg = C.ps[2 + (kc_[0] % 2)]; pb = C.ps[4 + (kc_[0] % 2)]; s = sg[kc_[0] % 2]; tm = tmp[kc_[0] % 2]
                kc_[0] += 1
                for kc in range(8):
                    P.mm(pg.v, xT[:, kc, :], wg[:, kc, b * 1024 + hf * 512: b * 1024 + (hf + 1) * 512],
                         start=(kc == 0), stop=(kc == 7))
                for kc in range(4):
                    P.mm(pb.v, yTs[b][t % 2][:, kc, :], wb[:, b * 4 + kc, hf * 512:(hf + 1) * 512],
                         start=(kc == 0), stop=(kc == 3))
                P.act(s.v, pg.v, AF.Sigmoid)
                msl = m[:, hf * 512:(hf + 1) * 512]
                if b == 0:
                    P.tt('dve', msl, s.v, pb.v, ALU.mult)
                else:
                    P.tt('dve', tm.v, s.v, pb.v, ALU.mult)
                    P.tt('pool', msl, msl, tm.v, ALU.add)

    def S2(t):
        xt = xts[t % 2]
        m = mg[t % 2]
        x_transpose(P, C, m.v, [(mT[t % 2].v, 'act')], [C.ps[6], C.ps[7]])
        r = rr[t % 2]
        for hf in range(2):
            po = C.ps[2 + (kc_[0] % 2)]
            kc_[0] += 1
            for kc in range(8):
                P.mm(po.v, mT[t % 2][:, kc, :], wo[:, kc, hf * 512:(hf + 1) * 512], start=(kc == 0), stop=(kc == 7))
            P.stt('dve', r[:, hf * 512:(hf + 1) * 512], xt[:, hf * 512:(hf + 1) * 512], DN_ALPHA, po.v,
                  ALU.mult, ALU.add)
        layer_norm(P, r.v, gB.v, bB.v, st.v, mv.v, rstd.v)
        P.dma('sp', x1_d[t * 128:(t + 1) * 128, :], r.v)

    ntl_ = NT // 128
    S1(0)
    for t in range(ntl_):
        if t + 1 < ntl_:
            S1(t + 1)
        S2(t)


def pass_moe(P, C, NT, x1_d, w_router, b_router, w_gate, b_gate, w_up, b_up, w_down, b_down, ln_g, ln_b, out_d,
             NE=NEXP, pfx="e", TGT=4, dbg=0):
    TG = TGT * 128
    gB = P.sb(pfx + "gB", [128, 1024]); bB = P.sb(pfx + "bB", [128, 1024])
    bcast_rows(P, gB.v, ln_g); bcast_rows(P, bB.v, ln_b)
    wr = P.sb(pfx + "wr", [128, 8, NE], F32R)
    wr0 = P.sb(pfx + "wr0", [128, 8, NE])
    P.dma('act', wr0.v, w_router.rearrange("(kc p) e -> p kc e", p=128))
    P.copy('dve', wr.v, wr0.v)
    brB = P.sb(pfx + "brB", [128, NE]); bcast_rows(P, brB.v, b_router)
    bgT = P.sb(pfx + "bgT", [128, NE, 8]); buT = P.sb(pfx + "buT", [128, NE, 8])
    bstage = P.sb(pfx + "bstage", [128, 128])
    if dbg in (3, 7):
        P.memset('dve', bgT.v, 0.0); P.memset('dve', buT.v, 0.0)
    for (dstT, src) in (((bgT, b_gate), (buT, b_up)) if dbg not in (3, 7) else ()):
        rows = NE * 8
        srcv = src.rearrange("e (fc p) -> (e fc) p", p=128)
        dv = dstT.v.re("p e fc -> p (e fc)")
        r0 = 0
        while r0 < rows:
            r1 = min(rows, r0 + 128)
            n = r1 - r0
            P.dma('act', bstage[0:n, :], srcv[r0:r1, :])
            pz = C.ps[7]
            P.tr(pz[:, 0:n], bstage[0:n, :], C.identf[0:n, 0:n])
            P.copy('dve', dv[:, r0:r1], pz[:, 0:n])
            r0 = r1
    bd = P.sb(pfx + "bd", [NE, 1024], F32R)
    bd0 = P.sb(pfx + "bd0", [NE, 1024])
    P.dma('act', bd0.v, b_down)
    P.copy('dve', bd.v, bd0.v)
    W = [[P.sb(pfx + "W%d_%d" % (j, i), [128, 8, 1024], BF16) for j in range(3)] for i in range(2)]
    xts = [P.sb(pfx + "xt%d" % i, [128, 1024]) for i in range(TGT)]
    xTg = P.sb(pfx + "xTg", [128, 8, TG], BF16)
    xT32 = P.sb(pfx + "xT32", [128, 8, 128], F32R)
    acc = P.sb(pfx + "acc", [128, TGT, 1024])
    pall = P.sb(pfx + "pall", [128, TGT, NE])
    actT = P.sb(pfx + "actT", [128, 8, TG], BF16)
    lg = P.sb(pfx + "lg", [128, NE]); t8 = P.sb(pfx + "t8", [128, 8]); msk = P.sb(pfx + "msk", [128, NE])
    ex = P.sb(pfx + "ex", [128, NE]); sm = P.sb(pfx + "sm", [128, 1]); nmx = P.sb(pfx + "nmx", [128, 1])
    pT = P.sb(pfx + "pT", [NE, 128], F32R)
    gt = [P.sb(pfx + "g%d" % i, [128, TG]) for i in range(2)]
    st_ = [P.sb(pfx + "s%d" % i, [128, TG]) for i in range(2)]
    ut = [P.sb(pfx + "u%d" % i, [128, TG]) for i in range(2)]
    st = P.sb(pfx + "st", [128, 2, 6]); mv = P.sb(pfx + "mv", [128, 2]); rstd = P.sb(pfx + "rstd", [128, 1])
    c1 = P.sb(pfx + "c1", [128, 1]); c7 = P.sb(pfx + "c7", [128, 1])
    P.memset('dve', c1.v, 1.0); P.memset('dve', c7.v, 7.0)
    rr = [P.sb(pfx + "rr%d" % i, [128, 1024]) for i in range(2)]
    tmpq = [P.sb(pfx + "tq%d" % i, [128, 512]) for i in range(2)]
    assert TG == 512
    if dbg == 5:
        P.wait_all('sp', [gB, bB, wr, brB, bd, bgT, buT])
        return
    wcnt = 0
    kk = 0
    for gi in range(NT // TG):
        for tt in range(TGT):
            t = gi * TGT + tt
            xt = xts[tt]
            P.dma('sp', xt.v, x1_d[t * 128:(t + 1) * 128, :])
            x_transpose(P, C, xt.v, [(xTg[:, :, tt * 128:(tt + 1) * 128], 'act')] + ([(xT32.v, 'dve')] if dbg != 3 else []), [C.ps[0], C.ps[1]])
            if dbg in (2, 3, 7, 8):
                P.memset('dve', acc[:, tt, :], 0.0)
                P.memset('dve', pall[:, tt, :], 0.25)
            else:
                pl = C.ps[6]
                for kc in range(8):
                    P.mm(pl[:, 0:NE], xT32[:, kc, :], wr[:, kc, :], start=(kc == 0), stop=(kc == 7))
                P.tt('dve', lg.v, pl[:, 0:NE], brB.v, ALU.add)
                a, b = t8.v.ap, lg.v.ap
                P.I('dve', (lambda a, b: (lambda e: e.max(out=a, in_=b)))(a, b), w=[t8], r=[lg])
                P.ts('dve', msk.v, lg.v, t8[:, 3:4], c1.v, op0=ALU.is_ge, op1=ALU.mult)
                P.ts('dve', nmx.v, t8[:, 0:1], -1.0, None, op0=ALU.mult)
                P.act(ex.v, lg.v, AF.Exp, bias=nmx.v)
                P.tt('dve', ex.v, ex.v, msk.v, ALU.mult)
                a2, b2 = sm.v.ap, ex.v.ap
                P.I('dve', (lambda a, b: (lambda e: e.reduce_sum(a, b, AX.X)))(a2, b2), w=[sm], r=[ex])
                a3 = sm.v.ap
                P.I('dve', (lambda a: (lambda e: e.reciprocal(a, a)))(a3), w=[sm], r=[sm])
                P.ts('dve', pall[:, tt, :], ex.v, sm.v, c1.v, op0=ALU.mult, op1=ALU.mult)
                P.tr(pl[0:NE, 128:256], pall[:, tt, :], C.identf.v)
                P.copy('dve', pT.v, pl[0:NE, 128:256])
                for hf in range(2):
                    pb = C.ps[7]
                    P.mm(pb.v, pT.v, bd[:, hf * 512:(hf + 1) * 512])
                    P.copy('dve', acc[:, tt, hf * 512:(hf + 1) * 512], pb.v)
        for e in range(NE if dbg not in (1, 3, 7, 8) else 0):
            Wg, Wu, Wd = W[wcnt % 2]
            wcnt += 1
            P.dma('pool', Wg.v, w_gate[e].rearrange("(kc p) f -> p kc f", p=128))
            P.dma('pool', Wu.v, w_up[e].rearrange("(kc p) f -> p kc f", p=128))
            P.dma('pool', Wd.v, w_down[e].rearrange("(kc p) f -> p kc f", p=128))
            for fc in range(8):
                pg = C.ps[(kk % 2) * 2]; pu = C.ps[(kk % 2) * 2 + 1]
                g = gt[kk % 2]; s = st_[kk % 2]; u = ut[kk % 2]
                kk += 1
                for kc in range(8):
                    P.mm(pg.v, Wg[:, kc, fc * 128:(fc + 1) * 128], xTg[:, kc, :], start=(kc == 0), stop=(kc == 7))
                for kc in range(8):
                    P.mm(pu.v, Wu[:, kc, fc * 128:(fc + 1) * 128], xTg[:, kc, :], start=(kc == 0), stop=(kc == 7))
                P.ts('dve', g.v, pg.v, bgT[:, e, fc:fc + 1], c7.v, op0=ALU.add, op1=ALU.min)
                P.act(s.v, g.v, AF.Sigmoid, scale=1.702)
                P.ts('dve', u.v, pu.v, buT[:, e, fc:fc + 1], c7.v, op0=ALU.add, op1=ALU.min)
                P.ts('dve', u.v, u.v, -7.0, 1.0, op0=ALU.max, op1=ALU.add)
                P.tt('dve', g.v, g.v, s.v, ALU.mult)
                P.tt('dve', actT[:, fc, :], g.v, u.v, ALU.mult)
            for tt in range(TGT):
                for hf in range(2):
                    py = C.ps[4 + (kk % 2)]
                    kk += 1
                    for fc in range(8):
                        P.mm(py.v, actT[:, fc, tt * 128:(tt + 1) * 128], Wd[:, fc, hf * 512:(hf + 1) * 512],
                             start=(fc == 0), stop=(fc == 7))
                    av = acc[:, tt, hf * 512:(hf + 1) * 512]
                    tq = tmpq[kk % 2]
                    P.ts('dve', tq.v, py.v, pall[:, tt, e:e + 1], c1.v, op0=ALU.mult, op1=ALU.mult)
                    P.tt('dve', av, av, tq.v, ALU.add)
        for tt in range(TGT):
            t = gi * TGT + tt
            r = rr[tt % 2].v
            P.stt('dve', r, xts[tt].v, DN_ALPHA, acc[:, tt, :], ALU.mult, ALU.add)
            layer_norm(P, r, gB.v, bB.v, st.v, mv.v, rstd.v)
            P.dma('sp', out_d[t * 128:(t + 1) * 128, :], r)


RET_GAMMA = [1.0 - 2.0 ** (-5.0 - h) for h in range(4)]


def host_consts_scan(NT):
    j = np.arange(128, dtype=np.float64)
    lg = np.log(np.array(RET_GAMMA, dtype=np.float64))
    aR = np.exp(lg[None, :] * (j[:, None] + 1.0)) * (64 ** -0.5)
    bR = np.exp(-lg[None, :] * (j[:, None] + 1.0))
    eR = np.zeros((128, 2)); gR = np.zeros((128, 2))
    for h in range(4):
        ps = (h % 2) * 64
        eR[ps:ps + 64, h // 2] = np.exp(lg[h] * 128.0)
        gR[ps:ps + 64, h // 2] = np.exp(lg[h] * float(NT))
    mask = (j[:, None] <= j[None, :]).astype(np.float64)
    return dict(aR=aR.astype(np.float32), bR=bR.astype(np.float32), eR=eR.astype(np.float32),
                gR=gR.astype(np.float32), mask=mask.astype(np.float32))


def host_blockdiag(w):
    out = np.zeros((4, 128, 128), dtype=np.float32)
    for h in range(4):
        for n in range(32):
            out[h, 4 * n:4 * n + 4, 4 * n:4 * n + 4] = w[32 * h + n]
    return out


class PSRot:
    def __init__(self, C, banks):
        self.C = C; self.banks = banks; self.i = 0

    def __call__(self):
        b = self.C.ps[self.banks[self.i % len(self.banks)]]
        self.i += 1
        return b


def small_T(P, C, dst, src2d, rows, stage, ps):
    P.dma('act', stage[0:rows, :], src2d)
    P.tr(ps[:, 0:rows], stage[0:rows, :], C.identf[0:rows, 0:rows])
    P.copy('dve', dst, ps[:, 0:rows])


def pass_scan(P, C, NT, x_d, xprev_d, w_in_l, prm, cst, init, outs, mode="full", pfx="s"):
    full = (mode == "full")
    NW = 2568
    W = P.sb(pfx + "W", [128, 8, NW], BF16)
    load_w_cast(P, W.v, w_in_l[:, 0:NW].rearrange("(kc p) c -> p kc c", p=128))
    BD = {}
    for nm in ("bdq", "bdk", "bdv"):
        BD[nm] = P.sb(pfx + nm, [128, 4, 128], BF16)
        P.dma('pool', BD[nm].v, prm[nm].rearrange("h i o -> i h o"))
    stage = P.sb(pfx + "stage", [128, 128])
    cwT = P.sb(pfx + "cwT", [128, 16]); cbT = P.sb(pfx + "cbT", [128, 4])
    small_T(P, C, cwT.v, prm["ml_conv_w"].rearrange("k (c p) -> (k c) p", p=128), 16, stage, C.ps[7])
    small_T(P, C, cbT.v, prm["ml_conv_b"].rearrange("(c p) -> c p", p=128), 4, stage, C.ps[7])
    biB = P.sb(pfx + "biB", [128, 4]); bfB = P.sb(pfx + "bfB", [128, 4])
    bcast_rows(P, biB.v, prm["ml_bi"]); bcast_rows(P, bfB.v, prm["ml_bf"])
    aR = P.sb(pfx + "aR", [128, 4]); bR = P.sb(pfx + "bR", [128, 4]); eR = P.sb(pfx + "eR", [128, 2]); gR = P.sb(pfx + "gR", [128, 2])
    for t_, n_ in ((aR, "aR"), (bR, "bR"), (eR, "eR"), (gR, "gR")):
        P.dma('act', t_.v, cst[n_])
    mask = P.sb(pfx + "mask", [128, 128]); P.dma('act', mask.v, cst["mask"])
    maskr = P.sb(pfx + "maskr", [128, 128], F32R); P.copy('dve', maskr.v, mask.v)
    onesr = P.sb(pfx + "onesr", [128, 128], F32R); P.copy('dve', onesr.v, C.onesf.v)
    c1 = P.sb(pfx + "c1", [128, 1]); P.memset('dve', c1.v, 1.0)
    if full:
        gnR = P.sb(pfx + "gnR", [128, 512]); gnM = P.sb(pfx + "gnM", [128, 512]); skM = P.sb(pfx + "skM", [128, 512])
        bcast_rows(P, gnR.v, prm["ret_gn"]); bcast_rows(P, gnM.v, prm["ml_gn"]); bcast_rows(P, skM.v, prm["ml_skip"])
    Sret = P.sb(pfx + "Sret", [128, 2, 128]); Sretb = P.sb(pfx + "Sretb", [128, 2, 128], BF16)
    Cml = P.sb(pfx + "Cml", [128, 4, 129]); Cmlb = P.sb(pfx + "Cmlb", [128, 4, 129], BF16)
    tmpS = P.sb(pfx + "tmpS", [128, 4, 129]); tmpS2 = P.sb(pfx + "tmpS2", [128, 4, 129])
    totacc = P.sb(pfx + "totacc", [128, 4])
    P.memset('dve', Sret.v, 0.0); P.memset('dve', Cml.v, 0.0); P.memset('dve', totacc.v, 0.0)
    if init is not None:
        sel = P.sb(pfx + "sel", [128, 3]); nsel = P.sb(pfx + "nsel", [128, 3])
        P.dma('act', sel.v, init["sel"]); P.dma('act', nsel.v, init["nsel"])
        Fr = P.sb(pfx + "Fr", [128, 2, 128]); Fm = P.sb(pfx + "Fm", [128, 4, 129]); tl = P.sb(pfx + "tl", [128, 4])
        Gm = P.sb(pfx + "Gm", [128, 4])
        for q in range(3):
            P.dma('act', Fr.v, init["Fret"][q]); P.dma('act', Fm.v, init["Fml"][q]); P.dma('act', tl.v, init["totL"][q])
            P.act(Gm.v, tl.v, AF.Exp, scale=-1.0)
            for hp in range(2):
                P.act(tmpS[:, hp, 0:128], Sret[:, hp, :], AF.Copy, scale=gR[:, hp:hp + 1])
                P.tt('dve', tmpS[:, hp, 0:128], tmpS[:, hp, 0:128], Fr[:, hp, :], ALU.add)
                P.act(tmpS[:, hp, 0:128], tmpS[:, hp, 0:128], AF.Copy, scale=sel[:, q:q + 1])
                P.act(tmpS2[:, hp, 0:128], Sret[:, hp, :], AF.Copy, scale=nsel[:, q:q + 1])
                P.tt('dve', Sret[:, hp, :], tmpS[:, hp, 0:128], tmpS2[:, hp, 0:128], ALU.add)
            for h in range(4):
                P.act(tmpS[:, h, :], Cml[:, h, :], AF.Copy, scale=Gm[:, h:h + 1])
                P.tt('dve', tmpS[:, h, :], tmpS[:, h, :], Fm[:, h, :], ALU.add)
                P.act(tmpS[:, h, :], tmpS[:, h, :], AF.Copy, scale=sel[:, q:q + 1])
                P.act(tmpS2[:, h, :], Cml[:, h, :], AF.Copy, scale=nsel[:, q:q + 1])
                P.tt('dve', Cml[:, h, :], tmpS[:, h, :], tmpS2[:, h, :], ALU.add)
    P.copy('dve', Sretb.v, Sret.v); P.copy('dve', Cmlb.v, Cml.v)
    SC = 512
    xts = [P.sb(pfx + "xt%d" % i, [128, 1024]) for i in range(2)]
    xT = P.sb(pfx + "xT", [128, 8, SC], BF16)
    rqT = P.sb(pfx + "rqT", [128, 2, SC], BF16); rkT = P.sb(pfx + "rkT", [128, 2, SC], BF16)
    mxT = P.sb(pfx + "mxT", [128, 4, 3 + SC]); mxb = P.sb(pfx + "mxb", [128, 4, SC], BF16)
    cva = P.sb(pfx + "cva", [128, SC]); cvb = P.sb(pfx + "cvb", [128, SC])
    mcT = P.sb(pfx + "mcT", [128, 4, SC], BF16)
    qmT = P.sb(pfx + "qmT", [128, 4, SC], BF16); kmT = P.sb(pfx + "kmT", [128, 4, SC], BF16)
    rk_tok = P.sb(pfx + "rk_tok", [128, 256], BF16); km_tok = P.sb(pfx + "km_tok", [128, 512], BF16)
    vpR = P.sb(pfx + "vpR", [128, 4, 128], BF16); vpM = P.sb(pfx + "vpM", [128, 4, 129], BF16)
    g8 = P.sb(pfx + "g8", [128, 8]); L1 = P.sb(pfx + "L1", [128, 4], F32R); e1 = P.sb(pfx + "e1", [128, 4])
    igt = P.sb(pfx + "igt", [128, 4]); aM = P.sb(pfx + "aM", [128, 4]); bM = P.sb(pfx + "bM", [128, 4]); eM = P.sb(pfx + "eM", [128, 4])
    tmp4 = P.sb(pfx + "tmp4", [128, 4])
    Pm = [P.sb(pfx + "Pm%d" % i, [128, 128], BF16) for i in range(2)]
    ot = [P.sb(pfx + "ot%d" % i, [128, 129]) for i in range(2)]
    hh = [P.sb(pfx + "hh%d" % i, [128, 128]) for i in range(2)]
    dn = P.sb(pfx + "dn", [128, 1]); st6 = P.sb(pfx + "st6", [128, 6]); mv = P.sb(pfx + "mv", [128, 2]); rs = P.sb(pfx + "rs", [128, 1])
    if full:
        yR = P.sb(pfx + "yR", [128, 512]); yM = P.sb(pfx + "yM", [128, 512])
        rg = P.sb(pfx + "rg", [128, 512]); mo = P.sb(pfx + "mo", [128, 512]); mct = P.sb(pfx + "mct", [128, 512])
        yTo = [P.sb(pfx + "yTo%d" % i, [128, 4, 128], BF16) for i in range(2)]
        ybf = P.sb(pfx + "ybf", [128, 512], BF16)
    nps = PSRot(C, [0, 1, 2, 3, 4, 5, 6, 7])
    P.dma('sp', xts[0].v, xprev_d)
    x_transpose(P, C, xts[0].v, [(xT[:, :, 0:128], 'act')], [nps(), nps()])
    for c in range(4):
        pz = nps()
        for kc in range(8):
            P.mm(pz[:, 0:128], W[:, kc, OFF['m_x'] + c * 128: OFF['m_x'] + (c + 1) * 128], xT[:, kc, 0:128],
                 start=(kc == 0), stop=(kc == 7))
        P.copy('dve', mxT[:, c, 0:3], pz[:, 125:128])
    lnscale = float(np.log(128 ** -0.5))
    for sc in range(NT // SC):
        for tt in range(4):
            t = sc * 4 + tt
            xt = xts[t % 2]
            P.dma('sp', xt.v, x_d[t * 128:(t + 1) * 128, :])
            x_transpose(P, C, xt.v, [(xT[:, :, tt * 128:(tt + 1) * 128], 'act')], [nps(), nps()])
        for (dst, off, nch, kind) in ((rqT, OFF['r_q'], 2, 'bf'), (rkT, OFF['r_k'], 2, 'bf'), (mxT, OFF['m_x'], 4, 'mx')):
            if not full and (dst is rqT or dst is rkT):
                continue
            for c in range(nch):
                pz = nps()
                for kc in range(8):
                    P.mm(pz.v, W[:, kc, off + c * 128: off + (c + 1) * 128], xT[:, kc, :], start=(kc == 0), stop=(kc == 7))
                if kind == 'bf':
                    P.copy('act', dst[:, c, :], pz.v)
                else:
                    P.copy('act', mxT[:, c, 3:3 + SC], pz.v)
                    P.copy('dve', mxb[:, c, :], pz.v)
        for c in range(4):
            P.ts('dve', cva.v, mxT[:, c, 3:3 + SC], cwT[:, 12 + c:13 + c], cbT[:, c:c + 1], op0=ALU.mult, op1=ALU.add)
            P.stt('dve', cvb.v, mxT[:, c, 2:2 + SC], cwT[:, 8 + c:9 + c], cva.v, ALU.mult, ALU.add)
            P.stt('dve', cva.v, mxT[:, c, 1:1 + SC], cwT[:, 4 + c:5 + c], cvb.v, ALU.mult, ALU.add)
            P.stt('dve', cvb.v, mxT[:, c, 0:SC], cwT[:, c:c + 1], cva.v, ALU.mult, ALU.add)
            P.act(mcT[:, c, :], cvb.v, AF.Silu)
            P.copy('dve', cva[:, 0:3], mxT[:, c, SC:SC + 3])
            P.copy('dve', mxT[:, c, 0:3], cva[:, 0:3])
        for (dst, bd) in ((qmT, BD["bdq"]), (kmT, BD["bdk"])):
            if not full:
                continue
            for h in range(4):
                pz = nps()
                P.mm(pz.v, bd[:, h, :], mcT[:, h, :])
                P.copy('act', dst[:, h, :], pz.v)
        for tt in range(4):
            t = sc * 4 + tt
            ts_ = slice(tt * 128, (tt + 1) * 128)

            def tok_proj(off, n):
                pz = nps()
                for kc in range(8):
                    P.mm(pz[:, 0:n], xT[:, kc, ts_], W[:, kc, off:off + n], start=(kc == 0), stop=(kc == 7))
                return pz
            p_rk = tok_proj(OFF['r_k'], 256)
            P.copy('act', rk_tok.v, p_rk[:, 0:256])
            p_g8 = tok_proj(OFF['m_i'], 8)
            P.copy('dve', g8.v, p_g8[:, 0:8])
            P.tt('dve', igt.v, g8[:, 0:4], biB.v, ALU.add)
            P.tt('dve', tmp4.v, g8[:, 4:8], bfB.v, ALU.add)
            P.act(e1.v, tmp4.v, AF.Exp, scale=-1.0)
            P.ts('dve', e1.v, e1.v, 1.0, None, op0=ALU.add)
            P.act(L1.v, e1.v, AF.Ln)
            pc = nps()
            P.mm(pc[:, 0:4], maskr.v, L1.v)
            P.mm(pc[:, 8:12], onesr.v, L1.v)
            P.act(aM.v, pc[:, 0:4], AF.Exp, scale=-1.0, bias=lnscale)
            P.tt('dve', tmp4.v, igt.v, pc[:, 0:4], ALU.add)
            P.act(bM.v, tmp4.v, AF.Exp)
            P.act(eM.v, pc[:, 8:12], AF.Exp, scale=-1.0)
            P.tt('dve', totacc.v, totacc.v, pc[:, 8:12], ALU.add)
            p_rv = tok_proj(OFF['r_v'], 512)
            for h in range(4):
                P.act(vpR[:, h, :], p_rv[:, h * 128:(h + 1) * 128], AF.Copy, scale=bR[:, h:h + 1])
            p_vm = nps()
            for h in range(4):
                P.mm(p_vm[:, h * 128:(h + 1) * 128], mxb[:, h, ts_], BD["bdv"][:, h, :])
            for h in range(4):
                P.act(vpM[:, h, 0:128], p_vm[:, h * 128:(h + 1) * 128], AF.Copy, scale=bM[:, h:h + 1])
            P.copy('dve', vpM[:, :, 128:129], bM.v.re("p (h o) -> p h o", o=1))
            p_km = nps()
            for h in range(4):
                P.mm(p_km[:, h * 128:(h + 1) * 128], mcT[:, h, ts_], BD["bdk"][:, h, :])
            P.copy('act', km_tok.v, p_km.v)
            if full:
                p_rg = tok_proj(OFF['r_g'], 512)
                P.act(rg.v, p_rg.v, AF.Silu)
                p_mo = tok_proj(OFF['m_o'], 512)
                P.act(mo.v, p_mo.v, AF.Sigmoid)
                p_mc = nps()
                pmb = p_mc.v.bitcast(BF16)
                for c in range(4):
                    P.tr(pmb[:, c * 128:(c + 1) * 128], mcT[:, c, ts_], C.identb.v)
                P.tt('dve', mct.v, pmb[:, 0:512], skM.v, ALU.mult)
            kq = 0
            for h in range(4):
                psl = slice((h % 2) * 64, (h % 2) * 64 + 64); hp = h // 2
                if full:
                    p_st = nps()
                    P.mm(p_st[:, 0:128], rkT[psl, hp, ts_], rqT[psl, hp, ts_])
                    pm = Pm[kq % 2]; o = ot[kq % 2]; hx = hh[kq % 2]; kq += 1
                    P.tt('dve', pm.v, p_st[:, 0:128], mask.v, ALU.mult)
                    p_o = nps()
                    P.mm(p_o[:, 0:128], pm.v, vpR[:, h, :], start=True, stop=False)
                    P.mm(p_o[:, 0:128], rqT[psl, hp, ts_], Sretb[psl, hp, :], start=False, stop=True)
                    P.act(o[:, 0:128], p_o[:, 0:128], AF.Copy, scale=aR[:, h:h + 1])
                    head_norm(P, o[:, 0:128], hx.v, st6, mv, rs)
                    P.tt('pool', hx.v, hx.v, gnR[:, h * 128:(h + 1) * 128], ALU.mult)
                    P.tt('pool', yR[:, h * 128:(h + 1) * 128], hx.v, rg[:, h * 128:(h + 1) * 128], ALU.mult)
                p_kv = nps()
                P.mm(p_kv[:, 0:128], rk_tok[:, hp * 128:(hp + 1) * 128], vpR[:, h, :])
                P.tt('dve', tmpS[psl, hp, 0:128], Sret[psl, hp, :], p_kv[psl, 0:128], ALU.add)
                P.act(Sret[psl, hp, :], tmpS[psl, hp, 0:128], AF.Copy, scale=eR[psl, hp:hp + 1])
                P.copy('dve', Sretb[psl, hp, :], Sret[psl, hp, :])
            for h in range(4):
                if full:
                    p_st = nps()
                    P.mm(p_st[:, 0:128], kmT[:, h, ts_], qmT[:, h, ts_])
                    pm = Pm[kq % 2]; o = ot[kq % 2]; hx = hh[kq % 2]; kq += 1
                    P.tt('dve', pm.v, p_st[:, 0:128], mask.v, ALU.mult)
                    p_o = nps()
                    P.mm(p_o[:, 0:129], pm.v, vpM[:, h, :], start=True, stop=False)
                    P.mm(p_o[:, 0:129], qmT[:, h, ts_], Cmlb[:, h, :], start=False, stop=True)
                    P.act(o.v, p_o[:, 0:129], AF.Copy, scale=aM[:, h:h + 1])
                    P.act(dn.v, o[:, 128:129], AF.Abs)
                    P.ts('dve', dn.v, dn.v, 1.0, None, op0=ALU.max)
                    a_ = dn.v.ap
                    P.I('dve', (lambda a_: (lambda e: e.reciprocal(a_, a_)))(a_), w=[dn], r=[dn])
                    P.act(o[:, 0:128], o[:, 0:128], AF.Copy, scale=dn.v)
                    head_norm(P, o[:, 0:128], hx.v, st6, mv, rs)
                    P.tt('pool', hx.v, hx.v, gnM[:, h * 128:(h + 1) * 128], ALU.mult)
                    P.tt('pool', hx.v, hx.v, mct[:, h * 128:(h + 1) * 128], ALU.add)
                    P.tt('pool', yM[:, h * 128:(h + 1) * 128], hx.v, mo[:, h * 128:(h + 1) * 128], ALU.mult)
                p_kv = nps()
                P.mm(p_kv[:, 0:129], km_tok[:, h * 128:(h + 1) * 128], vpM[:, h, :])
                P.tt('dve', tmpS[:, h, :], Cml[:, h, :], p_kv[:, 0:129], ALU.add)
                P.act(Cml[:, h, :], tmpS[:, h, :], AF.Copy, scale=eM[:, h:h + 1])
                P.copy('dve', Cmlb[:, h, :], Cml[:, h, :])
            if full:
                for (ysrc, ydst) in ((yR, outs[0]), (yM, outs[1])):
                    P.copy('act', ybf.v, ysrc.v)
                    pz = nps(); pzb = pz.v.bitcast(BF16)
                    for c in range(4):
                        P.tr(pzb[:, c * 128:(c + 1) * 128], ybf[:, c * 128:(c + 1) * 128], C.identb.v)
                    yo = yTo[kq % 2]; kq += 1
                    P.copy('dve', yo.v, pzb[:, 0:512].re("p (c t) -> p c t", c=4))
                    P.dma('sp', ydst[:, :, t * 128:(t + 1) * 128], yo.v)
    if not full:
        P.dma('sp', outs[0].v, Sret.v)
        P.dma('sp', outs[1].v, Cml.v)
        P.dma('sp', outs[2].v, totacc.v)


def head_norm(P, src, dst, st6, mv, rs):
    a, b = st6.v.ap, src.ap
    P.I('dve', lambda e: e.bn_stats(a, b), w=[st6], r=[src])
    a2, b2 = mv.v.ap, st6.v.ap
    P.I('dve', lambda e: e.bn_aggr(a2, b2), w=[mv], r=[st6])
    P.ts('dve', rs.v, mv[:, 1:2], EPS, None, op0=ALU.add)
    P.act(rs.v, rs.v, AF.Ln)
    P.act(rs.v, rs.v, AF.Exp, scale=-0.5)
    P.ts('dve', dst, src, mv[:, 0:1], rs.v, op0=ALU.subtract, op1=ALU.mult)


ATT_PAT = ((128, 1), (512, 4), (2048, 16))
HALO = 2048


def host_consts_attn():
    slopes = np.exp2(-8.0 * np.arange(1, 13, dtype=np.float64) / 12.0).reshape(3, 4)
    s = np.arange(128)[:, None]; i = np.arange(128)[None, :]
    out = np.zeros((3, 2, 128, 4, 128), dtype=np.float32)
    for g, (win, d) in enumerate(ATT_PAT):
        for h in range(4):
            dcur = i - s
            b = np.where((dcur >= 0), -slopes[g, h] * d * dcur, -30000.0)
            out[g, 1, :, h, :] = b
            dprev = i + 128 - s
            b = np.where((dprev <= 128), -slopes[g, h] * d * dprev, -30000.0)
            out[g, 0, :, h, :] = b
    return out.reshape(3, 2, 128, 512)


def pass_attn(P, C, NT, xext_d, w_in_l, bias_d, hv_d, yT_out, pfx="a", dbg=0):
    accN = P.sb(pfx + "accN", [128, 4, NT]); accD = P.sb(pfx + "accD", [128, 4, NT])
    for h_ in range(4):
        for j_ in range(NT // 2048):
            P.memset('dve', accN[:, h_, j_ * 2048:(j_ + 1) * 2048], 0.0 if dbg == 0 else 1.0)
            P.memset('dve', accD[:, h_, j_ * 2048:(j_ + 1) * 2048], 0.0 if dbg == 0 else 2.0)
    hv0 = P.sb(pfx + "hv0", [128, 128]); hvb = P.sb(pfx + "hvb", [128, 128], BF16)
    P.dma('act', hv0.v, hv_d); P.copy('dve', hvb.v, hv0.v)
    Wq = P.sb(pfx + "Wq", [128, 8, 256], BF16); Wk = P.sb(pfx + "Wk", [128, 8, 256], BF16); Wv = P.sb(pfx + "Wv", [128, 8, 512], BF16)
    bT = [P.sb(pfx + "bT%d" % i, [128, 512]) for i in range(2)]
    xts = [P.sb(pfx + "xt%d" % i, [128, 1024]) for i in range(2)]
    xTb = [P.sb(pfx + "xTb%d" % i, [128, 8, 128], BF16) for i in range(2)]
    kT = [P.sb(pfx + "kT%d" % i, [128, 2, 128], BF16) for i in range(4)]
    Vt = [P.sb(pfx + "V%d" % i, [128, 512], BF16) for i in range(4)]
    qT = [P.sb(pfx + "qT%d" % i, [128, 2, 128], BF16) for i in range(2)]
    tmp = [P.sb(pfx + "tmp%d" % i, [128, 512]) for i in range(2)]
    PT = [[P.sb(pfx + "PT%d_%d" % (i, j), [128, 512], BF16) for j in range(2)] for i in range(2)]
    nps = PSRot(C, [0, 1, 2, 3, 4, 5, 6, 7])
    win = w_in_l.rearrange("(kc p) c -> p kc c", p=128)
    nb = 0
    for g, (_, d) in enumerate(ATT_PAT if dbg not in (1, 2, 4, 5, 6) else (ATT_PAT[:1] if dbg in (2, 4, 5, 6) else ())):
        load_w_cast(P, Wq.v, win[:, :, OFF['a_q'] + g * 256: OFF['a_q'] + (g + 1) * 256])
        load_w_cast(P, Wk.v, win[:, :, OFF['a_k'] + g * 256: OFF['a_k'] + (g + 1) * 256])
        load_w_cast(P, Wv.v, win[:, :, OFF['a_v'] + g * 512: OFF['a_v'] + (g + 1) * 512])
        P.dma('act', bT[0].v, bias_d[g, 0]); P.dma('act', bT[1].v, bias_d[g, 1])
        NB = NT // (128 * d)
        blocks = [(r, m) for r in range(d) for m in range(-1, NB)]
        nblk = len(blocks)

        def Pst(i):
            r, m = blocks[i]
            s0 = HALO + m * 128 * d + r
            xt = xts[i % 2]; xb = xTb[i % 2]
            P.dma('sp', xt.v, xext_d[s0: s0 + 127 * d + 1: d, :])
            x_transpose(P, C, xt.v, [(xb.v, 'act')], [nps(), nps()])
            for c in range(2):
                pz = nps()
                for kc in range(8):
                    P.mm(pz[:, 0:128], Wk[:, kc, c * 128:(c + 1) * 128], xb[:, kc, :], start=(kc == 0), stop=(kc == 7))
                P.copy('act', kT[i % 4][:, c, :], pz[:, 0:128])
            pz = nps()
            for kc in range(8):
                P.mm(pz.v, xb[:, kc, :], Wv[:, kc, :], start=(kc == 0), stop=(kc == 7))
            P.copy('act', Vt[i % 4].v, pz.v)
            if m < 0:
                return
            for c in range(2):
                pz = nps()
                for kc in range(8):
                    P.mm(pz[:, 0:128], Wq[:, kc, c * 128:(c + 1) * 128], xb[:, kc, :], start=(kc == 0), stop=(kc == 7))
                P.copy('act', qT[i % 2][:, c, :], pz[:, 0:128])

        def Sst(i):
            r, m = blocks[i]
            if m < 0:
                return
            for pc, kb in ((0, (i - 1) % 4), (1, i % 4)):
                psAB = [nps(), nps()]
                for h in range(4):
                    psl = slice((h % 2) * 64, (h % 2) * 64 + 64); hp = h // 2
                    P.mm(psAB[h % 2][:, hp * 128:(hp + 1) * 128], kT[kb][psl, hp, :], qT[i % 2][psl, hp, :])
                tm = tmp[pc]
                for h in range(4):
                    hs = slice(h * 128, (h + 1) * 128); hp = h // 2
                    P.stt('dve', tm[:, hs], psAB[h % 2][:, hp * 128:(hp + 1) * 128], 0.125, bT[pc][:, hs], ALU.mult, ALU.add)
                P.act(PT[i % 2][pc].v, tm.v, AF.Exp)

        def Nst(i):
            r, m = blocks[i]
            if m < 0:
                return
            pn = nps()
            for h in range(4):
                hs = slice(h * 128, (h + 1) * 128)
                P.mm(pn[:, hs], Vt[(i - 1) % 4][:, hs], PT[i % 2][0][:, hs], start=True, stop=False)
                P.mm(pn[:, hs], Vt[i % 4][:, hs], PT[i % 2][1][:, hs], start=False, stop=True)
            pd = nps()
            P.mm(pd.v, (hvb.v if m == 0 else C.onesb.v), PT[i % 2][0].v, start=True, stop=False)
            P.mm(pd.v, C.onesb.v, PT[i % 2][1].v, start=False, stop=True)
            t0 = m * 128 * d + r
            for h in range(4):
                hs = slice(h * 128, (h + 1) * 128)
                av = accN[:, h, t0: t0 + 127 * d + 1: d]
                P.tt('dve', av, av, pn[:, hs], ALU.add)
                dv = accD[:, h, t0: t0 + 127 * d + 1: d]
                P.tt('dve', dv, dv, pd[:, hs], ALU.add)

        Pst(0)
        for i in range(nblk):
            if i + 1 < nblk:
                Pst(i + 1)
            Sst(i)
            if i >= 1:
                Nst(i - 1)
        Nst(nblk - 1)
    yo = [P.sb(pfx + "yo%d" % i, [128, 4, 512], BF16) for i in range(2)]
    rc = [P.sb(pfx + "rc%d" % i, [128, 4, 512]) for i in range(2)]
    for j in range(NT // 512):
        sl = slice(j * 512, (j + 1) * 512)
        for h in range(4):
            a_, b_ = rc[j % 2][:, h, :].ap, accD[:, h, sl].ap
            P.I('dve', (lambda a_, b_: (lambda e: e.reciprocal(a_, b_)))(a_, b_), w=[rc[j % 2]], r=[accD])
            P.tt('dve', yo[j % 2][:, h, :], accN[:, h, sl], rc[j % 2][:, h, :], ALU.mult)
        P.dma('sp', yT_out[:, :, sl], yo[j % 2].v)


NCORES = 8
NT_CORE = 4096
PRM_NAMES = ("ret_gn", "ml_conv_w", "ml_conv_b", "bdq", "bdk", "bdv", "ml_bi", "ml_bf", "ml_gn", "ml_skip")
PRM_SHAPES = dict(ret_gn=[512], ml_conv_w=[4, 512], ml_conv_b=[512], bdq=[4, 128, 128], bdk=[4, 128, 128], bdv=[4, 128, 128],
                  ml_bi=[4], ml_bf=[4], ml_gn=[512], ml_skip=[512])
CST_SHAPES = dict(aR=[128, 4], bR=[128, 4], eR=[128, 2], gR=[128, 2], mask=[128, 128])


def _scan_inputs(nc):
    def inp(n, sh):
        return nc.dram_tensor(n, sh, F32, kind="ExternalInput").ap()
    prm = {n: inp(n, PRM_SHAPES[n]) for n in PRM_NAMES}
    cst = {n: inp(n, CST_SHAPES[n]) for n in CST_SHAPES}
    return prm, cst


def build_A(NT=NT_CORE):
    nc = bass.Bass("TRN2", target_bir_lowering=False)
    with ExitStack() as es:
        P = Prog(nc, es)
        x_d = P.dram("x", [NT, D], F32, kind="ExternalInput")
        xprev = P.dram("xprev", [128, D], F32, kind="ExternalInput")
        w_in = nc.dram_tensor("w_in", [D, D_IN], F32, kind="ExternalInput").ap()
        prm, cst = _scan_inputs(nc)
        outs = [P.dram("oS", [128, 2, 128], F32, kind="ExternalOutput"), P.dram("oC", [128, 4, 129], F32, kind="ExternalOutput"),
                P.dram("oT", [128, 4], F32, kind="ExternalOutput")]
        C = make_ctx(P)
        pass_scan(P, C, NT, x_d, xprev, w_in, prm, cst, None, outs, mode="summary", pfx="s")
        P.wait_all('sp', outs)
        P.emit()
    return nc


def build_B(NT=NT_CORE, NE=NEXP):
    nc = bass.Bass("TRN2", target_bir_lowering=False)
    with ExitStack() as es:
        P = Prog(nc, es)

        def inp(n, sh):
            return nc.dram_tensor(n, sh, F32, kind="ExternalInput").ap()
        xext = P.dram("xext", [HALO + NT, D], F32, kind="ExternalInput")
        w_in = inp("w_in", [D, D_IN])
        prm, cst = _scan_inputs(nc)
        init = dict(Fret=inp("Fret", [3, 128, 2, 128]), Fml=inp("Fml", [3, 128, 4, 129]), totL=inp("totL", [3, 128, 4]),
                    sel=inp("sel", [128, 3]), nsel=inp("nsel", [128, 3]))
        abias = inp("abias", [3, 2, 128, 512]); hv = inp("hv", [128, 128])
        w_br = inp("w_branch", [3, MIXW, D]); w_out = inp("w_out", [D, D])
        ln1_g = inp("ln1_g", [D]); ln1_b = inp("ln1_b", [D]); ln2_g = inp("ln2_g", [D]); ln2_b = inp("ln2_b", [D])
        w_r = inp("w_router", [D, NE]); b_r = inp("b_router", [NE])
        w_g = inp("w_gate", [NE, D, D]); b_g = inp("b_gate", [NE, D])
        w_u = inp("w_up", [NE, D, D]); b_u = inp("b_up", [NE, D])
        w_d = inp("w_down", [NE, D, D]); b_d = inp("b_down", [NE, D])
        out = P.dram("out", [NT, D], F32, kind="ExternalOutput")
        yT = [P.dram("yT%d" % b, [128, 4, NT], BF16) for b in range(3)]
        x1s = P.dram("x1s", [NT, D], F32)
        C = make_ctx(P)
        x_own = Tile(P, xext.h[HALO:HALO + NT, :], "xown", "dram")
        x_own.lw, x_own.rd = xext.lw, xext.rd
        xprev = Tile(P, xext.h[HALO - 128:HALO, :], "xprv", "dram")
        xprev.lw, xprev.rd = xext.lw, xext.rd
        with P.scope():
            pass_attn(P, C, NT, xext, w_in, abias, hv, yT[2], pfx="a")
        with P.scope():
            pass_scan(P, C, NT, x_own, xprev, w_in, prm, cst, init, [yT[0], yT[1]], mode="full", pfx="s")
        with P.scope():
            pass_merge(P, C, NT, x_own, yT, w_in, w_br, w_out, ln1_g, ln1_b, x1s, pfx="m")
        NBLK = NT * 4 // 128 + NE
        Xs = P.dram("Xs", [NBLK * 128, D], BF16); Ys = P.dram("Ys", [NBLK * 128, D], F32)
        mc = {k: inp("mc_" + k, list(v.shape)) for k, v in host_consts_moe(NT, NE).items()}
        with P.scope():
            pass_moe2(P, C, NT, x1s, w_r, b_r, w_g, b_g, w_u, b_u, w_d, b_d, ln2_g, ln2_b, out, Xs, Ys, mc, NE=NE, pfx="f")
        P.wait_all('sp', [out])
        P.emit()
    return nc


def layer_params(inputs, l):
    g = lambda n: np.ascontiguousarray(np.asarray(inputs[n], dtype=np.float32)[l])
    prm = dict(ret_gn=g("ret_gn"), ml_conv_w=g("ml_conv_w"), ml_conv_b=g("ml_conv_b"),
               bdq=host_blockdiag(g("ml_wq")), bdk=host_blockdiag(g("ml_wk")), bdv=host_blockdiag(g("ml_wv")),
               ml_bi=g("ml_bi"), ml_bf=g("ml_bf"), ml_gn=g("ml_gn"), ml_skip=g("ml_skip"))
    big = dict(w_in=g("w_in"), w_branch=g("w_branch"), w_out=g("w_out"), ln1_g=g("ln1_g"), ln1_b=g("ln1_b"),
               ln2_g=g("ln2_g"), ln2_b=g("ln2_b"), w_router=g("w_router"), b_router=g("b_router"),
               w_gate=g("w_gate"), b_gate=g("b_gate"), w_up=g("w_up"), b_up=g("b_up"), w_down=g("w_down"), b_down=g("b_down"))
    return prm, big


def run_layer(ncA, ncB, xs, prm, big, cst, abias, QPB, NT):
    n = len(xs)
    zeros128 = np.zeros((128, D), np.float32)
    inA = []
    for c in range(n):
        q = c % QPB
        m = dict(prm); m.update(cst)
        m["w_in"] = big["w_in"]; m["x"] = xs[c]
        m["xprev"] = np.ascontiguousarray(xs[c - 1][-128:]) if q > 0 else zeros128
        inA.append(m)
    resA = run_bass_kernel_spmd(ncA, inA, core_ids=list(range(n))).results
    inB = []
    for c in range(n):
        q = c % QPB; b0 = c - q
        m = dict(prm); m.update(cst); m.update(big)
        halo = xs[c - 1][-HALO:] if q > 0 else np.zeros((HALO, D), np.float32)
        m["xext"] = np.ascontiguousarray(np.concatenate([halo, xs[c]], axis=0))
        Fret = np.zeros((3, 128, 2, 128), np.float32); Fml = np.zeros((3, 128, 4, 129), np.float32)
        totL = np.zeros((3, 128, 4), np.float32); sel = np.zeros((128, 3), np.float32)
        for qq in range(min(3, QPB)):
            Fret[qq] = resA[b0 + qq]["oS"]; Fml[qq] = resA[b0 + qq]["oC"]; totL[qq] = resA[b0 + qq]["oT"]
            if qq < q:
                sel[:, qq] = 1.0
        m.update(Fret=Fret, Fml=Fml, totL=totL, sel=sel, nsel=(1.0 - sel).astype(np.float32))
        m["abias"] = abias
        for k_, v_ in host_consts_moe(NT, big["w_router"].shape[1]).items():
            m["mc_" + k_] = v_
        m["hv"] = (np.ones((128, 128), np.float32) if q > 0 else np.zeros((128, 128), np.float32))
        inB.append(m)
    resB = run_bass_kernel_spmd(ncB, inB, core_ids=list(range(n))).results
    return [np.asarray(r["out"], dtype=np.float32) for r in resB]


def kernel(**inputs):
    x = np.asarray(inputs["x"], dtype=np.float32)
    B, S, _ = x.shape
    QPB = NCORES // B
    NT = S // QPB
    xs = [np.ascontiguousarray(x[c // QPB, (c % QPB) * NT:(c % QPB + 1) * NT]) for c in range(NCORES)]
    ncA = build_A(NT); ncB = build_B(NT)
    cst = host_consts_scan(NT); abias = host_consts_attn()
    L = np.asarray(inputs["w_in"]).shape[0]
    for l in range(L):
        prm, big = layer_params(inputs, l)
        xs = run_layer(ncA, ncB, xs, prm, big, cst, abias, QPB, NT)
    out = np.zeros((B, S, D), np.float32)
    for c in range(NCORES):
        out[c // QPB, (c % QPB) * NT:(c % QPB + 1) * NT] = xs[c]
    return out


BIGIDX = 4000000.0


def host_consts_moe(NT, NE=NEXP):
    NBLK = NT * 4 // 128 + NE
    p = np.arange(128, dtype=np.float32)
    d = dict(iota_p=p.reshape(128, 1).copy(),
             ustrict=(p[:, None] < p[None, :]).astype(np.float32),
             iotaJ=np.tile(np.arange(33, dtype=np.float32)[None, :], (128, 1)),
             iotaB=np.tile(np.arange(NBLK, dtype=np.float32)[None, :], (128, 1)))
    return d


def _breg(P, e, bound):
    if not hasattr(P, "_bregs"):
        P._bregs = {}
    if bound not in P._bregs:
        P._bregs[bound] = e.to_reg(int(bound))
    return P._bregs[bound]


def ind_dma(P, dst_v, src_tile, src_ap, idx_v, bound, scatter=False, extra_r=()):
    sb_tile = dst_v.tile
    key = P._tsem(sb_tile)
    d_ap, i_ap = dst_v.ap, idx_v.ap
    if not scatter:
        rt = [src_tile, idx_v.tile] + list(extra_r); wt = [sb_tile]

        def f(e):
            try:
                return e.indirect_dma_start(d_ap, None, src_ap, bass.IndirectOffsetOnAxis(i_ap, 0), bounds_check=_breg(P, e, bound), oob_is_err=False)
            except Exception:
                print("IND GATHER FAIL", d_ap, src_ap, i_ap, bound)
                raise
    else:
        rt = [sb_tile, idx_v.tile] + list(extra_r); wt = [src_tile]

        def f(e):
            try:
                return e.indirect_dma_start(src_ap, bass.IndirectOffsetOnAxis(i_ap, 0), d_ap, None, bounds_check=_breg(P, e, bound), oob_is_err=False)
            except Exception:
                print("IND SCATTER FAIL", d_ap, src_ap, i_ap, bound)
                raise
    waits = P._waits('pool', rt, wt)
    sb_tile.dcnt += 16
    P.stream['pool'].append((waits, f, (key, 16)))
    P._record((key, sb_tile.dcnt), rt, wt)
    P.ninst += 1


def pass_moe2(P, C, NT, x1_d, w_router, b_router, w_gate, b_gate, w_up, b_up, w_down, b_down, ln_g, ln_b, out_d,
              Xs, Ys, mc, NE=NEXP, pfx="f", dbg=0):
    NTL = NT // 128
    NBLK = NT * 4 // 128 + NE
    NSLOT = NBLK * 128
    gB = P.sb(pfx + "gB", [128, 1024]); bB = P.sb(pfx + "bB", [128, 1024])
    bcast_rows(P, gB.v, ln_g); bcast_rows(P, bB.v, ln_b)
    wr = P.sb(pfx + "wr", [128, 8, NE], F32R); wr0 = P.sb(pfx + "wr0", [128, 8, NE])
    P.dma('act', wr0.v, w_router.rearrange("(kc p) e -> p kc e", p=128)); P.copy('dve', wr.v, wr0.v)
    brB = P.sb(pfx + "brB", [128, NE]); bcast_rows(P, brB.v, b_router)
    c1 = P.sb(pfx + "c1", [128, 1]); P.memset('dve', c1.v, 1.0)
    iop = P.sb(pfx + "iop", [128, 1]); P.dma('act', iop.v, mc["iota_p"])
    us0 = P.sb(pfx + "us0", [128, 128]); P.dma('act', us0.v, mc["ustrict"])
    usb = P.sb(pfx + "usb", [128, 128], BF16); P.copy('dve', usb.v, us0.v)
    ioJ = P.sb(pfx + "ioJ", [128, 33]); P.dma('act', ioJ.v, mc["iotaJ"])
    ioB = P.sb(pfx + "ioB", [128, NBLK]); P.dma('act', ioB.v, mc["iotaB"])
    lg_all = P.sb(pfx + "lg_all", [128, NTL, NE]); t8_all = P.sb(pfx + "t8_all", [128, NTL, 8])
    pall = P.sb(pfx + "pall", [128, NTL, NE]); pos_all = P.sb(pfx + "pos_all", [128, NTL, NE])
    slot_f = P.sb(pfx + "slot_f", [128, NTL, 4]); slot_i = P.sb(pfx + "slot_i", [128, NTL * 4], I32)
    pk_all = P.sb(pfx + "pk_all", [128, NTL, 4])
    carry = P.sb(pfx + "carry", [128, NE]); P.memset('dve', carry.v, 0.0)
    xts = [P.sb(pfx + "xt%d" % i, [128, 1024]) for i in range(2)]
    xT32 = P.sb(pfx + "xT32", [128, 8, 128], F32R)
    msk = P.sb(pfx + "msk", [128, NE]); mskb = P.sb(pfx + "mskb", [128, NE], BF16)
    ex = P.sb(pfx + "ex", [128, NE]); sm = P.sb(pfx + "sm", [128, 1]); nmx = P.sb(pfx + "nmx", [128, 1])
    nps = PSRot(C, [0, 1, 2, 3, 4, 5, 6, 7])
    for t in range(NTL):
        xt = xts[t % 2]
        P.dma('sp', xt.v, x1_d[t * 128:(t + 1) * 128, :])
        x_transpose(P, C, xt.v, [(xT32.v, 'dve')], [nps(), nps()])
        pl = nps()
        for kc in range(8):
            P.mm(pl[:, 0:NE], xT32[:, kc, :], wr[:, kc, :], start=(kc == 0), stop=(kc == 7))
        lg = lg_all[:, t, :]; t8 = t8_all[:, t, :]
        P.tt('dve', lg, pl[:, 0:NE], brB.v, ALU.add)
        a, b = t8.ap, lg.ap
        P.I('dve', (lambda a, b: (lambda e: e.max(out=a, in_=b)))(a, b), w=[t8_all], r=[lg_all])
        P.ts('dve', msk.v, lg, t8_all[:, t, 3:4], c1.v, op0=ALU.is_ge, op1=ALU.mult)
        P.ts('dve', nmx.v, t8_all[:, t, 0:1], -1.0, None, op0=ALU.mult)
        P.act(ex.v, lg, AF.Exp, bias=nmx.v)
        P.tt('dve', ex.v, ex.v, msk.v, ALU.mult)
        a2, b2 = sm.v.ap, ex.v.ap
        P.I('dve', (lambda a, b: (lambda e: e.reduce_sum(a, b, AX.X)))(a2, b2), w=[sm], r=[ex])
        a3 = sm.v.ap
        P.I('dve', (lambda a: (lambda e: e.reciprocal(a, a)))(a3), w=[sm], r=[sm])
        P.ts('dve', pall[:, t, :], ex.v, sm.v, c1.v, op0=ALU.mult, op1=ALU.mult)
        P.copy('dve', mskb.v, msk.v)
        pr = nps()
        P.mm(pr[:, 0:NE], usb.v, mskb.v)
        P.mm(pr[:, 64:64 + NE], C.onesb.v, mskb.v)
        P.tt('dve', pos_all[:, t, :], carry.v, pr[:, 0:NE], ALU.add)
        P.tt('dve', carry.v, carry.v, pr[:, 64:64 + NE], ALU.add)
    q = P.sb(pfx + "q", [128, NE]); nb = P.sb(pfx + "nb", [128, NE]); bend = P.sb(pfx + "bend", [128, NE])
    pstart = P.sb(pfx + "pstart", [128, NE]); t33 = P.sb(pfx + "t33", [128, 33])
    P.ts('dve', q.v, carry.v, 1.0 / 128.0, None, op0=ALU.mult)
    for e in range(NE):
        P.ts('dve', t33.v, ioJ.v, q[:, e:e + 1], c1.v, op0=ALU.is_lt, op1=ALU.mult)
        a_, b_ = nb[:, e:e + 1].ap, t33.v.ap
        P.I('dve', (lambda a, b: (lambda e_: e_.reduce_sum(a, b, AX.X)))(a_, b_), w=[nb], r=[t33])
    P.copy('dve', bend[:, 0:1], nb[:, 0:1])
    for e in range(1, NE):
        P.tt('dve', bend[:, e:e + 1], bend[:, e - 1:e], nb[:, e:e + 1], ALU.add)
    P.tt('dve', pstart.v, bend.v, nb.v, ALU.subtract)
    P.ts('dve', pstart.v, pstart.v, 128.0, None, op0=ALU.mult)
    Eall = P.sb(pfx + "Eall", [128, NBLK]); tB = P.sb(pfx + "tB", [128, NBLK]); sk = P.sb(pfx + "sk", [128, NBLK])
    P.memset('dve', Eall.v, 0.0)
    for e in range(NE):
        P.ts('dve', tB.v, ioB.v, bend[:, e:e + 1], c1.v, op0=ALU.is_ge, op1=ALU.mult)
        P.tt('dve', Eall.v, Eall.v, tB.v, ALU.add)
    P.ts('dve', Eall.v, Eall.v, float(NE - 1), None, op0=ALU.min)
    P.memset('dve', sk.v, 0.0)
    P.tt('dve', sk[:, 2:NBLK], Eall[:, 2:NBLK], Eall[:, 0:NBLK - 2], ALU.is_equal)
    P.ts('dve', sk.v, sk.v, BIGIDX, None, op0=ALU.mult)
    idxWf = P.sb(pfx + "idxWf", [128, NBLK]); idxW = P.sb(pfx + "idxW", [128, 8 * NBLK], I32)
    idxBf = P.sb(pfx + "idxBf", [128, NBLK]); idxB = P.sb(pfx + "idxB", [128, NBLK], I32)
    P.tt('dve', idxBf.v, Eall.v, sk.v, ALU.add)
    P.copy('dve', idxB.v, idxBf.v)
    iop4 = P.sb(pfx + "iop4", [128, 1]); P.ts('dve', iop4.v, iop.v, 4.0, None, op0=ALU.mult)
    P.ts('dve', idxWf.v, Eall.v, 512.0, None, op0=ALU.mult)
    P.tt('dve', idxWf.v, idxWf.v, sk.v, ALU.add)
    P.ts('dve', idxWf.v, idxWf.v, iop4.v, c1.v, op0=ALU.add, op1=ALU.mult)
    for kq in range(4):
        P.ts('dve', tB.v, idxWf.v, float(kq), None, op0=ALU.add)
        P.copy('dve', idxW[:, kq * NBLK:(kq + 1) * NBLK], tB.v)
    OH = P.sb(pfx + "OH", [128, NBLK]); P.ts('dve', OH.v, Eall.v, iop.v, c1.v, op0=ALU.is_equal, op1=ALU.mult)
    if dbg == 2:
        return
    xb16 = [P.sb(pfx + "xb16_%d" % i, [128, 1024], BF16) for i in range(2)]
    tA = P.sb(pfx + "tA", [128, NE]); oh = P.sb(pfx + "oh", [128, NE]); pr1 = P.sb(pfx + "pr1", [128, NE])
    Xs2 = Xs.h[:]
    for t in range(NTL):
        xt = xts[t % 2]; xb = xb16[t % 2]
        P.dma('sp', xt.v, x1_d[t * 128:(t + 1) * 128, :])
        P.copy('act', xb.v, xt.v)
        P.tt('dve', tA.v, pos_all[:, t, :], pstart.v, ALU.add)
        for k in range(4):
            P.ts('dve', oh.v, lg_all[:, t, :], t8_all[:, t, k:k + 1], c1.v, op0=ALU.is_equal, op1=ALU.mult)
            P.tt('dve', pr1.v, oh.v, tA.v, ALU.mult)
            a_, b_ = slot_f[:, t, k:k + 1].ap, pr1.v.ap
            P.I('dve', (lambda a, b: (lambda e_: e_.reduce_sum(a, b, AX.X)))(a_, b_), w=[slot_f], r=[pr1])
            P.tt('dve', pr1.v, oh.v, pall[:, t, :], ALU.mult)
            a_, b_ = pk_all[:, t, k:k + 1].ap, pr1.v.ap
            P.I('dve', (lambda a, b: (lambda e_: e_.reduce_sum(a, b, AX.X)))(a_, b_), w=[pk_all], r=[pr1])
        P.copy('dve', slot_i[:, t * 4:(t + 1) * 4], slot_f[:, t, :])
        for k in range(4):
            ind_dma(P, xb.v, Xs, Xs2, slot_i[:, t * 4 + k: t * 4 + k + 1], NSLOT - 1, scatter=True)
    if dbg == 3:
        P.wait_all('sp', [Xs])
        return
    with P.scope():
        inv128 = P.sb(pfx + 'inv128', [128, 128], BF16); P.memset('dve', inv128.v, 1.0 / 128.0)
        Wq = [[[P.sb(pfx + "W%d_%d_%d" % (j, i, kq), [128, 2, 1024], BF16) for kq in range(4)] for j in range(3)] for i in range(2)]
        Wt = [[[Wq[i][j][kc // 2][:, kc % 2, :] for kc in range(8)] for j in range(3)] for i in range(2)]
        Ball = [P.sb(pfx + "Ball%d" % j, [NE, 1024], BF16) for j in range(3)]
        for j, bsrc_ in enumerate((b_gate, b_up, b_down)):
            P.dma('pool', Ball[j].v, bsrc_)
        OHb = [P.sb(pfx + "OHb%d" % i, [NE, 128], BF16) for i in range(2)]
        wsrc = [w_.rearrange("e (p kq r) f -> (e p kq) (r f)", p=128, kq=4, r=2) for w_ in (w_gate, w_up, w_down)]
        bsrc = [b_gate, b_up, b_down]
        wtile = [Tile(P, None, pfx + "wsrc%d" % j, "dram") for j in range(3)]
        xblk = [P.sb(pfx + "xblk%d" % i, [128, 1024], BF16) for i in range(2)]
        xbT = [P.sb(pfx + "xbT%d" % i, [128, 8, 128], BF16) for i in range(2)]
        actT = [P.sb(pfx + "actT%d" % i, [128, 8, 128], BF16) for i in range(2)]
        gt = [P.sb(pfx + "g%d" % i, [128, 512]) for i in range(2)]
        s_t = [P.sb(pfx + "s%d" % i, [128, 512]) for i in range(2)]
        ut = [P.sb(pfx + "u%d" % i, [128, 512]) for i in range(2)]
        atok = [P.sb(pfx + "atok%d" % i, [128, 1024], BF16) for i in range(2)]
        yb = [P.sb(pfx + "yb%d" % i, [128, 1024]) for i in range(2)]
        kkc = [0]

        def stageXg(b):
            cur = b % 2
            for j in range(3):
                for kq in range(4):
                    ind_dma(P, Wq[cur][j][kq].v.re("p r f -> p (r f)"), wtile[j], wsrc[j],
                            idxW[:, kq * NBLK + b: kq * NBLK + b + 1], NE * 512 - 1)
            Wg, Wu, Wd = Wt[cur]
            P.copy('dve', OHb[cur].v, OH[0:NE, b:b + 1].bc([NE, 128]))

        def stageXx(b):
            cur = b % 2
            xk = xblk[cur]
            if dbg != 9 or b < 2:
                P.dma('sp', xk.v, Xs[b * 128:(b + 1) * 128, :])
            xT_ = xbT[cur]
            for half in range(2):
                pz = nps(); pzb = pz.v.bitcast(BF16)
                for c in range(4):
                    kc = half * 4 + c
                    P.tr(pzb[:, c * 128:(c + 1) * 128], xk[:, kc:1024:8], C.identb.v)
                P.copy('act', xT_[:, half * 4:(half + 1) * 4, :], pzb[:, 0:512].re("p (c t) -> p c t", c=4))

        def stageG(b):
            cur = b % 2
            Wg, Wu, Wd = Wt[cur]
            xT_ = xbT[cur]
            aT = actT[cur]
            at = atok[cur]
            for hf in range(2 if dbg not in (6, 10) else 0):
                hs = slice(hf * 512, (hf + 1) * 512)
                pg = nps()
                for kc in range(8):
                    P.mm(pg.v, xT_[:, kc, :], Wg[kc][:, hs], start=(kc == 0), stop=False)
                P.mm(pg.v, OHb[cur].v, Ball[0][:, hs], start=False, stop=True)
                pu = nps()
                for kc in range(8):
                    P.mm(pu.v, xT_[:, kc, :], Wu[kc][:, hs], start=(kc == 0), stop=False)
                P.mm(pu.v, OHb[cur].v, Ball[1][:, hs], start=False, stop=True)
                g = gt[kkc[0] % 2]; s_ = s_t[kkc[0] % 2]; u = ut[kkc[0] % 2]; kkc[0] += 1
                P.ts('dve', g.v, pg.v, 7.0, None, op0=ALU.min)
                P.act(s_.v, g.v, AF.Sigmoid, scale=1.702)
                P.ts('dve', u.v, pu.v, 7.0, -7.0, op0=ALU.min, op1=ALU.max)
                P.tt('dve', g.v, g.v, s_.v, ALU.mult)
                P.stt('dve', at[:, hs], u.v, 1.0, g.v, ALU.add, ALU.mult)

        def stageT(b):
            cur = b % 2
            Wg, Wu, Wd = Wt[cur]
            aT = actT[cur]
            at = atok[cur]
            for half in range(2 if dbg not in (6, 10) else 0):
                pz = nps(); pzb = pz.v.bitcast(BF16)
                for c in range(4):
                    fc = half * 4 + c
                    P.tr(pzb[:, c * 128:(c + 1) * 128], at[:, fc:1024:8], C.identb.v)
                P.copy('act', aT[:, half * 4:(half + 1) * 4, :], pzb[:, 0:512].re("p (c t) -> p c t", c=4))

        def stageDn(b):
            cur = b % 2
            Wg, Wu, Wd = Wt[cur]
            aT = actT[cur]
            y = yb[cur]
            if dbg in (6, 10):
                P.memset('dve', y.v, 0.5)
            for hf in range(2 if dbg not in (6, 10) else 0):
                py = nps()
                hs = slice(hf * 512, (hf + 1) * 512)
                for fc in range(8):
                    P.mm(py.v, aT[:, fc, :], Wd[fc][:, hs], start=(fc == 0), stop=False)
                P.mm(py.v, OHb[cur].v, Ball[2][:, hs], start=False, stop=True)
                P.copy('act', y[:, hs], py.v)
            if dbg != 7:
                P.dma('sp', Ys[b * 128:(b + 1) * 128, :], y.v)
        nblk_ = NBLK if dbg != 11 else 10
        stageXx(0)
        for b in range(nblk_ + 1):
            if b < nblk_:
                stageXg(b)
                stageG(b)
            if b >= 1:
                stageT(b - 1)
            if b + 1 < nblk_:
                stageXx(b + 1)
            if b >= 1:
                stageDn(b - 1)
    if dbg == 4:
        return
    rk = [P.sb(pfx + "rk%d" % i, [128, 1024]) for i in range(4)]
    acA = P.sb(pfx + "acA", [128, 1024]); acB = P.sb(pfx + "acB", [128, 1024])
    st = P.sb(pfx + "st", [128, 2, 6]); mv = P.sb(pfx + "mv", [128, 2]); rstd = P.sb(pfx + "rstd", [128, 1])
    Ys2 = Ys.h[:]
    for t in range(NTL):
        xt = xts[t % 2]
        P.dma('sp', xt.v, x1_d[t * 128:(t + 1) * 128, :])
        for k in range(4):
            ind_dma(P, rk[k].v, Ys, Ys2, slot_i[:, t * 4 + k: t * 4 + k + 1], NSLOT - 1)
        P.ts('dve', acA.v, rk[0].v, pk_all[:, t, 0:1], c1.v, op0=ALU.mult, op1=ALU.mult)
        P.stt('dve', acB.v, rk[1].v, pk_all[:, t, 1:2], acA.v, ALU.mult, ALU.add)
        P.stt('dve', acA.v, rk[2].v, pk_all[:, t, 2:3], acB.v, ALU.mult, ALU.add)
        P.stt('dve', acB.v, rk[3].v, pk_all[:, t, 3:4], acA.v, ALU.mult, ALU.add)
        P.stt('dve', acA.v, xt.v, DN_ALPHA, acB.v, ALU.mult, ALU.add)
        layer_norm(P, acA.v, gB.v, bB.v, st.v, mv.v, rstd.v)
        P.dma('sp', out_d[t * 128:(t + 1) * 128, :], acA.v)
```

```python
import numpy as np
import concourse.bass as bass
import concourse.mybir as mybir
from concourse.bass_utils import run_bass_kernel_spmd
from contextlib import ExitStack

F32 = mybir.dt.float32
F32R = mybir.dt.float32r
BF16 = mybir.dt.bfloat16
I32 = mybir.dt.int32
U32 = mybir.dt.uint32
AF = mybir.ActivationFunctionType
ALU = mybir.AluOpType
AX = mybir.AxisListType

ENGS = ['pe', 'act', 'dve', 'pool', 'sp']


class Tile:
    def __init__(self, P, h, name, space='sb'):
        self.P = P
        self.h = h
        self.name = name
        self.space = space
        self.lw = {}
        self.rd = {}
        self.dsem = None
        self.dcnt = 0

    def __getitem__(self, k):
        return V(self, self.h[k])

    @property
    def v(self):
        return V(self, self.h[:])


class V:
    def __init__(self, tile, ap):
        self.tile = tile
        self.ap = ap

    def __getitem__(self, k):
        return V(self.tile, self.ap[k])

    def bitcast(self, dt):
        return V(self.tile, self.ap.bitcast(dt))

    def bc(self, shape):
        return V(self.tile, self.ap.to_broadcast(shape))

    def re(self, s, **kw):
        return V(self.tile, self.ap.rearrange(s, **kw))


def _ap(x):
    if isinstance(x, Tile):
        return x.h[:]
    return x.ap if isinstance(x, V) else x


class Prog:
    def __init__(self, nc, es, same_engine_sync=None):
        self.nc = nc
        self.es = es
        self.es_top = es
        self.all_tiles = []
        self.stream = {e: [] for e in ENGS}
        self.sems = {}
        self.cnt = {e: 0 for e in ENGS}
        self.known = {e: {} for e in ENGS}
        import os as _os
        self.same = (_os.environ.get('KSAME', '1') == '1') if same_engine_sync is None else same_engine_sync
        self.nsem = 0
        for e in ['pe', 'act', 'dve', 'pool']:
            self.sems[e] = es.enter_context(nc.semaphore("s_" + e))
            self.nsem += 1
        self.ninst = 0
        self.nwait = 0

    def sb(self, name, shape, dt=F32):
        h = self.es.enter_context(self.nc.sbuf_tensor(name, list(shape), dt))
        return Tile(self, h, name)

    def ps(self, name, shape, dt=F32):
        h = self.es.enter_context(self.nc.psum_tensor(name, list(shape), dt))
        return Tile(self, h, name, 'ps')

    def dram(self, name, shape, dt=F32, kind="Internal"):
        h = self.nc.dram_tensor(name, list(shape), dt, kind=kind)
        return Tile(self, h.ap(), name, 'dram')

    def _tsem(self, t):
        if t.dsem is None:
            key = "d_" + t.name
            self.sems[key] = self.es_top.enter_context(self.nc.semaphore(key))
            self.nsem += 1
            t.dsem = key
            self.all_tiles.append(t)
        return t.dsem

    def _waits(self, eng, rt, wt):
        need = {}
        for t in rt:
            for s, v in t.lw.items():
                need[s] = max(need.get(s, 0), v)
        for t in wt:
            for s, v in t.lw.items():
                need[s] = max(need.get(s, 0), v)
            for s, v in t.rd.items():
                need[s] = max(need.get(s, 0), v)
        out = []
        kn = self.known[eng]
        for s, v in need.items():
            if s == eng and (eng == 'pe' or not self.same):
                continue
            if kn.get(s, 0) < v:
                kn[s] = v
                out.append((s, v))
        return out

    def _record(self, ev, rt, wt):
        s, v = ev
        for t in wt:
            t.lw[s] = max(t.lw.get(s, 0), v)
            t.rd = {}
        for t in rt:
            if t in wt:
                continue
            t.rd[s] = max(t.rd.get(s, 0), v)

    @staticmethod
    def _tiles(xs):
        out = []
        for x in xs:
            if x is None:
                continue
            t = x.tile if isinstance(x, V) else x
            if isinstance(t, Tile) and t not in out:
                out.append(t)
        return out

    def I(self, eng, fn, w=(), r=()):
        wt = self._tiles(w)
        rt = self._tiles(r)
        for t in rt:
            if t.space == 'ps' and t not in wt and eng != 'pe':
                wt.append(t)
        waits = self._waits(eng, rt, wt)
        self.cnt[eng] += 1
        ev = (eng, self.cnt[eng])
        self.stream[eng].append((waits, fn, (eng, 1)))
        self._record(ev, rt, wt)
        self.ninst += 1
        self.nwait += len(waits)

    def dma(self, q, out, in_, **kw):
        wt = self._tiles([out])
        rt = self._tiles([in_])
        owner = None
        for x in (out, in_):
            t_ = x.tile if isinstance(x, V) else (x if isinstance(x, Tile) else None)
            if t_ is not None and t_.space == 'sb':
                owner = t_
        if owner is None:
            for x in (out, in_):
                t_ = x.tile if isinstance(x, V) else (x if isinstance(x, Tile) else None)
                if t_ is not None and owner is None:
                    owner = t_
        key = self._tsem(owner)
        waits = self._waits(q, rt, wt)
        owner.dcnt += 16
        ev = (key, owner.dcnt)
        o, i = _ap(out), _ap(in_)
        self.stream[q].append((waits, lambda e: e.dma_start(out=o, in_=i, **kw), (key, 16)))
        self._record(ev, rt, wt)
        self.ninst += 1
        self.nwait += len(waits)
        return ev

    def wait_all(self, eng, tiles):
        ts = self._tiles(tiles)
        waits = self._waits(eng, ts, ts)
        self.stream[eng].append((waits, None, None))

    def mm(self, out, lhsT, rhs, start=True, stop=True, **kw):
        o, a, b = _ap(out), _ap(lhsT), _ap(rhs)
        self.I('pe', lambda e: e.matmul(o, a, b, start=start, stop=stop, **kw), w=[out], r=[lhsT, rhs])

    def tr(self, out, in_, ident):
        o, a, b = _ap(out), _ap(in_), _ap(ident)
        self.I('pe', lambda e: e.transpose(o, a, b), w=[out], r=[in_, ident])

    def act(self, out, in_, func, bias=None, scale=1.0, accum_out=None, eng='act'):
        o, a = _ap(out), _ap(in_)
        kw = {}
        if bias is not None:
            kw['bias'] = _ap(bias)
        if accum_out is not None:
            kw['accum_out'] = _ap(accum_out)
        sc = _ap(scale)
        self.I(eng, lambda e: e.activation(o, a, func, scale=sc, **kw),
               w=[out, accum_out], r=[in_, bias, scale if isinstance(scale, V) else None])

    def tt(self, eng, out, in0, in1, op):
        o, a, b = _ap(out), _ap(in0), _ap(in1)
        self.I(eng, lambda e: e.tensor_tensor(o, a, b, op), w=[out], r=[in0, in1])

    def ts(self, eng, out, in0, s1, s2=None, op0=ALU.mult, op1=None, accum_out=None):
        o, a = _ap(out), _ap(in0)
        x1, x2 = _ap(s1), _ap(s2)
        kw = {}
        if op1 is not None:
            kw['op1'] = op1
        if accum_out is not None:
            kw['accum_out'] = _ap(accum_out)
        self.I(eng, lambda e: e.tensor_scalar(o, a, x1, x2, op0, **kw), w=[out, accum_out],
               r=[in0, s1 if isinstance(s1, V) else None, s2 if isinstance(s2, V) else None])

    def stt(self, eng, out, in0, scalar, in1, op0, op1):
        o, a, b = _ap(out), _ap(in0), _ap(in1)
        s = _ap(scalar)
        self.I(eng, lambda e: e.scalar_tensor_tensor(o, a, s, b, op0, op1), w=[out],
               r=[in0, in1, scalar if isinstance(scalar, V) else None])

    def copy(self, eng, out, in_):
        o, a = _ap(out), _ap(in_)
        if eng == 'act':
            self.I(eng, lambda e: e.copy(o, a), w=[out], r=[in_])
        else:
            self.I(eng, lambda e: e.tensor_copy(o, a), w=[out], r=[in_])

    def memset(self, eng, out, val):
        o = _ap(out)
        self.I(eng, lambda e: e.memset(o, val), w=[out])

    def barrier(self):
        evs = {e: self.cnt[e] for e in ['pe', 'act', 'dve', 'pool'] if self.cnt[e] > 0}
        for t in self.all_tiles:
            if t.dcnt > 0:
                evs[t.dsem] = t.dcnt
        for eng in ENGS:
            kn = self.known[eng]
            waits = []
            for s_, v in evs.items():
                if kn.get(s_, 0) < v:
                    kn[s_] = v
                    waits.append((s_, v))
            if waits:
                self.stream[eng].append((waits, None, None))

    def scope(self):
        P = self

        class _S:
            def __enter__(self_):
                self_.old = P.es
                self_.st = ExitStack()
                self_.st.__enter__()
                P.es = self_.st
                return self_

            def __exit__(self_, *a):
                P.barrier()
                P.emit()
                P.es = self_.old
                self_.st.__exit__(None, None, None)
                return False
        return _S()

    def emit(self):
        nc = self.nc
        sems = self.sems
        with nc.Block() as block:
            def run(engobj, name):
                for waits, fn, inc in self.stream[name]:
                    for s, v in waits:
                        engobj.wait_ge(sems[s], v)
                    if fn is not None:
                        ins = fn(engobj)
                        ins.then_inc(sems[inc[0]], inc[1])

            @block.tensor
            def _(e):
                run(e, 'pe')

            @block.scalar
            def _(e):
                run(e, 'act')

            @block.vector
            def _(e):
                run(e, 'dve')

            @block.gpsimd
            def _(e):
                run(e, 'pool')

            @block.sync
            def _(e):
                run(e, 'sp')
        self.stream = {e: [] for e in ENGS}


D = 1024
MIXW = 512
NEXP = 32
DN_ALPHA = (2.0 * 2) ** 0.25
EPS = 1e-5
OFF = dict(r_q=0, r_k=256, r_v=512, r_g=1024, m_x=1536, m_i=2048, m_f=2052, m_o=2056,
           a_q=2568, a_k=3336, a_v=4104, gates=5640)
D_IN = 8712


class Ctx:
    pass


def make_ctx(P):
    C = Ctx()
    C.identf = P.sb("identf", [128, 128], F32)
    C.identb = P.sb("identb", [128, 128], BF16)
    C.onesb = P.sb("onesb", [128, 128], BF16)
    C.onesf = P.sb("onesf", [128, 128], F32)
    P.memset('pool', C.identf.v, 1.0)
    o = C.identf.v.ap
    P.I('pool', lambda e: e.affine_select(o, o, [[-1, 128]], ALU.is_equal, 0.0, base=0, channel_multiplier=1),
        w=[C.identf], r=[C.identf])
    P.copy('pool', C.identb.v, C.identf.v)
    P.memset('pool', C.onesb.v, 1.0)
    P.memset('pool', C.onesf.v, 1.0)
    C.ps = [P.ps("psb%d" % i, [128, 512], F32) for i in range(8)]
    return C


def load_w_cast(P, dst, src, q='pool'):
    cols = src.shape[-1]
    c0 = 0
    while c0 < cols:
        c1 = min(cols, c0 + 1024)
        P.dma(q, dst[:, :, c0:c1], src[:, :, c0:c1])
        c0 = c1


def bcast_rows(P, dst, src1d, q='act'):
    P.dma(q, dst, src1d.partition_broadcast(128))


def x_transpose(P, C, xt, outs, psl):
    for half in range(2):
        pt = psl[half]
        for c in range(4):
            k = half * 4 + c
            P.tr(pt[:, c * 128:(c + 1) * 128], xt[:, k * 128:(k + 1) * 128], C.identf.v)
        for (o, eng) in outs:
            P.copy(eng, o[:, half * 4:(half + 1) * 4, :], pt.v.re("p (c t) -> p c t", c=4))


def layer_norm(P, r, g_b, b_b, st, mv, rstd, eng2='pool'):
    for hf in range(2):
        a, b = st[:, hf, :].ap, r[:, hf * 512:(hf + 1) * 512].ap
        P.I('dve', (lambda a, b: (lambda e: e.bn_stats(a, b)))(a, b), w=[st], r=[r])
    a, b = mv.ap, st.ap
    P.I('dve', lambda e: e.bn_aggr(a, b), w=[mv], r=[st])
    P.ts('dve', rstd, mv[:, 1:2], EPS, None, op0=ALU.add)
    P.act(rstd, rstd, AF.Ln)
    P.act(rstd, rstd, AF.Exp, scale=-0.5)
    P.ts('dve', r, r, mv[:, 0:1], rstd, op0=ALU.subtract, op1=ALU.mult)
    P.tt(eng2, r, r, g_b, ALU.mult)
    P.tt(eng2, r, r, b_b, ALU.add)


def pass_merge(P, C, NT, x_d, yT_d, w_in_l, w_branch_l, w_out_l, ln_g, ln_b, x1_d, pfx="m"):
    wg = P.sb(pfx + "wg", [128, 8, 3072], BF16)
    wb = P.sb(pfx + "wb", [128, 12, 1024], BF16)
    wo = P.sb(pfx + "wo", [128, 8, 1024], BF16)
    load_w_cast(P, wg.v, w_in_l[:, OFF['gates']:D_IN].rearrange("(kc p) c -> p kc c", p=128))
    load_w_cast(P, wb.v, w_branch_l.rearrange("b (kc p) c -> p (b kc) c", p=128))
    load_w_cast(P, wo.v, w_out_l.rearrange("(kc p) c -> p kc c", p=128))
    gB = P.sb(pfx + "gB", [128, 1024]); bB = P.sb(pfx + "bB", [128, 1024])
    bcast_rows(P, gB.v, ln_g); bcast_rows(P, bB.v, ln_b)
    xts = [P.sb(pfx + "xt%d" % i, [128, 1024]) for i in range(2)]
    xTs = [P.sb(pfx + "xT%d" % i, [128, 8, 128], BF16) for i in range(2)]
    yTs = [[P.sb(pfx + "yT%d_%d" % (b, i), [128, 4, 128], BF16) for i in range(2)] for b in range(3)]
    mg = [P.sb(pfx + "mg%d" % i, [128, 1024]) for i in range(2)]
    mT = [P.sb(pfx + "mT%d" % i, [128, 8, 128], BF16) for i in range(2)]
    sg = [P.sb(pfx + "sg%d" % i, [128, 512]) for i in range(2)]
    tmp = [P.sb(pfx + "tmp%d" % i, [128, 512]) for i in range(2)]
    rr = [P.sb(pfx + "rr%d" % i, [128, 1024]) for i in range(2)]
    st = P.sb(pfx + "st", [128, 2, 6]); mv = P.sb(pfx + "mv", [128, 2]); rstd = P.sb(pfx + "rstd", [128, 1])
    kc_ = [0]

    def S1(t):
        xt = xts[t % 2]; xT = xTs[t % 2]
        P.dma('sp', xt.v, x_d[t * 128:(t + 1) * 128, :])
        x_transpose(P, C, xt.v, [(xT.v, 'act')], [C.ps[0], C.ps[1]])
        for b in range(3):
            P.dma('act', yTs[b][t % 2].v, yT_d[b][:, :, t * 128:(t + 1) * 128])
        m = mg[t % 2]
        for b in range(3):
            for hf in range(2):
                pg = C.ps[2 + (kc_[0] % 2)]; pb = C.ps[4 + (kc_[0] % 2)]; s = sg[kc_[0] % 2]; tm = tmp[kc_[0] % 2]
                kc_[0] += 1
                for kc in range(8):
                    P.mm(pg.v, xT[:, kc, :], wg[:, kc, b * 1024 + hf * 512: b * 1024 + (hf + 1) * 512],
                         start=(kc == 0), stop=(kc == 7))
                for kc in range(4):
                    P.mm(pb.v, yTs[b][t % 2][:, kc, :], wb[:, b * 4 + kc, hf * 512:(hf + 1) * 512],
                         start=(kc == 0), stop=(kc == 3))
                P.act(s.v, pg.v, AF.Sigmoid)
                msl = m[:, hf * 512:(hf + 1) * 512]
                if b == 0:
                    P.tt('dve', msl, s.v, pb.v, ALU.mult)
                else:
                    P.tt('dve', tm.v, s.v, pb.v, ALU.mult)
                    P.tt('pool', msl, msl, tm.v, ALU.add)

    def S2(t):
        xt = xts[t % 2]
        m = mg[t % 2]
        x_transpose(P, C, m.v, [(mT[t % 2].v, 'act')], [C.ps[6], C.ps[7]])
        r = rr[t % 2]
        for hf in range(2):
            po = C.ps[2 + (kc_[0] % 2)]
            kc_[0] += 1
            for kc in range(8):
                P.mm(po.v, mT[t % 2][:, kc, :], wo[:, kc, hf * 512:(hf + 1) * 512], start=(kc == 0), stop=(kc == 7))
            P.stt('dve', r[:, hf * 512:(hf + 1) * 512], xt[:, hf * 512:(hf + 1) * 512], DN_ALPHA, po.v,
                  ALU.mult, ALU.add)
        layer_norm(P, r.v, gB.v, bB.v, st.v, mv.v, rstd.v)
        P.dma('sp', x1_d[t * 128:(t + 1) * 128, :], r.v)

    ntl_ = NT // 128
    S1(0)
    for t in range(ntl_):
        if t + 1 < ntl_:
            S1(t + 1)
        S2(t)


def pass_moe(P, C, NT, x1_d, w_router, b_router, w_gate, b_gate, w_up, b_up, w_down, b_down, ln_g, ln_b, out_d,
             NE=NEXP, pfx="e", TGT=4, dbg=0):
    TG = TGT * 128
    gB = P.sb(pfx + "gB", [128, 1024]); bB = P.sb(pfx + "bB", [128, 1024])
    bcast_rows(P, gB.v, ln_g); bcast_rows(P, bB.v, ln_b)
    wr = P.sb(pfx + "wr", [128, 8, NE], F32R)
    wr0 = P.sb(pfx + "wr0", [128, 8, NE])
    P.dma('act', wr0.v, w_router.rearrange("(kc p) e -> p kc e", p=128))
    P.copy('dve', wr.v, wr0.v)
    brB = P.sb(pfx + "brB", [128, NE]); bcast_rows(P, brB.v, b_router)
    bgT = P.sb(pfx + "bgT", [128, NE, 8]); buT = P.sb(pfx + "buT", [128, NE, 8])
    bstage = P.sb(pfx + "bstage", [128, 128])
    if dbg in (3, 7):
        P.memset('dve', bgT.v, 0.0); P.memset('dve', buT.v, 0.0)
    for (dstT, src) in (((bgT, b_gate), (buT, b_up)) if dbg not in (3, 7) else ()):
        rows = NE * 8
        srcv = src.rearrange("e (fc p) -> (e fc) p", p=128)
        dv = dstT.v.re("p e fc -> p (e fc)")
        r0 = 0
        while r0 < rows:
            r1 = min(rows, r0 + 128)
            n = r1 - r0
            P.dma('act', bstage[0:n, :], srcv[r0:r1, :])
            pz = C.ps[7]
            P.tr(pz[:, 0:n], bstage[0:n, :], C.identf[0:n, 0:n])
            P.copy('dve', dv[:, r0:r1], pz[:, 0:n])
            r0 = r1
    bd = P.sb(pfx + "bd", [NE, 1024], F32R)
    bd0 = P.sb(pfx + "bd0", [NE, 1024])
    P.dma('act', bd0.v, b_down)
    P.copy('dve', bd.v, bd0.v)
    W = [[P.sb(pfx + "W%d_%d" % (j, i), [128, 8, 1024], BF16) for j in range(3)] for i in range(2)]
    xts = [P.sb(pfx + "xt%d" % i, [128, 1024]) for i in range(TGT)]
    xTg = P.sb(pfx + "xTg", [128, 8, TG], BF16)
    xT32 = P.sb(pfx + "xT32", [128, 8, 128], F32R)
    acc = P.sb(pfx + "acc", [128, TGT, 1024])
    pall = P.sb(pfx + "pall", [128, TGT, NE])
    actT = P.sb(pfx + "actT", [128, 8, TG], BF16)
    lg = P.sb(pfx + "lg", [128, NE]); t8 = P.sb(pfx + "t8", [128, 8]); msk = P.sb(pfx + "msk", [128, NE])
    ex = P.sb(pfx + "ex", [128, NE]); sm = P.sb(pfx + "sm", [128, 1]); nmx = P.sb(pfx + "nmx", [128, 1])
    pT = P.sb(pfx + "pT", [NE, 128], F32R)
    gt = [P.sb(pfx + "g%d" % i, [128, TG]) for i in range(2)]
    st_ = [P.sb(pfx + "s%d" % i, [128, TG]) for i in range(2)]
    ut = [P.sb(pfx + "u%d" % i, [128, TG]) for i in range(2)]
    st = P.sb(pfx + "st", [128, 2, 6]); mv = P.sb(pfx + "mv", [128, 2]); rstd = P.sb(pfx + "rstd", [128, 1])
    c1 = P.sb(pfx + "c1", [128, 1]); c7 = P.sb(pfx + "c7", [128, 1])
    P.memset('dve', c1.v, 1.0); P.memset('dve', c7.v, 7.0)
    rr = [P.sb(pfx + "rr%d" % i, [128, 1024]) for i in range(2)]
    tmpq = [P.sb(pfx + "tq%d" % i, [128, 512]) for i in range(2)]
    assert TG == 512
    if dbg == 5:
        P.wait_all('sp', [gB, bB, wr, brB, bd, bgT, buT])
        return
    wcnt = 0
    kk = 0
    for gi in range(NT // TG):
        for tt in range(TGT):
            t = gi * TGT + tt
            xt = xts[tt]
            P.dma('sp', xt.v, x1_d[t * 128:(t + 1) * 128, :])
            x_transpose(P, C, xt.v, [(xTg[:, :, tt * 128:(tt + 1) * 128], 'act')] + ([(xT32.v, 'dve')] if dbg != 3 else []), [C.ps[0], C.ps[1]])
            if dbg in (2, 3, 7, 8):
                P.memset('dve', acc[:, tt, :], 0.0)
                P.memset('dve', pall[:, tt, :], 0.25)
            else:
                pl = C.ps[6]
                for kc in range(8):
                    P.mm(pl[:, 0:NE], xT32[:, kc, :], wr[:, kc, :], start=(kc == 0), stop=(kc == 7))
                P.tt('dve', lg.v, pl[:, 0:NE], brB.v, ALU.add)
                a, b = t8.v.ap, lg.v.ap
                P.I('dve', (lambda a, b: (lambda e: e.max(out=a, in_=b)))(a, b), w=[t8], r=[lg])
                P.ts('dve', msk.v, lg.v, t8[:, 3:4], c1.v, op0=ALU.is_ge, op1=ALU.mult)
                P.ts('dve', nmx.v, t8[:, 0:1], -1.0, None, op0=ALU.mult)
                P.act(ex.v, lg.v, AF.Exp, bias=nmx.v)
                P.tt('dve', ex.v, ex.v, msk.v, ALU.mult)
                a2, b2 = sm.v.ap, ex.v.ap
                P.I('dve', (lambda a, b: (lambda e: e.reduce_sum(a, b, AX.X)))(a2, b2), w=[sm], r=[ex])
                a3 = sm.v.ap
                P.I('dve', (lambda a: (lambda e: e.reciprocal(a, a)))(a3), w=[sm], r=[sm])
                P.ts('dve', pall[:, tt, :], ex.v, sm.v, c1.v, op0=ALU.mult, op1=ALU.mult)
                P.tr(pl[0:NE, 128:256], pall[:, tt, :], C.identf.v)
                P.copy('dve', pT.v, pl[0:NE, 128:256])
                for hf in range(2):
                    pb = C.ps[7]
                    P.mm(pb.v, pT.v, bd[:, hf * 512:(hf + 1) * 512])
                    P.copy('dve', acc[:, tt, hf * 512:(hf + 1) * 512], pb.v)
        for e in range(NE if dbg not in (1, 3, 7, 8) else 0):
            Wg, Wu, Wd = W[wcnt % 2]
            wcnt += 1
            P.dma('pool', Wg.v, w_gate[e].rearrange("(kc p) f -> p kc f", p=128))
            P.dma('pool', Wu.v, w_up[e].rearrange("(kc p) f -> p kc f", p=128))
            P.dma('pool', Wd.v, w_down[e].rearrange("(kc p) f -> p kc f", p=128))
            for fc in range(8):
                pg = C.ps[(kk % 2) * 2]; pu = C.ps[(kk % 2) * 2 + 1]
                g = gt[kk % 2]; s = st_[kk % 2]; u = ut[kk % 2]
                kk += 1
                for kc in range(8):
                    P.mm(pg.v, Wg[:, kc, fc * 128:(fc + 1) * 128], xTg[:, kc, :], start=(kc == 0), stop=(kc == 7))
                for kc in range(8):
                    P.mm(pu.v, Wu[:, kc, fc * 128:(fc + 1) * 128], xTg[:, kc, :], start=(kc == 0), stop=(kc == 7))
                P.ts('dve', g.v, pg.v, bgT[:, e, fc:fc + 1], c7.v, op0=ALU.add, op1=ALU.min)
                P.act(s.v, g.v, AF.Sigmoid, scale=1.702)
                P.ts('dve', u.v, pu.v, buT[:, e, fc:fc + 1], c7.v, op0=ALU.add, op1=ALU.min)
                P.ts('dve', u.v, u.v, -7.0, 1.0, op0=ALU.max, op1=ALU.add)
                P.tt('dve', g.v, g.v, s.v, ALU.mult)
                P.tt('dve', actT[:, fc, :], g.v, u.v, ALU.mult)
            for tt in range(TGT):
                for hf in range(2):
                    py = C.ps[4 + (kk % 2)]
                    kk += 1
                    for fc in range(8):
                        P.mm(py.v, actT[:, fc, tt * 128:(tt + 1) * 128], Wd[:, fc, hf * 512:(hf + 1) * 512],
                             start=(fc == 0), stop=(fc == 7))
                    av = acc[:, tt, hf * 512:(hf + 1) * 512]
                    tq = tmpq[kk % 2]
                    P.ts('dve', tq.v, py.v, pall[:, tt, e:e + 1], c1.v, op0=ALU.mult, op1=ALU.mult)
                    P.tt('dve', av, av, tq.v, ALU.add)
        for tt in range(TGT):
            t = gi * TGT + tt
            r = rr[tt % 2].v
            P.stt('dve', r, xts[tt].v, DN_ALPHA, acc[:, tt, :], ALU.mult, ALU.add)
            layer_norm(P, r, gB.v, bB.v, st.v, mv.v, rstd.v)
            P.dma('sp', out_d[t * 128:(t + 1) * 128, :], r)


RET_GAMMA = [1.0 - 2.0 ** (-5.0 - h) for h in range(4)]


def host_consts_scan(NT):
    j = np.arange(128, dtype=np.float64)
    lg = np.log(np.array(RET_GAMMA, dtype=np.float64))
    aR = np.exp(lg[None, :] * (j[:, None] + 1.0)) * (64 ** -0.5)
    bR = np.exp(-lg[None, :] * (j[:, None] + 1.0))
    eR = np.zeros((128, 2)); gR = np.zeros((128, 2))
    for h in range(4):
        ps = (h % 2) * 64
        eR[ps:ps + 64, h // 2] = np.exp(lg[h] * 128.0)
        gR[ps:ps + 64, h // 2] = np.exp(lg[h] * float(NT))
    mask = (j[:, None] <= j[None, :]).astype(np.float64)
    return dict(aR=aR.astype(np.float32), bR=bR.astype(np.float32), eR=eR.astype(np.float32),
                gR=gR.astype(np.float32), mask=mask.astype(np.float32))


def host_blockdiag(w):
    out = np.zeros((4, 128, 128), dtype=np.float32)
    for h in range(4):
        for n in range(32):
            out[h, 4 * n:4 * n + 4, 4 * n:4 * n + 4] = w[32 * h + n]
    return out


class PSRot:
    def __init__(self, C, banks):
        self.C = C; self.banks = banks; self.i = 0

    def __call__(self):
        b = self.C.ps[self.banks[self.i % len(self.banks)]]
        self.i += 1
        return b


def small_T(P, C, dst, src2d, rows, stage, ps):
    P.dma('act', stage[0:rows, :], src2d)
    P.tr(ps[:, 0:rows], stage[0:rows, :], C.identf[0:rows, 0:rows])
    P.copy('dve', dst, ps[:, 0:rows])


def pass_scan(P, C, NT, x_d, xprev_d, w_in_l, prm, cst, init, outs, mode="full", pfx="s"):
    full = (mode == "full")
    NW = 2568
    W = P.sb(pfx + "W", [128, 8, NW], BF16)
    load_w_cast(P, W.v, w_in_l[:, 0:NW].rearrange("(kc p) c -> p kc c", p=128))
    BD = {}
    for nm in ("bdq", "bdk", "bdv"):
        BD[nm] = P.sb(pfx + nm, [128, 4, 128], BF16)
        P.dma('pool', BD[nm].v, prm[nm].rearrange("h i o -> i h o"))
    stage = P.sb(pfx + "stage", [128, 128])
    cwT = P.sb(pfx + "cwT", [128, 16]); cbT = P.sb(pfx + "cbT", [128, 4])
    small_T(P, C, cwT.v, prm["ml_conv_w"].rearrange("k (c p) -> (k c) p", p=128), 16, stage, C.ps[7])
    small_T(P, C, cbT.v, prm["ml_conv_b"].rearrange("(c p) -> c p", p=128), 4, stage, C.ps[7])
    biB = P.sb(pfx + "biB", [128, 4]); bfB = P.sb(pfx + "bfB", [128, 4])
    bcast_rows(P, biB.v, prm["ml_bi"]); bcast_rows(P, bfB.v, prm["ml_bf"])
    aR = P.sb(pfx + "aR", [128, 4]); bR = P.sb(pfx + "bR", [128, 4]); eR = P.sb(pfx + "eR", [128, 2]); gR = P.sb(pfx + "gR", [128, 2])
    for t_, n_ in ((aR, "aR"), (bR, "bR"), (eR, "eR"), (gR, "gR")):
        P.dma('act', t_.v, cst[n_])
    mask = P.sb(pfx + "mask", [128, 128]); P.dma('act', mask.v, cst["mask"])
    maskr = P.sb(pfx + "maskr", [128, 128], F32R); P.copy('dve', maskr.v, mask.v)
    onesr = P.sb(pfx + "onesr", [128, 128], F32R); P.copy('dve', onesr.v, C.onesf.v)
    c1 = P.sb(pfx + "c1", [128, 1]); P.memset('dve', c1.v, 1.0)
    if full:
        gnR = P.sb(pfx + "gnR", [128, 512]); gnM = P.sb(pfx + "gnM", [128, 512]); skM = P.sb(pfx + "skM", [128, 512])
        bcast_rows(P, gnR.v, prm["ret_gn"]); bcast_rows(P, gnM.v, prm["ml_gn"]); bcast_rows(P, skM.v, prm["ml_skip"])
    Sret = P.sb(pfx + "Sret", [128, 2, 128]); Sretb = P.sb(pfx + "Sretb", [128, 2, 128], BF16)
    Cml = P.sb(pfx + "Cml", [128, 4, 129]); Cmlb = P.sb(pfx + "Cmlb", [128, 4, 129], BF16)
    tmpS = P.sb(pfx + "tmpS", [128, 4, 129]); tmpS2 = P.sb(pfx + "tmpS2", [128, 4, 129])
    totacc = P.sb(pfx + "totacc", [128, 4])
    P.memset('dve', Sret.v, 0.0); P.memset('dve', Cml.v, 0.0); P.memset('dve', totacc.v, 0.0)
    if init is not None:
        sel = P.sb(pfx + "sel", [128, 3]); nsel = P.sb(pfx + "nsel", [128, 3])
        P.dma('act', sel.v, init["sel"]); P.dma('act', nsel.v, init["nsel"])
        Fr = P.sb(pfx + "Fr", [128, 2, 128]); Fm = P.sb(pfx + "Fm", [128, 4, 129]); tl = P.sb(pfx + "tl", [128, 4])
        Gm = P.sb(pfx + "Gm", [128, 4])
        for q in range(3):
            P.dma('act', Fr.v, init["Fret"][q]); P.dma('act', Fm.v, init["Fml"][q]); P.dma('act', tl.v, init["totL"][q])
            P.act(Gm.v, tl.v, AF.Exp, scale=-1.0)
            for hp in range(2):
                P.act(tmpS[:, hp, 0:128], Sret[:, hp, :], AF.Copy, scale=gR[:, hp:hp + 1])
                P.tt('dve', tmpS[:, hp, 0:128], tmpS[:, hp, 0:128], Fr[:, hp, :], ALU.add)
                P.act(tmpS[:, hp, 0:128], tmpS[:, hp, 0:128], AF.Copy, scale=sel[:, q:q + 1])
                P.act(tmpS2[:, hp, 0:128], Sret[:, hp, :], AF.Copy, scale=nsel[:, q:q + 1])
                P.tt('dve', Sret[:, hp, :], tmpS[:, hp, 0:128], tmpS2[:, hp, 0:128], ALU.add)
            for h in range(4):
                P.act(tmpS[:, h, :], Cml[:, h, :], AF.Copy, scale=Gm[:, h:h + 1])
                P.tt('dve', tmpS[:, h, :], tmpS[:, h, :], Fm[:, h, :], ALU.add)
                P.act(tmpS[:, h, :], tmpS[:, h, :], AF.Copy, scale=sel[:, q:q + 1])
                P.act(tmpS2[:, h, :], Cml[:, h, :], AF.Copy, scale=nsel[:, q:q + 1])
                P.tt('dve', Cml[:, h, :], tmpS[:, h, :], tmpS2[:, h, :], ALU.add)
    P.copy('dve', Sretb.v, Sret.v); P.copy('dve', Cmlb.v, Cml.v)
    SC = 512
    xts = [P.sb(pfx + "xt%d" % i, [128, 1024]) for i in range(2)]
    xT = P.sb(pfx + "xT", [128, 8, SC], BF16)
    rqT = P.sb(pfx + "rqT", [128, 2, SC], BF16); rkT = P.sb(pfx + "rkT", [128, 2, SC], BF16)
    mxT = P.sb(pfx + "mxT", [128, 4, 3 + SC]); mxb = P.sb(pfx + "mxb", [128, 4, SC], BF16)
    cva = P.sb(pfx + "cva", [128, SC]); cvb = P.sb(pfx + "cvb", [128, SC])
    mcT = P.sb(pfx + "mcT", [128, 4, SC], BF16)
    qmT = P.sb(pfx + "qmT", [128, 4, SC], BF16); kmT = P.sb(pfx + "kmT", [128, 4, SC], BF16)
    rk_tok = P.sb(pfx + "rk_tok", [128, 256], BF16); km_tok = P.sb(pfx + "km_tok", [128, 512], BF16)
    vpR = P.sb(pfx + "vpR", [128, 4, 128], BF16); vpM = P.sb(pfx + "vpM", [128, 4, 129], BF16)
    g8 = P.sb(pfx + "g8", [128, 8]); L1 = P.sb(pfx + "L1", [128, 4], F32R); e1 = P.sb(pfx + "e1", [128, 4])
    igt = P.sb(pfx + "igt", [128, 4]); aM = P.sb(pfx + "aM", [128, 4]); bM = P.sb(pfx + "bM", [128, 4]); eM = P.sb(pfx + "eM", [128, 4])
    tmp4 = P.sb(pfx + "tmp4", [128, 4])
    Pm8 = [P.sb(pfx + "Pm%d" % i, [128, 128], BF16) for i in range(8)]
    ot8 = [P.sb(pfx + "ot%d" % i, [128, 129]) for i in range(8)]
    hh = [P.sb(pfx + "hh%d" % i, [128, 128]) for i in range(2)]
    dn = P.sb(pfx + "dn", [128, 1]); st6 = P.sb(pfx + "st6", [128, 6]); mv = P.sb(pfx + "mv", [128, 2]); rs = P.sb(pfx + "rs", [128, 1])
    if full:
        yR = P.sb(pfx + "yR", [128, 512]); yM = P.sb(pfx + "yM", [128, 512])
        rg = P.sb(pfx + "rg", [128, 512]); mo = P.sb(pfx + "mo", [128, 512]); mct = P.sb(pfx + "mct", [128, 512])
        yTo = [P.sb(pfx + "yTo%d" % i, [128, 4, 128], BF16) for i in range(2)]
        ybf = P.sb(pfx + "ybf", [128, 512], BF16)
    nps = PSRot(C, [0, 1, 2, 3, 4, 5, 6, 7])
    P.dma('sp', xts[0].v, xprev_d)
    x_transpose(P, C, xts[0].v, [(xT[:, :, 0:128], 'act')], [nps(), nps()])
    for c in range(4):
        pz = nps()
        for kc in range(8):
            P.mm(pz[:, 0:128], W[:, kc, OFF['m_x'] + c * 128: OFF['m_x'] + (c + 1) * 128], xT[:, kc, 0:128],
                 start=(kc == 0), stop=(kc == 7))
        P.copy('dve', mxT[:, c, 0:3], pz[:, 125:128])
    lnscale = float(np.log(128 ** -0.5))
    for sc in range(NT // SC):
        for tt in range(4):
            t = sc * 4 + tt
            xt = xts[t % 2]
            P.dma('sp', xt.v, x_d[t * 128:(t + 1) * 128, :])
            x_transpose(P, C, xt.v, [(xT[:, :, tt * 128:(tt + 1) * 128], 'act')], [nps(), nps()])
        for (dst, off, nch, kind) in ((rqT, OFF['r_q'], 2, 'bf'), (rkT, OFF['r_k'], 2, 'bf'), (mxT, OFF['m_x'], 4, 'mx')):
            if not full and (dst is rqT or dst is rkT):
                continue
            for c in range(nch):
                pz = nps()
                for kc in range(8):
                    P.mm(pz.v, W[:, kc, off + c * 128: off + (c + 1) * 128], xT[:, kc, :], start=(kc == 0), stop=(kc == 7))
                if kind == 'bf':
                    P.copy('act', dst[:, c, :], pz.v)
                else:
                    P.copy('act', mxT[:, c, 3:3 + SC], pz.v)
                    P.copy('dve', mxb[:, c, :], pz.v)
        for c in range(4):
            P.ts('dve', cva.v, mxT[:, c, 3:3 + SC], cwT[:, 12 + c:13 + c], cbT[:, c:c + 1], op0=ALU.mult, op1=ALU.add)
            P.stt('dve', cvb.v, mxT[:, c, 2:2 + SC], cwT[:, 8 + c:9 + c], cva.v, ALU.mult, ALU.add)
            P.stt('dve', cva.v, mxT[:, c, 1:1 + SC], cwT[:, 4 + c:5 + c], cvb.v, ALU.mult, ALU.add)
            P.stt('dve', cvb.v, mxT[:, c, 0:SC], cwT[:, c:c + 1], cva.v, ALU.mult, ALU.add)
            P.act(mcT[:, c, :], cvb.v, AF.Silu)
            P.copy('dve', cva[:, 0:3], mxT[:, c, SC:SC + 3])
            P.copy('dve', mxT[:, c, 0:3], cva[:, 0:3])
        for (dst, bd) in ((qmT, BD["bdq"]), (kmT, BD["bdk"])):
            if not full:
                continue
            for h in range(4):
                pz = nps()
                P.mm(pz.v, bd[:, h, :], mcT[:, h, :])
                P.copy('act', dst[:, h, :], pz.v)
        for tt in range(4):
            t = sc * 4 + tt
            ts_ = slice(tt * 128, (tt + 1) * 128)

            def tok_proj(off, n):
                pz = nps()
                for kc in range(8):
                    P.mm(pz[:, 0:n], xT[:, kc, ts_], W[:, kc, off:off + n], start=(kc == 0), stop=(kc == 7))
                return pz
            p_rk = tok_proj(OFF['r_k'], 256)
            P.copy('act', rk_tok.v, p_rk[:, 0:256])
            p_g8 = tok_proj(OFF['m_i'], 8)
            P.copy('dve', g8.v, p_g8[:, 0:8])
            P.tt('dve', igt.v, g8[:, 0:4], biB.v, ALU.add)
            P.tt('dve', tmp4.v, g8[:, 4:8], bfB.v, ALU.add)
            P.act(e1.v, tmp4.v, AF.Exp, scale=-1.0)
            P.ts('dve', e1.v, e1.v, 1.0, None, op0=ALU.add)
            P.act(L1.v, e1.v, AF.Ln)
            pc = nps()
            P.mm(pc[:, 0:4], maskr.v, L1.v)
            P.mm(pc[:, 8:12], onesr.v, L1.v)
            P.act(aM.v, pc[:, 0:4], AF.Exp, scale=-1.0, bias=lnscale)
            P.tt('dve', tmp4.v, igt.v, pc[:, 0:4], ALU.add)
            P.act(bM.v, tmp4.v, AF.Exp)
            P.act(eM.v, pc[:, 8:12], AF.Exp, scale=-1.0)
            P.tt('dve', totacc.v, totacc.v, pc[:, 8:12], ALU.add)
            p_rv = tok_proj(OFF['r_v'], 512)
            for h in range(4):
                P.act(vpR[:, h, :], p_rv[:, h * 128:(h + 1) * 128], AF.Copy, scale=bR[:, h:h + 1])
            p_vm = nps()
            for h in range(4):
                P.mm(p_vm[:, h * 128:(h + 1) * 128], mxb[:, h, ts_], BD["bdv"][:, h, :])
            for h in range(4):
                P.act(vpM[:, h, 0:128], p_vm[:, h * 128:(h + 1) * 128], AF.Copy, scale=bM[:, h:h + 1])
            P.copy('dve', vpM[:, :, 128:129], bM.v.re("p (h o) -> p h o", o=1))
            p_km = nps()
            for h in range(4):
                P.mm(p_km[:, h * 128:(h + 1) * 128], mcT[:, h, ts_], BD["bdk"][:, h, :])
            P.copy('act', km_tok.v, p_km.v)
            if full:
                p_rg = tok_proj(OFF['r_g'], 512)
                P.act(rg.v, p_rg.v, AF.Silu)
                p_mo = tok_proj(OFF['m_o'], 512)
                P.act(mo.v, p_mo.v, AF.Sigmoid)
                p_mc = nps()
                pmb = p_mc.v.bitcast(BF16)
                for c in range(4):
                    P.tr(pmb[:, c * 128:(c + 1) * 128], mcT[:, c, ts_], C.identb.v)
                P.tt('dve', mct.v, pmb[:, 0:512], skM.v, ALU.mult)
            kq = 0
            if full:
                for h in range(4):
                    psl = slice((h % 2) * 64, (h % 2) * 64 + 64); hp = h // 2
                    p_st = nps()
                    P.mm(p_st[:, 0:128], rkT[psl, hp, ts_], rqT[psl, hp, ts_])
                    P.tt('dve', Pm8[h].v, p_st[:, 0:128], mask.v, ALU.mult)
                for h in range(4):
                    p_st = nps()
                    P.mm(p_st[:, 0:128], kmT[:, h, ts_], qmT[:, h, ts_])
                    P.tt('dve', Pm8[4 + h].v, p_st[:, 0:128], mask.v, ALU.mult)
                for h in range(4):
                    psl = slice((h % 2) * 64, (h % 2) * 64 + 64); hp = h // 2
                    p_o = nps()
                    P.mm(p_o[:, 0:128], Pm8[h].v, vpR[:, h, :], start=True, stop=False)
                    P.mm(p_o[:, 0:128], rqT[psl, hp, ts_], Sretb[psl, hp, :], start=False, stop=True)
                    P.act(ot8[h][:, 0:128], p_o[:, 0:128], AF.Copy, scale=aR[:, h:h + 1])
                for h in range(4):
                    p_o = nps()
                    P.mm(p_o[:, 0:129], Pm8[4 + h].v, vpM[:, h, :], start=True, stop=False)
                    P.mm(p_o[:, 0:129], qmT[:, h, ts_], Cmlb[:, h, :], start=False, stop=True)
                    P.act(ot8[4 + h].v, p_o[:, 0:129], AF.Copy, scale=aM[:, h:h + 1])
            for h in range(4):
                psl = slice((h % 2) * 64, (h % 2) * 64 + 64); hp = h // 2
                p_kv = nps()
                P.mm(p_kv[:, 0:128], rk_tok[:, hp * 128:(hp + 1) * 128], vpR[:, h, :])
                P.tt('dve', tmpS[psl, hp, 0:128], Sret[psl, hp, :], p_kv[psl, 0:128], ALU.add)
                P.act(Sret[psl, hp, :], tmpS[psl, hp, 0:128], AF.Copy, scale=eR[psl, hp:hp + 1])
                P.copy('dve', Sretb[psl, hp, :], Sret[psl, hp, :])
            for h in range(4):
                p_kv = nps()
                P.mm(p_kv[:, 0:129], km_tok[:, h * 128:(h + 1) * 128], vpM[:, h, :])
                P.tt('dve', tmpS[:, h, :], Cml[:, h, :], p_kv[:, 0:129], ALU.add)
                P.act(Cml[:, h, :], tmpS[:, h, :], AF.Copy, scale=eM[:, h:h + 1])
                P.copy('dve', Cmlb[:, h, :], Cml[:, h, :])
            if full:
                for h in range(4):
                    o = ot8[h]; hx = hh[kq % 2]; kq += 1
                    head_norm(P, o[:, 0:128], hx.v, st6, mv, rs)
                    P.tt('pool', hx.v, hx.v, gnR[:, h * 128:(h + 1) * 128], ALU.mult)
                    P.tt('pool', yR[:, h * 128:(h + 1) * 128], hx.v, rg[:, h * 128:(h + 1) * 128], ALU.mult)
                for h in range(4):
                    o = ot8[4 + h]; hx = hh[kq % 2]; kq += 1
                    P.act(dn.v, o[:, 128:129], AF.Abs)
                    P.ts('dve', dn.v, dn.v, 1.0, None, op0=ALU.max)
                    a_ = dn.v.ap
                    P.I('dve', (lambda a_: (lambda e: e.reciprocal(a_, a_)))(a_), w=[dn], r=[dn])
                    P.act(o[:, 0:128], o[:, 0:128], AF.Copy, scale=dn.v)
                    head_norm(P, o[:, 0:128], hx.v, st6, mv, rs)
                    P.tt('pool', hx.v, hx.v, gnM[:, h * 128:(h + 1) * 128], ALU.mult)
                    P.tt('pool', hx.v, hx.v, mct[:, h * 128:(h + 1) * 128], ALU.add)
                    P.tt('pool', yM[:, h * 128:(h + 1) * 128], hx.v, mo[:, h * 128:(h + 1) * 128], ALU.mult)
            if full:
                for (ysrc, ydst) in ((yR, outs[0]), (yM, outs[1])):
                    P.copy('act', ybf.v, ysrc.v)
                    pz = nps(); pzb = pz.v.bitcast(BF16)
                    for c in range(4):
                        P.tr(pzb[:, c * 128:(c + 1) * 128], ybf[:, c * 128:(c + 1) * 128], C.identb.v)
                    yo = yTo[kq % 2]; kq += 1
                    P.copy('dve', yo.v, pzb[:, 0:512].re("p (c t) -> p c t", c=4))
                    P.dma('sp', ydst[:, :, t * 128:(t + 1) * 128], yo.v)
    if not full:
        P.dma('sp', outs[0].v, Sret.v)
        P.dma('sp', outs[1].v, Cml.v)
        P.dma('sp', outs[2].v, totacc.v)


def head_norm(P, src, dst, st6, mv, rs):
    a, b = st6.v.ap, src.ap
    P.I('dve', lambda e: e.bn_stats(a, b), w=[st6], r=[src])
    a2, b2 = mv.v.ap, st6.v.ap
    P.I('dve', lambda e: e.bn_aggr(a2, b2), w=[mv], r=[st6])
    P.ts('dve', rs.v, mv[:, 1:2], EPS, None, op0=ALU.add)
    P.act(rs.v, rs.v, AF.Ln)
    P.act(rs.v, rs.v, AF.Exp, scale=-0.5)
    P.ts('dve', dst, src, mv[:, 0:1], rs.v, op0=ALU.subtract, op1=ALU.mult)


ATT_PAT = ((128, 1), (512, 4), (2048, 16))
HALO = 2048


def host_consts_attn():
    slopes = np.exp2(-8.0 * np.arange(1, 13, dtype=np.float64) / 12.0).reshape(3, 4)
    s = np.arange(128)[:, None]; i = np.arange(128)[None, :]
    out = np.zeros((3, 2, 128, 4, 128), dtype=np.float32)
    for g, (win, d) in enumerate(ATT_PAT):
        for h in range(4):
            dcur = i - s
            b = np.where((dcur >= 0), -slopes[g, h] * d * dcur, -30000.0)
            out[g, 1, :, h, :] = b
            dprev = i + 128 - s
            b = np.where((dprev <= 128), -slopes[g, h] * d * dprev, -30000.0)
            out[g, 0, :, h, :] = b
    return out.reshape(3, 2, 128, 512)


def pass_attn(P, C, NT, xext_d, w_in_l, bias_d, hv_d, yT_out, pfx="a", dbg=0):
    accN = P.sb(pfx + "accN", [128, 4, NT]); accD = P.sb(pfx + "accD", [128, 4, NT])
    for h_ in range(4):
        for j_ in range(NT // 2048):
            P.memset('dve', accN[:, h_, j_ * 2048:(j_ + 1) * 2048], 0.0 if dbg == 0 else 1.0)
            P.memset('dve', accD[:, h_, j_ * 2048:(j_ + 1) * 2048], 0.0 if dbg == 0 else 2.0)
    hv0 = P.sb(pfx + "hv0", [128, 128]); hvb = P.sb(pfx + "hvb", [128, 128], BF16)
    P.dma('act', hv0.v, hv_d); P.copy('dve', hvb.v, hv0.v)
    Wq = P.sb(pfx + "Wq", [128, 8, 256], BF16); Wk = P.sb(pfx + "Wk", [128, 8, 256], BF16); Wv = P.sb(pfx + "Wv", [128, 8, 512], BF16)
    bT = [P.sb(pfx + "bT%d" % i, [128, 512]) for i in range(2)]
    xts = [P.sb(pfx + "xt%d" % i, [128, 1024]) for i in range(2)]
    xTb = [P.sb(pfx + "xTb%d" % i, [128, 8, 128], BF16) for i in range(2)]
    kT = [P.sb(pfx + "kT%d" % i, [128, 2, 128], BF16) for i in range(4)]
    Vt = [P.sb(pfx + "V%d" % i, [128, 512], BF16) for i in range(4)]
    qT = [P.sb(pfx + "qT%d" % i, [128, 2, 128], BF16) for i in range(2)]
    tmp = [P.sb(pfx + "tmp%d" % i, [128, 512]) for i in range(2)]
    PT = [[P.sb(pfx + "PT%d_%d" % (i, j), [128, 512], BF16) for j in range(2)] for i in range(2)]
    nps = PSRot(C, [0, 1, 2, 3, 4, 5, 6, 7])
    win = w_in_l.rearrange("(kc p) c -> p kc c", p=128)
    nb = 0
    for g, (_, d) in enumerate(ATT_PAT if dbg not in (1, 2, 4, 5, 6) else (ATT_PAT[:1] if dbg in (2, 4, 5, 6) else ())):
        load_w_cast(P, Wq.v, win[:, :, OFF['a_q'] + g * 256: OFF['a_q'] + (g + 1) * 256])
        load_w_cast(P, Wk.v, win[:, :, OFF['a_k'] + g * 256: OFF['a_k'] + (g + 1) * 256])
        load_w_cast(P, Wv.v, win[:, :, OFF['a_v'] + g * 512: OFF['a_v'] + (g + 1) * 512])
        P.dma('act', bT[0].v, bias_d[g, 0]); P.dma('act', bT[1].v, bias_d[g, 1])
        NB = NT // (128 * d)
        blocks = [(r, m) for r in range(d) for m in range(-1, NB)]
        nblk = len(blocks)

        def Pst(i):
            r, m = blocks[i]
            s0 = HALO + m * 128 * d + r
            xt = xts[i % 2]; xb = xTb[i % 2]
            P.dma('sp', xt.v, xext_d[s0: s0 + 127 * d + 1: d, :])
            x_transpose(P, C, xt.v, [(xb.v, 'act')], [nps(), nps()])
            for c in range(2):
                pz = nps()
                for kc in range(8):
                    P.mm(pz[:, 0:128], Wk[:, kc, c * 128:(c + 1) * 128], xb[:, kc, :], start=(kc == 0), stop=(kc == 7))
                P.copy('act', kT[i % 4][:, c, :], pz[:, 0:128])
            pz = nps()
            for kc in range(8):
                P.mm(pz.v, xb[:, kc, :], Wv[:, kc, :], start=(kc == 0), stop=(kc == 7))
            P.copy('act', Vt[i % 4].v, pz.v)
            if m < 0:
                return
            for c in range(2):
                pz = nps()
                for kc in range(8):
                    P.mm(pz[:, 0:128], Wq[:, kc, c * 128:(c + 1) * 128], xb[:, kc, :], start=(kc == 0), stop=(kc == 7))
                P.copy('act', qT[i % 2][:, c, :], pz[:, 0:128])

        def Sst(i):
            r, m = blocks[i]
            if m < 0:
                return
            for pc, kb in ((0, (i - 1) % 4), (1, i % 4)):
                psAB = [nps(), nps()]
                for h in range(4):
                    psl = slice((h % 2) * 64, (h % 2) * 64 + 64); hp = h // 2
                    P.mm(psAB[h % 2][:, hp * 128:(hp + 1) * 128], kT[kb][psl, hp, :], qT[i % 2][psl, hp, :])
                tm = tmp[pc]
                for h in range(4):
                    hs = slice(h * 128, (h + 1) * 128); hp = h // 2
                    P.stt('dve', tm[:, hs], psAB[h % 2][:, hp * 128:(hp + 1) * 128], 0.125, bT[pc][:, hs], ALU.mult, ALU.add)
                P.act(PT[i % 2][pc].v, tm.v, AF.Exp)

        def Nst(i):
            r, m = blocks[i]
            if m < 0:
                return
            pn = nps()
            for h in range(4):
                hs = slice(h * 128, (h + 1) * 128)
                P.mm(pn[:, hs], Vt[(i - 1) % 4][:, hs], PT[i % 2][0][:, hs], start=True, stop=False)
                P.mm(pn[:, hs], Vt[i % 4][:, hs], PT[i % 2][1][:, hs], start=False, stop=True)
            pd = nps()
            P.mm(pd.v, (hvb.v if m == 0 else C.onesb.v), PT[i % 2][0].v, start=True, stop=False)
            P.mm(pd.v, C.onesb.v, PT[i % 2][1].v, start=False, stop=True)
            t0 = m * 128 * d + r
            for h in range(4):
                hs = slice(h * 128, (h + 1) * 128)
                av = accN[:, h, t0: t0 + 127 * d + 1: d]
                P.tt('dve', av, av, pn[:, hs], ALU.add)
                dv = accD[:, h, t0: t0 + 127 * d + 1: d]
                P.tt('dve', dv, dv, pd[:, hs], ALU.add)

        Pst(0)
        for i in range(nblk):
            if i + 1 < nblk:
                Pst(i + 1)
            Sst(i)
            if i >= 1:
                Nst(i - 1)
        Nst(nblk - 1)
    yo = [P.sb(pfx + "yo%d" % i, [128, 4, 512], BF16) for i in range(2)]
    rc = [P.sb(pfx + "rc%d" % i, [128, 4, 512]) for i in range(2)]
    for j in range(NT // 512):
        sl = slice(j * 512, (j + 1) * 512)
        for h in range(4):
            a_, b_ = rc[j % 2][:, h, :].ap, accD[:, h, sl].ap
            P.I('dve', (lambda a_, b_: (lambda e: e.reciprocal(a_, b_)))(a_, b_), w=[rc[j % 2]], r=[accD])
            P.tt('dve', yo[j % 2][:, h, :], accN[:, h, sl], rc[j % 2][:, h, :], ALU.mult)
        P.dma('sp', yT_out[:, :, sl], yo[j % 2].v)


NCORES = 8
NT_CORE = 4096
PRM_NAMES = ("ret_gn", "ml_conv_w", "ml_conv_b", "bdq", "bdk", "bdv", "ml_bi", "ml_bf", "ml_gn", "ml_skip")
PRM_SHAPES = dict(ret_gn=[512], ml_conv_w=[4, 512], ml_conv_b=[512], bdq=[4, 128, 128], bdk=[4, 128, 128], bdv=[4, 128, 128],
                  ml_bi=[4], ml_bf=[4], ml_gn=[512], ml_skip=[512])
CST_SHAPES = dict(aR=[128, 4], bR=[128, 4], eR=[128, 2], gR=[128, 2], mask=[128, 128])


def _scan_inputs(nc):
    def inp(n, sh):
        return nc.dram_tensor(n, sh, F32, kind="ExternalInput").ap()
    prm = {n: inp(n, PRM_SHAPES[n]) for n in PRM_NAMES}
    cst = {n: inp(n, CST_SHAPES[n]) for n in CST_SHAPES}
    return prm, cst


def build_A(NT=NT_CORE):
    nc = bass.Bass("TRN2", target_bir_lowering=False)
    with ExitStack() as es:
        P = Prog(nc, es)
        x_d = P.dram("x", [NT, D], F32, kind="ExternalInput")
        xprev = P.dram("xprev", [128, D], F32, kind="ExternalInput")
        w_in = nc.dram_tensor("w_in", [D, D_IN], F32, kind="ExternalInput").ap()
        prm, cst = _scan_inputs(nc)
        outs = [P.dram("oS", [128, 2, 128], F32, kind="ExternalOutput"), P.dram("oC", [128, 4, 129], F32, kind="ExternalOutput"),
                P.dram("oT", [128, 4], F32, kind="ExternalOutput")]
        C = make_ctx(P)
        pass_scan(P, C, NT, x_d, xprev, w_in, prm, cst, None, outs, mode="summary", pfx="s")
        P.wait_all('sp', outs)
        P.emit()
    return nc


def build_B(NT=NT_CORE, NE=NEXP):
    nc = bass.Bass("TRN2", target_bir_lowering=False)
    with ExitStack() as es:
        P = Prog(nc, es)

        def inp(n, sh):
            return nc.dram_tensor(n, sh, F32, kind="ExternalInput").ap()
        xext = P.dram("xext", [HALO + NT, D], F32, kind="ExternalInput")
        w_in = inp("w_in", [D, D_IN])
        prm, cst = _scan_inputs(nc)
        init = dict(Fret=inp("Fret", [3, 128, 2, 128]), Fml=inp("Fml", [3, 128, 4, 129]), totL=inp("totL", [3, 128, 4]),
                    sel=inp("sel", [128, 3]), nsel=inp("nsel", [128, 3]))
        abias = inp("abias", [3, 2, 128, 512]); hv = inp("hv", [128, 128])
        w_br = inp("w_branch", [3, MIXW, D]); w_out = inp("w_out", [D, D])
        ln1_g = inp("ln1_g", [D]); ln1_b = inp("ln1_b", [D]); ln2_g = inp("ln2_g", [D]); ln2_b = inp("ln2_b", [D])
        w_r = inp("w_router", [D, NE]); b_r = inp("b_router", [NE])
        w_g = inp("w_gate", [NE, D, D]); b_g = inp("b_gate", [NE, D])
        w_u = inp("w_up", [NE, D, D]); b_u = inp("b_up", [NE, D])
        w_d = inp("w_down", [NE, D, D]); b_d = inp("b_down", [NE, D])
        out = P.dram("out", [NT, D], F32, kind="ExternalOutput")
        yT = [P.dram("yT%d" % b, [128, 4, NT], BF16) for b in range(3)]
        x1s = P.dram("x1s", [NT, D], F32)
        C = make_ctx(P)
        x_own = Tile(P, xext.h[HALO:HALO + NT, :], "xown", "dram")
        x_own.lw, x_own.rd = xext.lw, xext.rd
        xprev = Tile(P, xext.h[HALO - 128:HALO, :], "xprv", "dram")
        xprev.lw, xprev.rd = xext.lw, xext.rd
        with P.scope():
            pass_attn(P, C, NT, xext, w_in, abias, hv, yT[2], pfx="a")
        with P.scope():
            pass_scan(P, C, NT, x_own, xprev, w_in, prm, cst, init, [yT[0], yT[1]], mode="full", pfx="s")
        with P.scope():
            pass_merge(P, C, NT, x_own, yT, w_in, w_br, w_out, ln1_g, ln1_b, x1s, pfx="m")
        NBLK = NT * 4 // 128 + NE
        Xs = P.dram("Xs", [NBLK * 128, D], BF16); Ys = P.dram("Ys", [NBLK * 128, D], F32)
        mc = {k: inp("mc_" + k, list(v.shape)) for k, v in host_consts_moe(NT, NE).items()}
        with P.scope():
            pass_moe2(P, C, NT, x1s, w_r, b_r, w_g, b_g, w_u, b_u, w_d, b_d, ln2_g, ln2_b, out, Xs, Ys, mc, NE=NE, pfx="f")
        P.wait_all('sp', [out])
        P.emit()
    return nc


def layer_params(inputs, l):
    g = lambda n: np.ascontiguousarray(np.asarray(inputs[n], dtype=np.float32)[l])
    prm = dict(ret_gn=g("ret_gn"), ml_conv_w=g("ml_conv_w"), ml_conv_b=g("ml_conv_b"),
               bdq=host_blockdiag(g("ml_wq")), bdk=host_blockdiag(g("ml_wk")), bdv=host_blockdiag(g("ml_wv")),
               ml_bi=g("ml_bi"), ml_bf=g("ml_bf"), ml_gn=g("ml_gn"), ml_skip=g("ml_skip"))
    big = dict(w_in=g("w_in"), w_branch=g("w_branch"), w_out=g("w_out"), ln1_g=g("ln1_g"), ln1_b=g("ln1_b"),
               ln2_g=g("ln2_g"), ln2_b=g("ln2_b"), w_router=g("w_router"), b_router=g("b_router"),
               w_gate=g("w_gate"), b_gate=g("b_gate"), w_up=g("w_up"), b_up=g("b_up"), w_down=g("w_down"), b_down=g("b_down"))
    return prm, big


def run_layer(ncA, ncB, xs, prm, big, cst, abias, QPB, NT):
    n = len(xs)
    zeros128 = np.zeros((128, D), np.float32)
    inA = []
    for c in range(n):
        q = c % QPB
        m = dict(prm); m.update(cst)
        m["w_in"] = big["w_in"]; m["x"] = xs[c]
        m["xprev"] = np.ascontiguousarray(xs[c - 1][-128:]) if q > 0 else zeros128
        inA.append(m)
    resA = run_bass_kernel_spmd(ncA, inA, core_ids=list(range(n))).results
    inB = []
    for c in range(n):
        q = c % QPB; b0 = c - q
        m = dict(prm); m.update(cst); m.update(big)
        halo = xs[c - 1][-HALO:] if q > 0 else np.zeros((HALO, D), np.float32)
        m["xext"] = np.ascontiguousarray(np.concatenate([halo, xs[c]], axis=0))
        Fret = np.zeros((3, 128, 2, 128), np.float32); Fml = np.zeros((3, 128, 4, 129), np.float32)
        totL = np.zeros((3, 128, 4), np.float32); sel = np.zeros((128, 3), np.float32)
        for qq in range(min(3, QPB)):
            Fret[qq] = resA[b0 + qq]["oS"]; Fml[qq] = resA[b0 + qq]["oC"]; totL[qq] = resA[b0 + qq]["oT"]
            if qq < q:
                sel[:, qq] = 1.0
        m.update(Fret=Fret, Fml=Fml, totL=totL, sel=sel, nsel=(1.0 - sel).astype(np.float32))
        m["abias"] = abias
        for k_, v_ in host_consts_moe(NT, big["w_router"].shape[1]).items():
            m["mc_" + k_] = v_
        m["hv"] = (np.ones((128, 128), np.float32) if q > 0 else np.zeros((128, 128), np.float32))
        inB.append(m)
    resB = run_bass_kernel_spmd(ncB, inB, core_ids=list(range(n))).results
    return [np.asarray(r["out"], dtype=np.float32) for r in resB]


def kernel(**inputs):
    x = np.asarray(inputs["x"], dtype=np.float32)
    B, S, _ = x.shape
    QPB = NCORES // B
    NT = S // QPB
    xs = [np.ascontiguousarray(x[c // QPB, (c % QPB) * NT:(c % QPB + 1) * NT]) for c in range(NCORES)]
    ncA = build_A(NT); ncB = build_B(NT)
    cst = host_consts_scan(NT); abias = host_consts_attn()
    L = np.asarray(inputs["w_in"]).shape[0]
    for l in range(L):
        prm, big = layer_params(inputs, l)
        xs = run_layer(ncA, ncB, xs, prm, big, cst, abias, QPB, NT)
    out = np.zeros((B, S, D), np.float32)
    for c in range(NCORES):
        out[c // QPB, (c % QPB) * NT:(c % QPB + 1) * NT] = xs[c]
    return out


BIGIDX = 4000000.0


def host_consts_moe(NT, NE=NEXP):
    NBLK = NT * 4 // 128 + NE
    p = np.arange(128, dtype=np.float32)
    d = dict(iota_p=p.reshape(128, 1).copy(),
             ustrict=(p[:, None] < p[None, :]).astype(np.float32),
             iotaJ=np.tile(np.arange(33, dtype=np.float32)[None, :], (128, 1)),
             iotaB=np.tile(np.arange(NBLK, dtype=np.float32)[None, :], (128, 1)))
    return d


def _breg(P, e, bound):
    if not hasattr(P, "_bregs"):
        P._bregs = {}
    if bound not in P._bregs:
        P._bregs[bound] = e.to_reg(int(bound))
    return P._bregs[bound]


def ind_dma(P, dst_v, src_tile, src_ap, idx_v, bound, scatter=False, extra_r=()):
    sb_tile = dst_v.tile
    key = P._tsem(sb_tile)
    d_ap, i_ap = dst_v.ap, idx_v.ap
    if not scatter:
        rt = [src_tile, idx_v.tile] + list(extra_r); wt = [sb_tile]

        def f(e):
            try:
                return e.indirect_dma_start(d_ap, None, src_ap, bass.IndirectOffsetOnAxis(i_ap, 0), bounds_check=_breg(P, e, bound), oob_is_err=False)
            except Exception:
                print("IND GATHER FAIL", d_ap, src_ap, i_ap, bound)
                raise
    else:
        rt = [sb_tile, idx_v.tile] + list(extra_r); wt = [src_tile]

        def f(e):
            try:
                return e.indirect_dma_start(src_ap, bass.IndirectOffsetOnAxis(i_ap, 0), d_ap, None, bounds_check=_breg(P, e, bound), oob_is_err=False)
            except Exception:
                print("IND SCATTER FAIL", d_ap, src_ap, i_ap, bound)
                raise
    waits = P._waits('pool', rt, wt)
    sb_tile.dcnt += 16
    P.stream['pool'].append((waits, f, (key, 16)))
    P._record((key, sb_tile.dcnt), rt, wt)
    P.ninst += 1


def pass_moe2(P, C, NT, x1_d, w_router, b_router, w_gate, b_gate, w_up, b_up, w_down, b_down, ln_g, ln_b, out_d,
              Xs, Ys, mc, NE=NEXP, pfx="f", dbg=0):
    NTL = NT // 128
    NBLK = NT * 4 // 128 + NE
    NSLOT = NBLK * 128
    gB = P.sb(pfx + "gB", [128, 1024]); bB = P.sb(pfx + "bB", [128, 1024])
    bcast_rows(P, gB.v, ln_g); bcast_rows(P, bB.v, ln_b)
    wr = P.sb(pfx + "wr", [128, 8, NE], F32R); wr0 = P.sb(pfx + "wr0", [128, 8, NE])
    P.dma('act', wr0.v, w_router.rearrange("(kc p) e -> p kc e", p=128)); P.copy('dve', wr.v, wr0.v)
    brB = P.sb(pfx + "brB", [128, NE]); bcast_rows(P, brB.v, b_router)
    c1 = P.sb(pfx + "c1", [128, 1]); P.memset('dve', c1.v, 1.0)
    iop = P.sb(pfx + "iop", [128, 1]); P.dma('act', iop.v, mc["iota_p"])
    us0 = P.sb(pfx + "us0", [128, 128]); P.dma('act', us0.v, mc["ustrict"])
    usb = P.sb(pfx + "usb", [128, 128], BF16); P.copy('dve', usb.v, us0.v)
    ioJ = P.sb(pfx + "ioJ", [128, 33]); P.dma('act', ioJ.v, mc["iotaJ"])
    ioB = P.sb(pfx + "ioB", [128, NBLK]); P.dma('act', ioB.v, mc["iotaB"])
    lg_all = P.sb(pfx + "lg_all", [128, NTL, NE]); t8_all = P.sb(pfx + "t8_all", [128, NTL, 8])
    pall = P.sb(pfx + "pall", [128, NTL, NE]); pos_all = P.sb(pfx + "pos_all", [128, NTL, NE])
    slot_f = P.sb(pfx + "slot_f", [128, NTL, 4]); slot_i = P.sb(pfx + "slot_i", [128, NTL * 4], I32)
    pk_all = P.sb(pfx + "pk_all", [128, NTL, 4])
    carry = P.sb(pfx + "carry", [128, NE]); P.memset('dve', carry.v, 0.0)
    xts = [P.sb(pfx + "xt%d" % i, [128, 1024]) for i in range(2)]
    xT32 = P.sb(pfx + "xT32", [128, 8, 128], F32R)
    msk = P.sb(pfx + "msk", [128, NE]); mskb = P.sb(pfx + "mskb", [128, NE], BF16)
    ex = P.sb(pfx + "ex", [128, NE]); sm = P.sb(pfx + "sm", [128, 1]); nmx = P.sb(pfx + "nmx", [128, 1])
    nps = PSRot(C, [0, 1, 2, 3, 4, 5, 6, 7])
    for t in range(NTL):
        xt = xts[t % 2]
        P.dma('sp', xt.v, x1_d[t * 128:(t + 1) * 128, :])
        x_transpose(P, C, xt.v, [(xT32.v, 'dve')], [nps(), nps()])
        pl = nps()
        for kc in range(8):
            P.mm(pl[:, 0:NE], xT32[:, kc, :], wr[:, kc, :], start=(kc == 0), stop=(kc == 7))
        lg = lg_all[:, t, :]; t8 = t8_all[:, t, :]
        P.tt('dve', lg, pl[:, 0:NE], brB.v, ALU.add)
        a, b = t8.ap, lg.ap
        P.I('dve', (lambda a, b: (lambda e: e.max(out=a, in_=b)))(a, b), w=[t8_all], r=[lg_all])
        P.ts('dve', msk.v, lg, t8_all[:, t, 3:4], c1.v, op0=ALU.is_ge, op1=ALU.mult)
        P.ts('dve', nmx.v, t8_all[:, t, 0:1], -1.0, None, op0=ALU.mult)
        P.act(ex.v, lg, AF.Exp, bias=nmx.v)
        P.tt('dve', ex.v, ex.v, msk.v, ALU.mult)
        a2, b2 = sm.v.ap, ex.v.ap
        P.I('dve', (lambda a, b: (lambda e: e.reduce_sum(a, b, AX.X)))(a2, b2), w=[sm], r=[ex])
        a3 = sm.v.ap
        P.I('dve', (lambda a: (lambda e: e.reciprocal(a, a)))(a3), w=[sm], r=[sm])
        P.ts('dve', pall[:, t, :], ex.v, sm.v, c1.v, op0=ALU.mult, op1=ALU.mult)
        P.copy('dve', mskb.v, msk.v)
        pr = nps()
        P.mm(pr[:, 0:NE], usb.v, mskb.v)
        P.mm(pr[:, 64:64 + NE], C.onesb.v, mskb.v)
        P.tt('dve', pos_all[:, t, :], carry.v, pr[:, 0:NE], ALU.add)
        P.tt('dve', carry.v, carry.v, pr[:, 64:64 + NE], ALU.add)
    q = P.sb(pfx + "q", [128, NE]); nb = P.sb(pfx + "nb", [128, NE]); bend = P.sb(pfx + "bend", [128, NE])
    pstart = P.sb(pfx + "pstart", [128, NE]); t33 = P.sb(pfx + "t33", [128, 33])
    P.ts('dve', q.v, carry.v, 1.0 / 128.0, None, op0=ALU.mult)
    for e in range(NE):
        P.ts('dve', t33.v, ioJ.v, q[:, e:e + 1], c1.v, op0=ALU.is_lt, op1=ALU.mult)
        a_, b_ = nb[:, e:e + 1].ap, t33.v.ap
        P.I('dve', (lambda a, b: (lambda e_: e_.reduce_sum(a, b, AX.X)))(a_, b_), w=[nb], r=[t33])
    P.copy('dve', bend[:, 0:1], nb[:, 0:1])
    for e in range(1, NE):
        P.tt('dve', bend[:, e:e + 1], bend[:, e - 1:e], nb[:, e:e + 1], ALU.add)
    P.tt('dve', pstart.v, bend.v, nb.v, ALU.subtract)
    P.ts('dve', pstart.v, pstart.v, 128.0, None, op0=ALU.mult)
    Eall = P.sb(pfx + "Eall", [128, NBLK]); tB = P.sb(pfx + "tB", [128, NBLK]); sk = P.sb(pfx + "sk", [128, NBLK])
    P.memset('dve', Eall.v, 0.0)
    for e in range(NE):
        P.ts('dve', tB.v, ioB.v, bend[:, e:e + 1], c1.v, op0=ALU.is_ge, op1=ALU.mult)
        P.tt('dve', Eall.v, Eall.v, tB.v, ALU.add)
    P.ts('dve', Eall.v, Eall.v, float(NE - 1), None, op0=ALU.min)
    P.memset('dve', sk.v, 0.0)
    P.tt('dve', sk[:, 2:NBLK], Eall[:, 2:NBLK], Eall[:, 0:NBLK - 2], ALU.is_equal)
    P.ts('dve', sk.v, sk.v, BIGIDX, None, op0=ALU.mult)
    idxWf = P.sb(pfx + "idxWf", [128, NBLK]); idxW = P.sb(pfx + "idxW", [128, 8 * NBLK], I32)
    idxBf = P.sb(pfx + "idxBf", [128, NBLK]); idxB = P.sb(pfx + "idxB", [128, NBLK], I32)
    P.tt('dve', idxBf.v, Eall.v, sk.v, ALU.add)
    P.copy('dve', idxB.v, idxBf.v)
    iop4 = P.sb(pfx + "iop4", [128, 1]); P.ts('dve', iop4.v, iop.v, 4.0, None, op0=ALU.mult)
    P.ts('dve', idxWf.v, Eall.v, 512.0, None, op0=ALU.mult)
    P.tt('dve', idxWf.v, idxWf.v, sk.v, ALU.add)
    P.ts('dve', idxWf.v, idxWf.v, iop4.v, c1.v, op0=ALU.add, op1=ALU.mult)
    for kq in range(4):
        P.ts('dve', tB.v, idxWf.v, float(kq), None, op0=ALU.add)
        P.copy('dve', idxW[:, kq * NBLK:(kq + 1) * NBLK], tB.v)
    OH = P.sb(pfx + "OH", [128, NBLK]); P.ts('dve', OH.v, Eall.v, iop.v, c1.v, op0=ALU.is_equal, op1=ALU.mult)
    if dbg == 2:
        return
    xb16 = [P.sb(pfx + "xb16_%d" % i, [128, 1024], BF16) for i in range(2)]
    tA = P.sb(pfx + "tA", [128, NE]); oh = P.sb(pfx + "oh", [128, NE]); pr1 = P.sb(pfx + "pr1", [128, NE])
    Xs2 = Xs.h[:]
    for t in range(NTL):
        xt = xts[t % 2]; xb = xb16[t % 2]
        P.dma('sp', xt.v, x1_d[t * 128:(t + 1) * 128, :])
        P.copy('act', xb.v, xt.v)
        P.tt('dve', tA.v, pos_all[:, t, :], pstart.v, ALU.add)
        for k in range(4):
            P.ts('dve', oh.v, lg_all[:, t, :], t8_all[:, t, k:k + 1], c1.v, op0=ALU.is_equal, op1=ALU.mult)
            P.tt('dve', pr1.v, oh.v, tA.v, ALU.mult)
            a_, b_ = slot_f[:, t, k:k + 1].ap, pr1.v.ap
            P.I('dve', (lambda a, b: (lambda e_: e_.reduce_sum(a, b, AX.X)))(a_, b_), w=[slot_f], r=[pr1])
            P.tt('dve', pr1.v, oh.v, pall[:, t, :], ALU.mult)
            a_, b_ = pk_all[:, t, k:k + 1].ap, pr1.v.ap
            P.I('dve', (lambda a, b: (lambda e_: e_.reduce_sum(a, b, AX.X)))(a_, b_), w=[pk_all], r=[pr1])
        P.copy('dve', slot_i[:, t * 4:(t + 1) * 4], slot_f[:, t, :])
        for k in range(4):
            ind_dma(P, xb.v, Xs, Xs2, slot_i[:, t * 4 + k: t * 4 + k + 1], NSLOT - 1, scatter=True)
    if dbg == 3:
        P.wait_all('sp', [Xs])
        return
    with P.scope():
        inv128 = P.sb(pfx + 'inv128', [128, 128], BF16); P.memset('dve', inv128.v, 1.0 / 128.0)
        Wq = [[[P.sb(pfx + "W%d_%d_%d" % (j, i, kq), [128, 2, 1024], BF16) for kq in range(4)] for j in range(3)] for i in range(2)]
        Wt = [[[Wq[i][j][kc // 2][:, kc % 2, :] for kc in range(8)] for j in range(3)] for i in range(2)]
        Ball = [P.sb(pfx + "Ball%d" % j, [NE, 1024], BF16) for j in range(3)]
        for j, bsrc_ in enumerate((b_gate, b_up, b_down)):
            P.dma('pool', Ball[j].v, bsrc_)
        OHb = [P.sb(pfx + "OHb%d" % i, [NE, 128], BF16) for i in range(2)]
        wsrc = [w_.rearrange("e (p kq r) f -> (e p kq) (r f)", p=128, kq=4, r=2) for w_ in (w_gate, w_up, w_down)]
        bsrc = [b_gate, b_up, b_down]
        wtile = [Tile(P, None, pfx + "wsrc%d" % j, "dram") for j in range(3)]
        xblk = [P.sb(pfx + "xblk%d" % i, [128, 1024], BF16) for i in range(2)]
        xbT = [P.sb(pfx + "xbT%d" % i, [128, 8, 128], BF16) for i in range(2)]
        actT = [P.sb(pfx + "actT%d" % i, [128, 8, 128], BF16) for i in range(2)]
        gt = [P.sb(pfx + "g%d" % i, [128, 512]) for i in range(2)]
        s_t = [P.sb(pfx + "s%d" % i, [128, 512]) for i in range(2)]
        ut = [P.sb(pfx + "u%d" % i, [128, 512]) for i in range(2)]
        atok = [P.sb(pfx + "atok%d" % i, [128, 1024], BF16) for i in range(2)]
        yb = [P.sb(pfx + "yb%d" % i, [128, 1024]) for i in range(2)]
        kkc = [0]

        def stageXg(b):
            cur = b % 2
            for j in range(3):
                for kq in range(4):
                    ind_dma(P, Wq[cur][j][kq].v.re("p r f -> p (r f)"), wtile[j], wsrc[j],
                            idxW[:, kq * NBLK + b: kq * NBLK + b + 1], NE * 512 - 1)
            Wg, Wu, Wd = Wt[cur]
            P.copy('dve', OHb[cur].v, OH[0:NE, b:b + 1].bc([NE, 128]))

        def stageXx(b):
            cur = b % 2
            xk = xblk[cur]
            if dbg != 9 or b < 2:
                P.dma('sp', xk.v, Xs[b * 128:(b + 1) * 128, :])
            xT_ = xbT[cur]
            for half in range(2):
                pz = nps(); pzb = pz.v.bitcast(BF16)
                for c in range(4):
                    kc = half * 4 + c
                    P.tr(pzb[:, c * 128:(c + 1) * 128], xk[:, kc:1024:8], C.identb.v)
                P.copy('act', xT_[:, half * 4:(half + 1) * 4, :], pzb[:, 0:512].re("p (c t) -> p c t", c=4))

        def stageG(b):
            cur = b % 2
            Wg, Wu, Wd = Wt[cur]
            xT_ = xbT[cur]
            aT = actT[cur]
            at = atok[cur]
            for hf in range(2 if dbg not in (6, 10) else 0):
                hs = slice(hf * 512, (hf + 1) * 512)
                pg = nps()
                for kc in range(8):
                    P.mm(pg.v, xT_[:, kc, :], Wg[kc][:, hs], start=(kc == 0), stop=False)
                P.mm(pg.v, OHb[cur].v, Ball[0][:, hs], start=False, stop=True)
                pu = nps()
                for kc in range(8):
                    P.mm(pu.v, xT_[:, kc, :], Wu[kc][:, hs], start=(kc == 0), stop=False)
                P.mm(pu.v, OHb[cur].v, Ball[1][:, hs], start=False, stop=True)
                g = gt[kkc[0] % 2]; s_ = s_t[kkc[0] % 2]; u = ut[kkc[0] % 2]; kkc[0] += 1
                P.ts('dve', g.v, pg.v, 7.0, None, op0=ALU.min)
                P.act(s_.v, g.v, AF.Sigmoid, scale=1.702)
                P.ts('dve', u.v, pu.v, 7.0, -7.0, op0=ALU.min, op1=ALU.max)
                P.tt('dve', g.v, g.v, s_.v, ALU.mult)
                P.stt('dve', at[:, hs], u.v, 1.0, g.v, ALU.add, ALU.mult)

        def stageT(b):
            cur = b % 2
            Wg, Wu, Wd = Wt[cur]
            aT = actT[cur]
            at = atok[cur]
            for half in range(2 if dbg not in (6, 10) else 0):
                pz = nps(); pzb = pz.v.bitcast(BF16)
                for c in range(4):
                    fc = half * 4 + c
                    P.tr(pzb[:, c * 128:(c + 1) * 128], at[:, fc:1024:8], C.identb.v)
                P.copy('act', aT[:, half * 4:(half + 1) * 4, :], pzb[:, 0:512].re("p (c t) -> p c t", c=4))

        def stageDn(b):
            cur = b % 2
            Wg, Wu, Wd = Wt[cur]
            aT = actT[cur]
            y = yb[cur]
            if dbg in (6, 10):
                P.memset('dve', y.v, 0.5)
            for hf in range(2 if dbg not in (6, 10) else 0):
                py = nps()
                hs = slice(hf * 512, (hf + 1) * 512)
                for fc in range(8):
                    P.mm(py.v, aT[:, fc, :], Wd[fc][:, hs], start=(fc == 0), stop=False)
                P.mm(py.v, OHb[cur].v, Ball[2][:, hs], start=False, stop=True)
                P.copy('act', y[:, hs], py.v)
            if dbg != 7:
                P.dma('sp', Ys[b * 128:(b + 1) * 128, :], y.v)
        nblk_ = NBLK if dbg != 11 else 10
        stageXx(0)
        for b in range(nblk_ + 1):
            if b < nblk_:
                stageXg(b)
                stageG(b)
            if b >= 1:
                stageT(b - 1)
            if b + 1 < nblk_:
                stageXx(b + 1)
            if b >= 1:
                stageDn(b - 1)
    if dbg == 4:
        return
    rk = [P.sb(pfx + "rk%d" % i, [128, 1024]) for i in range(4)]
    acA = P.sb(pfx + "acA", [128, 1024]); acB = P.sb(pfx + "acB", [128, 1024])
    st = P.sb(pfx + "st", [128, 2, 6]); mv = P.sb(pfx + "mv", [128, 2]); rstd = P.sb(pfx + "rstd", [128, 1])
    Ys2 = Ys.h[:]
    for t in range(NTL):
        xt = xts[t % 2]
        P.dma('sp', xt.v, x1_d[t * 128:(t + 1) * 128, :])
        for k in range(4):
            ind_dma(P, rk[k].v, Ys, Ys2, slot_i[:, t * 4 + k: t * 4 + k + 1], NSLOT - 1)
        P.ts('dve', acA.v, rk[0].v, pk_all[:, t, 0:1], c1.v, op0=ALU.mult, op1=ALU.mult)
        P.stt('dve', acB.v, rk[1].v, pk_all[:, t, 1:2], acA.v, ALU.mult, ALU.add)
        P.stt('dve', acA.v, rk[2].v, pk_all[:, t, 2:3], acB.v, ALU.mult, ALU.add)
        P.stt('dve', acB.v, rk[3].v, pk_all[:, t, 3:4], acA.v, ALU.mult, ALU.add)
        P.stt('dve', acA.v, xt.v, DN_ALPHA, acB.v, ALU.mult, ALU.add)
        layer_norm(P, acA.v, gB.v, bB.v, st.v, mv.v, rstd.v)
        P.dma('sp', out_d[t * 128:(t + 1) * 128, :], acA.v)
```

```python
import numpy as np
import concourse.bass as bass
import concourse.mybir as mybir
from concourse.bass_utils import run_bass_kernel_spmd
from contextlib import ExitStack

F32 = mybir.dt.float32
F32R = mybir.dt.float32r
BF16 = mybir.dt.bfloat16
I32 = mybir.dt.int32
U32 = mybir.dt.uint32
AF = mybir.ActivationFunctionType
ALU = mybir.AluOpType
AX = mybir.AxisListType

ENGS = ['pe', 'act', 'dve', 'pool', 'sp']


class Tile:
    def __init__(self, P, h, name, space='sb'):
        self.P = P
        self.h = h
        self.name = name
        self.space = space
        self.lw = {}
        self.rd = {}
        self.dsem = None
        self.dcnt = 0

    def __getitem__(self, k):
        return V(self, self.h[k])

    @property
    def v(self):
        return V(self, self.h[:])


class V:
    def __init__(self, tile, ap):
        self.tile = tile
        self.ap = ap

    def __getitem__(self, k):
        return V(self.tile, self.ap[k])

    def bitcast(self, dt):
        return V(self.tile, self.ap.bitcast(dt))

    def bc(self, shape):
        return V(self.tile, self.ap.to_broadcast(shape))

    def re(self, s, **kw):
        return V(self.tile, self.ap.rearrange(s, **kw))


def _ap(x):
    if isinstance(x, Tile):
        return x.h[:]
    return x.ap if isinstance(x, V) else x


class Prog:
    def __init__(self, nc, es, same_engine_sync=None):
        self.nc = nc
        self.es = es
        self.es_top = es
        self.all_tiles = []
        self.stream = {e: [] for e in ENGS}
        self.sems = {}
        self.cnt = {e: 0 for e in ENGS}
        self.known = {e: {} for e in ENGS}
        import os as _os
        self.same = (_os.environ.get('KSAME', '1') == '1') if same_engine_sync is None else same_engine_sync
        self.nsem = 0
        for e in ['pe', 'act', 'dve', 'pool']:
            self.sems[e] = es.enter_context(nc.semaphore("s_" + e))
            self.nsem += 1
        self.ninst = 0
        self.nwait = 0

    def sb(self, name, shape, dt=F32):
        h = self.es.enter_context(self.nc.sbuf_tensor(name, list(shape), dt))
        return Tile(self, h, name)

    def ps(self, name, shape, dt=F32):
        h = self.es.enter_context(self.nc.psum_tensor(name, list(shape), dt))
        return Tile(self, h, name, 'ps')

    def dram(self, name, shape, dt=F32, kind="Internal"):
        h = self.nc.dram_tensor(name, list(shape), dt, kind=kind)
        return Tile(self, h.ap(), name, 'dram')

    def _tsem(self, t):
        if t.dsem is None:
            key = "d_" + t.name
            self.sems[key] = self.es_top.enter_context(self.nc.semaphore(key))
            self.nsem += 1
            t.dsem = key
            self.all_tiles.append(t)
        return t.dsem

    def _waits(self, eng, rt, wt):
        need = {}
        for t in rt:
            for s, v in t.lw.items():
                need[s] = max(need.get(s, 0), v)
        for t in wt:
            for s, v in t.lw.items():
                need[s] = max(need.get(s, 0), v)
            for s, v in t.rd.items():
                need[s] = max(need.get(s, 0), v)
        out = []
        kn = self.known[eng]
        for s, v in need.items():
            if s == eng and (eng == 'pe' or not self.same):
                continue
            if kn.get(s, 0) < v:
                kn[s] = v
                out.append((s, v))
        return out

    def _record(self, ev, rt, wt):
        s, v = ev
        for t in wt:
            t.lw[s] = max(t.lw.get(s, 0), v)
            t.rd = {}
        for t in rt:
            if t in wt:
                continue
            t.rd[s] = max(t.rd.get(s, 0), v)

    @staticmethod
    def _tiles(xs):
        out = []
        for x in xs:
            if x is None:
                continue
            t = x.tile if isinstance(x, V) else x
            if isinstance(t, Tile) and t not in out:
                out.append(t)
        return out

    def I(self, eng, fn, w=(), r=()):
        wt = self._tiles(w)
        rt = self._tiles(r)
        for t in rt:
            if t.space == 'ps' and t not in wt and eng != 'pe':
                wt.append(t)
        waits = self._waits(eng, rt, wt)
        self.cnt[eng] += 1
        ev = (eng, self.cnt[eng])
        self.stream[eng].append((waits, fn, (eng, 1)))
        self._record(ev, rt, wt)
        self.ninst += 1
        self.nwait += len(waits)

    def dma(self, q, out, in_, **kw):
        wt = self._tiles([out])
        rt = self._tiles([in_])
        owner = None
        for x in (out, in_):
            t_ = x.tile if isinstance(x, V) else (x if isinstance(x, Tile) else None)
            if t_ is not None and t_.space == 'sb':
                owner = t_
        if owner is None:
            for x in (out, in_):
                t_ = x.tile if isinstance(x, V) else (x if isinstance(x, Tile) else None)
                if t_ is not None and owner is None:
                    owner = t_
        key = self._tsem(owner)
        waits = self._waits(q, rt, wt)
        owner.dcnt += 16
        ev = (key, owner.dcnt)
        o, i = _ap(out), _ap(in_)
        self.stream[q].append((waits, lambda e: e.dma_start(out=o, in_=i, **kw), (key, 16)))
        self._record(ev, rt, wt)
        self.ninst += 1
        self.nwait += len(waits)
        return ev

    def wait_all(self, eng, tiles):
        ts = self._tiles(tiles)
        waits = self._waits(eng, ts, ts)
        self.stream[eng].append((waits, None, None))

    def mm(self, out, lhsT, rhs, start=True, stop=True, **kw):
        o, a, b = _ap(out), _ap(lhsT), _ap(rhs)
        self.I('pe', lambda e: e.matmul(o, a, b, start=start, stop=stop, **kw), w=[out], r=[lhsT, rhs])

    def tr(self, out, in_, ident):
        o, a, b = _ap(out), _ap(in_), _ap(ident)
        self.I('pe', lambda e: e.transpose(o, a, b), w=[out], r=[in_, ident])

    def act(self, out, in_, func, bias=None, scale=1.0, accum_out=None, eng='act'):
        o, a = _ap(out), _ap(in_)
        kw = {}
        if bias is not None:
            kw['bias'] = _ap(bias)
        if accum_out is not None:
            kw['accum_out'] = _ap(accum_out)
        sc = _ap(scale)
        self.I(eng, lambda e: e.activation(o, a, func, scale=sc, **kw),
               w=[out, accum_out], r=[in_, bias, scale if isinstance(scale, V) else None])

    def tt(self, eng, out, in0, in1, op):
        o, a, b = _ap(out), _ap(in0), _ap(in1)
        self.I(eng, lambda e: e.tensor_tensor(o, a, b, op), w=[out], r=[in0, in1])

    def ts(self, eng, out, in0, s1, s2=None, op0=ALU.mult, op1=None, accum_out=None):
        o, a = _ap(out), _ap(in0)
        x1, x2 = _ap(s1), _ap(s2)
        kw = {}
        if op1 is not None:
            kw['op1'] = op1
        if accum_out is not None:
            kw['accum_out'] = _ap(accum_out)
        self.I(eng, lambda e: e.tensor_scalar(o, a, x1, x2, op0, **kw), w=[out, accum_out],
               r=[in0, s1 if isinstance(s1, V) else None, s2 if isinstance(s2, V) else None])

    def stt(self, eng, out, in0, scalar, in1, op0, op1):
        o, a, b = _ap(out), _ap(in0), _ap(in1)
        s = _ap(scalar)
        self.I(eng, lambda e: e.scalar_tensor_tensor(o, a, s, b, op0, op1), w=[out],
               r=[in0, in1, scalar if isinstance(scalar, V) else None])

    def copy(self, eng, out, in_):
        o, a = _ap(out), _ap(in_)
        if eng == 'act':
            self.I(eng, lambda e: e.copy(o, a), w=[out], r=[in_])
        else:
            self.I(eng, lambda e: e.tensor_copy(o, a), w=[out], r=[in_])

    def memset(self, eng, out, val):
        o = _ap(out)
        self.I(eng, lambda e: e.memset(o, val), w=[out])

    def barrier(self):
        evs = {e: self.cnt[e] for e in ['pe', 'act', 'dve', 'pool'] if self.cnt[e] > 0}
        for t in self.all_tiles:
            if t.dcnt > 0:
                evs[t.dsem] = t.dcnt
        for eng in ENGS:
            kn = self.known[eng]
            waits = []
            for s_, v in evs.items():
                if kn.get(s_, 0) < v:
                    kn[s_] = v
                    waits.append((s_, v))
            if waits:
                self.stream[eng].append((waits, None, None))

    def scope(self):
        P = self

        class _S:
            def __enter__(self_):
                self_.old = P.es
                self_.st = ExitStack()
                self_.st.__enter__()
                P.es = self_.st
                return self_

            def __exit__(self_, *a):
                P.barrier()
                P.emit()
                P.es = self_.old
                self_.st.__exit__(None, None, None)
                return False
        return _S()

    def emit(self):
        nc = self.nc
        sems = self.sems
        with nc.Block() as block:
            def run(engobj, name):
                for waits, fn, inc in self.stream[name]:
                    for s, v in waits:
                        engobj.wait_ge(sems[s], v)
                    if fn is not None:
                        ins = fn(engobj)
                        ins.then_inc(sems[inc[0]], inc[1])

            @block.tensor
            def _(e):
                run(e, 'pe')

            @block.scalar
            def _(e):
                run(e, 'act')

            @block.vector
            def _(e):
                run(e, 'dve')

            @block.gpsimd
            def _(e):
                run(e, 'pool')

            @block.sync
            def _(e):
                run(e, 'sp')
        self.stream = {e: [] for e in ENGS}


D = 1024
MIXW = 512
NEXP = 32
DN_ALPHA = (2.0 * 2) ** 0.25
EPS = 1e-5
OFF = dict(r_q=0, r_k=256, r_v=512, r_g=1024, m_x=1536, m_i=2048, m_f=2052, m_o=2056,
           a_q=2568, a_k=3336, a_v=4104, gates=5640)
D_IN = 8712


class Ctx:
    pass


def make_ctx(P):
    C = Ctx()
    C.identf = P.sb("identf", [128, 128], F32)
    C.identb = P.sb("identb", [128, 128], BF16)
    C.onesb = P.sb("onesb", [128, 128], BF16)
    C.onesf = P.sb("onesf", [128, 128], F32)
    P.memset('pool', C.identf.v, 1.0)
    o = C.identf.v.ap
    P.I('pool', lambda e: e.affine_select(o, o, [[-1, 128]], ALU.is_equal, 0.0, base=0, channel_multiplier=1),
        w=[C.identf], r=[C.identf])
    P.copy('pool', C.identb.v, C.identf.v)
    P.memset('pool', C.onesb.v, 1.0)
    P.memset('pool', C.onesf.v, 1.0)
    C.ps = [P.ps("psb%d" % i, [128, 512], F32) for i in range(8)]
    return C


def load_w_cast(P, dst, src, q='pool'):
    cols = src.shape[-1]
    c0 = 0
    while c0 < cols:
        c1 = min(cols, c0 + 1024)
        P.dma(q, dst[:, :, c0:c1], src[:, :, c0:c1])
        c0 = c1


def bcast_rows(P, dst, src1d, q='act'):
    P.dma(q, dst, src1d.partition_broadcast(128))


def x_transpose(P, C, xt, outs, psl):
    for half in range(2):
        pt = psl[half]
        for c in range(4):
            k = half * 4 + c
            P.tr(pt[:, c * 128:(c + 1) * 128], xt[:, k * 128:(k + 1) * 128], C.identf.v)
        for (o, eng) in outs:
            P.copy(eng, o[:, half * 4:(half + 1) * 4, :], pt.v.re("p (c t) -> p c t", c=4))


def layer_norm(P, r, g_b, b_b, st, mv, rstd, eng2='pool'):
    for hf in range(2):
        a, b = st[:, hf, :].ap, r[:, hf * 512:(hf + 1) * 512].ap
        P.I('dve', (lambda a, b: (lambda e: e.bn_stats(a, b)))(a, b), w=[st], r=[r])
    a, b = mv.ap, st.ap
    P.I('dve', lambda e: e.bn_aggr(a, b), w=[mv], r=[st])
    P.ts('dve', rstd, mv[:, 1:2], EPS, None, op0=ALU.add)
    P.act(rstd, rstd, AF.Ln)
    P.act(rstd, rstd, AF.Exp, scale=-0.5)
    P.ts('dve', r, r, mv[:, 0:1], rstd, op0=ALU.subtract, op1=ALU.mult)
    P.tt(eng2, r, r, g_b, ALU.mult)
    P.tt(eng2, r, r, b_b, ALU.add)


def pass_merge(P, C, NT, x_d, yT_d, w_in_l, w_branch_l, w_out_l, ln_g, ln_b, x1_d, pfx="m"):
    wg = P.sb(pfx + "wg", [128, 8, 3072], BF16)
    wb = P.sb(pfx + "wb", [128, 12, 1024], BF16)
    wo = P.sb(pfx + "wo", [128, 8, 1024], BF16)
    load_w_cast(P, wg.v, w_in_l[:, OFF['gates']:D_IN].rearrange("(kc p) c -> p kc c", p=128))
    load_w_cast(P, wb.v, w_branch_l.rearrange("b (kc p) c -> p (b kc) c", p=128))
    load_w_cast(P, wo.v, w_out_l.rearrange("(kc p) c -> p kc c", p=128))
    gB = P.sb(pfx + "gB", [128, 1024]); bB = P.sb(pfx + "bB", [128, 1024])
    bcast_rows(P, gB.v, ln_g); bcast_rows(P, bB.v, ln_b)
    xts = [P.sb(pfx + "xt%d" % i, [128, 1024]) for i in range(2)]
    xTs = [P.sb(pfx + "xT%d" % i, [128, 8, 128], BF16) for i in range(2)]
    yTs = [[P.sb(pfx + "yT%d_%d" % (b, i), [128, 4, 128], BF16) for i in range(2)] for b in range(3)]
    mg = [P.sb(pfx + "mg%d" % i, [128, 1024]) for i in range(2)]
    mT = [P.sb(pfx + "mT%d" % i, [128, 8, 128], BF16) for i in range(2)]
    sg = [P.sb(pfx + "sg%d" % i, [128, 512]) for i in range(2)]
    tmp = [P.sb(pfx + "tmp%d" % i, [128, 512]) for i in range(2)]
    rr = [P.sb(pfx + "rr%d" % i, [128, 1024]) for i in range(2)]
    st = P.sb(pfx + "st", [128, 2, 6]); mv = P.sb(pfx + "mv", [128, 2]); rstd = P.sb(pfx + "rstd", [128, 1])
    kc_ = [0]

    def S1(t):
        xt = xts[t % 2]; xT = xTs[t % 2]
        P.dma('sp', xt.v, x_d[t * 128:(t + 1) * 128, :])
        x_transpose(P, C, xt.v, [(xT.v, 'act')], [C.ps[0], C.ps[1]])
        for b in range(3):
            P.dma('act', yTs[b][t % 2].v, yT_d[b][:, :, t * 128:(t + 1) * 128])
        m = mg[t % 2]
        for b in range(3):
            for hf in range(2):
                pg = C.ps[2 + (kc_[0] % 2)]; pb = C.ps[4 + (kc_[0] % 2)]; s = sg[kc_[0] % 2]; tm = tmp[kc_[0] % 2]
                kc_[0] += 1
                for kc in range(8):
                    P.mm(pg.v, xT[:, kc, :], wg[:, kc, b * 1024 + hf * 512: b * 1024 + (hf + 1) * 512],
                         start=(kc == 0), stop=(kc == 7))
                for kc in range(4):
                    P.mm(pb.v, yTs[b][t % 2][:, kc, :], wb[:, b * 4 + kc, hf * 512:(hf + 1) * 512],
                         start=(kc == 0), stop=(kc == 3))
                P.act(s.v, pg.v, AF.Sigmoid)
                msl = m[:, hf * 512:(hf + 1) * 512]
                if b == 0:
                    P.tt('dve', msl, s.v, pb.v, ALU.mult)
                else:
                    P.tt('dve', tm.v, s.v, pb.v, ALU.mult)
                    P.tt('pool', msl, msl, tm.v, ALU.add)

    def S2(t):
        xt = xts[t % 2]
        m = mg[t % 2]
        x_transpose(P, C, m.v, [(mT[t % 2].v, 'act')], [C.ps[6], C.ps[7]])
        r = rr[t % 2]
        for hf in range(2):
            po = C.ps[2 + (kc_[0] % 2)]
            kc_[0] += 1
            for kc in range(8):
                P.mm(po.v, mT[t % 2][:, kc, :], wo[:, kc, hf * 512:(hf + 1) * 512], start=(kc == 0), stop=(kc == 7))
            P.stt('dve', r[:, hf * 512:(hf + 1) * 512], xt[:, hf * 512:(hf + 1) * 512], DN_ALPHA, po.v,
                  ALU.mult, ALU.add)
        layer_norm(P, r.v, gB.v, bB.v, st.v, mv.v, rstd.v)
        P.dma('sp', x1_d[t * 128:(t + 1) * 128, :], r.v)

    ntl_ = NT // 128
    S1(0)
    for t in range(ntl_):
        if t + 1 < ntl_:
            S1(t + 1)
        S2(t)


def pass_moe(P, C, NT, x1_d, w_router, b_router, w_gate, b_gate, w_up, b_up, w_down, b_down, ln_g, ln_b, out_d,
             NE=NEXP, pfx="e", TGT=4, dbg=0):
    TG = TGT * 128
    gB = P.sb(pfx + "gB", [128, 1024]); bB = P.sb(pfx + "bB", [128, 1024])
    bcast_rows(P, gB.v, ln_g); bcast_rows(P, bB.v, ln_b)
    wr = P.sb(pfx + "wr", [128, 8, NE], F32R)
    wr0 = P.sb(pfx + "wr0", [128, 8, NE])
    P.dma('act', wr0.v, w_router.rearrange("(kc p) e -> p kc e", p=128))
    P.copy('dve', wr.v, wr0.v)
    brB = P.sb(pfx + "brB", [128, NE]); bcast_rows(P, brB.v, b_router)
    bgT = P.sb(pfx + "bgT", [128, NE, 8]); buT = P.sb(pfx + "buT", [128, NE, 8])
    bstage = P.sb(pfx + "bstage", [128, 128])
    if dbg in (3, 7):
        P.memset('dve', bgT.v, 0.0); P.memset('dve', buT.v, 0.0)
    for (dstT, src) in (((bgT, b_gate), (buT, b_up)) if dbg not in (3, 7) else ()):
        rows = NE * 8
        srcv = src.rearrange("e (fc p) -> (e fc) p", p=128)
        dv = dstT.v.re("p e fc -> p (e fc)")
        r0 = 0
        while r0 < rows:
            r1 = min(rows, r0 + 128)
            n = r1 - r0
            P.dma('act', bstage[0:n, :], srcv[r0:r1, :])
            pz = C.ps[7]
            P.tr(pz[:, 0:n], bstage[0:n, :], C.identf[0:n, 0:n])
            P.copy('dve', dv[:, r0:r1], pz[:, 0:n])
            r0 = r1
    bd = P.sb(pfx + "bd", [NE, 1024], F32R)
    bd0 = P.sb(pfx + "bd0", [NE, 1024])
    P.dma('act', bd0.v, b_down)
    P.copy('dve', bd.v, bd0.v)
    W = [[P.sb(pfx + "W%d_%d" % (j, i), [128, 8, 1024], BF16) for j in range(3)] for i in range(2)]
    xts = [P.sb(pfx + "xt%d" % i, [128, 1024]) for i in range(TGT)]
    xTg = P.sb(pfx + "xTg", [128, 8, TG], BF16)
    xT32 = P.sb(pfx + "xT32", [128, 8, 128], F32R)
    acc = P.sb(pfx + "acc", [128, TGT, 1024])
    pall = P.sb(pfx + "pall", [128, TGT, NE])
    actT = P.sb(pfx + "actT", [128, 8, TG], BF16)
    lg = P.sb(pfx + "lg", [128, NE]); t8 = P.sb(pfx + "t8", [128, 8]); msk = P.sb(pfx + "msk", [128, NE])
    ex = P.sb(pfx + "ex", [128, NE]); sm = P.sb(pfx + "sm", [128, 1]); nmx = P.sb(pfx + "nmx", [128, 1])
    pT = P.sb(pfx + "pT", [NE, 128], F32R)
    gt = [P.sb(pfx + "g%d" % i, [128, TG]) for i in range(2)]
    st_ = [P.sb(pfx + "s%d" % i, [128, TG]) for i in range(2)]
    ut = [P.sb(pfx + "u%d" % i, [128, TG]) for i in range(2)]
    st = P.sb(pfx + "st", [128, 2, 6]); mv = P.sb(pfx + "mv", [128, 2]); rstd = P.sb(pfx + "rstd", [128, 1])
    c1 = P.sb(pfx + "c1", [128, 1]); c7 = P.sb(pfx + "c7", [128, 1])
    P.memset('dve', c1.v, 1.0); P.memset('dve', c7.v, 7.0)
    rr = [P.sb(pfx + "rr%d" % i, [128, 1024]) for i in range(2)]
    tmpq = [P.sb(pfx + "tq%d" % i, [128, 512]) for i in range(2)]
    assert TG == 512
    if dbg == 5:
        P.wait_all('sp', [gB, bB, wr, brB, bd, bgT, buT])
        return
    wcnt = 0
    kk = 0
    for gi in range(NT // TG):
        for tt in range(TGT):
            t = gi * TGT + tt
            xt = xts[tt]
            P.dma('sp', xt.v, x1_d[t * 128:(t + 1) * 128, :])
            x_transpose(P, C, xt.v, [(xTg[:, :, tt * 128:(tt + 1) * 128], 'act')] + ([(xT32.v, 'dve')] if dbg != 3 else []), [C.ps[0], C.ps[1]])
            if dbg in (2, 3, 7, 8):
                P.memset('dve', acc[:, tt, :], 0.0)
                P.memset('dve', pall[:, tt, :], 0.25)
            else:
                pl = C.ps[6]
                for kc in range(8):
                    P.mm(pl[:, 0:NE], xT32[:, kc, :], wr[:, kc, :], start=(kc == 0), stop=(kc == 7))
                P.tt('dve', lg.v, pl[:, 0:NE], brB.v, ALU.add)
                a, b = t8.v.ap, lg.v.ap
                P.I('dve', (lambda a, b: (lambda e: e.max(out=a, in_=b)))(a, b), w=[t8], r=[lg])
                P.ts('dve', msk.v, lg.v, t8[:, 3:4], c1.v, op0=ALU.is_ge, op1=ALU.mult)
                P.ts('dve', nmx.v, t8[:, 0:1], -1.0, None, op0=ALU.mult)
                P.act(ex.v, lg.v, AF.Exp, bias=nmx.v)
                P.tt('dve', ex.v, ex.v, msk.v, ALU.mult)
                a2, b2 = sm.v.ap, ex.v.ap
                P.I('dve', (lambda a, b: (lambda e: e.reduce_sum(a, b, AX.X)))(a2, b2), w=[sm], r=[ex])
                a3 = sm.v.ap
                P.I('dve', (lambda a: (lambda e: e.reciprocal(a, a)))(a3), w=[sm], r=[sm])
                P.ts('dve', pall[:, tt, :], ex.v, sm.v, c1.v, op0=ALU.mult, op1=ALU.mult)
                P.tr(pl[0:NE, 128:256], pall[:, tt, :], C.identf.v)
                P.copy('dve', pT.v, pl[0:NE, 128:256])
                for hf in range(2):
                    pb = C.ps[7]
                    P.mm(pb.v, pT.v, bd[:, hf * 512:(hf + 1) * 512])
                    P.copy('dve', acc[:, tt, hf * 512:(hf + 1) * 512], pb.v)
        for e in range(NE if dbg not in (1, 3, 7, 8) else 0):
            Wg, Wu, Wd = W[wcnt % 2]
            wcnt += 1
            P.dma('pool', Wg.v, w_gate[e].rearrange("(kc p) f -> p kc f", p=128))
            P.dma('pool', Wu.v, w_up[e].rearrange("(kc p) f -> p kc f", p=128))
            P.dma('pool', Wd.v, w_down[e].rearrange("(kc p) f -> p kc f", p=128))
            for fc in range(8):
                pg = C.ps[(kk % 2) * 2]; pu = C.ps[(kk % 2) * 2 + 1]
                g = gt[kk % 2]; s = st_[kk % 2]; u = ut[kk % 2]
                kk += 1
                for kc in range(8):
                    P.mm(pg.v, Wg[:, kc, fc * 128:(fc + 1) * 128], xTg[:, kc, :], start=(kc == 0), stop=(kc == 7))
                for kc in range(8):
                    P.mm(pu.v, Wu[:, kc, fc * 128:(fc + 1) * 128], xTg[:, kc, :], start=(kc == 0), stop=(kc == 7))
                P.ts('dve', g.v, pg.v, bgT[:, e, fc:fc + 1], c7.v, op0=ALU.add, op1=ALU.min)
                P.act(s.v, g.v, AF.Sigmoid, scale=1.702)
                P.ts('dve', u.v, pu.v, buT[:, e, fc:fc + 1], c7.v, op0=ALU.add, op1=ALU.min)
                P.ts('dve', u.v, u.v, -7.0, 1.0, op0=ALU.max, op1=ALU.add)
                P.tt('dve', g.v, g.v, s.v, ALU.mult)
                P.tt('dve', actT[:, fc, :], g.v, u.v, ALU.mult)
            for tt in range(TGT):
                for hf in range(2):
                    py = C.ps[4 + (kk % 2)]
                    kk += 1
                    for fc in range(8):
                        P.mm(py.v, actT[:, fc, tt * 128:(tt + 1) * 128], Wd[:, fc, hf * 512:(hf + 1) * 512],
                             start=(fc == 0), stop=(fc == 7))
                    av = acc[:, tt, hf * 512:(hf + 1) * 512]
                    tq = tmpq[kk % 2]
                    P.ts('dve', tq.v, py.v, pall[:, tt, e:e + 1], c1.v, op0=ALU.mult, op1=ALU.mult)
                    P.tt('dve', av, av, tq.v, ALU.add)
        for tt in range(TGT):
            t = gi * TGT + tt
            r = rr[tt % 2].v
            P.stt('dve', r, xts[tt].v, DN_ALPHA, acc[:, tt, :], ALU.mult, ALU.add)
            layer_norm(P, r, gB.v, bB.v, st.v, mv.v, rstd.v)
            P.dma('sp', out_d[t * 128:(t + 1) * 128, :], r)


RET_GAMMA = [1.0 - 2.0 ** (-5.0 - h) for h in range(4)]


def host_consts_scan(NT):
    j = np.arange(128, dtype=np.float64)
    lg = np.log(np.array(RET_GAMMA, dtype=np.float64))
    aR = np.exp(lg[None, :] * (j[:, None] + 1.0)) * (64 ** -0.5)
    bR = np.exp(-lg[None, :] * (j[:, None] + 1.0))
    eR = np.zeros((128, 2)); gR = np.zeros((128, 2))
    for h in range(4):
        ps = (h % 2) * 64
        eR[ps:ps + 64, h // 2] = np.exp(lg[h] * 128.0)
        gR[ps:ps + 64, h // 2] = np.exp(lg[h] * float(NT))
    mask = (j[:, None] <= j[None, :]).astype(np.float64)
    return dict(aR=aR.astype(np.float32), bR=bR.astype(np.float32), eR=eR.astype(np.float32),
                gR=gR.astype(np.float32), mask=mask.astype(np.float32))


def host_blockdiag(w):
    out = np.zeros((4, 128, 128), dtype=np.float32)
    for h in range(4):
        for n in range(32):
            out[h, 4 * n:4 * n + 4, 4 * n:4 * n + 4] = w[32 * h + n]
    return out


class PSRot:
    def __init__(self, C, banks):
        self.C = C; self.banks = banks; self.i = 0

    def __call__(self):
        b = self.C.ps[self.banks[self.i % len(self.banks)]]
        self.i += 1
        return b


def small_T(P, C, dst, src2d, rows, stage, ps):
    P.dma('act', stage[0:rows, :], src2d)
    P.tr(ps[:, 0:rows], stage[0:rows, :], C.identf[0:rows, 0:rows])
    P.copy('dve', dst, ps[:, 0:rows])


def pass_scan(P, C, NT, x_d, xprev_d, w_in_l, prm, cst, init, outs, mode="full", pfx="s"):
    full = (mode == "full")
    NW = 2568
    W = P.sb(pfx + "W", [128, 8, NW], BF16)
    load_w_cast(P, W.v, w_in_l[:, 0:NW].rearrange("(kc p) c -> p kc c", p=128))
    BD = {}
    for nm in ("bdq", "bdk", "bdv"):
        BD[nm] = P.sb(pfx + nm, [128, 4, 128], BF16)
        P.dma('pool', BD[nm].v, prm[nm].rearrange("h i o -> i h o"))
    stage = P.sb(pfx + "stage", [128, 128])
    cwT = P.sb(pfx + "cwT", [128, 16]); cbT = P.sb(pfx + "cbT", [128, 4])
    small_T(P, C, cwT.v, prm["ml_conv_w"].rearrange("k (c p) -> (k c) p", p=128), 16, stage, C.ps[7])
    small_T(P, C, cbT.v, prm["ml_conv_b"].rearrange("(c p) -> c p", p=128), 4, stage, C.ps[7])
    biB = P.sb(pfx + "biB", [128, 4]); bfB = P.sb(pfx + "bfB", [128, 4])
    bcast_rows(P, biB.v, prm["ml_bi"]); bcast_rows(P, bfB.v, prm["ml_bf"])
    aR = P.sb(pfx + "aR", [128, 4]); bR = P.sb(pfx + "bR", [128, 4]); eR = P.sb(pfx + "eR", [128, 2]); gR = P.sb(pfx + "gR", [128, 2])
    for t_, n_ in ((aR, "aR"), (bR, "bR"), (eR, "eR"), (gR, "gR")):
        P.dma('act', t_.v, cst[n_])
    mask = P.sb(pfx + "mask", [128, 128]); P.dma('act', mask.v, cst["mask"])
    maskr = P.sb(pfx + "maskr", [128, 128], F32R); P.copy('dve', maskr.v, mask.v)
    onesr = P.sb(pfx + "onesr", [128, 128], F32R); P.copy('dve', onesr.v, C.onesf.v)
    c1 = P.sb(pfx + "c1", [128, 1]); P.memset('dve', c1.v, 1.0)
    if full:
        gnR = P.sb(pfx + "gnR", [128, 512]); gnM = P.sb(pfx + "gnM", [128, 512]); skM = P.sb(pfx + "skM", [128, 512])
        bcast_rows(P, gnR.v, prm["ret_gn"]); bcast_rows(P, gnM.v, prm["ml_gn"]); bcast_rows(P, skM.v, prm["ml_skip"])
    Sret = P.sb(pfx + "Sret", [128, 2, 128]); Sretb = P.sb(pfx + "Sretb", [128, 2, 128], BF16)
    Cml = P.sb(pfx + "Cml", [128, 4, 129]); Cmlb = P.sb(pfx + "Cmlb", [128, 4, 129], BF16)
    tmpS = P.sb(pfx + "tmpS", [128, 4, 129]); tmpS2 = P.sb(pfx + "tmpS2", [128, 4, 129])
    totacc = P.sb(pfx + "totacc", [128, 4])
    P.memset('dve', Sret.v, 0.0); P.memset('dve', Cml.v, 0.0); P.memset('dve', totacc.v, 0.0)
    if init is not None:
        sel = P.sb(pfx + "sel", [128, 3]); nsel = P.sb(pfx + "nsel", [128, 3])
        P.dma('act', sel.v, init["sel"]); P.dma('act', nsel.v, init["nsel"])
        Fr = P.sb(pfx + "Fr", [128, 2, 128]); Fm = P.sb(pfx + "Fm", [128, 4, 129]); tl = P.sb(pfx + "tl", [128, 4])
        Gm = P.sb(pfx + "Gm", [128, 4])
        for q in range(3):
            P.dma('act', Fr.v, init["Fret"][q]); P.dma('act', Fm.v, init["Fml"][q]); P.dma('act', tl.v, init["totL"][q])
            P.act(Gm.v, tl.v, AF.Exp, scale=-1.0)
            for hp in range(2):
                P.act(tmpS[:, hp, 0:128], Sret[:, hp, :], AF.Copy, scale=gR[:, hp:hp + 1])
                P.tt('dve', tmpS[:, hp, 0:128], tmpS[:, hp, 0:128], Fr[:, hp, :], ALU.add)
                P.act(tmpS[:, hp, 0:128], tmpS[:, hp, 0:128], AF.Copy, scale=sel[:, q:q + 1])
                P.act(tmpS2[:, hp, 0:128], Sret[:, hp, :], AF.Copy, scale=nsel[:, q:q + 1])
                P.tt('dve', Sret[:, hp, :], tmpS[:, hp, 0:128], tmpS2[:, hp, 0:128], ALU.add)
            for h in range(4):
                P.act(tmpS[:, h, :], Cml[:, h, :], AF.Copy, scale=Gm[:, h:h + 1])
                P.tt('dve', tmpS[:, h, :], tmpS[:, h, :], Fm[:, h, :], ALU.add)
                P.act(tmpS[:, h, :], tmpS[:, h, :], AF.Copy, scale=sel[:, q:q + 1])
                P.act(tmpS2[:, h, :], Cml[:, h, :], AF.Copy, scale=nsel[:, q:q + 1])
                P.tt('dve', Cml[:, h, :], tmpS[:, h, :], tmpS2[:, h, :], ALU.add)
    P.copy('dve', Sretb.v, Sret.v); P.copy('dve', Cmlb.v, Cml.v)
    SC = 512
    xts = [P.sb(pfx + "xt%d" % i, [128, 1024]) for i in range(2)]
    xT = P.sb(pfx + "xT", [128, 8, SC], BF16)
    rqT = P.sb(pfx + "rqT", [128, 2, SC], BF16); rkT = P.sb(pfx + "rkT", [128, 2, SC], BF16)
    mxT = P.sb(pfx + "mxT", [128, 4, 3 + SC]); mxb = P.sb(pfx + "mxb", [128, 4, SC], BF16)
    cva = P.sb(pfx + "cva", [128, SC]); cvb = P.sb(pfx + "cvb", [128, SC])
    mcT = P.sb(pfx + "mcT", [128, 4, SC], BF16)
    qmT = P.sb(pfx + "qmT", [128, 4, SC], BF16); kmT = P.sb(pfx + "kmT", [128, 4, SC], BF16)
    rk_tok = P.sb(pfx + "rk_tok", [128, 256], BF16); km_tok = P.sb(pfx + "km_tok", [128, 512], BF16)
    vpR = P.sb(pfx + "vpR", [128, 4, 128], BF16); vpM = P.sb(pfx + "vpM", [128, 4, 129], BF16)
    g8 = P.sb(pfx + "g8", [128, 8]); L1 = P.sb(pfx + "L1", [128, 4], F32R); e1 = P.sb(pfx + "e1", [128, 4])
    igt = P.sb(pfx + "igt", [128, 4]); aM = P.sb(pfx + "aM", [128, 4]); bM = P.sb(pfx + "bM", [128, 4]); eM = P.sb(pfx + "eM", [128, 4])
    tmp4 = P.sb(pfx + "tmp4", [128, 4])
    Pm8 = [P.sb(pfx + "Pm%d" % i, [128, 128], BF16) for i in range(8)]
    ot8 = [P.sb(pfx + "ot%d" % i, [128, 129]) for i in range(8)]
    hh = [P.sb(pfx + "hh%d" % i, [128, 128]) for i in range(2)]
    dn = P.sb(pfx + "dn", [128, 1]); st6 = P.sb(pfx + "st6", [128, 6]); mv = P.sb(pfx + "mv", [128, 2]); rs = P.sb(pfx + "rs", [128, 1])
    if full:
        yR = P.sb(pfx + "yR", [128, 512]); yM = P.sb(pfx + "yM", [128, 512])
        rg = P.sb(pfx + "rg", [128, 512]); mo = P.sb(pfx + "mo", [128, 512]); mct = P.sb(pfx + "mct", [128, 512])
        yTo = [P.sb(pfx + "yTo%d" % i, [128, 4, 128], BF16) for i in range(2)]
        ybf = P.sb(pfx + "ybf", [128, 512], BF16)
    nps = PSRot(C, [0, 1, 2, 3, 4, 5, 6, 7])
    P.dma('sp', xts[0].v, xprev_d)
    x_transpose(P, C, xts[0].v, [(xT[:, :, 0:128], 'act')], [nps(), nps()])
    for c in range(4):
        pz = nps()
        for kc in range(8):
            P.mm(pz[:, 0:128], W[:, kc, OFF['m_x'] + c * 128: OFF['m_x'] + (c + 1) * 128], xT[:, kc, 0:128],
                 start=(kc == 0), stop=(kc == 7))
        P.copy('dve', mxT[:, c, 0:3], pz[:, 125:128])
    lnscale = float(np.log(128 ** -0.5))
    for sc in range(NT // SC):
        for tt in range(4):
            t = sc * 4 + tt
            xt = xts[t % 2]
            P.dma('sp', xt.v, x_d[t * 128:(t + 1) * 128, :])
            x_transpose(P, C, xt.v, [(xT[:, :, tt * 128:(tt + 1) * 128], 'act')], [nps(), nps()])
        for (dst, off, nch, kind) in ((rqT, OFF['r_q'], 2, 'bf'), (rkT, OFF['r_k'], 2, 'bf'), (mxT, OFF['m_x'], 4, 'mx')):
            if not full and (dst is rqT or dst is rkT):
                continue
            for c in range(nch):
                pz = nps()
                for kc in range(8):
                    P.mm(pz.v, W[:, kc, off + c * 128: off + (c + 1) * 128], xT[:, kc, :], start=(kc == 0), stop=(kc == 7))
                if kind == 'bf':
                    P.copy('act', dst[:, c, :], pz.v)
                else:
                    P.copy('act', mxT[:, c, 3:3 + SC], pz.v)
                    P.copy('dve', mxb[:, c, :], pz.v)
        for c in range(4):
            P.ts('dve', cva.v, mxT[:, c, 3:3 + SC], cwT[:, 12 + c:13 + c], cbT[:, c:c + 1], op0=ALU.mult, op1=ALU.add)
            P.stt('dve', cvb.v, mxT[:, c, 2:2 + SC], cwT[:, 8 + c:9 + c], cva.v, ALU.mult, ALU.add)
            P.stt('dve', cva.v, mxT[:, c, 1:1 + SC], cwT[:, 4 + c:5 + c], cvb.v, ALU.mult, ALU.add)
            P.stt('dve', cvb.v, mxT[:, c, 0:SC], cwT[:, c:c + 1], cva.v, ALU.mult, ALU.add)
            P.act(mcT[:, c, :], cvb.v, AF.Silu)
            P.copy('dve', cva[:, 0:3], mxT[:, c, SC:SC + 3])
            P.copy('dve', mxT[:, c, 0:3], cva[:, 0:3])
        for (dst, bd) in ((qmT, BD["bdq"]), (kmT, BD["bdk"])):
            if not full:
                continue
            for h in range(4):
                pz = nps()
                P.mm(pz.v, bd[:, h, :], mcT[:, h, :])
                P.copy('act', dst[:, h, :], pz.v)
        for tt in range(4):
            t = sc * 4 + tt
            ts_ = slice(tt * 128, (tt + 1) * 128)

            def tok_proj(off, n):
                pz = nps()
                for kc in range(8):
                    P.mm(pz[:, 0:n], xT[:, kc, ts_], W[:, kc, off:off + n], start=(kc == 0), stop=(kc == 7))
                return pz
            p_rk = tok_proj(OFF['r_k'], 256)
            P.copy('act', rk_tok.v, p_rk[:, 0:256])
            p_g8 = tok_proj(OFF['m_i'], 8)
            P.copy('dve', g8.v, p_g8[:, 0:8])
            P.tt('dve', igt.v, g8[:, 0:4], biB.v, ALU.add)
            P.tt('dve', tmp4.v, g8[:, 4:8], bfB.v, ALU.add)
            P.act(e1.v, tmp4.v, AF.Exp, scale=-1.0)
            P.ts('dve', e1.v, e1.v, 1.0, None, op0=ALU.add)
            P.act(L1.v, e1.v, AF.Ln)
            pc = nps()
            P.mm(pc[:, 0:4], maskr.v, L1.v)
            P.mm(pc[:, 8:12], onesr.v, L1.v)
            P.act(aM.v, pc[:, 0:4], AF.Exp, scale=-1.0, bias=lnscale)
            P.tt('dve', tmp4.v, igt.v, pc[:, 0:4], ALU.add)
            P.act(bM.v, tmp4.v, AF.Exp)
            P.act(eM.v, pc[:, 8:12], AF.Exp, scale=-1.0)
            P.tt('dve', totacc.v, totacc.v, pc[:, 8:12], ALU.add)
            p_rv = tok_proj(OFF['r_v'], 512)
            for h in range(4):
                P.act(vpR[:, h, :], p_rv[:, h * 128:(h + 1) * 128], AF.Copy, scale=bR[:, h:h + 1])
            p_vm = nps()
            for h in range(4):
                P.mm(p_vm[:, h * 128:(h + 1) * 128], mxb[:, h, ts_], BD["bdv"][:, h, :])
            for h in range(4):
                P.act(vpM[:, h, 0:128], p_vm[:, h * 128:(h + 1) * 128], AF.Copy, scale=bM[:, h:h + 1])
            P.copy('dve', vpM[:, :, 128:129], bM.v.re("p (h o) -> p h o", o=1))
            p_km = nps()
            for h in range(4):
                P.mm(p_km[:, h * 128:(h + 1) * 128], mcT[:, h, ts_], BD["bdk"][:, h, :])
            P.copy('act', km_tok.v, p_km.v)
            if full:
                p_rg = tok_proj(OFF['r_g'], 512)
                P.act(rg.v, p_rg.v, AF.Silu)
                p_mo = tok_proj(OFF['m_o'], 512)
                P.act(mo.v, p_mo.v, AF.Sigmoid)
                p_mc = nps()
                pmb = p_mc.v.bitcast(BF16)
                for c in range(4):
                    P.tr(pmb[:, c * 128:(c + 1) * 128], mcT[:, c, ts_], C.identb.v)
                P.tt('dve', mct.v, pmb[:, 0:512], skM.v, ALU.mult)
            kq = 0
            if full:
                for h in range(4):
                    psl = slice((h % 2) * 64, (h % 2) * 64 + 64); hp = h // 2
                    p_st = nps()
                    P.mm(p_st[:, 0:128], rkT[psl, hp, ts_], rqT[psl, hp, ts_])
                    P.tt('dve', Pm8[h].v, p_st[:, 0:128], mask.v, ALU.mult)
                for h in range(4):
                    p_st = nps()
                    P.mm(p_st[:, 0:128], kmT[:, h, ts_], qmT[:, h, ts_])
                    P.tt('dve', Pm8[4 + h].v, p_st[:, 0:128], mask.v, ALU.mult)
                for h in range(4):
                    psl = slice((h % 2) * 64, (h % 2) * 64 + 64); hp = h // 2
                    p_o = nps()
                    P.mm(p_o[:, 0:128], Pm8[h].v, vpR[:, h, :], start=True, stop=False)
                    P.mm(p_o[:, 0:128], rqT[psl, hp, ts_], Sretb[psl, hp, :], start=False, stop=True)
                    P.act(ot8[h][:, 0:128], p_o[:, 0:128], AF.Copy, scale=aR[:, h:h + 1])
                for h in range(4):
                    p_o = nps()
                    P.mm(p_o[:, 0:129], Pm8[4 + h].v, vpM[:, h, :], start=True, stop=False)
                    P.mm(p_o[:, 0:129], qmT[:, h, ts_], Cmlb[:, h, :], start=False, stop=True)
                    P.act(ot8[4 + h].v, p_o[:, 0:129], AF.Copy, scale=aM[:, h:h + 1])
            for h in range(4):
                psl = slice((h % 2) * 64, (h % 2) * 64 + 64); hp = h // 2
                p_kv = nps()
                P.mm(p_kv[:, 0:128], rk_tok[:, hp * 128:(hp + 1) * 128], vpR[:, h, :])
                P.tt('dve', tmpS[psl, hp, 0:128], Sret[psl, hp, :], p_kv[psl, 0:128], ALU.add)
                P.act(Sret[psl, hp, :], tmpS[psl, hp, 0:128], AF.Copy, scale=eR[psl, hp:hp + 1])
                P.copy('dve', Sretb[psl, hp, :], Sret[psl, hp, :])
            for h in range(4):
                p_kv = nps()
                P.mm(p_kv[:, 0:129], km_tok[:, h * 128:(h + 1) * 128], vpM[:, h, :])
                P.tt('dve', tmpS[:, h, :], Cml[:, h, :], p_kv[:, 0:129], ALU.add)
                P.act(Cml[:, h, :], tmpS[:, h, :], AF.Copy, scale=eM[:, h:h + 1])
                P.copy('dve', Cmlb[:, h, :], Cml[:, h, :])
            if full:
                for h in range(4):
                    o = ot8[h]; hx = hh[kq % 2]; kq += 1
                    head_norm(P, o[:, 0:128], hx.v, st6, mv, rs)
                    P.tt('pool', hx.v, hx.v, gnR[:, h * 128:(h + 1) * 128], ALU.mult)
                    P.tt('pool', yR[:, h * 128:(h + 1) * 128], hx.v, rg[:, h * 128:(h + 1) * 128], ALU.mult)
                for h in range(4):
                    o = ot8[4 + h]; hx = hh[kq % 2]; kq += 1
                    P.act(dn.v, o[:, 128:129], AF.Abs)
                    P.ts('dve', dn.v, dn.v, 1.0, None, op0=ALU.max)
                    a_ = dn.v.ap
                    P.I('dve', (lambda a_: (lambda e: e.reciprocal(a_, a_)))(a_), w=[dn], r=[dn])
                    P.act(o[:, 0:128], o[:, 0:128], AF.Copy, scale=dn.v)
                    head_norm(P, o[:, 0:128], hx.v, st6, mv, rs)
                    P.tt('pool', hx.v, hx.v, gnM[:, h * 128:(h + 1) * 128], ALU.mult)
                    P.tt('pool', hx.v, hx.v, mct[:, h * 128:(h + 1) * 128], ALU.add)
                    P.tt('pool', yM[:, h * 128:(h + 1) * 128], hx.v, mo[:, h * 128:(h + 1) * 128], ALU.mult)
            if full:
                for (ysrc, ydst) in ((yR, outs[0]), (yM, outs[1])):
                    P.copy('act', ybf.v, ysrc.v)
                    pz = nps(); pzb = pz.v.bitcast(BF16)
                    for c in range(4):
                        P.tr(pzb[:, c * 128:(c + 1) * 128], ybf[:, c * 128:(c + 1) * 128], C.identb.v)
                    yo = yTo[kq % 2]; kq += 1
                    P.copy('dve', yo.v, pzb[:, 0:512].re("p (c t) -> p c t", c=4))
                    P.dma('sp', ydst[:, :, t * 128:(t + 1) * 128], yo.v)
    if not full:
        P.dma('sp', outs[0].v, Sret.v)
        P.dma('sp', outs[1].v, Cml.v)
        P.dma('sp', outs[2].v, totacc.v)


def head_norm(P, src, dst, st6, mv, rs):
    a, b = st6.v.ap, src.ap
    P.I('dve', lambda e: e.bn_stats(a, b), w=[st6], r=[src])
    a2, b2 = mv.v.ap, st6.v.ap
    P.I('dve', lambda e: e.bn_aggr(a2, b2), w=[mv], r=[st6])
    P.ts('dve', rs.v, mv[:, 1:2], EPS, None, op0=ALU.add)
    P.act(rs.v, rs.v, AF.Ln)
    P.act(rs.v, rs.v, AF.Exp, scale=-0.5)
    P.ts('dve', dst, src, mv[:, 0:1], rs.v, op0=ALU.subtract, op1=ALU.mult)


ATT_PAT = ((128, 1), (512, 4), (2048, 16))
HALO = 2048


def host_consts_attn():
    slopes = np.exp2(-8.0 * np.arange(1, 13, dtype=np.float64) / 12.0).reshape(3, 4)
    s = np.arange(128)[:, None]; i = np.arange(128)[None, :]
    out = np.zeros((3, 2, 128, 4, 128), dtype=np.float32)
    for g, (win, d) in enumerate(ATT_PAT):
        for h in range(4):
            dcur = i - s
            b = np.where((dcur >= 0), -slopes[g, h] * d * dcur, -30000.0)
            out[g, 1, :, h, :] = b
            dprev = i + 128 - s
            b = np.where((dprev <= 128), -slopes[g, h] * d * dprev, -30000.0)
            out[g, 0, :, h, :] = b
    return out.reshape(3, 2, 128, 512)


def pass_attn(P, C, NT, xext_d, w_in_l, bias_d, hv_d, yT_out, pfx="a", dbg=0):
    accN = P.sb(pfx + "accN", [128, 4, NT]); accD = P.sb(pfx + "accD", [128, 4, NT])
    for h_ in range(4):
        for j_ in range(NT // 2048):
            P.memset('dve', accN[:, h_, j_ * 2048:(j_ + 1) * 2048], 0.0 if dbg == 0 else 1.0)
            P.memset('dve', accD[:, h_, j_ * 2048:(j_ + 1) * 2048], 0.0 if dbg == 0 else 2.0)
    hv0 = P.sb(pfx + "hv0", [128, 128]); hvb = P.sb(pfx + "hvb", [128, 128], BF16)
    P.dma('act', hv0.v, hv_d); P.copy('dve', hvb.v, hv0.v)
    Wq = P.sb(pfx + "Wq", [128, 8, 256], BF16); Wk = P.sb(pfx + "Wk", [128, 8, 256], BF16); Wv = P.sb(pfx + "Wv", [128, 8, 512], BF16)
    bT = [P.sb(pfx + "bT%d" % i, [128, 512]) for i in range(2)]
    xts = [P.sb(pfx + "xt%d" % i, [128, 1024]) for i in range(2)]
    xTb = [P.sb(pfx + "xTb%d" % i, [128, 8, 128], BF16) for i in range(2)]
    kT = [P.sb(pfx + "kT%d" % i, [128, 2, 128], BF16) for i in range(4)]
    Vt = [P.sb(pfx + "V%d" % i, [128, 512], BF16) for i in range(4)]
    qT = [P.sb(pfx + "qT%d" % i, [128, 2, 128], BF16) for i in range(2)]
    tmp = [P.sb(pfx + "tmp%d" % i, [128, 512]) for i in range(2)]
    PT = [[P.sb(pfx + "PT%d_%d" % (i, j), [128, 512], BF16) for j in range(2)] for i in range(2)]
    nps = PSRot(C, [0, 1, 2, 3, 4, 5, 6, 7])
    win = w_in_l.rearrange("(kc p) c -> p kc c", p=128)
    nb = 0
    for g, (_, d) in enumerate(ATT_PAT if dbg not in (1, 2, 4, 5, 6) else (ATT_PAT[:1] if dbg in (2, 4, 5, 6) else ())):
        load_w_cast(P, Wq.v, win[:, :, OFF['a_q'] + g * 256: OFF['a_q'] + (g + 1) * 256])
        load_w_cast(P, Wk.v, win[:, :, OFF['a_k'] + g * 256: OFF['a_k'] + (g + 1) * 256])
        load_w_cast(P, Wv.v, win[:, :, OFF['a_v'] + g * 512: OFF['a_v'] + (g + 1) * 512])
        P.dma('act', bT[0].v, bias_d[g, 0]); P.dma('act', bT[1].v, bias_d[g, 1])
        NB = NT // (128 * d)
        blocks = [(r, m) for r in range(d) for m in range(-1, NB)]
        nblk = len(blocks)

        def Pst(i):
            r, m = blocks[i]
            s0 = HALO + m * 128 * d + r
            xt = xts[i % 2]; xb = xTb[i % 2]
            P.dma('sp', xt.v, xext_d[s0: s0 + 127 * d + 1: d, :])
            x_transpose(P, C, xt.v, [(xb.v, 'act')], [nps(), nps()])
            for c in range(2):
                pz = nps()
                for kc in range(8):
                    P.mm(pz[:, 0:128], Wk[:, kc, c * 128:(c + 1) * 128], xb[:, kc, :], start=(kc == 0), stop=(kc == 7))
                P.copy('act', kT[i % 4][:, c, :], pz[:, 0:128])
            pz = nps()
            for kc in range(8):
                P.mm(pz.v, xb[:, kc, :], Wv[:, kc, :], start=(kc == 0), stop=(kc == 7))
            P.copy('act', Vt[i % 4].v, pz.v)
            if m < 0:
                return
            for c in range(2):
                pz = nps()
                for kc in range(8):
                    P.mm(pz[:, 0:128], Wq[:, kc, c * 128:(c + 1) * 128], xb[:, kc, :], start=(kc == 0), stop=(kc == 7))
                P.copy('act', qT[i % 2][:, c, :], pz[:, 0:128])

        def Sst(i):
            r, m = blocks[i]
            if m < 0:
                return
            for pc, kb in ((0, (i - 1) % 4), (1, i % 4)):
                psAB = [nps(), nps()]
                for h in range(4):
                    psl = slice((h % 2) * 64, (h % 2) * 64 + 64); hp = h // 2
                    P.mm(psAB[h % 2][:, hp * 128:(hp + 1) * 128], kT[kb][psl, hp, :], qT[i % 2][psl, hp, :])
                tm = tmp[pc]
                for h in range(4):
                    hs = slice(h * 128, (h + 1) * 128); hp = h // 2
                    P.stt('dve', tm[:, hs], psAB[h % 2][:, hp * 128:(hp + 1) * 128], 0.125, bT[pc][:, hs], ALU.mult, ALU.add)
                P.act(PT[i % 2][pc].v, tm.v, AF.Exp)

        def Nst(i):
            r, m = blocks[i]
            if m < 0:
                return
            pn = nps()
            for h in range(4):
                hs = slice(h * 128, (h + 1) * 128)
                P.mm(pn[:, hs], Vt[(i - 1) % 4][:, hs], PT[i % 2][0][:, hs], start=True, stop=False)
                P.mm(pn[:, hs], Vt[i % 4][:, hs], PT[i % 2][1][:, hs], start=False, stop=True)
            pd = nps()
            P.mm(pd.v, (hvb.v if m == 0 else C.onesb.v), PT[i % 2][0].v, start=True, stop=False)
            P.mm(pd.v, C.onesb.v, PT[i % 2][1].v, start=False, stop=True)
            t0 = m * 128 * d + r
            for h in range(4):
                hs = slice(h * 128, (h + 1) * 128)
                av = accN[:, h, t0: t0 + 127 * d + 1: d]
                P.tt('dve', av, av, pn[:, hs], ALU.add)
                dv = accD[:, h, t0: t0 + 127 * d + 1: d]
                P.tt('dve', dv, dv, pd[:, hs], ALU.add)

        Pst(0)
        for i in range(nblk):
            if i + 1 < nblk:
                Pst(i + 1)
            Sst(i)
            if i >= 1:
                Nst(i - 1)
        Nst(nblk - 1)
    yo = [P.sb(pfx + "yo%d" % i, [128, 4, 512], BF16) for i in range(2)]
    rc = [P.sb(pfx + "rc%d" % i, [128, 4, 512]) for i in range(2)]
    for j in range(NT // 512):
        sl = slice(j * 512, (j + 1) * 512)
        for h in range(4):
            a_, b_ = rc[j % 2][:, h, :].ap, accD[:, h, sl].ap
            P.I('dve', (lambda a_, b_: (lambda e: e.reciprocal(a_, b_)))(a_, b_), w=[rc[j % 2]], r=[accD])
            P.tt('dve', yo[j % 2][:, h, :], accN[:, h, sl], rc[j % 2][:, h, :], ALU.mult)
        P.dma('sp', yT_out[:, :, sl], yo[j % 2].v)


NCORES = 8
NT_CORE = 4096
PRM_NAMES = ("ret_gn", "ml_conv_w", "ml_conv_b", "bdq", "bdk", "bdv", "ml_bi", "ml_bf", "ml_gn", "ml_skip")
PRM_SHAPES = dict(ret_gn=[512], ml_conv_w=[4, 512], ml_conv_b=[512], bdq=[4, 128, 128], bdk=[4, 128, 128], bdv=[4, 128, 128],
                  ml_bi=[4], ml_bf=[4], ml_gn=[512], ml_skip=[512])
CST_SHAPES = dict(aR=[128, 4], bR=[128, 4], eR=[128, 2], gR=[128, 2], mask=[128, 128])


def _scan_inputs(nc):
    def inp(n, sh):
        return nc.dram_tensor(n, sh, F32, kind="ExternalInput").ap()
    prm = {n: inp(n, PRM_SHAPES[n]) for n in PRM_NAMES}
    cst = {n: inp(n, CST_SHAPES[n]) for n in CST_SHAPES}
    return prm, cst


def build_A(NT=NT_CORE):
    nc = bass.Bass("TRN2", target_bir_lowering=False)
    with ExitStack() as es:
        P = Prog(nc, es)
        x_d = P.dram("x", [NT, D], F32, kind="ExternalInput")
        xprev = P.dram("xprev", [128, D], F32, kind="ExternalInput")
        w_in = nc.dram_tensor("w_in", [D, D_IN], F32, kind="ExternalInput").ap()
        prm, cst = _scan_inputs(nc)
        outs = [P.dram("oS", [128, 2, 128], F32, kind="ExternalOutput"), P.dram("oC", [128, 4, 129], F32, kind="ExternalOutput"),
                P.dram("oT", [128, 4], F32, kind="ExternalOutput")]
        C = make_ctx(P)
        pass_scan(P, C, NT, x_d, xprev, w_in, prm, cst, None, outs, mode="summary", pfx="s")
        P.wait_all('sp', outs)
        P.emit()
    return nc


def build_B(NT=NT_CORE, NE=NEXP):
    nc = bass.Bass("TRN2", target_bir_lowering=False)
    with ExitStack() as es:
        P = Prog(nc, es)

        def inp(n, sh):
            return nc.dram_tensor(n, sh, F32, kind="ExternalInput").ap()
        xext = P.dram("xext", [HALO + NT, D], F32, kind="ExternalInput")
        w_in = inp("w_in", [D, D_IN])
        prm, cst = _scan_inputs(nc)
        init = dict(Fret=inp("Fret", [3, 128, 2, 128]), Fml=inp("Fml", [3, 128, 4, 129]), totL=inp("totL", [3, 128, 4]),
                    sel=inp("sel", [128, 3]), nsel=inp("nsel", [128, 3]))
        abias = inp("abias", [3, 2, 128, 512]); hv = inp("hv", [128, 128])
        w_br = inp("w_branch", [3, MIXW, D]); w_out = inp("w_out", [D, D])
        ln1_g = inp("ln1_g", [D]); ln1_b = inp("ln1_b", [D]); ln2_g = inp("ln2_g", [D]); ln2_b = inp("ln2_b", [D])
        w_r = inp("w_router", [D, NE]); b_r = inp("b_router", [NE])
        w_g = inp("w_gate", [NE, D, D]); b_g = inp("b_gate", [NE, D])
        w_u = inp("w_up", [NE, D, D]); b_u = inp("b_up", [NE, D])
        w_d = inp("w_down", [NE, D, D]); b_d = inp("b_down", [NE, D])
        out = P.dram("out", [NT, D], F32, kind="ExternalOutput")
        yT = [P.dram("yT%d" % b, [128, 4, NT], BF16) for b in range(3)]
        x1s = P.dram("x1s", [NT, D], F32)
        C = make_ctx(P)
        x_own = Tile(P, xext.h[HALO:HALO + NT, :], "xown", "dram")
        x_own.lw, x_own.rd = xext.lw, xext.rd
        xprev = Tile(P, xext.h[HALO - 128:HALO, :], "xprv", "dram")
        xprev.lw, xprev.rd = xext.lw, xext.rd
        with P.scope():
            pass_attn(P, C, NT, xext, w_in, abias, hv, yT[2], pfx="a")
        with P.scope():
            pass_scan(P, C, NT, x_own, xprev, w_in, prm, cst, init, [yT[0], yT[1]], mode="full", pfx="s")
        with P.scope():
            pass_merge(P, C, NT, x_own, yT, w_in, w_br, w_out, ln1_g, ln1_b, x1s, pfx="m")
        NBLK = NT * 4 // 128 + NE
        Xs = P.dram("Xs", [NBLK * 128, D], BF16); Ys = P.dram("Ys", [NBLK * 128, D], F32)
        mc = {k: inp("mc_" + k, list(v.shape)) for k, v in host_consts_moe(NT, NE).items()}
        with P.scope():
            pass_moe2(P, C, NT, x1s, w_r, b_r, w_g, b_g, w_u, b_u, w_d, b_d, ln2_g, ln2_b, out, Xs, Ys, mc, NE=NE, pfx="f")
        P.wait_all('sp', [out])
        P.emit()
    return nc


def layer_params(inputs, l):
    g = lambda n: np.ascontiguousarray(np.asarray(inputs[n], dtype=np.float32)[l])
    prm = dict(ret_gn=g("ret_gn"), ml_conv_w=g("ml_conv_w"), ml_conv_b=g("ml_conv_b"),
               bdq=host_blockdiag(g("ml_wq")), bdk=host_blockdiag(g("ml_wk")), bdv=host_blockdiag(g("ml_wv")),
               ml_bi=g("ml_bi"), ml_bf=g("ml_bf"), ml_gn=g("ml_gn"), ml_skip=g("ml_skip"))
    big = dict(w_in=g("w_in"), w_branch=g("w_branch"), w_out=g("w_out"), ln1_g=g("ln1_g"), ln1_b=g("ln1_b"),
               ln2_g=g("ln2_g"), ln2_b=g("ln2_b"), w_router=g("w_router"), b_router=g("b_router"),
               w_gate=g("w_gate"), b_gate=g("b_gate"), w_up=g("w_up"), b_up=g("b_up"), w_down=g("w_down"), b_down=g("b_down"))
    return prm, big


def run_layer(ncA, ncB, xs, prm, big, cst, abias, QPB, NT):
    n = len(xs)
    zeros128 = np.zeros((128, D), np.float32)
    inA = []
    for c in range(n):
        q = c % QPB
        m = dict(prm); m.update(cst)
        m["w_in"] = big["w_in"]; m["x"] = xs[c]
        m["xprev"] = np.ascontiguousarray(xs[c - 1][-128:]) if q > 0 else zeros128
        inA.append(m)
    resA = run_bass_kernel_spmd(ncA, inA, core_ids=list(range(n))).results
    inB = []
    for c in range(n):
        q = c % QPB; b0 = c - q
        m = dict(prm); m.update(cst); m.update(big)
        halo = xs[c - 1][-HALO:] if q > 0 else np.zeros((HALO, D), np.float32)
        m["xext"] = np.ascontiguousarray(np.concatenate([halo, xs[c]], axis=0))
        Fret = np.zeros((3, 128, 2, 128), np.float32); Fml = np.zeros((3, 128, 4, 129), np.float32)
        totL = np.zeros((3, 128, 4), np.float32); sel = np.zeros((128, 3), np.float32)
        for qq in range(min(3, QPB)):
            Fret[qq] = resA[b0 + qq]["oS"]; Fml[qq] = resA[b0 + qq]["oC"]; totL[qq] = resA[b0 + qq]["oT"]
            if qq < q:
                sel[:, qq] = 1.0
        m.update(Fret=Fret, Fml=Fml, totL=totL, sel=sel, nsel=(1.0 - sel).astype(np.float32))
        m["abias"] = abias
        for k_, v_ in host_consts_moe(NT, big["w_router"].shape[1]).items():
            m["mc_" + k_] = v_
        m["hv"] = (np.ones((128, 128), np.float32) if q > 0 else np.zeros((128, 128), np.float32))
        inB.append(m)
    resB = run_bass_kernel_spmd(ncB, inB, core_ids=list(range(n))).results
    return [np.asarray(r["out"], dtype=np.float32) for r in resB]


def kernel(**inputs):
    x = np.asarray(inputs["x"], dtype=np.float32)
    B, S, _ = x.shape
    QPB = NCORES // B
    NT = S // QPB
    xs = [np.ascontiguousarray(x[c // QPB, (c % QPB) * NT:(c % QPB + 1) * NT]) for c in range(NCORES)]
    ncA = build_A(NT); ncB = build_B(NT)
    cst = host_consts_scan(NT); abias = host_consts_attn()
    L = np.asarray(inputs["w_in"]).shape[0]
    for l in range(L):
        prm, big = layer_params(inputs, l)
        xs = run_layer(ncA, ncB, xs, prm, big, cst, abias, QPB, NT)
    out = np.zeros((B, S, D), np.float32)
    for c in range(NCORES):
        out[c // QPB, (c % QPB) * NT:(c % QPB + 1) * NT] = xs[c]
    return out


BIGIDX = 4000000.0


def host_consts_moe(NT, NE=NEXP):
    NBLK = NT * 4 // 128 + NE
    p = np.arange(128, dtype=np.float32)
    d = dict(iota_p=p.reshape(128, 1).copy(),
             ustrict=(p[:, None] < p[None, :]).astype(np.float32),
             iotaJ=np.tile(np.arange(33, dtype=np.float32)[None, :], (128, 1)),
             iotaB=np.tile(np.arange(NBLK, dtype=np.float32)[None, :], (128, 1)))
    return d


def _breg(P, e, bound):
    if not hasattr(P, "_bregs"):
        P._bregs = {}
    if bound not in P._bregs:
        P._bregs[bound] = e.to_reg(int(bound))
    return P._bregs[bound]


def ind_dma(P, dst_v, src_tile, src_ap, idx_v, bound, scatter=False, extra_r=()):
    sb_tile = dst_v.tile
    key = P._tsem(sb_tile)
    d_ap, i_ap = dst_v.ap, idx_v.ap
    if not scatter:
        rt = [src_tile, idx_v.tile] + list(extra_r); wt = [sb_tile]

        def f(e):
            try:
                return e.indirect_dma_start(d_ap, None, src_ap, bass.IndirectOffsetOnAxis(i_ap, 0), bounds_check=_breg(P, e, bound), oob_is_err=False)
            except Exception:
                print("IND GATHER FAIL", d_ap, src_ap, i_ap, bound)
                raise
    else:
        rt = [sb_tile, idx_v.tile] + list(extra_r); wt = [src_tile]

        def f(e):
            try:
                return e.indirect_dma_start(src_ap, bass.IndirectOffsetOnAxis(i_ap, 0), d_ap, None, bounds_check=_breg(P, e, bound), oob_is_err=False)
            except Exception:
                print("IND SCATTER FAIL", d_ap, src_ap, i_ap, bound)
                raise
    waits = P._waits('pool', rt, wt)
    sb_tile.dcnt += 16
    P.stream['pool'].append((waits, f, (key, 16)))
    P._record((key, sb_tile.dcnt), rt, wt)
    P.ninst += 1


def pass_moe2(P, C, NT, x1_d, w_router, b_router, w_gate, b_gate, w_up, b_up, w_down, b_down, ln_g, ln_b, out_d,
              Xs, Ys, mc, NE=NEXP, pfx="f", dbg=0):
    NTL = NT // 128
    NBLK = NT * 4 // 128 + NE
    NSLOT = NBLK * 128
    gB = P.sb(pfx + "gB", [128, 1024]); bB = P.sb(pfx + "bB", [128, 1024])
    bcast_rows(P, gB.v, ln_g); bcast_rows(P, bB.v, ln_b)
    wr = P.sb(pfx + "wr", [128, 8, NE], F32R); wr0 = P.sb(pfx + "wr0", [128, 8, NE])
    P.dma('act', wr0.v, w_router.rearrange("(kc p) e -> p kc e", p=128)); P.copy('dve', wr.v, wr0.v)
    brB = P.sb(pfx + "brB", [128, NE]); bcast_rows(P, brB.v, b_router)
    c1 = P.sb(pfx + "c1", [128, 1]); P.memset('dve', c1.v, 1.0)
    iop = P.sb(pfx + "iop", [128, 1]); P.dma('act', iop.v, mc["iota_p"])
    us0 = P.sb(pfx + "us0", [128, 128]); P.dma('act', us0.v, mc["ustrict"])
    usb = P.sb(pfx + "usb", [128, 128], BF16); P.copy('dve', usb.v, us0.v)
    ioJ = P.sb(pfx + "ioJ", [128, 33]); P.dma('act', ioJ.v, mc["iotaJ"])
    ioB = P.sb(pfx + "ioB", [128, NBLK]); P.dma('act', ioB.v, mc["iotaB"])
    lg_all = P.sb(pfx + "lg_all", [128, NTL, NE]); t8_all = P.sb(pfx + "t8_all", [128, NTL, 8])
    pall = P.sb(pfx + "pall", [128, NTL, NE]); pos_all = P.sb(pfx + "pos_all", [128, NTL, NE])
    slot_f = P.sb(pfx + "slot_f", [128, NTL, 4]); slot_i = P.sb(pfx + "slot_i", [128, NTL * 4], I32)
    pk_all = P.sb(pfx + "pk_all", [128, NTL, 4])
    carry = P.sb(pfx + "carry", [128, NE]); P.memset('dve', carry.v, 0.0)
    xts = [P.sb(pfx + "xt%d" % i, [128, 1024]) for i in range(2)]
    xT32 = P.sb(pfx + "xT32", [128, 8, 128], F32R)
    msk = P.sb(pfx + "msk", [128, NE]); mskb = P.sb(pfx + "mskb", [128, NE], BF16)
    ex = P.sb(pfx + "ex", [128, NE]); sm = P.sb(pfx + "sm", [128, 1]); nmx = P.sb(pfx + "nmx", [128, 1])
    nps = PSRot(C, [0, 1, 2, 3, 4, 5, 6, 7])
    for t in range(NTL):
        xt = xts[t % 2]
        P.dma('sp', xt.v, x1_d[t * 128:(t + 1) * 128, :])
        x_transpose(P, C, xt.v, [(xT32.v, 'dve')], [nps(), nps()])
        pl = nps()
        for kc in range(8):
            P.mm(pl[:, 0:NE], xT32[:, kc, :], wr[:, kc, :], start=(kc == 0), stop=(kc == 7))
        lg = lg_all[:, t, :]; t8 = t8_all[:, t, :]
        P.tt('dve', lg, pl[:, 0:NE], brB.v, ALU.add)
        a, b = t8.ap, lg.ap
        P.I('dve', (lambda a, b: (lambda e: e.max(out=a, in_=b)))(a, b), w=[t8_all], r=[lg_all])
        P.ts('dve', msk.v, lg, t8_all[:, t, 3:4], c1.v, op0=ALU.is_ge, op1=ALU.mult)
        P.ts('dve', nmx.v, t8_all[:, t, 0:1], -1.0, None, op0=ALU.mult)
        P.act(ex.v, lg, AF.Exp, bias=nmx.v)
        P.tt('dve', ex.v, ex.v, msk.v, ALU.mult)
        a2, b2 = sm.v.ap, ex.v.ap
        P.I('dve', (lambda a, b: (lambda e: e.reduce_sum(a, b, AX.X)))(a2, b2), w=[sm], r=[ex])
        a3 = sm.v.ap
        P.I('dve', (lambda a: (lambda e: e.reciprocal(a, a)))(a3), w=[sm], r=[sm])
        P.ts('dve', pall[:, t, :], ex.v, sm.v, c1.v, op0=ALU.mult, op1=ALU.mult)
        P.copy('dve', mskb.v, msk.v)
        pr = nps()
        P.mm(pr[:, 0:NE], usb.v, mskb.v)
        P.mm(pr[:, 64:64 + NE], C.onesb.v, mskb.v)
        P.tt('dve', pos_all[:, t, :], carry.v, pr[:, 0:NE], ALU.add)
        P.tt('dve', carry.v, carry.v, pr[:, 64:64 + NE], ALU.add)
    q = P.sb(pfx + "q", [128, NE]); nb = P.sb(pfx + "nb", [128, NE]); bend = P.sb(pfx + "bend", [128, NE])
    pstart = P.sb(pfx + "pstart", [128, NE]); t33 = P.sb(pfx + "t33", [128, 33])
    P.ts('dve', q.v, carry.v, 1.0 / 128.0, None, op0=ALU.mult)
    for e in range(NE):
        P.ts('dve', t33.v, ioJ.v, q[:, e:e + 1], c1.v, op0=ALU.is_lt, op1=ALU.mult)
        a_, b_ = nb[:, e:e + 1].ap, t33.v.ap
        P.I('dve', (lambda a, b: (lambda e_: e_.reduce_sum(a, b, AX.X)))(a_, b_), w=[nb], r=[t33])
    P.copy('dve', bend[:, 0:1], nb[:, 0:1])
    for e in range(1, NE):
        P.tt('dve', bend[:, e:e + 1], bend[:, e - 1:e], nb[:, e:e + 1], ALU.add)
    P.tt('dve', pstart.v, bend.v, nb.v, ALU.subtract)
    P.ts('dve', pstart.v, pstart.v, 128.0, None, op0=ALU.mult)
    Eall = P.sb(pfx + "Eall", [128, NBLK]); tB = P.sb(pfx + "tB", [128, NBLK]); sk = P.sb(pfx + "sk", [128, NBLK])
    P.memset('dve', Eall.v, 0.0)
    for e in range(NE):
        P.ts('dve', tB.v, ioB.v, bend[:, e:e + 1], c1.v, op0=ALU.is_ge, op1=ALU.mult)
        P.tt('dve', Eall.v, Eall.v, tB.v, ALU.add)
    P.ts('dve', Eall.v, Eall.v, float(NE - 1), None, op0=ALU.min)
    P.memset('dve', sk.v, 0.0)
    P.tt('dve', sk[:, 2:NBLK], Eall[:, 2:NBLK], Eall[:, 0:NBLK - 2], ALU.is_equal)
    P.ts('dve', sk.v, sk.v, BIGIDX, None, op0=ALU.mult)
    idxWf = P.sb(pfx + "idxWf", [128, NBLK]); idxW = P.sb(pfx + "idxW", [128, 8 * NBLK], I32)
    idxBf = P.sb(pfx + "idxBf", [128, NBLK]); idxB = P.sb(pfx + "idxB", [128, NBLK], I32)
    P.tt('dve', idxBf.v, Eall.v, sk.v, ALU.add)
    P.copy('dve', idxB.v, idxBf.v)
    iop4 = P.sb(pfx + "iop4", [128, 1]); P.ts('dve', iop4.v, iop.v, 4.0, None, op0=ALU.mult)
    P.ts('dve', idxWf.v, Eall.v, 512.0, None, op0=ALU.mult)
    P.tt('dve', idxWf.v, idxWf.v, sk.v, ALU.add)
    P.ts('dve', idxWf.v, idxWf.v, iop4.v, c1.v, op0=ALU.add, op1=ALU.mult)
    for kq in range(4):
        P.ts('dve', tB.v, idxWf.v, float(kq), None, op0=ALU.add)
        P.copy('dve', idxW[:, kq * NBLK:(kq + 1) * NBLK], tB.v)
    OH = P.sb(pfx + "OH", [128, NBLK]); P.ts('dve', OH.v, Eall.v, iop.v, c1.v, op0=ALU.is_equal, op1=ALU.mult)
    if dbg == 2:
        return
    xb16 = [P.sb(pfx + "xb16_%d" % i, [128, 1024], BF16) for i in range(2)]
    tA = P.sb(pfx + "tA", [128, NE]); oh = P.sb(pfx + "oh", [128, NE]); pr1 = P.sb(pfx + "pr1", [128, NE])
    Xs2 = Xs.h[:]
    for t in range(NTL):
        xt = xts[t % 2]; xb = xb16[t % 2]
        P.dma('sp', xt.v, x1_d[t * 128:(t + 1) * 128, :])
        P.copy('act', xb.v, xt.v)
        P.tt('dve', tA.v, pos_all[:, t, :], pstart.v, ALU.add)
        for k in range(4):
            P.ts('dve', oh.v, lg_all[:, t, :], t8_all[:, t, k:k + 1], c1.v, op0=ALU.is_equal, op1=ALU.mult)
            P.tt('dve', pr1.v, oh.v, tA.v, ALU.mult)
            a_, b_ = slot_f[:, t, k:k + 1].ap, pr1.v.ap
            P.I('dve', (lambda a, b: (lambda e_: e_.reduce_sum(a, b, AX.X)))(a_, b_), w=[slot_f], r=[pr1])
            P.tt('dve', pr1.v, oh.v, pall[:, t, :], ALU.mult)
            a_, b_ = pk_all[:, t, k:k + 1].ap, pr1.v.ap
            P.I('dve', (lambda a, b: (lambda e_: e_.reduce_sum(a, b, AX.X)))(a_, b_), w=[pk_all], r=[pr1])
        P.copy('dve', slot_i[:, t * 4:(t + 1) * 4], slot_f[:, t, :])
        for k in range(4):
            ind_dma(P, xb.v, Xs, Xs2, slot_i[:, t * 4 + k: t * 4 + k + 1], NSLOT - 1, scatter=True)
    if dbg == 3:
        P.wait_all('sp', [Xs])
        return
    with P.scope():
        inv128 = P.sb(pfx + 'inv128', [128, 128], BF16); P.memset('dve', inv128.v, 1.0 / 128.0)
        Wq = [[[P.sb(pfx + "W%d_%d_%d" % (j, i, kq), [128, 2, 1024], BF16) for kq in range(4)] for j in range(3)] for i in range(2)]
        Wt = [[[Wq[i][j][kc // 2][:, kc % 2, :] for kc in range(8)] for j in range(3)] for i in range(2)]
        Ball = [P.sb(pfx + "Ball%d" % j, [NE, 1024], BF16) for j in range(3)]
        for j, bsrc_ in enumerate((b_gate, b_up, b_down)):
            P.dma('pool', Ball[j].v, bsrc_)
        OHb = [P.sb(pfx + "OHb%d" % i, [NE, 128], BF16) for i in range(2)]
        wsrc = [w_.rearrange("e (p kq r) f -> (e p kq) (r f)", p=128, kq=4, r=2) for w_ in (w_gate, w_up, w_down)]
        bsrc = [b_gate, b_up, b_down]
        wtile = [Tile(P, None, pfx + "wsrc%d" % j, "dram") for j in range(3)]
        xblk = [P.sb(pfx + "xblk%d" % i, [128, 1024], BF16) for i in range(2)]
        xbT = [P.sb(pfx + "xbT%d" % i, [128, 8, 128], BF16) for i in range(2)]
        actT = [P.sb(pfx + "actT%d" % i, [128, 8, 128], BF16) for i in range(2)]
        gt = [P.sb(pfx + "g%d" % i, [128, 512]) for i in range(2)]
        s_t = [P.sb(pfx + "s%d" % i, [128, 512]) for i in range(2)]
        ut = [P.sb(pfx + "u%d" % i, [128, 512]) for i in range(2)]
        atok = [P.sb(pfx + "atok%d" % i, [128, 1024], BF16) for i in range(2)]
        yb = [P.sb(pfx + "yb%d" % i, [128, 1024]) for i in range(2)]
        kkc = [0]

        def stageXg(b):
            cur = b % 2
            for j in range(3):
                for kq in range(4):
                    ind_dma(P, Wq[cur][j][kq].v.re("p r f -> p (r f)"), wtile[j], wsrc[j],
                            idxW[:, kq * NBLK + b: kq * NBLK + b + 1], NE * 512 - 1)
            Wg, Wu, Wd = Wt[cur]
            P.copy('dve', OHb[cur].v, OH[0:NE, b:b + 1].bc([NE, 128]))

        def stageXx(b):
            cur = b % 2
            xk = xblk[cur]
            if dbg != 9 or b < 2:
                P.dma('sp', xk.v, Xs[b * 128:(b + 1) * 128, :])
            xT_ = xbT[cur]
            for half in range(2):
                pz = nps(); pzb = pz.v.bitcast(BF16)
                for c in range(4):
                    kc = half * 4 + c
                    P.tr(pzb[:, c * 128:(c + 1) * 128], xk[:, kc:1024:8], C.identb.v)
                P.copy('act', xT_[:, half * 4:(half + 1) * 4, :], pzb[:, 0:512].re("p (c t) -> p c t", c=4))

        def stageG(b):
            cur = b % 2
            Wg, Wu, Wd = Wt[cur]
            xT_ = xbT[cur]
            aT = actT[cur]
            at = atok[cur]
            for hf in range(2 if dbg not in (6, 10) else 0):
                hs = slice(hf * 512, (hf + 1) * 512)
                pg = nps()
                for kc in range(8):
                    P.mm(pg.v, xT_[:, kc, :], Wg[kc][:, hs], start=(kc == 0), stop=False)
                P.mm(pg.v, OHb[cur].v, Ball[0][:, hs], start=False, stop=True)
                pu = nps()
                for kc in range(8):
                    P.mm(pu.v, xT_[:, kc, :], Wu[kc][:, hs], start=(kc == 0), stop=False)
                P.mm(pu.v, OHb[cur].v, Ball[1][:, hs], start=False, stop=True)
                g = gt[kkc[0] % 2]; s_ = s_t[kkc[0] % 2]; u = ut[kkc[0] % 2]; kkc[0] += 1
                P.ts('dve', g.v, pg.v, 7.0, None, op0=ALU.min)
                P.act(s_.v, g.v, AF.Sigmoid, scale=1.702)
                P.ts('dve', u.v, pu.v, 7.0, -7.0, op0=ALU.min, op1=ALU.max)
                P.tt('dve', g.v, g.v, s_.v, ALU.mult)
                P.stt('dve', at[:, hs], u.v, 1.0, g.v, ALU.add, ALU.mult)

        def stageT(b):
            cur = b % 2
            Wg, Wu, Wd = Wt[cur]
            aT = actT[cur]
            at = atok[cur]
            for half in range(2 if dbg not in (6, 10) else 0):
                pz = nps(); pzb = pz.v.bitcast(BF16)
                for c in range(4):
                    fc = half * 4 + c
                    P.tr(pzb[:, c * 128:(c + 1) * 128], at[:, fc:1024:8], C.identb.v)
                P.copy('act', aT[:, half * 4:(half + 1) * 4, :], pzb[:, 0:512].re("p (c t) -> p c t", c=4))

        def stageDn(b):
            cur = b % 2
            Wg, Wu, Wd = Wt[cur]
            aT = actT[cur]
            y = yb[cur]
            if dbg in (6, 10):
                P.memset('dve', y.v, 0.5)
            for hf in range(2 if dbg not in (6, 10) else 0):
                py = nps()
                hs = slice(hf * 512, (hf + 1) * 512)
                for fc in range(8):
                    P.mm(py.v, aT[:, fc, :], Wd[fc][:, hs], start=(fc == 0), stop=False)
                P.mm(py.v, OHb[cur].v, Ball[2][:, hs], start=False, stop=True)
                P.copy('act', y[:, hs], py.v)
            if dbg != 7:
                P.dma('sp', Ys[b * 128:(b + 1) * 128, :], y.v)
        nblk_ = NBLK if dbg != 11 else 10
        stageXx(0)
        for b in range(nblk_ + 1):
            if b < nblk_:
                stageXg(b)
                stageG(b)
            if b >= 1:
                stageT(b - 1)
            if b + 1 < nblk_:
                stageXx(b + 1)
            if b >= 1:
                stageDn(b - 1)
    if dbg == 4:
        return
    rk = [P.sb(pfx + "rkq%d" % j, [128, 4 * 1024]) for j in range(2)]
    acA = P.sb(pfx + "acA", [128, 1024]); acB = P.sb(pfx + "acB", [128, 1024])
    st = P.sb(pfx + "st", [128, 2, 6]); mv = P.sb(pfx + "mv", [128, 2]); rstd = P.sb(pfx + "rstd", [128, 1])
    Ys2 = Ys.h[:]

    def F1(t):
        xt = xts[t % 2]
        P.dma('sp', xt.v, x1_d[t * 128:(t + 1) * 128, :])
        for k in range(4):
            ind_dma(P, rk[t % 2][:, k * 1024:(k + 1) * 1024], Ys, Ys2, slot_i[:, t * 4 + k: t * 4 + k + 1], NSLOT - 1)

    def F2(t):
        xt = xts[t % 2]; r_ = rk[t % 2]
        P.ts('dve', acA.v, r_[:, 0:1024], pk_all[:, t, 0:1], c1.v, op0=ALU.mult, op1=ALU.mult)
        P.stt('dve', acB.v, r_[:, 1024:2048], pk_all[:, t, 1:2], acA.v, ALU.mult, ALU.add)
        P.stt('dve', acA.v, r_[:, 2048:3072], pk_all[:, t, 2:3], acB.v, ALU.mult, ALU.add)
        P.stt('dve', acB.v, r_[:, 3072:4096], pk_all[:, t, 3:4], acA.v, ALU.mult, ALU.add)
        P.stt('dve', acA.v, xt.v, DN_ALPHA, acB.v, ALU.mult, ALU.add)
        layer_norm(P, acA.v, gB.v, bB.v, st.v, mv.v, rstd.v)
        P.dma('sp', out_d[t * 128:(t + 1) * 128, :], acA.v)

    F1(0)
    for t in range(NTL):
        if t + 1 < NTL:
            F1(t + 1)
        F2(t)
```
